# Optimizing a Trainium2 kernel written in Bass

```python
import jax, jax.numpy as jnp
from jax import lax
import numpy as np

D_MODEL = 2048
BATCH = 2
SEQ = 8192
DEPTH = 2

PLE_DIM = 256
N_BRANCH = 3
BRANCH_WIDTH = 1024
SWA_Q_HEADS = 16
SWA_KV_HEADS = 4
SWA_HEAD_DIM = 64
SWA_WINDOW = 128
SWA_BLOCK = 128
DN_HEADS = 8
DN_HEAD_DIM = 128
DN_WIDTH = DN_HEADS * DN_HEAD_DIM
DN_CONV = 4
DN_CHUNK = 64
MLA_HEADS = 8
MLA_Q_LORA = 512
MLA_KV_LORA = 512
MLA_NOPE = 128
MLA_ROPE = 64
MLA_V = 128
MLA_QBLOCK = 128
ROPE_THETA = 10000.0
D_FF = 7168
N_EXPERTS = 8
TOP_K = 2
D_FF_EXPERT = 7168
MOE_BLOCK = 512
N_DENSE = (DEPTH + 1) // 2
N_MOE = DEPTH // 2
NORM_EPS = 1e-6
IN_SPLITS = (
    SWA_Q_HEADS * SWA_HEAD_DIM,
    SWA_KV_HEADS * SWA_HEAD_DIM,
    SWA_KV_HEADS * SWA_HEAD_DIM,
    3 * DN_WIDTH,
    DN_WIDTH,
    DN_HEADS,
    DN_HEADS,
    MLA_Q_LORA,
    MLA_KV_LORA,
    MLA_ROPE,
    N_BRANCH * D_MODEL,
)
IN_COLS = sum(IN_SPLITS)

kernel_name = "hybrid_swa_deltanet_mla_moe_block"


def rms_norm(x, gain):
    xf = x.astype(jnp.float32)
    inv = lax.rsqrt(jnp.mean(xf * xf, axis=-1, keepdims=True) + NORM_EPS)
    return (xf * inv * gain.astype(jnp.float32)).astype(x.dtype)


def l2_normalize(x):
    return x * lax.rsqrt(jnp.sum(x * x, axis=-1, keepdims=True) + 1e-6)


def rotary(x, positions):
    half = x.shape[-1] // 2
    inv_freq = ROPE_THETA ** (-jnp.arange(half, dtype=jnp.float32) / half)
    ang = positions.astype(jnp.float32)[:, :, None] * inv_freq
    cos = jnp.cos(ang)[:, :, None, :]
    sin = jnp.sin(ang)[:, :, None, :]
    xf = x.astype(jnp.float32)
    x1, x2 = xf[..., :half], xf[..., half:]
    return jnp.concatenate([x1 * cos - x2 * sin, x2 * cos + x1 * sin], axis=-1).astype(x.dtype)


def causal_depthwise_conv(x, w):
    width, channels = w.shape
    return lax.conv_general_dilated(
        x, w[:, None, :].astype(x.dtype), window_strides=(1,), padding=((width - 1, 0),),
        dimension_numbers=('NWC', 'WIO', 'NWC'), feature_group_count=channels)


def sliding_window_attention(q, k, v, sinks):
    B, S, Hq, dh = q.shape
    Hkv = k.shape[2]
    G = Hq // Hkv
    L = SWA_BLOCK
    NB = S // L
    qb = q.reshape(B, NB, L, Hkv, G, dh)

    def band(t):
        tp = jnp.pad(t, ((0, 0), (L, 0), (0, 0), (0, 0))).reshape(B, NB + 1, L, Hkv, dh)
        return jnp.concatenate([tp[:, :-1], tp[:, 1:]], axis=2)

    kb, vb = band(k), band(v)
    s = jnp.einsum('bnqhgd,bnkhd->bnhgqk', qb, kb,
                   preferred_element_type=jnp.float32) * (dh ** -0.5)
    qi = jnp.arange(L)[:, None]
    kj = jnp.arange(2 * L)[None, :]
    rel = qi + L - kj
    key_pos = jnp.arange(NB)[:, None, None] * L + kj - L
    valid = (rel >= 0) & (rel < SWA_WINDOW) & (key_pos >= 0)
    s = jnp.where(valid[None, :, None, None], s, -jnp.inf)
    sink = sinks.astype(jnp.float32).reshape(1, 1, Hkv, G, 1, 1)
    m = jnp.maximum(jnp.max(s, axis=-1, keepdims=True), sink)
    e = jnp.exp(s - m)
    probs = e / (jnp.sum(e, axis=-1, keepdims=True) + jnp.exp(sink - m))
    o = jnp.einsum('bnhgqk,bnkhd->bnqhgd', probs.astype(v.dtype), vb)
    return o.reshape(B, S, Hq * dh)


def gated_delta_rule(q, k, v, g, beta):
    B, S, H, dk = q.shape
    dv = v.shape[-1]
    C = DN_CHUNK
    N = S // C

    def chunk(t):
        t = t.reshape((B, N, C, H) + t.shape[3:])
        return jnp.moveaxis(t, (1, 3), (0, 2))

    qc, kc, vc = chunk(q), chunk(k), chunk(v)
    gc = jnp.cumsum(chunk(g), axis=-1)
    bc = chunk(beta)
    idx = jnp.arange(C)
    incl = idx[:, None] >= idx[None, :]
    strict = idx[:, None] > idx[None, :]
    decay = jnp.exp(jnp.where(incl, gc[..., :, None] - gc[..., None, :], -jnp.inf))
    k_beta = kc * bc[..., None]
    kk = jnp.einsum('nbhid,nbhjd->nbhij', k_beta, kc) * decay
    eye = jnp.eye(C, dtype=jnp.float32)
    a_mat = eye + jnp.where(strict, kk, 0.0)
    t_mat = lax.linalg.triangular_solve(a_mat, jnp.broadcast_to(eye, a_mat.shape),
                                        left_side=True, lower=True)
    u = t_mat @ (vc * bc[..., None])
    w = t_mat @ (k_beta * jnp.exp(gc)[..., None])
    qk = jnp.where(incl, jnp.einsum('nbhid,nbhjd->nbhij', qc, kc) * decay, 0.0)

    def step(state, inp):
        q_i, k_i, u_i, w_i, g_i, qk_i = inp
        v_new = u_i - w_i @ state
        o = (q_i * jnp.exp(g_i)[..., None]) @ state + qk_i @ v_new
        g_last = g_i[..., -1:]
        state = state * jnp.exp(g_last)[..., None] + jnp.einsum(
            'bhcd,bhce->bhde', k_i * jnp.exp(g_last - g_i)[..., None], v_new)
        return state, o

    state0 = jnp.zeros((B, H, dk, dv), jnp.float32)
    _, o = lax.scan(step, state0, (qc, kc, u, w, gc, qk))
    return jnp.moveaxis(o, (0, 2), (1, 3)).reshape(B, S, H, dv)


def causal_block_attention(q, k, v):
    B, S, H, dq = q.shape
    Lq = MLA_QBLOCK
    NB = S // Lq
    scale = dq ** -0.5
    qb = jnp.moveaxis(q.reshape(B, NB, Lq, H, dq), 1, 0)
    key_pos = jnp.arange(S)

    def one_block(args):
        q_blk, n = args
        s = jnp.einsum('bqhd,bkhd->bhqk', q_blk, k, preferred_element_type=jnp.float32) * scale
        q_pos = n * Lq + jnp.arange(Lq)
        s = jnp.where(key_pos[None, :] <= q_pos[:, None], s, -jnp.inf)
        probs = jax.nn.softmax(s, axis=-1)
        return jnp.einsum('bhqk,bkhd->bqhd', probs.astype(v.dtype), v)

    o = lax.map(one_block, (qb, jnp.arange(NB)))
    return jnp.moveaxis(o, 0, 1).reshape(B, S, H * v.shape[-1])


def hybrid_mixer(h, positions, w_in, conv_w, dn_a_log, dn_dt_bias, dn_norm, swa_sinks,
                 mla_q_norm, w_uq, mla_kv_norm, w_ukv, w_branch, w_out):
    B, S, _ = h.shape
    proj = h @ w_in
    cuts = np.cumsum(IN_SPLITS)[:-1].tolist()
    (a_q, a_k, a_v, b_qkv, b_z, b_beta, b_decay,
     c_q, c_kv, c_kr, gate_logits) = jnp.split(proj, cuts, axis=-1)

    y_a = sliding_window_attention(
        a_q.reshape(B, S, SWA_Q_HEADS, SWA_HEAD_DIM),
        a_k.reshape(B, S, SWA_KV_HEADS, SWA_HEAD_DIM),
        a_v.reshape(B, S, SWA_KV_HEADS, SWA_HEAD_DIM), swa_sinks)

    qkv = jax.nn.silu(causal_depthwise_conv(b_qkv, conv_w))
    dq, dk_, dv_ = jnp.split(qkv, 3, axis=-1)
    dq = l2_normalize(dq.reshape(B, S, DN_HEADS, DN_HEAD_DIM).astype(jnp.float32)) * (DN_HEAD_DIM ** -0.5)
    dk_ = l2_normalize(dk_.reshape(B, S, DN_HEADS, DN_HEAD_DIM).astype(jnp.float32))
    dv_ = dv_.reshape(B, S, DN_HEADS, DN_HEAD_DIM).astype(jnp.float32)
    beta = jax.nn.sigmoid(b_beta.astype(jnp.float32))
    g = -jnp.exp(dn_a_log.astype(jnp.float32)) * jax.nn.softplus(
        b_decay.astype(jnp.float32) + dn_dt_bias.astype(jnp.float32))
    o_b = gated_delta_rule(dq, dk_, dv_, g, beta)
    z = b_z.reshape(B, S, DN_HEADS, DN_HEAD_DIM)
    y_b = (rms_norm(o_b, dn_norm).astype(h.dtype) * jax.nn.silu(z)).reshape(B, S, DN_WIDTH)

    q_c = (rms_norm(c_q, mla_q_norm) @ w_uq).reshape(B, S, MLA_HEADS, MLA_NOPE + MLA_ROPE)
    q_c = jnp.concatenate([q_c[..., :MLA_NOPE], rotary(q_c[..., MLA_NOPE:], positions)], axis=-1)
    kv = (rms_norm(c_kv, mla_kv_norm) @ w_ukv).reshape(B, S, MLA_HEADS, MLA_NOPE + MLA_V)
    k_nope, v_c = kv[..., :MLA_NOPE], kv[..., MLA_NOPE:]
    k_rope = rotary(c_kr[:, :, None, :], positions)
    k_c = jnp.concatenate([k_nope, jnp.broadcast_to(k_rope, (B, S, MLA_HEADS, MLA_ROPE))], axis=-1)
    y_c = causal_block_attention(q_c, k_c, v_c)

    branches = jnp.stack([y_a, y_b, y_c], axis=2)
    branch_d = jnp.einsum('bsnw,nwd->bsnd', branches, w_branch)
    gates = jax.nn.sigmoid(gate_logits.reshape(B, S, N_BRANCH, D_MODEL))
    merged = jnp.sum(gates * branch_d, axis=2)
    return merged @ w_out


def swiglu(h, w_gate, w_up, w_down):
    return (jax.nn.silu(h @ w_gate) * (h @ w_up)) @ w_down


def moe_swiglu(h, w_router, w_gate, w_up, w_down):
    B, S, D = h.shape
    T = B * S
    hf = h.reshape(T, D)
    logits = jnp.dot(hf, w_router, preferred_element_type=jnp.float32)
    top_logit, top_idx = lax.top_k(logits, TOP_K)
    top_w = jax.nn.softmax(top_logit, axis=-1)
    flat_e = top_idx.reshape(-1)
    flat_tok = jnp.repeat(jnp.arange(T, dtype=jnp.int32), TOP_K)
    flat_w = top_w.reshape(-1)
    order = jnp.argsort(flat_e)
    e_sorted, tok_sorted, w_sorted = flat_e[order], flat_tok[order], flat_w[order]
    counts = jnp.bincount(flat_e, length=N_EXPERTS)
    padded = (counts + MOE_BLOCK - 1) // MOE_BLOCK * MOE_BLOCK
    start = jnp.cumsum(counts) - counts
    pad_end = jnp.cumsum(padded)
    pad_start = pad_end - padded
    dest = pad_start[e_sorted] + jnp.arange(T * TOP_K) - start[e_sorted]
    n_rows = -(-(T * TOP_K + N_EXPERTS * (MOE_BLOCK - 1)) // MOE_BLOCK) * MOE_BLOCK
    n_blocks = n_rows // MOE_BLOCK
    row_tok = jnp.zeros((n_rows,), jnp.int32).at[dest].set(tok_sorted)
    row_w = jnp.zeros((n_rows,), jnp.float32).at[dest].set(w_sorted)
    block_e = jnp.minimum(
        jnp.sum(jnp.arange(n_blocks)[:, None] * MOE_BLOCK >= pad_end[None, :], axis=1),
        N_EXPERTS - 1)
    xb = hf[row_tok].reshape(n_blocks, MOE_BLOCK, D)

    def expert_block(args):
        x_blk, e = args
        return (jax.nn.silu(x_blk @ w_gate[e]) * (x_blk @ w_up[e])) @ w_down[e]

    yb = lax.map(expert_block, (xb, block_e)).reshape(n_rows, D)
    y = jnp.zeros((T, D), h.dtype).at[row_tok].add(yb * row_w[:, None].astype(h.dtype))
    return y.reshape(B, S, D)


def setup_inputs(seed: int = 0) -> dict:
    key = jax.random.key(seed)
    ks = iter(jax.random.split(key, 40))
    f32 = jnp.float32

    def nrm(shape, scale):
        return jax.random.normal(next(ks), shape, f32) * scale

    def gain(shape):
        return 1.0 + 0.02 * jax.random.normal(next(ks), shape, f32)

    x = nrm((BATCH, SEQ, D_MODEL), 1.0)
    p = nrm((DEPTH, BATCH, SEQ, PLE_DIM), 1.0)
    offset = jax.random.randint(next(ks), (BATCH, 1), 0, 1024, dtype=jnp.int32)
    positions = offset + jnp.arange(SEQ, dtype=jnp.int32)[None, :]
    norm_mix = gain((DEPTH, D_MODEL))
    w_in = nrm((DEPTH, D_MODEL, IN_COLS), D_MODEL ** -0.5)
    conv_w = nrm((DEPTH, DN_CONV, 3 * DN_WIDTH), DN_CONV ** -0.5)
    dn_a_log = jnp.log(jax.random.uniform(next(ks), (DEPTH, DN_HEADS), f32, 1.0, 16.0))
    dt = jnp.exp(jax.random.uniform(next(ks), (DEPTH, DN_HEADS), f32,
                                    float(np.log(1e-3)), float(np.log(1e-1))))
    dn_dt_bias = dt + jnp.log(-jnp.expm1(-dt))
    dn_norm = gain((DEPTH, DN_HEAD_DIM))
    swa_sinks = nrm((DEPTH, SWA_Q_HEADS), 1.0)
    mla_q_norm = gain((DEPTH, MLA_Q_LORA))
    w_uq = nrm((DEPTH, MLA_Q_LORA, MLA_HEADS * (MLA_NOPE + MLA_ROPE)), MLA_Q_LORA ** -0.5)
    mla_kv_norm = gain((DEPTH, MLA_KV_LORA))
    w_ukv = nrm((DEPTH, MLA_KV_LORA, MLA_HEADS * (MLA_NOPE + MLA_V)), MLA_KV_LORA ** -0.5)
    w_branch = nrm((DEPTH, N_BRANCH, BRANCH_WIDTH, D_MODEL), BRANCH_WIDTH ** -0.5)
    w_out = nrm((DEPTH, D_MODEL, D_MODEL), D_MODEL ** -0.5)
    norm_ffn = gain((DEPTH, D_MODEL))
    w_ffn_gate = nrm((N_DENSE, D_MODEL, D_FF), D_MODEL ** -0.5)
    w_ffn_up = nrm((N_DENSE, D_MODEL, D_FF), D_MODEL ** -0.5)
    w_ffn_down = nrm((N_DENSE, D_FF, D_MODEL), D_FF ** -0.5)
    w_router = nrm((N_MOE, D_MODEL, N_EXPERTS), D_MODEL ** -0.5)
    w_exp_gate = nrm((N_MOE, N_EXPERTS, D_MODEL, D_FF_EXPERT), D_MODEL ** -0.5)
    w_exp_up = nrm((N_MOE, N_EXPERTS, D_MODEL, D_FF_EXPERT), D_MODEL ** -0.5)
    w_exp_down = nrm((N_MOE, N_EXPERTS, D_FF_EXPERT, D_MODEL), D_FF_EXPERT ** -0.5)
    norm_ple = gain((DEPTH, D_MODEL))
    w_ple_gate = nrm((DEPTH, D_MODEL, D_MODEL), D_MODEL ** -0.5)
    w_ple_proj = nrm((DEPTH, PLE_DIM, D_MODEL), PLE_DIM ** -0.5)
    final_norm = gain((D_MODEL,))
    return {
        "x": x, "p": p, "positions": positions,
        "norm_mix": norm_mix, "w_in": w_in, "conv_w": conv_w,
        "dn_a_log": dn_a_log, "dn_dt_bias": dn_dt_bias, "dn_norm": dn_norm,
        "swa_sinks": swa_sinks, "mla_q_norm": mla_q_norm, "w_uq": w_uq,
        "mla_kv_norm": mla_kv_norm, "w_ukv": w_ukv, "w_branch": w_branch, "w_out": w_out,
        "norm_ffn": norm_ffn, "w_ffn_gate": w_ffn_gate, "w_ffn_up": w_ffn_up,
        "w_ffn_down": w_ffn_down, "w_router": w_router, "w_exp_gate": w_exp_gate,
        "w_exp_up": w_exp_up, "w_exp_down": w_exp_down, "norm_ple": norm_ple,
        "w_ple_gate": w_ple_gate, "w_ple_proj": w_ple_proj, "final_norm": final_norm,
    }


def reference(x, p, positions, norm_mix, w_in, conv_w, dn_a_log, dn_dt_bias, dn_norm,
              swa_sinks, mla_q_norm, w_uq, mla_kv_norm, w_ukv, w_branch, w_out,
              norm_ffn, w_ffn_gate, w_ffn_up, w_ffn_down, w_router, w_exp_gate,
              w_exp_up, w_exp_down, norm_ple, w_ple_gate, w_ple_proj, final_norm):
    for i in range(DEPTH):
        h = rms_norm(x, norm_mix[i])
        x = x + hybrid_mixer(h, positions, w_in[i], conv_w[i], dn_a_log[i], dn_dt_bias[i],
                             dn_norm[i], swa_sinks[i], mla_q_norm[i], w_uq[i],
                             mla_kv_norm[i], w_ukv[i], w_branch[i], w_out[i])
        h = rms_norm(x, norm_ffn[i])
        j = i // 2
        if i % 2 == 0:
            x = x + swiglu(h, w_ffn_gate[j], w_ffn_up[j], w_ffn_down[j])
        else:
            x = x + moe_swiglu(h, w_router[j], w_exp_gate[j], w_exp_up[j], w_exp_down[j])
        hp = rms_norm(x, norm_ple[i])
        x = x + (p[i] @ w_ple_proj[i]) * jax.nn.sigmoid(hp @ w_ple_gate[i])
    return rms_norm(x, final_norm)
```

```python
import contextlib
import numpy as np
import concourse.bass as bass
import concourse.mybir as mybir
from concourse.bass_utils import run_bass_kernel_spmd

F32 = mybir.dt.float32
BF16 = mybir.dt.bfloat16
I32 = mybir.dt.int32
ALU = mybir.AluOpType
AF = mybir.ActivationFunctionType
AX = mybir.AxisListType

D = 2048
KD = D // 128
IN_COLS = 12880
EPS = 1e-6

C_AQ, C_AK, C_AV = 0, 1024, 1280
C_BQKV, C_BZ, C_BB, C_BD = 1536, 4608, 5632, 5640
C_CQ, C_CKV, C_CKR, C_G = 5648, 6160, 6672, 6736


class Op:
    __slots__ = ("eng", "fn", "reads", "writes", "dma", "deps", "signal", "val", "sem", "lhs")

    def __init__(self, eng, fn, reads, writes, dma):
        self.eng, self.fn, self.reads, self.writes, self.dma = eng, fn, reads, writes, dma
        self.deps = ()
        self.signal = False
        self.val = 0
        self.sem = None
        self.lhs = None


def _norm(k):
    if isinstance(k, tuple):
        return tuple(_norm(x) for x in k)
    if isinstance(k, (str, int)):
        return k
    return "@" + k.name


class Prog:
    COMPUTE = ("pe", "act", "dve", "pool")

    def __init__(self, nc, stack):
        self.nc = nc
        self.stack = stack
        self.gstack = stack
        self.ops = []
        self.last_w = {}
        self.readers = {}
        self.n_alloc = 0

    def sb(self, shape, dtype, name=None):
        self.n_alloc += 1
        return self.stack.enter_context(self.nc.sbuf_tensor((name or "sb") + f"_{self.n_alloc}", list(shape), dtype))

    def ps(self, shape, dtype=F32, name=None):
        self.n_alloc += 1
        return self.stack.enter_context(self.nc.psum_tensor(name or f"ps{self.n_alloc}", list(shape), dtype))

    def dram(self, shape, dtype, name=None):
        self.n_alloc += 1
        return self.nc.dram_tensor(name or f"dr{self.n_alloc}", list(shape), dtype).ap()

    def _add(self, eng, fn, reads, writes, dma):
        rr = [_norm(r) for r in reads]
        ww = [_norm(w) for w in writes]
        ww += [r for r in rr if isinstance(r, tuple) and r[0] == "bank"]
        rr = [r for r in rr if not (isinstance(r, tuple) and r[0] == "bank")]
        op = Op(eng, fn, tuple(rr), tuple(ww), dma)
        deps = set()
        me = len(self.ops)
        for r in op.reads:
            w = self.last_w.get(r)
            if w is not None:
                deps.add((w, True))
        for w_ in op.writes:
            w = self.last_w.get(w_)
            if w is not None:
                deps.add((w, False))
            for rd in self.readers.get(w_, ()):
                deps.add((rd, False))
        keep = set()
        for (d, raw) in deps:
            dop = self.ops[d]
            if not dop.dma and dop.eng == eng and not dma:
                if eng == "pe" or not raw:
                    continue
            keep.add(d)
        op.deps = tuple(sorted(keep))
        if eng == "pe" and op.reads:
            lw = self.last_w.get(op.reads[0])
            if lw is not None and lw in keep:
                op.lhs = lw
        for d in op.deps:
            self.ops[d].signal = True
        for w_ in op.writes:
            self.last_w[w_] = me
            self.readers[w_] = []
        for r in op.reads:
            lst = self.readers.setdefault(r, [])
            if not dma:
                lst[:] = [x for x in lst if self.ops[x].dma or self.ops[x].eng != eng]
            lst.append(me)
        self.ops.append(op)
        return op

    def pe(self, fn, reads=(), writes=()):
        return self._add("pe", fn, reads, writes, False)

    def act(self, fn, reads=(), writes=()):
        return self._add("act", fn, reads, writes, False)

    def dve(self, fn, reads=(), writes=()):
        return self._add("dve", fn, reads, writes, False)

    def pool(self, fn, reads=(), writes=()):
        return self._add("pool", fn, reads, writes, False)

    def indirect(self, fn, reads=(), writes=()):
        op = self._add("pool", fn, reads, writes, True)
        op.lhs = "ind"
        return op

    def dma(self, out, in_, reads=(), writes=(), q="sp", slow=False):
        if slow:
            return self._add(q, lambda e: e.dma_start(out=out, in_=in_, allow_slow_non_contiguous=True), reads, writes, True)
        return self._add(q, lambda e: e.dma_start(out=out, in_=in_), reads, writes, True)

    def barrier(self):
        last = {}
        for i in range(len(self.ops) - 1, -1, -1):
            op = self.ops[i]
            if op.eng == "bar":
                break
            if not op.dma and op.eng not in last:
                last[op.eng] = i
                op.signal = True
            if len(last) == 4:
                break
        b = Op("bar", None, (), (), False)
        b.deps = tuple(last.values())
        self.ops.append(b)
        self.last_w = {}
        self.readers = {}

    @contextlib.contextmanager
    def phase(self):
        st = contextlib.ExitStack()
        old = self.stack
        self.stack = st
        try:
            yield
        finally:
            self.barrier()
            self.flush()
            st.close()
            self.stack = old

    def _init_emit(self, n_dma_sems=40):
        nc = self.nc
        self.engs = {"pe": nc.tensor, "act": nc.scalar, "dve": nc.vector, "pool": nc.gpsimd, "sp": nc.sync}
        st = self.gstack
        self.esem = {e: st.enter_context(nc.semaphore(f"s_{e}")) for e in self.COMPUTE}
        self.ecnt = {e: 0 for e in self.COMPUTE}
        self.dsem = [st.enter_context(nc.semaphore(f"s_d{i}")) for i in range(n_dma_sems)]
        self.dcnt = [0] * n_dma_sems
        self.dnext = 0
        self.waited = {}
        self.emitted = 0
        self.ind_hist = []

    def _wait(self, eng_name, sem, key, val):
        if self.waited.get((eng_name, key), 0) >= val:
            return
        self.waited[(eng_name, key)] = val
        self.engs[eng_name].wait_ge(sem, val)

    def flush(self):
        if not hasattr(self, "engs"):
            self._init_emit()
        engs, esem, ecnt, dsem, dcnt = self.engs, self.esem, self.ecnt, self.dsem, self.dcnt
        nd = len(dsem)
        for idx in range(self.emitted, len(self.ops)):
            op = self.ops[idx]
            en = op.eng
            if en == "bar":
                for q in ("pe", "act", "dve", "pool", "sp"):
                    for d in op.deps:
                        dop = self.ops[d]
                        if dop.eng != q:
                            self._wait(q, dop.sem[1], dop.sem[0], dop.val)
                    for k in range(nd):
                        if dcnt[k] > 0:
                            self._wait(q, dsem[k], ("d", k), 16 * dcnt[k])
                continue
            need = {}
            for d in op.deps:
                dop = self.ops[d]
                key, sem = dop.sem
                if key not in need or need[key][1] < dop.val:
                    need[key] = (sem, dop.val)
            for key, (sem, val) in need.items():
                self._wait(en, sem, key, val)
            if op.dma:
                if op.lhs == "ind":
                    self.ind_hist.append(None)
                    if len(self.ind_hist) > 6 and self.ind_hist[-7] is not None:
                        ksem, kkey, kval = self.ind_hist[-7]
                        self._wait(en, ksem, kkey, kval)
                k = self.dnext
                self.dnext = (self.dnext + 1) % nd
                if dcnt[k] > 0:
                    self._wait(en, dsem[k], ("d", k), 16 * dcnt[k])
                ins = op.fn(engs[en])
                dcnt[k] += 1
                ins.then_inc(dsem[k], 16)
                op.sem = (("d", k), dsem[k])
                op.val = 16 * dcnt[k]
                if op.lhs == "ind":
                    self.ind_hist[-1] = (dsem[k], ("d", k), op.val)
            else:
                ins = op.fn(engs[en])
                if op.lhs is not None:
                    dop = self.ops[op.lhs]
                    ins._wait_ge(dop.sem[1], dop.val)
                if op.signal:
                    ecnt[en] += 1
                    ins.then_inc(esem[en], 1)
                    op.sem = (("e", en), esem[en])
                    op.val = ecnt[en]
            op.fn = None
        self.emitted = len(self.ops)

    def finish(self):
        self.barrier()
        self.flush()
        self.counts = dict(self.ecnt)


def host_consts():
    c = {}
    c["ident_in"] = np.eye(128, dtype=np.float32)
    k = np.arange(128)[:, None]
    q = np.arange(128)[None, :]
    cur = (k <= q).astype(np.float32)
    prev = (k > q).astype(np.float32)
    c["m_swa"] = np.ascontiguousarray(np.stack([np.tile(prev, (1, 4)), np.tile(cur, (1, 4))]))
    q5 = np.arange(512)[None, :]
    c["m_mla"] = np.ascontiguousarray(np.stack([((128 * jj + k) <= q5).astype(np.float32) for jj in range(4)]))
    rot = np.zeros((64, 64), np.float32)
    for m in range(32):
        rot[m + 32, m] = -1.0
    for m in range(32, 64):
        rot[m - 32, m] = 1.0
    c["rot_in"] = rot
    c["tri_in"] = np.ascontiguousarray(np.stack([cur, prev]))
    sel = np.zeros((8, 8, 128), np.float32)
    for e_ in range(8):
        sel[e_, e_, :] = 1.0
    c["sel_in"] = sel
    c["tokid_in"] = (np.arange(64)[None, :] * 128 + np.arange(128)[:, None]).astype(np.float32)
    c["inv_freq"] = (10000.0 ** (-(np.arange(64) % 32) / 32.0)).astype(np.float32)[:, None]
    return c


def build(S, n_layers=2, dbg=None, phases=("p1", "swa", "mla", "dn", "p3")):
    dbg = dbg or {}
    nc = bass.Bass("TRN2", target_bir_lowering=False)
    NG = S // 512
    NT = S // 128
    stack = contextlib.ExitStack()
    P = Prog(nc, stack)
    L = n_layers

    def ext_in(name, shape, dt=F32):
        return nc.dram_tensor(name, list(shape), dt, kind="ExternalInput").ap()

    def ext_out(name, shape, dt=F32):
        return nc.dram_tensor(name, list(shape), dt, kind="ExternalOutput").ap()

    x_in = ext_in("x", [S, D])
    pos_in = ext_in("positions", [1, S], I32)
    ident_in = ext_in("ident_in", [128, 128])
    m_swa_in = ext_in("m_swa", [2, 128, 512])
    m_mla_in = ext_in("m_mla", [4, 128, 512])
    rot_in = ext_in("rot_in", [64, 64])
    inv_freq_in = ext_in("inv_freq", [64, 1])
    norm_mix = ext_in("norm_mix", [L, D])
    w_in = ext_in("w_in", [L, D, IN_COLS])
    swa_sinks = ext_in("swa_sinks", [L, 16])
    mla_q_norm = ext_in("mla_q_norm", [L, 512])
    mla_kv_norm = ext_in("mla_kv_norm", [L, 512])
    w_uq = ext_in("w_uq", [L, 512, 1536])
    w_ukv = ext_in("w_ukv", [L, 512, 2048])
    tri_in = ext_in("tri_in", [2, 128, 128])
    sel_in = ext_in("sel_in", [8, 8, 128])
    tokid_in = ext_in("tokid_in", [128, 64])
    p_in = ext_in("p", [L, S, 256])
    w_branch = ext_in("w_branch", [L, 3, 1024, D])
    w_out = ext_in("w_out", [L, D, D])
    norm_ffn = ext_in("norm_ffn", [L, D])
    w_ffn_gate = ext_in("w_ffn_gate", [1, D, 7168])
    w_ffn_up = ext_in("w_ffn_up", [1, D, 7168])
    w_ffn_down = ext_in("w_ffn_down", [1, 7168, D])
    norm_ple = ext_in("norm_ple", [L, D])
    w_ple_gate = ext_in("w_ple_gate", [L, D, D])
    w_ple_proj = ext_in("w_ple_proj", [L, 256, D])
    final_norm = ext_in("final_norm", [1, D])
    if L > 1 or "moe" in phases:
        w_router = ext_in("w_router", [1, D, 8])
        w_exp_gate = ext_in("w_exp_gate", [1, 8, D, 7168])
        w_exp_up = ext_in("w_exp_up", [1, 8, D, 7168])
        w_exp_down = ext_in("w_exp_down", [1, 8, 7168, D])
    out_ap = ext_out("out", [S, D])
    conv_w = ext_in("conv_w", [L, 4, 3072])
    dn_a_log = ext_in("dn_a_log", [L, 8])
    dn_dt_bias = ext_in("dn_dt_bias", [L, 8])
    dn_norm = ext_in("dn_norm", [L, 128])

    bank = [P.ps([128, 512], F32, f"bank{i}") for i in range(8)]

    ident = P.sb([128, 128], F32, "ident")
    P.dma(ident[:], ident_in[:, :], writes=[ident])
    eps_t = P.sb([128, 1], F32, "eps_t")
    P.pool(lambda e: e.memset(eps_t[:], EPS), writes=[eps_t])
    ones_bf = P.sb([128, 128], BF16, "ones_bf")
    P.pool(lambda e: e.memset(ones_bf[:], 1.0), writes=[ones_bf])
    m_swa = P.sb([128, 2, 512], BF16, "m_swa_t")
    m_mla = P.sb([128, 4, 512], BF16, "m_mla_t")
    for i in range(2):
        P.dma(m_swa[:, i, :], m_swa_in[i], writes=[m_swa], q="pool")
    for i in range(4):
        P.dma(m_mla[:, i, :], m_mla_in[i], writes=[m_mla], q="pool")
    rot = P.sb([64, 64], F32, "rot_t")
    P.dma(rot[:], rot_in[:, :], writes=[rot])
    inv_freq = P.sb([64, 1], F32, "inv_freq_t")
    P.dma(inv_freq[:], inv_freq_in[:, :], writes=[inv_freq])

    xT = P.dram([D, S], F32, "xT")
    projT = P.dram([C_G, S], F32, "projT")
    gatesT = P.dram([3 * D, S], BF16, "gatesT")
    vA = P.dram([S, 256], BF16, "vA")
    bd = P.dram([S, 16], F32, "bd")
    yT = P.dram([3 * 1024, S], BF16, "yT")
    cosT = P.dram([64, S], F32, "cosT")
    sinT = P.dram([64, S], F32, "sinT")
    qnT = P.dram([1024, S], BF16, "qnT")
    qrT = P.dram([512, S], BF16, "qrT")
    knT = P.dram([1024, S], BF16, "knT")
    krT = P.dram([64, S], BF16, "krT")
    vC = P.dram([S, 1024], BF16, "vC")
    w_in_bf = [P.dram([D, IN_COLS], BF16, f"w_in_bf{l}") for l in range(L)]
    w_branch_bf = [P.dram([3072, D], BF16, f"w_branch_bf{l}") for l in range(L)]
    w_out_bf = [P.dram([D, D], BF16, f"w_out_bf{l}") for l in range(L)]
    w_pg_bf = [P.dram([D, D], BF16, f"w_pg_bf{l}") for l in range(L)]
    w_pp_bf = [P.dram([256, D], BF16, f"w_pp_bf{l}") for l in range(L)]
    w_ffn_bf = [P.dram([D, 7168], BF16, "w_ffn_bf_g"), P.dram([D, 7168], BF16, "w_ffn_bf_u"), P.dram([7168, D], BF16, "w_ffn_bf_d")]
    if L > 1 or "moe" in phases:
        w_exp_bf = [P.dram([8, D, 7168], BF16, "w_exp_bf_g"), P.dram([8, D, 7168], BF16, "w_exp_bf_u"), P.dram([8, 7168, D], BF16, "w_exp_bf_d")]
    pT = [P.dram([256, S], BF16, f"pT{l}") for l in range(L)]

    ALLG = list(range(NG))

    def keys(name, gs=None):
        return [(name, g) for g in (ALLG if gs is None else gs)]

    def cast(dst, src, rows, key, step=256):
        for r in range(0, rows, step):
            P.dma(dst[r:r + step, :], src[r:r + step, :], writes=[key], q="pool")

    def casts_layer(l):
        cast(w_in_bf[l], w_in[l], D, ("w_in_bf", l))
        if "p3" in phases:
            cast(w_branch_bf[l], w_branch[l].rearrange("n k d -> (n k) d"), 3072, ("w_branch_bf", l), 512)
            cast(w_out_bf[l], w_out[l], D, ("w_out_bf", l), 512)
            if l == 0:
                cast(w_ffn_bf[0], w_ffn_gate[0], D, "w_ffn_bf")
                cast(w_ffn_bf[1], w_ffn_up[0], D, "w_ffn_bf")
                cast(w_ffn_bf[2], w_ffn_down[0], 7168, "w_ffn_bf", 512)
            else:
                for ex in range(8):
                    cast(w_exp_bf[0][ex], w_exp_gate[0, ex], D, "w_exp_bf")
                    cast(w_exp_bf[1][ex], w_exp_up[0, ex], D, "w_exp_bf")
                    cast(w_exp_bf[2][ex], w_exp_down[0, ex], 7168, "w_exp_bf", 512)
            cast(w_pg_bf[l], w_ple_gate[l], D, ("w_pg_bf", l), 512)
            cast(w_pp_bf[l], w_ple_proj[l], 256, ("w_pp_bf", l))

    with P.phase():
        xin4 = [P.sb([128, D], F32, f"xin4_{i}") for i in range(4)]
        tp_sb = [P.sb([128, 512], F32, f"tp_sb{i}") for i in range(2)]
        for g in range(NG):
            for t in range(4):
                tok0 = g * 512 + t * 128
                P.dma(xin4[t][:], x_in[tok0:tok0 + 128, :], writes=[xin4[t]])
            for k in range(KD):
                pp = bank[k % 2]
                sbuf = tp_sb[k % 2]
                for t in range(4):
                    P.pe(lambda e, pp=pp, t=t, k=k: e.transpose(pp[:, t * 128:(t + 1) * 128], xin4[t][:, k * 128:(k + 1) * 128], ident[:]),
                         reads=[xin4[t], ident], writes=[pp])
                P.act(lambda e, pp=pp, sbuf=sbuf: e.copy(out=sbuf[:], in_=pp[:]), reads=[pp], writes=[sbuf])
                P.dma(xT[k * 128:(k + 1) * 128, g * 512:(g + 1) * 512], sbuf[:], reads=[sbuf], writes=[("xT", g)], q="pool")
        if "p3" in phases:
            pin = [P.sb([128, 256], F32, f"pin{i}") for i in range(4)]
            psb = [P.sb([128, 512], BF16, f"psb{i}") for i in range(2)]
            np_ = 0
            for l in range(L):
                for g in range(NG):
                    for t in range(4):
                        tok0 = g * 512 + t * 128
                        P.dma(pin[t][:], p_in[l, tok0:tok0 + 128, :], writes=[pin[t]])
                    for kk in range(2):
                        pp = bank[2 + np_ % 2]
                        sbuf = psb[np_ % 2]
                        np_ += 1
                        for t in range(4):
                            P.pe(lambda e, pp=pp, t=t, kk=kk: e.transpose(pp[:, t * 128:(t + 1) * 128], pin[t][:, kk * 128:(kk + 1) * 128], ident[:]),
                                 reads=[pin[t], ident], writes=[pp])
                        P.act(lambda e, pp=pp, sbuf=sbuf: e.copy(out=sbuf[:], in_=pp[:]), reads=[pp], writes=[sbuf])
                        P.dma(pT[l][kk * 128:(kk + 1) * 128, g * 512:(g + 1) * 512], sbuf[:], reads=[sbuf], writes=[("pT", l)], q="pool")

    if "mla" in phases:
      with P.phase():
        pos_i = P.sb([64, 512], I32, "pos_i")
        ang = P.sb([64, 512], F32, "ang")
        ang2 = P.sb([64, 512], F32, "ang2")
        pos_k = P.sb([64, 512], I32, "pos_k")
        trig = [P.sb([64, 512], F32, f"trig{i}") for i in range(2)]
        negpi = P.sb([64, 1], F32, "negpi")
        P.pool(lambda e: e.memset(negpi[:], -float(np.pi)), writes=[negpi])
        TWO_PI = float(2 * np.pi)
        for g in range(NG):
            P.dma(pos_i[:], pos_in[0:1, g * 512:(g + 1) * 512].partition_broadcast(64), writes=[pos_i], slow=True)
            P.dve(lambda e: e.tensor_copy(out=ang[:], in_=pos_i[:]), reads=[pos_i], writes=[ang])
            P.dve(lambda e: e.tensor_scalar(out=ang[:], in0=ang[:], scalar1=inv_freq[:, 0:1], scalar2=1.0, op0=ALU.mult, op1=ALU.mult),
                  reads=[ang, inv_freq], writes=[ang])
            for which, shift, dst_d in ((0, 0.0, sinT), (1, float(np.pi / 2), cosT)):
                tr = trig[which]
                P.dve(lambda e, shift=shift: e.tensor_scalar(out=ang2[:], in0=ang[:], scalar1=shift, scalar2=float(1.0 / (2 * np.pi)), op0=ALU.add, op1=ALU.mult),
                      reads=[ang], writes=[ang2])
                P.dve(lambda e: e.tensor_copy(out=pos_k[:], in_=ang2[:]), reads=[ang2], writes=[pos_k])
                P.dve(lambda e: e.tensor_copy(out=ang2[:], in_=pos_k[:]), reads=[pos_k], writes=[ang2])
                P.dve(lambda e, tr=tr, shift=shift: e.tensor_scalar(out=tr[:], in0=ang[:], scalar1=shift, scalar2=1.0, op0=ALU.add, op1=ALU.mult),
                      reads=[ang], writes=[tr])
                P.dve(lambda e, tr=tr: e.scalar_tensor_tensor(out=tr[:], in0=ang2[:], scalar=-6.28125, in1=tr[:], op0=ALU.mult, op1=ALU.add),
                      reads=[ang2, tr], writes=[tr])
                P.dve(lambda e, tr=tr: e.scalar_tensor_tensor(out=tr[:], in0=ang2[:], scalar=-0.0019353071795864769, in1=tr[:], op0=ALU.mult, op1=ALU.add),
                      reads=[ang2, tr], writes=[tr])
                P.dve(lambda e, tr=tr: e.tensor_scalar(out=ang2[:], in0=tr[:], scalar1=float(np.pi), scalar2=TWO_PI, op0=ALU.is_gt, op1=ALU.mult),
                      reads=[tr], writes=[ang2])
                P.dve(lambda e, tr=tr: e.tensor_tensor(out=tr[:], in0=tr[:], in1=ang2[:], op=ALU.subtract), reads=[tr, ang2], writes=[tr])
                P.dve(lambda e, tr=tr: e.tensor_scalar(out=ang2[:], in0=tr[:], scalar1=-float(np.pi), scalar2=TWO_PI, op0=ALU.is_lt, op1=ALU.mult),
                      reads=[tr], writes=[ang2])
                P.dve(lambda e, tr=tr: e.tensor_tensor(out=tr[:], in0=tr[:], in1=ang2[:], op=ALU.add), reads=[tr, ang2], writes=[tr])
                P.dve(lambda e, tr=tr: e.tensor_scalar(out=tr[:], in0=tr[:], scalar1=float(np.pi), scalar2=-float(np.pi), op0=ALU.min, op1=ALU.max),
                      reads=[tr], writes=[tr])
                P.act(lambda e, tr=tr: e.activation(out=tr[:], in_=tr[:], func=AF.Sin), reads=[tr], writes=[tr])
                P.dma(dst_d[:, g * 512:(g + 1) * 512], tr[:], reads=[tr], writes=[("sinT" if which == 0 else "cosT", g)], q="pool")

    gain_mix = P.sb([128, L, KD], F32, "gain_mix")
    gain_q = P.sb([128, L, 4], F32, "gain_q")
    gain_kv = P.sb([128, L, 4], F32, "gain_kv")
    esink = P.sb([64, L, 16], F32, "esink")
    gain_ffn = P.sb([128, L, KD], F32, "gain_ffn")
    gain_ple = P.sb([128, L, KD], F32, "gain_ple")
    gain_fin = P.sb([128, KD], F32, "gain_fin")
    Wall = P.sb([128, NT, 8], F32, "Wall")
    Iall = P.sb([128, NT, 8], I32, "Iall")
    P.dma(gain_fin[:, :], final_norm[0].rearrange("(k p) -> p k", p=128), writes=[gain_fin], slow=True)
    for l in range(L):
        P.dma(gain_mix[:, l, :], norm_mix[l].rearrange("(k p) -> p k", p=128), writes=[gain_mix], slow=True)
        P.dma(gain_ffn[:, l, :], norm_ffn[l].rearrange("(k p) -> p k", p=128), writes=[gain_ffn], slow=True)
        P.dma(gain_ple[:, l, :], norm_ple[l].rearrange("(k p) -> p k", p=128), writes=[gain_ple], slow=True)
        P.dma(gain_q[:, l, :], mla_q_norm[l].rearrange("(k p) -> p k", p=128), writes=[gain_q], slow=True)
        P.dma(gain_kv[:, l, :], mla_kv_norm[l].rearrange("(k p) -> p k", p=128), writes=[gain_kv], slow=True)
        P.dma(esink[:, l, :], swa_sinks[l:l + 1, :].partition_broadcast(64), writes=[esink], slow=True)
    P.act(lambda e: e.activation(out=esink[:], in_=esink[:], func=AF.Exp), reads=[esink], writes=[esink])

    def rms_T(src, dst, nk, dim, gain, gkey, ss_bank, sq, inv):
        for k in range(nk):
            P.act(lambda e, k=k: e.activation(out=sq[:, k, :], in_=src[:, k, :], func=AF.Square), reads=[(src, k)], writes=[(sq, k)])
        for k in range(nk):
            P.pe(lambda e, k=k: e.matmul(ss_bank[:], lhsT=ones_bf[:], rhs=sq[:, k, :], start=(k == 0), stop=(k == nk - 1)),
                 reads=[ones_bf, (sq, k)], writes=[ss_bank])
        P.act(lambda e: e.activation(out=inv[:], in_=ss_bank[:], func=AF.Sqrt, scale=1.0 / dim, bias=eps_t[:, 0:1]),
              reads=[ss_bank, eps_t], writes=[inv])
        P.dve(lambda e: e.reciprocal(out=inv[:], in_=inv[:]), reads=[inv], writes=[inv])
        for k in range(nk):
            P.dve(lambda e, k=k: e.scalar_tensor_tensor(out=dst[:, k, :], in0=src[:, k, :], scalar=gain[:, k:k + 1], in1=inv[:],
                                                        op0=ALU.mult, op1=ALU.mult),
                  reads=[(src, k), inv, gkey], writes=[(dst, k)])

    blocks = [(0, 512, "f"), (512, 512, "f"), (C_AK, 256, "f"), (C_AV, 256, "t")] + \
             [(c, 512, "f") for c in range(C_BQKV, C_BB, 512)] + \
             [(C_BB, 16, "t"), (C_CQ, 512, "f"), (C_CKV, 512, "f"), (C_CKR, 64, "f")] + \
             [(c, 512, "f") for c in range(C_G, IN_COLS, 512)]

    cnt = {"blk": 0, "ev": 0}

    def p1(l):
        xg = P.sb([128, KD, 512], F32, "xg")
        sq = P.sb([128, KD, 512], BF16, "sq")
        hT = P.sb([128, KD, 512], BF16, "hT")
        inv = P.sb([128, 512], F32, "inv")
        wblk = [P.sb([128, KD, 512], BF16, f"wblk{i}") for i in range(2)]
        ev_sb = [P.sb([128, 512], F32, f"ev_sb{i}") for i in range(3)]
        evb_sb = [P.sb([128, 512], BF16, f"evb_sb{i}") for i in range(3)]
        for g in range(NG):
            for k in range(KD):
                P.dma(xg[:, k, :], xT[k * 128:(k + 1) * 128, g * 512:(g + 1) * 512], reads=[("xT", g)], writes=[(xg, k)])
            rms_T(xg, hT, KD, D, gain_mix[:, l, :], gain_mix, bank[2], sq, inv)
            for (c0, wd, mode) in blocks:
                wb = wblk[cnt["blk"] % 2]
                cnt["blk"] += 1
                for k in range(KD):
                    P.dma(wb[:, k, 0:wd], w_in_bf[l][k * 128:(k + 1) * 128, c0:c0 + wd], reads=[("w_in_bf", l)], writes=[wb])
                if mode == "t":
                    for t in range(4):
                        pp = bank[cnt["ev"] % 2]
                        for k in range(KD):
                            P.pe(lambda e, pp=pp, wb=wb, k=k, t=t, wd=wd: e.matmul(pp[:, 0:wd], lhsT=hT[:, k, t * 128:(t + 1) * 128], rhs=wb[:, k, 0:wd],
                                                                                     start=(k == 0), stop=(k == KD - 1)),
                                 reads=[(hT, k), wb], writes=[pp])
                        tok0 = g * 512 + t * 128
                        if c0 == C_AV:
                            ob = evb_sb[cnt["ev"] % 3]
                            P.act(lambda e, pp=pp, ob=ob, wd=wd: e.copy(out=ob[:, 0:wd], in_=pp[:, 0:wd]), reads=[pp], writes=[ob])
                            P.dma(vA[tok0:tok0 + 128, :], ob[:, 0:wd], reads=[ob], writes=[("vA", g)], q="pool")
                        else:
                            ob = ev_sb[cnt["ev"] % 3]
                            P.act(lambda e, pp=pp, ob=ob, wd=wd: e.copy(out=ob[:, 0:wd], in_=pp[:, 0:wd]), reads=[pp], writes=[ob])
                            P.dma(bd[tok0:tok0 + 128, :], ob[:, 0:wd], reads=[ob], writes=[("bd", g)], q="pool")
                        cnt["ev"] += 1
                    continue
                for j in range(0, wd, 128):
                    cw = min(128, wd - j)
                    pp = bank[cnt["ev"] % 2]
                    for k in range(KD):
                        P.pe(lambda e, pp=pp, wb=wb, k=k, j=j, cw=cw: e.matmul(pp[0:cw, :], lhsT=wb[:, k, j:j + cw], rhs=hT[:, k, :],
                                                                                start=(k == 0), stop=(k == KD - 1)),
                             reads=[wb, (hT, k)], writes=[pp])
                    col = c0 + j
                    if col >= C_G:
                        ob = evb_sb[cnt["ev"] % 3]
                        P.act(lambda e, pp=pp, ob=ob, cw=cw: e.activation(out=ob[0:cw, :], in_=pp[0:cw, :], func=AF.Sigmoid),
                              reads=[pp], writes=[ob])
                        P.dma(gatesT[col - C_G:col - C_G + cw, g * 512:(g + 1) * 512], ob[0:cw, :], reads=[ob],
                              writes=[("gatesT", g)], q="pool")
                    else:
                        ob = ev_sb[cnt["ev"] % 3]
                        P.act(lambda e, pp=pp, ob=ob, cw=cw: e.copy(out=ob[0:cw, :], in_=pp[0:cw, :]), reads=[pp], writes=[ob])
                        P.dma(projT[col:col + cw, g * 512:(g + 1) * 512], ob[0:cw, :], reads=[ob], writes=[("projT", g)], q="pool")
                    cnt["ev"] += 1

    def swa(l):
        swa_kT = P.sb([64, S], BF16, "swa_kT")
        swa_v = P.sb([128, NT, 64], BF16, "swa_v")
        swa_q = [P.sb([64, 4, 512], BF16, f"swa_q{i}") for i in range(2)]
        swa_p = [P.sb([128, 512], BF16, f"swa_p{i}") for i in range(4)]
        swa_den = P.sb([64, 512], F32, "swa_den")
        swa_y = [P.sb([64, 4, 512], BF16, f"swa_y{i}") for i in range(2)]

        npt = 0
        for hk in range(4):
            P.dma(swa_kT[:, :], projT[C_AK + hk * 64:C_AK + (hk + 1) * 64, :], reads=keys("projT"), writes=[swa_kT], q="pool")
            P.dma(swa_v[:, :, :], vA[:, hk * 64:(hk + 1) * 64].rearrange("(t p) d -> p t d", p=128), reads=keys("vA"), writes=[swa_v], slow=True)
            for g in range(NG):
                qt = swa_q[g % 2]
                yb = swa_y[g % 2]
                P.dma(qt[:, :, :], projT[hk * 256:(hk + 1) * 256, g * 512:(g + 1) * 512].rearrange("(i d) t -> d i t", d=64),
                      reads=[("projT", g)], writes=[qt], q="pool")
                for b in range(4):
                    n = g * 4 + b
                    o_ps = bank[4 + (n % 2)]
                    s_ps = bank[6 + (n % 2)]
                    jl = [j for j in (n - 1, n) if j >= 0]
                    for idx, j in enumerate(jl):
                        sc = bank[npt % 4]
                        pt = swa_p[npt % 4]
                        npt += 1
                        P.pe(lambda e, sc=sc, j=j, qt=qt, b=b: e.matmul(sc[:, :].rearrange("p (i t) -> p i t", i=4), lhsT=swa_kT[:, j * 128:(j + 1) * 128],
                                                                         rhs=qt[:, :, b * 128:(b + 1) * 128], start=True, stop=True),
                             reads=[swa_kT, qt], writes=[sc])
                        P.act(lambda e, sc=sc, pt=pt: e.activation(out=pt[:], in_=sc[:], func=AF.Exp, scale=0.125), reads=[sc], writes=[pt])
                        mi = 1 if j == n else 0
                        P.dve(lambda e, pt=pt, mi=mi: e.tensor_tensor(out=pt[:], in0=pt[:], in1=m_swa[:, mi, :], op=ALU.mult),
                              reads=[pt, m_swa], writes=[pt])
                        first, last = idx == 0, idx == len(jl) - 1
                        P.pe(lambda e, o_ps=o_ps, pt=pt, j=j, first=first, last=last: e.matmul(o_ps[0:64, :], lhsT=swa_v[:, j, :], rhs=pt[:], start=first, stop=last),
                             reads=[swa_v, pt], writes=[o_ps])
                        P.pe(lambda e, s_ps=s_ps, pt=pt, first=first, last=last: e.matmul(s_ps[0:64, :], lhsT=ones_bf[:, 0:64], rhs=pt[:], start=first, stop=last),
                             reads=[ones_bf, pt], writes=[s_ps])
                    for i in range(4):
                        h = hk * 4 + i
                        P.dve(lambda e, s_ps=s_ps, i=i, h=h: e.tensor_scalar(out=swa_den[:, i * 128:(i + 1) * 128], in0=s_ps[0:64, i * 128:(i + 1) * 128],
                                                                               scalar1=esink[:, l, h:h + 1], scalar2=1.0, op0=ALU.add, op1=ALU.mult),
                              reads=[s_ps, esink], writes=[swa_den])
                    P.dve(lambda e: e.reciprocal(out=swa_den[:], in_=swa_den[:]), reads=[swa_den], writes=[swa_den])
                    P.dve(lambda e, o_ps=o_ps, yb=yb, b=b: e.tensor_tensor(out=yb[:, :, b * 128:(b + 1) * 128],
                                                                           in0=o_ps[0:64, :].rearrange("p (i t) -> p i t", i=4),
                                                                           in1=swa_den[:, :].rearrange("p (i t) -> p i t", i=4), op=ALU.mult),
                          reads=[o_ps, swa_den], writes=[yb])
                P.dma(yT[hk * 256:(hk + 1) * 256, g * 512:(g + 1) * 512].rearrange("(i d) t -> d i t", d=64), yb[:, :, :],
                      reads=[yb], writes=[("yT_a", g)], q="pool")

    def mla1(l):
        wuq_sb = P.sb([128, 4, 1536], BF16, "wuq_sb")
        wukv_sb = P.sb([128, 4, 2048], BF16, "wukv_sb")
        lat = P.sb([128, 4, 512], F32, "lat")
        latn = [P.sb([128, 4, 512], BF16, f"latn{i}") for i in range(2)]
        sq = P.sb([128, 4, 512], BF16, "sq_m")
        inv = P.sb([128, 512], F32, "inv_m")
        rope_x = P.sb([64, 512], F32, "rope_x")
        rope_t = P.sb([64, 512], F32, "rope_t")
        rope_o = [P.sb([64, 512], BF16, f"rope_o{i}") for i in range(2)]
        cs_sb = P.sb([64, 2, 512], F32, "cs_sb")
        evb_sb = [P.sb([128, 512], BF16, f"evb_m{i}") for i in range(3)]
        def rope(src_ps_or_sb, src_key, nrows_dummy, dst, g):
            P.pe(lambda e: e.matmul(bank[3][0:64, :], lhsT=rot[:, :], rhs=rope_x[:, :], start=True, stop=True), reads=[rot, rope_x], writes=[bank[3]])
            P.dve(lambda e: e.tensor_tensor(out=rope_t[:], in0=bank[3][0:64, :], in1=cs_sb[:, 1, :], op=ALU.mult), reads=[bank[3], cs_sb], writes=[rope_t])
            P.dve(lambda e: e.tensor_tensor(out=rope_x[:], in0=rope_x[:], in1=cs_sb[:, 0, :], op=ALU.mult), reads=[rope_x, cs_sb], writes=[rope_x])
            P.dve(lambda e, dst=dst: e.tensor_tensor(out=dst[:], in0=rope_x[:], in1=rope_t[:], op=ALU.add), reads=[rope_x, rope_t], writes=[dst])

        for k in range(4):
            P.dma(wuq_sb[:, k, :], w_uq[l, k * 128:(k + 1) * 128, :], writes=[wuq_sb], q="pool")
            P.dma(wukv_sb[:, k, :], w_ukv[l, k * 128:(k + 1) * 128, :], writes=[wukv_sb], q="pool")
        nev = 0
        for g in range(NG):
            gs = slice(g * 512, (g + 1) * 512)
            P.dma(cs_sb[:, 0, :], cosT[:, gs], reads=[("cosT", g)], writes=[cs_sb])
            P.dma(cs_sb[:, 1, :], sinT[:, gs], reads=[("sinT", g)], writes=[cs_sb])
            for k in range(4):
                P.dma(lat[:, k, :], projT[C_CQ + k * 128:C_CQ + (k + 1) * 128, gs], reads=[("projT", g)], writes=[(lat, k)])
            rms_T(lat, latn[0], 4, 512, gain_q[:, l, :], gain_q, bank[2], sq, inv)
            for h in range(8):
                pp = bank[nev % 2]
                for k in range(4):
                    P.pe(lambda e, pp=pp, k=k, h=h: e.matmul(pp[:, :], lhsT=wuq_sb[:, k, h * 192:h * 192 + 128], rhs=latn[0][:, k, :], start=(k == 0), stop=(k == 3)),
                         reads=[wuq_sb, (latn[0], k)], writes=[pp])
                ob = evb_sb[nev % 3]
                P.act(lambda e, pp=pp, ob=ob: e.copy(out=ob[:], in_=pp[:]), reads=[pp], writes=[ob])
                P.dma(qnT[h * 128:(h + 1) * 128, gs], ob[:], reads=[ob], writes=[("qnT", g)], q="pool")
                nev += 1
                pp = bank[nev % 2]
                for k in range(4):
                    P.pe(lambda e, pp=pp, k=k, h=h: e.matmul(pp[0:64, :], lhsT=wuq_sb[:, k, h * 192 + 128:h * 192 + 192], rhs=latn[0][:, k, :], start=(k == 0), stop=(k == 3)),
                         reads=[wuq_sb, (latn[0], k)], writes=[pp])
                P.act(lambda e, pp=pp: e.copy(out=rope_x[:], in_=pp[0:64, :]), reads=[pp], writes=[rope_x])
                ro = rope_o[nev % 2]
                rope(None, None, None, ro, g)
                P.dma(qrT[h * 64:(h + 1) * 64, gs], ro[:], reads=[ro], writes=[("qrT", g)], q="pool")
                nev += 1
            P.dma(rope_x[:], projT[C_CKR:C_CKR + 64, gs], reads=[("projT", g)], writes=[rope_x])
            ro = rope_o[nev % 2]
            rope(None, None, None, ro, g)
            P.dma(krT[:, gs], ro[:], reads=[ro], writes=[("krT", g)], q="pool")
            nev += 1
            for k in range(4):
                P.dma(lat[:, k, :], projT[C_CKV + k * 128:C_CKV + (k + 1) * 128, gs], reads=[("projT", g)], writes=[(lat, k)])
            rms_T(lat, latn[1], 4, 512, gain_kv[:, l, :], gain_kv, bank[2], sq, inv)
            for h in range(8):
                pp = bank[nev % 2]
                for k in range(4):
                    P.pe(lambda e, pp=pp, k=k, h=h: e.matmul(pp[:, :], lhsT=wukv_sb[:, k, h * 256:h * 256 + 128], rhs=latn[1][:, k, :], start=(k == 0), stop=(k == 3)),
                         reads=[wukv_sb, (latn[1], k)], writes=[pp])
                ob = evb_sb[nev % 3]
                P.act(lambda e, pp=pp, ob=ob: e.copy(out=ob[:], in_=pp[:]), reads=[pp], writes=[ob])
                P.dma(knT[h * 128:(h + 1) * 128, gs], ob[:], reads=[ob], writes=[("knT", g)], q="pool")
                nev += 1
            for t in range(4):
                tok0 = g * 512 + t * 128
                for hh in range(2):
                    pp = bank[nev % 2]
                    for k in range(4):
                        P.pe(lambda e, pp=pp, k=k, t=t, hh=hh: e.matmul(pp[:, :].rearrange("p (h d) -> p h d", h=4), lhsT=latn[1][:, k, t * 128:(t + 1) * 128],
                                                                         rhs=wukv_sb[:, k, :].rearrange("p (h c) -> p h c", h=8)[:, hh * 4:(hh + 1) * 4, 128:256],
                                                                         start=(k == 0), stop=(k == 3)),
                             reads=[(latn[1], k), wukv_sb], writes=[pp])
                    ob = evb_sb[nev % 3]
                    P.act(lambda e, pp=pp, ob=ob: e.copy(out=ob[:], in_=pp[:]), reads=[pp], writes=[ob])
                    P.dma(vC[tok0:tok0 + 128, hh * 512:(hh + 1) * 512], ob[:], reads=[ob], writes=[("vC", g)], q="pool")
                    nev += 1

    def mla2(l):
        mla_k = P.sb([128, S], BF16, "mla_k")
        mla_kr = P.sb([64, S], BF16, "mla_kr")
        mla_v = P.sb([128, NT, 128], BF16, "mla_v")
        mla_qn = [P.sb([128, 512], BF16, f"mla_qn{i}") for i in range(2)]
        mla_qr = [P.sb([64, 512], BF16, f"mla_qr{i}") for i in range(2)]
        mla_p = [P.sb([128, 512], BF16, f"mla_p{i}") for i in range(4)]
        mla_rs = P.sb([128, 512], F32, "mla_rs")
        mla_y = [P.sb([128, 512], BF16, f"mla_y{i}") for i in range(2)]
        P.dma(mla_kr[:, :], krT[:, :], reads=keys("krT"), writes=[mla_kr])
        scale = float(192 ** -0.5)
        npt = 0
        nq = 0
        for h in range(8):
            P.dma(mla_k[:, :], knT[h * 128:(h + 1) * 128, :], reads=keys("knT"), writes=[mla_k])
            P.dma(mla_v[:, :, :], vC[:, h * 128:(h + 1) * 128].rearrange("(t p) d -> p t d", p=128), reads=keys("vC"), writes=[mla_v], slow=True)
            for g in range(NG):
                gs = slice(g * 512, (g + 1) * 512)
                qn = mla_qn[nq % 2]
                qr = mla_qr[nq % 2]
                yb = mla_y[nq % 2]
                o_ps = bank[4 + (nq % 2)]
                s_ps = bank[6 + (nq % 2)]
                nq += 1
                P.dma(qn[:, :], qnT[h * 128:(h + 1) * 128, gs], reads=[("qnT", g)], writes=[qn])
                P.dma(qr[:, :], qrT[h * 64:(h + 1) * 64, gs], reads=[("qrT", g)], writes=[qr])
                nj = 4 * g + 4
                for j in range(nj):
                    sc = bank[npt % 4]
                    pt = mla_p[npt % 4]
                    npt += 1
                    P.pe(lambda e, sc=sc, j=j, qn=qn: e.matmul(sc[:, :], lhsT=mla_k[:, j * 128:(j + 1) * 128], rhs=qn[:, :], start=True, stop=False),
                         reads=[mla_k, qn], writes=[sc])
                    P.pe(lambda e, sc=sc, j=j, qr=qr: e.matmul(sc[:, :], lhsT=mla_kr[:, j * 128:(j + 1) * 128], rhs=qr[:, :], start=False, stop=True),
                         reads=[mla_kr, qr], writes=[sc])
                    P.act(lambda e, sc=sc, pt=pt: e.activation(out=pt[:], in_=sc[:], func=AF.Exp, scale=scale), reads=[sc], writes=[pt])
                    if j >= 4 * g:
                        jj = j - 4 * g
                        P.dve(lambda e, pt=pt, jj=jj: e.tensor_tensor(out=pt[:], in0=pt[:], in1=m_mla[:, jj, :], op=ALU.mult),
                              reads=[pt, m_mla], writes=[pt])
                    first, last = j == 0, j == nj - 1
                    P.pe(lambda e, o_ps=o_ps, pt=pt, j=j, first=first, last=last: e.matmul(o_ps[:, :], lhsT=mla_v[:, j, :], rhs=pt[:], start=first, stop=last),
                         reads=[mla_v, pt], writes=[o_ps])
                    P.pe(lambda e, s_ps=s_ps, pt=pt, first=first, last=last: e.matmul(s_ps[:, :], lhsT=ones_bf[:, :], rhs=pt[:], start=first, stop=last),
                         reads=[ones_bf, pt], writes=[s_ps])
                P.dve(lambda e, s_ps=s_ps: e.reciprocal(out=mla_rs[:], in_=s_ps[:]), reads=[s_ps], writes=[mla_rs])
                P.dve(lambda e, o_ps=o_ps, yb=yb: e.tensor_tensor(out=yb[:], in0=o_ps[:], in1=mla_rs[:], op=ALU.mult), reads=[o_ps, mla_rs], writes=[yb])
                P.dma(yT[2048 + h * 128:2048 + (h + 1) * 128, gs], yb[:], reads=[yb], writes=[("yT_c", g)], q="pool")

    def dn(l):
        NB = 4
        stop = dbg.get('dn_stop', 99)
        ones_f = P.sb([128, 128], F32, "ones_f")
        P.pool(lambda e: e.memset(ones_f[:], 1.0), writes=[ones_f])
        one_t = P.sb([128, 1], F32, "one_t")
        P.pool(lambda e: e.memset(one_t[:], 1.0), writes=[one_t])
        eps6 = P.sb([128, 1], F32, "eps6")
        P.pool(lambda e: e.memset(eps6[:], 1e-6), writes=[eps6])
        tri = P.sb([128, 2, 128], F32, "tri")
        P.dma(tri[:, 0, :], tri_in[0], writes=[tri])
        P.dma(tri[:, 1, :], tri_in[1], writes=[tri])
        U = tri[:, 0, :]
        TL = tri[:, 1, :]
        convw = P.sb([128, 4, 24], F32, "convw")
        for j in range(4):
            P.dma(convw[:, j, :], conv_w[l, j].rearrange("(c p) -> p c", p=128), writes=[convw], slow=True)
        dtb = P.sb([128, 8], F32, "dtb")
        nA = P.sb([128, 8], F32, "nA")
        gn = P.sb([128, 1], F32, "gn")
        P.dma(dtb[:, :], dn_dt_bias[l:l + 1, :].partition_broadcast(128), writes=[dtb], slow=True)
        P.dma(nA[:, :], dn_a_log[l:l + 1, :].partition_broadcast(128), writes=[nA], slow=True)
        P.dma(gn[:, :], dn_norm[l].rearrange("(p o) -> p o", o=1), writes=[gn], slow=True)
        P.act(lambda e: e.activation(out=nA[:], in_=nA[:], func=AF.Exp), reads=[nA], writes=[nA])
        P.dve(lambda e: e.tensor_scalar(out=nA[:], in0=nA[:], scalar1=-1.0, scalar2=0.0, op0=ALU.mult, op1=ALU.add), reads=[nA], writes=[nA])
        bdt = P.sb([128, NT, 16], F32, "bdt")
        P.dma(bdt[:, :, :], bd[:, :].rearrange("(t p) c -> p t c", p=128), reads=keys("bd"), writes=[bdt], slow=True)
        betat = P.sb([128, NT, 8], F32, "betat")
        gt = P.sb([128, NT, 8], F32, "gt")
        P.act(lambda e: e.activation(out=betat[:, :, :], in_=bdt[:, :, 0:8], func=AF.Sigmoid), reads=[bdt], writes=[betat])
        for t in range(NT):
            P.dve(lambda e, t=t: e.tensor_tensor(out=gt[:, t, :], in0=bdt[:, t, 8:16], in1=dtb[:, :], op=ALU.add), reads=[bdt, dtb], writes=[gt])
        P.act(lambda e: e.activation(out=gt[:, :, :], in_=gt[:, :, :], func=AF.Exp), reads=[gt], writes=[gt])
        P.act(lambda e: e.activation(out=gt[:, :, :], in_=gt[:, :, :], func=AF.Ln, bias=one_t[:, 0:1], scale=1.0), reads=[gt, one_t], writes=[gt])
        for t in range(NT):
            P.dve(lambda e, t=t: e.tensor_tensor(out=gt[:, t, :], in0=gt[:, t, :], in1=nA[:, :], op=ALU.mult), reads=[gt, nA], writes=[gt])

        if stop <= 1:
            return
        if dbg.get("dn_dump"):
            dd = {n: ext_out("o_dn" + n, [1024, S]) for n in ("q", "k", "v", "o")}
            dgb = ext_out("o_dngb", [128, NT, 16])
            P.dma(dgb[:, :, 0:8], gt[:, :, :], reads=[gt], writes=["o_dngb"])
            P.dma(dgb[:, :, 8:16], betat[:, :, :], reads=[betat], writes=["o_dngb"])
        xin = [P.sb([128, 515], F32, f"dn_xin{i}") for i in range(2)]
        cacc = P.sb([128, 512], F32, "dn_cacc")
        qkvT = [P.sb([128, 512], F32, f"dn_qkvT{i}") for i in range(3)]
        sqb = P.sb([128, 512], BF16, "dn_sqb")
        rinv = P.sb([128, 512], F32, "dn_rinv")
        zT = P.sb([128, 512], F32, "dn_zT")
        oT = P.sb([128, 512], F32, "dn_oT")
        yb = [P.sb([128, 512], BF16, f"dn_yb{i}") for i in range(2)]
        St = P.sb([128, 128], F32, "dn_S")
        names = ["ktok", "vtok", "gcrow", "gccol", "t1", "t2", "Lm", "NTm", "Pa", "PaT", "Pb", "PbT", "R", "qkT", "qgT", "kbg", "vb", "kd",
                 "egrow", "sc1", "sc2", "u", "wT", "vnew"]
        W = [{n: P.sb([128, 128] if n not in ("gccol", "sc1", "sc2") else [128, 1], F32, f"dn_{n}{b}") for n in names} for b in range(NB)]
        nps = [0]

        def psq():
            i = nps[0] % 28
            nps[0] += 1
            bi, qi = i % 7, (i // 7) % 4
            return bank[bi][:, qi * 128:(qi + 1) * 128], ("bank", bi)

        def mmf(lhsT, rhs, rd, n=128):
            pp, key = psq()
            P.pe(lambda e, pp=pp, lhsT=lhsT, rhs=rhs, n=n: e.matmul(pp[:, 0:n], lhsT=lhsT, rhs=rhs, start=True, stop=True), reads=rd, writes=[key])
            return pp, key

        def tpf(src, rd):
            pp, key = psq()
            P.pe(lambda e, pp=pp, src=src: e.transpose(pp, src, ident[:]), reads=rd + [ident], writes=[key])
            return pp, key

        for h in range(8):
            P.dve(lambda e: e.memset(St[:], 0.0), writes=[St])
            for g in range(NG):
                gs = slice(g * 512, (g + 1) * 512)
                for ci in range(3):
                    chunk = ci * 8 + h
                    row0 = C_BQKV + chunk * 128
                    xi = xin[ci % 2]
                    if g == 0:
                        P.dve(lambda e, xi=xi: e.memset(xi[:, 0:3], 0.0), writes=[xi])
                        P.dma(xi[:, 3:515], projT[row0:row0 + 128, 0:512], reads=[("projT", 0)], writes=[xi])
                    else:
                        P.dma(xi[:, 0:515], projT[row0:row0 + 128, g * 512 - 3:(g + 1) * 512], reads=[("projT", g - 1), ("projT", g)], writes=[xi])
                    P.act(lambda e, xi=xi, chunk=chunk: e.activation(out=cacc[:], in_=xi[:, 0:512], func=AF.Copy, scale=convw[:, 0, chunk:chunk + 1]),
                          reads=[xi, convw], writes=[cacc])
                    for j in range(1, 4):
                        P.dve(lambda e, xi=xi, chunk=chunk, j=j: e.scalar_tensor_tensor(out=cacc[:], in0=xi[:, j:j + 512], scalar=convw[:, j, chunk:chunk + 1], in1=cacc[:],
                                                                                       op0=ALU.mult, op1=ALU.add), reads=[xi, convw, cacc], writes=[cacc])
                    dst = qkvT[ci]
                    P.act(lambda e, dst=dst: e.activation(out=dst[:], in_=cacc[:], func=AF.Silu), reads=[cacc], writes=[dst])
                    if ci < 2:
                        P.act(lambda e, dst=dst: e.activation(out=sqb[:], in_=dst[:], func=AF.Square), reads=[dst], writes=[sqb])
                        P.pe(lambda e: e.matmul(bank[7][:, :], lhsT=ones_bf[:], rhs=sqb[:], start=True, stop=True), reads=[ones_bf, sqb], writes=[("bank", 7)])
                        P.act(lambda e: e.activation(out=rinv[:], in_=bank[7][:, :], func=AF.Sqrt, bias=eps6[:, 0:1], scale=1.0),
                              reads=[("bank", 7), eps6], writes=[rinv])
                        P.dve(lambda e: e.reciprocal(out=rinv[:], in_=rinv[:]), reads=[rinv], writes=[rinv])
                        sc = float(128 ** -0.5) if ci == 0 else 1.0
                        P.dve(lambda e, dst=dst, sc=sc: e.scalar_tensor_tensor(out=dst[:], in0=dst[:], scalar=sc, in1=rinv[:], op0=ALU.mult, op1=ALU.mult),
                              reads=[dst, rinv], writes=[dst])
                qT_, kT_, vT_ = qkvT
                if dbg.get("dn_dump"):
                    for n_, t_ in (("q", qT_), ("k", kT_), ("v", vT_)):
                        P.dma(dd[n_][h * 128:(h + 1) * 128, gs], t_[:, :], reads=[t_], writes=["o_dn" + n_], q="pool")
                P.dma(zT[:, :], projT[C_BZ + h * 128:C_BZ + (h + 1) * 128, gs], reads=[("projT", g)], writes=[zT])
                P.act(lambda e: e.activation(out=zT[:], in_=zT[:], func=AF.Silu), reads=[zT], writes=[zT])
                if stop <= 2:
                    return
                for b in range(NB):
                    w = W[b]
                    t = g * 4 + b
                    cs = slice(b * 128, (b + 1) * 128)
                    gcol = gt[:, t, h:h + 1]
                    bcol = betat[:, t, h:h + 1]
                    pp, key = tpf(kT_[:, cs], [kT_])
                    P.act(lambda e, pp=pp, w=w: e.copy(out=w["ktok"][:], in_=pp), reads=[key], writes=[w["ktok"]])
                    pp, key = tpf(vT_[:, cs], [vT_])
                    P.act(lambda e, pp=pp, w=w: e.copy(out=w["vtok"][:], in_=pp), reads=[key], writes=[w["vtok"]])
                    P.dve(lambda e, w=w, gcol=gcol: e.tensor_scalar(out=w["t1"][:], in0=U, scalar1=gcol, scalar2=1.0, op0=ALU.mult, op1=ALU.mult), reads=[tri, gt], writes=[w["t1"]])
                    pp, key = mmf(ones_f[:, :], w["t1"][:], [ones_f, w["t1"]])
                    P.act(lambda e, pp=pp, w=w: e.copy(out=w["gcrow"][:], in_=pp), reads=[key], writes=[w["gcrow"]])
                    P.dve(lambda e, w=w: e.tensor_tensor(out=w["t2"][:], in0=w["gcrow"][:], in1=ident[:], op=ALU.mult), reads=[w["gcrow"], ident], writes=[w["t2"]])
                    P.dve(lambda e, w=w: e.reduce_sum(out=w["gccol"][:], in_=w["t2"][:], axis=AX.X), reads=[w["t2"]], writes=[w["gccol"]])
                    P.dve(lambda e, w=w: e.tensor_scalar(out=w["t1"][:], in0=w["gcrow"][:], scalar1=-1.0, scalar2=w["gccol"][:, 0:1], op0=ALU.mult, op1=ALU.add),
                          reads=[w["gcrow"], w["gccol"]], writes=[w["t1"]])
                    P.dve(lambda e, w=w: e.tensor_scalar(out=w["t1"][:], in0=w["t1"][:], scalar1=0.0, scalar2=1.0, op0=ALU.min, op1=ALU.mult), reads=[w["t1"]], writes=[w["t1"]])
                    P.act(lambda e, w=w: e.activation(out=w["t1"][:], in_=w["t1"][:], func=AF.Exp), reads=[w["t1"]], writes=[w["t1"]])
                    P.dve(lambda e, w=w, bcol=bcol: e.scalar_tensor_tensor(out=w["t1"][:], in0=w["t1"][:], scalar=bcol, in1=TL, op0=ALU.mult, op1=ALU.mult),
                          reads=[w["t1"], betat, tri], writes=[w["t1"]])
                    pp, key = mmf(kT_[:, cs], kT_[:, cs], [kT_])
                    P.dve(lambda e, pp=pp, w=w: e.tensor_tensor(out=w["Lm"][:], in0=pp, in1=w["t1"][:], op=ALU.mult), reads=[key, w["t1"]], writes=[w["Lm"]])
                    P.dve(lambda e, w=w: e.tensor_scalar(out=w["t2"][:], in0=w["gcrow"][:], scalar1=w["gccol"][:, 0:1], scalar2=0.0, op0=ALU.subtract, op1=ALU.min),
                          reads=[w["gcrow"], w["gccol"]], writes=[w["t2"]])
                    P.act(lambda e, w=w: e.activation(out=w["t2"][:], in_=w["t2"][:], func=AF.Exp), reads=[w["t2"]], writes=[w["t2"]])
                    P.dve(lambda e, w=w: e.tensor_tensor(out=w["t2"][:], in0=w["t2"][:], in1=U, op=ALU.mult), reads=[w["t2"], tri], writes=[w["t2"]])
                    pp, key = mmf(kT_[:, cs], qT_[:, cs], [kT_, qT_])
                    P.dve(lambda e, pp=pp, w=w: e.tensor_tensor(out=w["qkT"][:], in0=pp, in1=w["t2"][:], op=ALU.mult), reads=[key, w["t2"]], writes=[w["qkT"]])
                    P.act(lambda e, w=w: e.activation(out=w["egrow"][:], in_=w["gcrow"][:], func=AF.Exp), reads=[w["gcrow"]], writes=[w["egrow"]])
                    P.dve(lambda e, w=w, cs=cs: e.tensor_tensor(out=w["qgT"][:], in0=qT_[:, cs], in1=w["egrow"][:], op=ALU.mult), reads=[qT_, w["egrow"]], writes=[w["qgT"]])
                    P.act(lambda e, w=w: e.activation(out=w["sc1"][:], in_=w["gccol"][:], func=AF.Exp), reads=[w["gccol"]], writes=[w["sc1"]])
                    P.dve(lambda e, w=w, bcol=bcol: e.tensor_tensor(out=w["sc1"][:], in0=w["sc1"][:], in1=bcol, op=ALU.mult), reads=[w["sc1"], betat], writes=[w["sc1"]])
                    P.dve(lambda e, w=w: e.tensor_scalar(out=w["kbg"][:], in0=w["ktok"][:], scalar1=w["sc1"][:, 0:1], scalar2=1.0, op0=ALU.mult, op1=ALU.mult),
                          reads=[w["ktok"], w["sc1"]], writes=[w["kbg"]])
                    P.dve(lambda e, w=w, bcol=bcol: e.tensor_scalar(out=w["vb"][:], in0=w["vtok"][:], scalar1=bcol, scalar2=1.0, op0=ALU.mult, op1=ALU.mult),
                          reads=[w["vtok"], betat], writes=[w["vb"]])
                    P.act(lambda e, w=w: e.activation(out=w["sc2"][:], in_=w["gccol"][:], func=AF.Exp, scale=-1.0, bias=w["gcrow"][:, 127:128]),
                          reads=[w["gccol"], w["gcrow"]], writes=[w["sc2"]])
                    P.dve(lambda e, w=w: e.tensor_scalar(out=w["kd"][:], in0=w["ktok"][:], scalar1=w["sc2"][:, 0:1], scalar2=1.0, op0=ALU.mult, op1=ALU.mult),
                          reads=[w["ktok"], w["sc2"]], writes=[w["kd"]])
                    pp, key = tpf(w["Lm"][:], [w["Lm"]])
                    P.act(lambda e, pp=pp, w=w: e.copy(out=w["NTm"][:], in_=pp), reads=[key], writes=[w["NTm"]])
                    P.dve(lambda e, pp=pp, w=w: e.tensor_tensor(out=w["R"][:], in0=ident[:], in1=pp, op=ALU.subtract), reads=[key, ident], writes=[w["R"]])
                if stop <= 3:
                    return
                cur = [(W[b]["Lm"], W[b]["NTm"]) for b in range(NB)]
                for m in range(1, 7):
                    nxt = []
                    res = []
                    for b in range(NB):
                        w = W[b]
                        M_, MT_ = cur[b]
                        p1_, k1 = mmf(MT_[:], M_[:], [MT_, M_])
                        if m < 6:
                            p2_, k2 = mmf(M_[:], MT_[:], [M_, MT_])
                        else:
                            p2_, k2 = None, None
                        res.append((p1_, k1, p2_, k2))
                    for b in range(NB):
                        w = W[b]
                        p1_, k1, p2_, k2 = res[b]
                        Pn = w["Pa"] if m % 2 == 1 else w["Pb"]
                        PnT = w["PaT"] if m % 2 == 1 else w["PbT"]
                        P.act(lambda e, p1_=p1_, Pn=Pn: e.copy(out=Pn[:], in_=p1_), reads=[k1], writes=[Pn])
                        if m < 6:
                            P.dve(lambda e, p2_=p2_, PnT=PnT: e.tensor_copy(out=PnT[:], in_=p2_), reads=[k2], writes=[PnT])
                        nxt.append((Pn, PnT))
                    if dbg.get("dn_bar"):
                        P.barrier()
                    if dbg.get("dn_dump") and h == 1 and g == 0:
                        if m == 1:
                            oP = ext_out("o_dnP", [6, 4, 128, 128]); oPT = ext_out("o_dnPT", [6, 4, 128, 128]); oR = ext_out("o_dnR", [6, 4, 128, 128])
                        for b_ in range(NB):
                            P.dma(oP[m - 1, b_], nxt[b_][0][:, :], reads=[nxt[b_][0]], writes=["o_dnP"], q="pool")
                            if m < 6:
                                P.dma(oPT[m - 1, b_], nxt[b_][1][:, :], reads=[nxt[b_][1]], writes=["o_dnPT"], q="pool")
                            P.dma(oR[m - 1, b_], W[b_]["R"][:, :], reads=[W[b_]["R"]], writes=["o_dnR"], q="pool")
                    res = []
                    for b in range(NB):
                        w = W[b]
                        Pn, PnT = nxt[b]
                        res.append(mmf(Pn[:], w["R"][:], [Pn, w["R"]]))
                    for b in range(NB):
                        w = W[b]
                        pp, key = res[b]
                        P.dve(lambda e, pp=pp, w=w: e.tensor_tensor(out=w["R"][:], in0=w["R"][:], in1=pp, op=ALU.add), reads=[key, w["R"]], writes=[w["R"]])
                    cur = nxt
                if stop <= 4:
                    return
                if dbg.get("dn_dump") and h == 1 and g == 0:
                    for nm_ in ("R", "Lm", "NTm", "Pa", "PaT", "Pb", "PbT"):
                        oo = ext_out("o_dnW_" + nm_, [4, 128, 128])
                        for b_ in range(NB):
                            P.dma(oo[b_], W[b_][nm_][:, :], reads=[W[b_][nm_]], writes=["o_dnW_" + nm_], q="pool")
                for b in range(NB):
                    w = W[b]
                    pp, key = mmf(w["R"][:], w["vb"][:], [w["R"], w["vb"]])
                    P.act(lambda e, pp=pp, w=w: e.copy(out=w["u"][:], in_=pp), reads=[key], writes=[w["u"]])
                    pp, key = mmf(w["kbg"][:], w["R"][:], [w["kbg"], w["R"]])
                    P.act(lambda e, pp=pp, w=w: e.copy(out=w["wT"][:], in_=pp), reads=[key], writes=[w["wT"]])
                if stop <= 5:
                    return
                for b in range(NB):
                    w = W[b]
                    cs = slice(b * 128, (b + 1) * 128)
                    pp, key = mmf(w["wT"][:], St[:], [w["wT"], St])
                    P.dve(lambda e, pp=pp, w=w: e.tensor_tensor(out=w["vnew"][:], in0=w["u"][:], in1=pp, op=ALU.subtract), reads=[key, w["u"]], writes=[w["vnew"]])
                    po, keyo = psq()
                    P.pe(lambda e, po=po, w=w: e.matmul(po, lhsT=St[:], rhs=w["qgT"][:], start=True, stop=False), reads=[St, w["qgT"]], writes=[keyo])
                    P.pe(lambda e, po=po, w=w: e.matmul(po, lhsT=w["vnew"][:], rhs=w["qkT"][:], start=False, stop=True), reads=[w["vnew"], w["qkT"]], writes=[keyo])
                    P.act(lambda e, po=po, cs=cs: e.copy(out=oT[:, cs], in_=po), reads=[keyo], writes=[oT])
                    pp, key = mmf(w["kd"][:], w["vnew"][:], [w["kd"], w["vnew"]])
                    P.dve(lambda e, pp=pp, w=w: e.scalar_tensor_tensor(out=St[:], in0=St[:], scalar=w["egrow"][:, 127:128], in1=pp, op0=ALU.mult, op1=ALU.add),
                          reads=[key, St, w["egrow"]], writes=[St])
                if stop <= 6:
                    return
                if dbg.get("dn_dump"):
                    P.dma(dd["o"][h * 128:(h + 1) * 128, gs], oT[:, :], reads=[oT], writes=["o_dno"], q="pool")
                P.act(lambda e: e.activation(out=sqb[:], in_=oT[:], func=AF.Square), reads=[oT], writes=[sqb])
                bkeys = [("bank", 7)]
                P.pe(lambda e: e.matmul(bank[7][:, :], lhsT=ones_bf[:], rhs=sqb[:], start=True, stop=True), reads=[ones_bf, sqb], writes=bkeys)
                P.act(lambda e: e.activation(out=rinv[:], in_=bank[7][:, :], func=AF.Sqrt, bias=eps_t[:, 0:1], scale=1.0 / 128), reads=bkeys + [eps_t], writes=[rinv])
                P.dve(lambda e: e.reciprocal(out=rinv[:], in_=rinv[:]), reads=[rinv], writes=[rinv])
                P.dve(lambda e: e.scalar_tensor_tensor(out=oT[:], in0=oT[:], scalar=gn[:, 0:1], in1=rinv[:], op0=ALU.mult, op1=ALU.mult), reads=[oT, gn, rinv], writes=[oT])
                ybt = yb[g % 2]
                P.dve(lambda e, ybt=ybt: e.tensor_tensor(out=ybt[:], in0=oT[:], in1=zT[:], op=ALU.mult), reads=[oT, zT], writes=[ybt])
                P.dma(yT[1024 + h * 128:1024 + (h + 1) * 128, gs], ybt[:], reads=[ybt], writes=[("yT_b", g)], q="pool")

    def dump(name, src, shape, dt, rkeys):
        o = ext_out("o_" + name, shape, dt)
        P.dma(o, src, reads=rkeys, writes=["o_" + name])

    def load_x(xg, g):
        for k in range(KD):
            P.dma(xg[:, k, :], xT[k * 128:(k + 1) * 128, g * 512:(g + 1) * 512], reads=[("xT", g)], writes=[(xg, k)])

    def store_x(xg, g):
        for k in range(KD):
            P.dma(xT[k * 128:(k + 1) * 128, g * 512:(g + 1) * 512], xg[:, k, :], reads=[(xg, k)], writes=[("xT", g)], q="pool")

    def p3a(l):
        xg = P.sb([128, KD, 512], F32, "xg")
        yTt = P.sb([128, 24, 512], BF16, "yTt")
        gat = P.sb([128, KD, 512], BF16, "gat")
        merged = P.sb([128, KD, 512], F32, "merged")
        mergedb = P.sb([128, KD, 512], BF16, "mergedb")
        tmp = [P.sb([128, 512], F32, f"p3tmp{i}") for i in range(2)]
        wblk = [P.sb([128, KD, 512], BF16, f"wblk{i}") for i in range(2)]
        nb = 0
        ne = 0
        for g in range(NG):
            gs = slice(g * 512, (g + 1) * 512)
            load_x(xg, g)
            for c in range(24):
                P.dma(yTt[:, c, :], yT[c * 128:(c + 1) * 128, gs], reads=[("yT_a", g), ("yT_b", g), ("yT_c", g)], writes=[(yTt, c)])
            for n in range(3):
                for c in range(KD):
                    P.dma(gat[:, c, :], gatesT[n * D + c * 128:n * D + (c + 1) * 128, gs], reads=[("gatesT", g)], writes=[(gat, c)])
                for cb in range(4):
                    wb = wblk[nb % 2]
                    nb += 1
                    for k in range(8):
                        P.dma(wb[:, k, :], w_branch_bf[l][n * 1024 + k * 128:n * 1024 + (k + 1) * 128, cb * 512:(cb + 1) * 512],
                              reads=[("w_branch_bf", l)], writes=[wb])
                    for j in range(4):
                        dc = cb * 4 + j
                        pp = bank[ne % 4]
                        ne += 1
                        for k in range(8):
                            P.pe(lambda e, pp=pp, wb=wb, k=k, j=j, n=n: e.matmul(pp[:, :], lhsT=wb[:, k, j * 128:(j + 1) * 128], rhs=yTt[:, n * 8 + k, :],
                                                                                start=(k == 0), stop=(k == 7)), reads=[wb, (yTt, n * 8 + k)], writes=[pp])
                        if n == 0:
                            P.dve(lambda e, pp=pp, dc=dc: e.tensor_tensor(out=merged[:, dc, :], in0=pp[:, :], in1=gat[:, dc, :], op=ALU.mult),
                                  reads=[pp, (gat, dc)], writes=[(merged, dc)])
                        else:
                            tt = tmp[ne % 2]
                            P.dve(lambda e, pp=pp, dc=dc, tt=tt: e.tensor_tensor(out=tt[:], in0=pp[:, :], in1=gat[:, dc, :], op=ALU.mult),
                                  reads=[pp, (gat, dc)], writes=[tt])
                            P.pool(lambda e, dc=dc, tt=tt: e.tensor_tensor(out=merged[:, dc, :], in0=merged[:, dc, :], in1=tt[:], op=ALU.add),
                                   reads=[tt, (merged, dc)], writes=[(merged, dc)])
            for dc in range(KD):
                P.act(lambda e, dc=dc: e.copy(out=mergedb[:, dc, :], in_=merged[:, dc, :]), reads=[(merged, dc)], writes=[(mergedb, dc)])
            for cb in range(4):
                wb = wblk[nb % 2]
                nb += 1
                for k in range(KD):
                    P.dma(wb[:, k, :], w_out_bf[l][k * 128:(k + 1) * 128, cb * 512:(cb + 1) * 512], reads=[("w_out_bf", l)], writes=[wb])
                for j in range(4):
                    dc = cb * 4 + j
                    pp = bank[ne % 4]
                    ne += 1
                    for k in range(KD):
                        P.pe(lambda e, pp=pp, wb=wb, k=k, j=j: e.matmul(pp[:, :], lhsT=wb[:, k, j * 128:(j + 1) * 128], rhs=mergedb[:, k, :],
                                                                         start=(k == 0), stop=(k == KD - 1)), reads=[wb, (mergedb, k)], writes=[pp])
                    P.dve(lambda e, pp=pp, dc=dc: e.tensor_tensor(out=xg[:, dc, :], in0=xg[:, dc, :], in1=pp[:, :], op=ALU.add),
                          reads=[pp, (xg, dc)], writes=[(xg, dc)])
            store_x(xg, g)

    def ffn_core(wg_bf, wu_bf, wd_bf, wkey, hT, xg, hid, wblk, sgt, st, wbc=None):
        for fb in range(14):
            wbg = wblk[st["nb"] % len(wblk)]
            wbu = wblk[(st["nb"] + 1) % len(wblk)]
            st["nb"] += 2
            for k in range(KD):
                P.dma(wbg[:, k, :], wg_bf[k * 128:(k + 1) * 128, fb * 512:(fb + 1) * 512], reads=[wkey], writes=[wbg])
                P.dma(wbu[:, k, :], wu_bf[k * 128:(k + 1) * 128, fb * 512:(fb + 1) * 512], reads=[wkey], writes=[wbu])
            for j in range(4):
                fc = fb * 4 + j
                pg = bank[(st["ne"] % 2) * 2]
                pu = bank[(st["ne"] % 2) * 2 + 1]
                sg = sgt[st["ne"] % 2]
                st["ne"] += 1
                for k in range(KD):
                    P.pe(lambda e, pg=pg, wbg=wbg, k=k, j=j: e.matmul(pg[:, :], lhsT=wbg[:, k, j * 128:(j + 1) * 128], rhs=hT[:, k, :],
                                                                       start=(k == 0), stop=(k == KD - 1)), reads=[wbg, (hT, k)], writes=[pg])
                for k in range(KD):
                    P.pe(lambda e, pu=pu, wbu=wbu, k=k, j=j: e.matmul(pu[:, :], lhsT=wbu[:, k, j * 128:(j + 1) * 128], rhs=hT[:, k, :],
                                                                       start=(k == 0), stop=(k == KD - 1)), reads=[wbu, (hT, k)], writes=[pu])
                P.act(lambda e, pg=pg, sg=sg: e.activation(out=sg[:], in_=pg[:, :], func=AF.Silu), reads=[pg], writes=[sg])
                if wbc is not None:
                    P.pool(lambda e, sg=sg: e.tensor_tensor(out=sg[:], in0=sg[:], in1=wbc, op=ALU.mult), reads=[sg, "wbc"], writes=[sg])
                P.dve(lambda e, pu=pu, sg=sg, fc=fc: e.tensor_tensor(out=hid[:, fc, :], in0=pu[:, :], in1=sg[:], op=ALU.mult),
                      reads=[pu, sg], writes=[(hid, fc)])
        for cb in range(4):
            for seg in range(4):
                k0 = seg * 16
                nk = min(16, 56 - k0)
                wb = wblk[st["nb"] % len(wblk)]
                st["nb"] += 1
                for k in range(nk):
                    P.dma(wb[:, k, :], wd_bf[(k0 + k) * 128:(k0 + k + 1) * 128, cb * 512:(cb + 1) * 512], reads=[wkey], writes=[wb])
                for j in range(4):
                    pp = bank[4 + j]
                    for k in range(nk):
                        kk = k0 + k
                        P.pe(lambda e, pp=pp, wb=wb, k=k, j=j, kk=kk: e.matmul(pp[:, :], lhsT=wb[:, k, j * 128:(j + 1) * 128], rhs=hid[:, kk, :],
                                                                                start=(kk == 0), stop=(kk == 55)), reads=[wb, (hid, kk)], writes=[pp])
            for j in range(4):
                dc = cb * 4 + j
                pp = bank[4 + j]
                P.dve(lambda e, pp=pp, dc=dc: e.tensor_tensor(out=xg[:, dc, :], in0=xg[:, dc, :], in1=pp[:, :], op=ALU.add),
                      reads=[pp, (xg, dc)], writes=[(xg, dc)])

    def p3b_dense(l):
        xg = P.sb([128, KD, 512], F32, "xg")
        sq = P.sb([128, KD, 512], BF16, "sq")
        hT = P.sb([128, KD, 512], BF16, "hT")
        inv = P.sb([128, 512], F32, "inv")
        hid = P.sb([128, 56, 512], BF16, "hid")
        wblk = [P.sb([128, KD, 512], BF16, f"wblk{i}") for i in range(4)]
        sgt = [P.sb([128, 512], F32, f"sgt{i}") for i in range(2)]
        st = {"nb": 0, "ne": 0}
        for g in range(NG):
            load_x(xg, g)
            rms_T(xg, hT, KD, D, gain_ffn[:, l, :], gain_ffn, bank[0], sq, inv)
            ffn_core(w_ffn_bf[0], w_ffn_bf[1], w_ffn_bf[2], "w_ffn_bf", hT, xg, hid, wblk, sgt, st)
            store_x(xg, g)

    def p3b_moe(l):
        xg = P.sb([128, KD, 512], F32, "xg")
        sq = P.sb([128, KD, 512], BF16, "sq")
        hT = P.sb([128, KD, 512], BF16, "hT")
        inv = P.sb([128, 512], F32, "inv")
        hid = P.sb([128, 56, 512], BF16, "hid")
        wblk = [P.sb([128, KD, 512], BF16, f"wblk{i}") for i in range(3)]
        sgt = [P.sb([128, 512], F32, f"sgt{i}") for i in range(2)]
        hf = [P.sb([128, 512], F32, f"hf{i}") for i in range(2)]
        wr = P.sb([128, KD, 8], F32, "wr")
        P.dma(wr[:, :, :], w_router[0].rearrange("(k p) e -> p k e", p=128), writes=[wr], slow=True)
        sel = P.sb([8, 8, 128], F32, "sel")
        P.dma(sel[:, :, :], sel_in[:, :, :], writes=[sel])
        lg = P.sb([128, 4, 8], F32, "lg")
        m8 = P.sb([128, 8], F32, "m8")
        nm1 = P.sb([128, 1], F32, "nm1")
        msk = P.sb([128, 8], F32, "msk")
        den = P.sb([128, 1], F32, "den")
        wT8 = P.sb([8, 512], F32, "wT8")
        wbc = P.sb([128, 8, 512], BF16, "wbc")
        st = {"nb": 0, "ne": 0}
        for g in range(NG):
            load_x(xg, g)
            rms_T(xg, hT, KD, D, gain_ffn[:, l, :], gain_ffn, bank[0], sq, inv)
            for k in range(KD):
                hb = hf[k % 2]
                P.dve(lambda e, k=k, hb=hb: e.scalar_tensor_tensor(out=hb[:], in0=xg[:, k, :], scalar=gain_ffn[:, l, k:k + 1], in1=inv[:], op0=ALU.mult, op1=ALU.mult),
                      reads=[(xg, k), inv, gain_ffn], writes=[hb])
                for t in range(4):
                    P.pe(lambda e, k=k, hb=hb, t=t: e.matmul(bank[4 + t][:, 0:8], lhsT=hb[:, t * 128:(t + 1) * 128], rhs=wr[:, k, :], start=(k == 0), stop=(k == KD - 1)),
                         reads=[hb, wr], writes=[bank[4 + t]])
            for t in range(4):
                P.act(lambda e, t=t: e.copy(out=lg[:, t, :], in_=bank[4 + t][:, 0:8]), reads=[bank[4 + t]], writes=[lg])
            for t in range(4):
                P.dve(lambda e, t=t: e.max(out=m8[:, :], in_=lg[:, t, :]), reads=[lg], writes=[m8])
                P.dve(lambda e, t=t: e.tensor_scalar(out=msk[:, :], in0=lg[:, t, :], scalar1=m8[:, 1:2], scalar2=1.0, op0=ALU.is_ge, op1=ALU.mult), reads=[lg, m8], writes=[msk])
                P.dve(lambda e: e.tensor_scalar(out=nm1[:, :], in0=m8[:, 0:1], scalar1=-1.0, scalar2=0.0, op0=ALU.mult, op1=ALU.add), reads=[m8], writes=[nm1])
                P.act(lambda e, t=t: e.activation(out=lg[:, t, :], in_=lg[:, t, :], func=AF.Exp, bias=nm1[:, 0:1], scale=1.0), reads=[lg, nm1], writes=[lg])
                P.dve(lambda e, t=t: e.tensor_tensor(out=lg[:, t, :], in0=lg[:, t, :], in1=msk[:, :], op=ALU.mult), reads=[lg, msk], writes=[lg])
                P.dve(lambda e, t=t: e.reduce_sum(out=den[:, :], in_=lg[:, t, :], axis=AX.X), reads=[lg], writes=[den])
                P.dve(lambda e: e.reciprocal(out=den[:, :], in_=den[:, :]), reads=[den], writes=[den])
                P.dve(lambda e, t=t: e.tensor_scalar(out=lg[:, t, :], in0=lg[:, t, :], scalar1=den[:, 0:1], scalar2=1.0, op0=ALU.mult, op1=ALU.mult), reads=[lg, den], writes=[lg])
                P.pe(lambda e, t=t: e.transpose(bank[0][0:8, t * 128:(t + 1) * 128], lg[:, t, :], ident[:]), reads=[lg, ident], writes=[bank[0]])
            P.act(lambda e: e.copy(out=wT8[:, :], in_=bank[0][0:8, :]), reads=[bank[0]], writes=[wT8])
            for ex in range(8):
                pp = bank[ex % 4]
                P.pe(lambda e, pp=pp, ex=ex: e.matmul(pp[:, :], lhsT=sel[:, ex, :], rhs=wT8[:, :], start=True, stop=True), reads=[sel, wT8], writes=[pp])
                P.act(lambda e, pp=pp, ex=ex: e.copy(out=wbc[:, ex, :], in_=pp[:, :]), reads=[pp], writes=["wbc"])
            for ex in range(8):
                ffn_core(w_exp_bf[0][ex], w_exp_bf[1][ex], w_exp_bf[2][ex], "w_exp_bf", hT, xg, hid, wblk, sgt, st, wbc=wbc[:, ex, :])
            store_x(xg, g)

    CAP = max(512, -(-(S * 5 // 16) // 512) * 512)
    NSG = CAP // 512

    def moe_routed(l):
        Hsel = P.dram([8 * CAP + 128, D], BF16, "Hsel")
        hrow = P.dram([S, D], BF16, "hrow")
        Tslot = P.dram([8 * CAP + 128, 2], I32, "Tslot")
        Ytok = P.dram([2 * S + 128, D], F32, "Ytok")
        BIG2 = 2 * S
        pbf = bank[7][:, :].bitcast(BF16)
        with P.phase():
            xg = P.sb([128, KD, 512], F32, "xg")
            sq = P.sb([128, KD, 512], BF16, "sq")
            hT = P.sb([128, KD, 512], BF16, "hT")
            inv = P.sb([128, 512], F32, "inv")
            hf = [P.sb([128, 512], F32, f"hf{i}") for i in range(2)]
            wr = P.sb([128, KD, 8], F32, "wr")
            P.dma(wr[:, :, :], w_router[0].rearrange("(k p) e -> p k e", p=128), writes=[wr], slow=True)
            ident_bf = P.sb([128, 128], BF16, "ident_bf")
            P.dve(lambda e: e.tensor_copy(out=ident_bf[:], in_=ident[:]), reads=[ident], writes=[ident_bf])
            lg = P.sb([128, 4, 8], F32, "lg")
            m8 = P.sb([128, 8], F32, "m8")
            nm1 = P.sb([128, 1], F32, "nm1")
            den = P.sb([128, 1], F32, "den")
            Mall = P.sb([128, NT, 8], F32, "Mall")
            Tall = P.sb([128, NT, 8], F32, "Tall")
            TW = P.sb([128, NT, 8, 2], I32, "TW")
            tokid = P.sb([128, 64], F32, "tokid")
            P.dma(tokid[:, :], tokid_in[:, :], writes=[tokid])
            bigt = P.sb([128, 2 * 8 * CAP // 128], I32, "bigt")
            P.dve(lambda e: e.memset(bigt[:], BIG2), writes=[bigt])
            P.dma(Tslot[0:8 * CAP, :].rearrange("(p c) o -> p (c o)", p=128), bigt[:, :], reads=[bigt], writes=["Tslot"], q="pool")
            hro = [P.sb([128, D], BF16, f"hro{i}") for i in range(2)]
            nh = 0
            for g in range(NG):
                load_x(xg, g)
                rms_T(xg, hT, KD, D, gain_ffn[:, l, :], gain_ffn, bank[0], sq, inv)
                for k in range(KD):
                    hb = hf[k % 2]
                    P.dve(lambda e, k=k, hb=hb: e.scalar_tensor_tensor(out=hb[:], in0=xg[:, k, :], scalar=gain_ffn[:, l, k:k + 1], in1=inv[:], op0=ALU.mult, op1=ALU.mult),
                          reads=[(xg, k), inv, gain_ffn], writes=[hb])
                    for t in range(4):
                        P.pe(lambda e, k=k, hb=hb, t=t: e.matmul(bank[1 + t][:, 0:8], lhsT=hb[:, t * 128:(t + 1) * 128], rhs=wr[:, k, :], start=(k == 0), stop=(k == KD - 1)),
                             reads=[hb, wr], writes=[bank[1 + t]])
                for t in range(4):
                    P.act(lambda e, t=t: e.copy(out=lg[:, t, :], in_=bank[1 + t][:, 0:8]), reads=[bank[1 + t]], writes=[lg])
                for t in range(4):
                    tt = g * 4 + t
                    P.dve(lambda e, t=t: e.max(out=m8[:, :], in_=lg[:, t, :]), reads=[lg], writes=[m8])
                    P.dve(lambda e, t=t, tt=tt: e.tensor_scalar(out=Mall[:, tt, :], in0=lg[:, t, :], scalar1=m8[:, 1:2], scalar2=1.0, op0=ALU.is_ge, op1=ALU.mult),
                          reads=[lg, m8], writes=[Mall])
                    P.dve(lambda e, t=t, tt=tt: e.tensor_scalar(out=Tall[:, tt, :], in0=lg[:, t, :], scalar1=m8[:, 0:1], scalar2=float(-S), op0=ALU.is_ge, op1=ALU.mult),
                          reads=[lg, m8], writes=[Tall])
                    P.dve(lambda e, tt=tt: e.tensor_scalar(out=Tall[:, tt, :], in0=Tall[:, tt, :], scalar1=tokid[:, tt:tt + 1], scalar2=float(S), op0=ALU.add, op1=ALU.add),
                          reads=[Tall, tokid], writes=[Tall])
                    P.dve(lambda e: e.tensor_scalar(out=nm1[:, :], in0=m8[:, 0:1], scalar1=-1.0, scalar2=0.0, op0=ALU.mult, op1=ALU.add), reads=[m8], writes=[nm1])
                    P.act(lambda e, t=t: e.activation(out=lg[:, t, :], in_=lg[:, t, :], func=AF.Exp, bias=nm1[:, 0:1], scale=1.0), reads=[lg, nm1], writes=[lg])
                    P.dve(lambda e, t=t, tt=tt: e.tensor_tensor(out=lg[:, t, :], in0=lg[:, t, :], in1=Mall[:, tt, :], op=ALU.mult), reads=[lg, Mall], writes=[lg])
                    P.dve(lambda e, t=t: e.reduce_sum(out=den[:, :], in_=lg[:, t, :], axis=AX.X), reads=[lg], writes=[den])
                    P.dve(lambda e: e.reciprocal(out=den[:, :], in_=den[:, :]), reads=[den], writes=[den])
                    P.dve(lambda e, t=t, tt=tt: e.tensor_scalar(out=Wall[:, tt, :], in0=lg[:, t, :], scalar1=den[:, 0:1], scalar2=1.0, op0=ALU.mult, op1=ALU.mult),
                          reads=[lg, den], writes=[Wall])
                for t in range(4):
                    hr = hro[nh % 2]
                    nh += 1
                    for kb in range(2):
                        for kq in range(8):
                            k = kb * 8 + kq
                            P.pe(lambda e, k=k, kq=kq, t=t: e.transpose(pbf[:, kq * 128:(kq + 1) * 128], hT[:, k, t * 128:(t + 1) * 128], ident_bf[:]),
                                 reads=[(hT, k), ident_bf], writes=[bank[7]])
                        P.act(lambda e, hr=hr, kb=kb: e.copy(out=hr[:, kb * 1024:(kb + 1) * 1024], in_=pbf[:, :]), reads=[bank[7]], writes=[hr])
                    tok0 = g * 512 + t * 128
                    P.dma(hrow[tok0:tok0 + 128, :], hr[:, :], reads=[hr], writes=["hrow"], q="pool")
            us = P.sb([128, 128], F32, "us")
            ones_f = P.sb([128, 128], F32, "ones_f")
            P.pool(lambda e: e.memset(ones_f[:], 1.0), writes=[ones_f])
            P.dma(us[:, :], tri_in[0], writes=[us])
            P.dve(lambda e: e.tensor_tensor(out=us[:], in0=us[:], in1=ident[:], op=ALU.subtract), reads=[us, ident], writes=[us])
            cum = P.sb([128, NT, 8], F32, "cum")
            tot = P.sb([128, NT, 8], F32, "tot")
            base = P.sb([128, NT, 8], F32, "base")
            for c0 in range(0, NT * 8, 512):
                c1 = min(NT * 8, c0 + 512)
                mflat = Mall[:, :, :].rearrange("p t e -> p (t e)")
                P.pe(lambda e, c0=c0, c1=c1, mflat=mflat: e.matmul(bank[1][:, 0:c1 - c0], lhsT=us[:, :], rhs=mflat[:, c0:c1], start=True, stop=True), reads=[us, Mall], writes=[bank[1]])
                P.act(lambda e, c0=c0, c1=c1: e.copy(out=cum[:, :, :].rearrange("p t e -> p (t e)")[:, c0:c1], in_=bank[1][:, 0:c1 - c0]), reads=[bank[1]], writes=[cum])
                P.pe(lambda e, c0=c0, c1=c1, mflat=mflat: e.matmul(bank[2][:, 0:c1 - c0], lhsT=ones_f[:, :], rhs=mflat[:, c0:c1], start=True, stop=True), reads=[ones_f, Mall], writes=[bank[2]])
                P.act(lambda e, c0=c0, c1=c1: e.copy(out=tot[:, :, :].rearrange("p t e -> p (t e)")[:, c0:c1], in_=bank[2][:, 0:c1 - c0]), reads=[bank[2]], writes=[tot])
            for ex in range(8):
                P.dve(lambda e, ex=ex: e.memset(base[:, 0, ex:ex + 1], float(ex * CAP)), writes=[base])
            for t in range(1, NT):
                P.dve(lambda e, t=t: e.tensor_tensor(out=base[:, t, :], in0=base[:, t - 1, :], in1=tot[:, t - 1, :], op=ALU.add), reads=[base, tot], writes=[base])
            BIG = float(8 * CAP)
            P.dve(lambda e: e.tensor_tensor(out=cum[:, :, :], in0=cum[:, :, :], in1=base[:, :, :], op=ALU.add), reads=[cum, base], writes=[cum])
            P.dve(lambda e: e.tensor_scalar(out=cum[:, :, :], in0=cum[:, :, :], scalar1=-BIG, scalar2=1.0, op0=ALU.add, op1=ALU.mult), reads=[cum], writes=[cum])
            P.dve(lambda e: e.tensor_tensor(out=cum[:, :, :], in0=cum[:, :, :], in1=Mall[:, :, :], op=ALU.mult), reads=[cum, Mall], writes=[cum])
            P.dve(lambda e: e.tensor_scalar(out=cum[:, :, :], in0=cum[:, :, :], scalar1=BIG, scalar2=1.0, op0=ALU.add, op1=ALU.mult), reads=[cum], writes=[cum])
            P.dve(lambda e: e.tensor_copy(out=Iall[:, :, :], in_=cum[:, :, :]), reads=[cum], writes=[Iall])
            P.dve(lambda e: e.tensor_copy(out=TW[:, :, :, 0], in_=Tall[:, :, :]), reads=[Tall], writes=[TW])
            P.dve(lambda e: e.tensor_copy(out=TW[:, :, :, :].bitcast(F32)[:, :, :, 1], in_=Wall[:, :, :]), reads=[Wall, TW], writes=[TW])
            for t in range(NT):
                hr = hro[nh % 2]
                nh += 1
                P.dma(hr[:, :], hrow[t * 128:(t + 1) * 128, :], reads=["hrow"], writes=[hr])
                for ex in range(8):
                    P.indirect(lambda e, hr=hr, t=t, ex=ex: e.indirect_dma_start(
                        out=Hsel[:, :], out_offset=bass.IndirectOffsetOnAxis(ap=Iall[:, t, ex:ex + 1], axis=0),
                        in_=hr[:, :], in_offset=None),
                        [hr, Iall], ["Hsel"])
                    P.indirect(lambda e, t=t, ex=ex: e.indirect_dma_start(
                        out=Tslot[:, :], out_offset=bass.IndirectOffsetOnAxis(ap=Iall[:, t, ex:ex + 1], axis=0),
                        in_=TW[:, t, ex, :], in_offset=None),
                        [TW, Iall, "Tslot"], ["Tslot"])
        with P.phase():
            ident_bf = P.sb([128, 128], BF16, "ident_bf")
            P.dve(lambda e: e.tensor_copy(out=ident_bf[:], in_=ident[:]), reads=[ident], writes=[ident_bf])
            hs = P.sb([128, 4, D], BF16, "hs")
            hselT = P.sb([128, KD, 512], BF16, "hselT")
            hid = P.sb([128, 56, 512], BF16, "hid")
            wblk = [P.sb([128, KD, 512], BF16, f"wblk{i}") for i in range(3)]
            sgt = [P.sb([128, 512], F32, f"sgt{i}") for i in range(2)]
            osl = P.sb([128, 4, D], F32, "osl")
            tsl = P.sb([128, 4, 2], I32, "tsl")
            st = {"nb": 0, "ne": 0}
            for ex in range(8):
                for sg in range(NSG):
                    r0 = ex * CAP + sg * 512
                    for s4 in range(4):
                        P.dma(hs[:, s4, :], Hsel[r0 + s4 * 128:r0 + (s4 + 1) * 128, :], reads=["Hsel"], writes=[(hs, s4)])
                    P.dma(tsl[:, :, :], Tslot[r0:r0 + 512, :].rearrange("(s p) o -> p s o", p=128), reads=["Tslot"], writes=[tsl], slow=True)
                    for kp in range(KD // 2):
                        for kq in range(2):
                            k = kp * 2 + kq
                            for s4 in range(4):
                                P.pe(lambda e, k=k, kq=kq, s4=s4: e.transpose(pbf[:, kq * 512 + s4 * 128:kq * 512 + (s4 + 1) * 128], hs[:, s4, k * 128:(k + 1) * 128], ident_bf[:]),
                                     reads=[(hs, s4), ident_bf], writes=[bank[7]])
                        P.act(lambda e, kp=kp: e.copy(out=hselT[:, kp * 2:kp * 2 + 2, :].rearrange("p a b -> p (a b)"), in_=pbf[:, :]), reads=[bank[7]],
                              writes=[(hselT, kp * 2), (hselT, kp * 2 + 1)])
                    wg_bf, wu_bf, wd_bf = w_exp_bf[0][ex], w_exp_bf[1][ex], w_exp_bf[2][ex]
                    for fb in range(14):
                        wbg = wblk[st["nb"] % 3]
                        wbu = wblk[(st["nb"] + 1) % 3]
                        st["nb"] += 2
                        for k in range(KD):
                            P.dma(wbg[:, k, :], wg_bf[k * 128:(k + 1) * 128, fb * 512:(fb + 1) * 512], reads=["w_exp_bf"], writes=[wbg])
                            P.dma(wbu[:, k, :], wu_bf[k * 128:(k + 1) * 128, fb * 512:(fb + 1) * 512], reads=["w_exp_bf"], writes=[wbu])
                        for j in range(4):
                            fc = fb * 4 + j
                            pg = bank[(st["ne"] % 2) * 2]
                            pu = bank[(st["ne"] % 2) * 2 + 1]
                            sgq = sgt[st["ne"] % 2]
                            st["ne"] += 1
                            for k in range(KD):
                                P.pe(lambda e, pg=pg, wbg=wbg, k=k, j=j: e.matmul(pg[:, :], lhsT=wbg[:, k, j * 128:(j + 1) * 128], rhs=hselT[:, k, :],
                                                                                   start=(k == 0), stop=(k == KD - 1)), reads=[wbg, (hselT, k)], writes=[pg])
                            for k in range(KD):
                                P.pe(lambda e, pu=pu, wbu=wbu, k=k, j=j: e.matmul(pu[:, :], lhsT=wbu[:, k, j * 128:(j + 1) * 128], rhs=hselT[:, k, :],
                                                                                   start=(k == 0), stop=(k == KD - 1)), reads=[wbu, (hselT, k)], writes=[pu])
                            P.act(lambda e, pg=pg, sgq=sgq: e.activation(out=sgq[:], in_=pg[:, :], func=AF.Silu), reads=[pg], writes=[sgq])
                            P.dve(lambda e, pu=pu, sgq=sgq, fc=fc: e.tensor_tensor(out=hid[:, fc, :], in0=pu[:, :], in1=sgq[:], op=ALU.mult),
                                  reads=[pu, sgq], writes=[(hid, fc)])
                    for cb in range(4):
                        for seg in range(4):
                            k0 = seg * 16
                            nk = min(16, 56 - k0)
                            wb = wblk[st["nb"] % 3]
                            st["nb"] += 1
                            for k in range(nk):
                                P.dma(wb[:, k, :], wd_bf[(k0 + k) * 128:(k0 + k + 1) * 128, cb * 512:(cb + 1) * 512], reads=["w_exp_bf"], writes=[wb])
                            for s4 in range(4):
                                pp = bank[3 + s4]
                                for k in range(nk):
                                    kk = k0 + k
                                    P.pe(lambda e, pp=pp, wb=wb, k=k, s4=s4, kk=kk: e.matmul(pp[:, :], lhsT=hid[:, kk, s4 * 128:(s4 + 1) * 128], rhs=wb[:, k, :],
                                                                                          start=(kk == 0), stop=(kk == 55)), reads=[(hid, kk), wb], writes=[pp])
                        for s4 in range(4):
                            pp = bank[3 + s4]
                            P.act(lambda e, pp=pp, s4=s4, cb=cb: e.activation(out=osl[:, s4, cb * 512:(cb + 1) * 512], in_=pp[:, :], func=AF.Copy, scale=tsl[:, s4, 1:2].bitcast(F32)),
                                  reads=[pp, tsl], writes=[(osl, s4)])
                    for s4 in range(4):
                        P.indirect(lambda e, s4=s4: e.indirect_dma_start(
                            out=Ytok[:, :], out_offset=bass.IndirectOffsetOnAxis(ap=tsl[:, s4, 0:1], axis=0),
                            in_=osl[:, s4, :], in_offset=None),
                            [(osl, s4), tsl], ["Ytok"])
        with P.phase():
            xg = P.sb([128, KD, 512], F32, "xg")
            y1 = [P.sb([128, D], F32, f"y1_{i}") for i in range(4)]
            y2 = [P.sb([128, D], F32, f"y2_{i}") for i in range(4)]
            for g in range(NG):
                load_x(xg, g)
                for t in range(4):
                    tok0 = g * 512 + t * 128
                    P.dma(y1[t][:, :], Ytok[tok0:tok0 + 128, :], reads=["Ytok"], writes=[y1[t]])
                    P.dma(y2[t][:, :], Ytok[S + tok0:S + tok0 + 128, :], reads=["Ytok"], writes=[y2[t]])
                    P.pool(lambda e, t=t: e.tensor_tensor(out=y1[t][:], in0=y1[t][:], in1=y2[t][:], op=ALU.add), reads=[y1[t], y2[t]], writes=[y1[t]])
                for k in range(KD):
                    pp = bank[k % 4]
                    for t in range(4):
                        P.pe(lambda e, pp=pp, t=t, k=k: e.transpose(pp[:, t * 128:(t + 1) * 128], y1[t][:, k * 128:(k + 1) * 128], ident[:]),
                             reads=[y1[t], ident], writes=[pp])
                    P.dve(lambda e, pp=pp, k=k: e.tensor_tensor(out=xg[:, k, :], in0=xg[:, k, :], in1=pp[:, :], op=ALU.add), reads=[pp, (xg, k)], writes=[(xg, k)])
                store_x(xg, g)

    def p3c(l, last):
        xg = P.sb([128, KD, 512], F32, "xg")
        sq = P.sb([128, KD, 512], BF16, "sq")
        hT = P.sb([128, KD, 512], BF16, "hT")
        inv = P.sb([128, 512], F32, "inv")
        pTt = P.sb([128, 2, 512], BF16, "pTt")
        wblk = [P.sb([128, KD, 512], BF16, f"wblk{i}") for i in range(2)]
        wpb = [P.sb([128, 2, 512], BF16, f"wpb{i}") for i in range(2)]
        sgt = [P.sb([128, 512], F32, f"sgt{i}") for i in range(2)]
        if last:
            hfin = P.sb([128, KD, 512], F32, "hfin")
            osb = [P.sb([128, D], F32, f"osb{i}") for i in range(2)]
        nb = 0
        ne = 0
        no = 0
        for g in range(NG):
            gs = slice(g * 512, (g + 1) * 512)
            load_x(xg, g)
            rms_T(xg, hT, KD, D, gain_ple[:, l, :], gain_ple, bank[0], sq, inv)
            for kk in range(2):
                P.dma(pTt[:, kk, :], pT[l][kk * 128:(kk + 1) * 128, gs], reads=[("pT", l)], writes=[pTt])
            for cb in range(4):
                wb = wblk[nb % 2]
                wp = wpb[nb % 2]
                nb += 1
                for k in range(KD):
                    P.dma(wb[:, k, :], w_pg_bf[l][k * 128:(k + 1) * 128, cb * 512:(cb + 1) * 512], reads=[("w_pg_bf", l)], writes=[wb])
                for kk in range(2):
                    P.dma(wp[:, kk, :], w_pp_bf[l][kk * 128:(kk + 1) * 128, cb * 512:(cb + 1) * 512], reads=[("w_pp_bf", l)], writes=[wp])
                for j in range(4):
                    dc = cb * 4 + j
                    pgt = bank[2 + (ne % 2) * 2]
                    ppr = bank[3 + (ne % 2) * 2]
                    sg = sgt[ne % 2]
                    ne += 1
                    for k in range(KD):
                        P.pe(lambda e, pgt=pgt, wb=wb, k=k, j=j: e.matmul(pgt[:, :], lhsT=wb[:, k, j * 128:(j + 1) * 128], rhs=hT[:, k, :],
                                                                           start=(k == 0), stop=(k == KD - 1)), reads=[wb, (hT, k)], writes=[pgt])
                    for kk in range(2):
                        P.pe(lambda e, ppr=ppr, wp=wp, kk=kk, j=j: e.matmul(ppr[:, :], lhsT=wp[:, kk, j * 128:(j + 1) * 128], rhs=pTt[:, kk, :],
                                                                             start=(kk == 0), stop=(kk == 1)), reads=[wp, pTt], writes=[ppr])
                    P.act(lambda e, pgt=pgt, sg=sg: e.activation(out=sg[:], in_=pgt[:, :], func=AF.Sigmoid), reads=[pgt], writes=[sg])
                    P.dve(lambda e, ppr=ppr, sg=sg: e.tensor_tensor(out=sg[:], in0=ppr[:, :], in1=sg[:], op=ALU.mult), reads=[ppr, sg], writes=[sg])
                    P.pool(lambda e, sg=sg, dc=dc: e.tensor_tensor(out=xg[:, dc, :], in0=xg[:, dc, :], in1=sg[:], op=ALU.add), reads=[sg, (xg, dc)], writes=[(xg, dc)])
            if not last:
                store_x(xg, g)
                continue
            for k in range(KD):
                P.act(lambda e, k=k: e.activation(out=sq[:, k, :], in_=xg[:, k, :], func=AF.Square), reads=[(xg, k)], writes=[(sq, k)])
            for k in range(KD):
                P.pe(lambda e, k=k: e.matmul(bank[0][:], lhsT=ones_bf[:], rhs=sq[:, k, :], start=(k == 0), stop=(k == KD - 1)), reads=[ones_bf, (sq, k)], writes=[bank[0]])
            P.act(lambda e: e.activation(out=inv[:], in_=bank[0][:], func=AF.Sqrt, scale=1.0 / D, bias=eps_t[:, 0:1]), reads=[bank[0], eps_t], writes=[inv])
            P.dve(lambda e: e.reciprocal(out=inv[:], in_=inv[:]), reads=[inv], writes=[inv])
            for k in range(KD):
                P.dve(lambda e, k=k: e.scalar_tensor_tensor(out=hfin[:, k, :], in0=xg[:, k, :], scalar=gain_fin[:, k:k + 1], in1=inv[:], op0=ALU.mult, op1=ALU.mult),
                      reads=[(xg, k), inv, gain_fin], writes=[(hfin, k)])
            for t in range(4):
                ob = osb[no % 2]
                no += 1
                for kb in range(4):
                    pp = bank[4 + (kb % 4)]
                    for kq in range(4):
                        k = kb * 4 + kq
                        P.pe(lambda e, pp=pp, k=k, kq=kq, t=t: e.transpose(pp[:, kq * 128:(kq + 1) * 128], hfin[:, k, t * 128:(t + 1) * 128], ident[:]),
                             reads=[(hfin, k), ident], writes=[pp])
                    P.act(lambda e, pp=pp, ob=ob, kb=kb: e.copy(out=ob[:, kb * 512:(kb + 1) * 512], in_=pp[:, :]), reads=[pp], writes=[ob])
                tok0 = g * 512 + t * 128
                P.dma(out_ap[tok0:tok0 + 128, :], ob[:, :], reads=[ob], writes=["out"], q="pool")

    if "moe" in phases:
        for ex in range(8):
            cast(w_exp_bf[0][ex], w_exp_gate[0, ex], D, "w_exp_bf")
            cast(w_exp_bf[1][ex], w_exp_up[0, ex], D, "w_exp_bf")
            cast(w_exp_bf[2][ex], w_exp_down[0, ex], 7168, "w_exp_bf", 512)
        if dbg.get("dense_moe"):
            with P.phase():
                p3b_moe(0)
        else:
            moe_routed(0)
        dump("x2T", xT[:, :], [D, S], F32, [])
    for l in range(L):
        casts_layer(l)
    for l in range(L):
        if "p1" in phases:
            with P.phase():
                p1(l)
        if "swa" in phases:
            with P.phase():
                swa(l)
        if "mla" in phases:
            with P.phase():
                mla1(l)
            with P.phase():
                mla2(l)
        if "dn" in phases:
            with P.phase():
                dn(l)
        if "p3" in phases:
            with P.phase():
                p3a(l)
            if dbg.get("x1") and l == 0:
                dump("x1T", xT[:, :], [D, S], F32, [])
            if l % 2 == 0:
                with P.phase():
                    p3b_dense(l)
            elif dbg.get("dense_moe"):
                with P.phase():
                    p3b_moe(l)
            else:
                moe_routed(l)
            if dbg.get("x1") and l == 0:
                dump("x2T", xT[:, :], [D, S], F32, [])
            with P.phase():
                p3c(l, l == L - 1)

    if dbg.get("projT"):
        dump("projT", projT[:, :], [C_G, S], F32, keys("projT"))
        dump("gatesT", gatesT[:, :], [3 * D, S], BF16, keys("gatesT"))
    if dbg.get("xT"):
        dump("xT", xT[:, :], [D, S], F32, keys("xT"))
    if dbg.get("yT"):
        dump("yT", yT[:, :], [3072, S], BF16, keys("yT_a") + keys("yT_b") + keys("yT_c"))
    if dbg.get("mlaqk"):
        dump("qnT", qnT[:, :], [1024, S], BF16, keys("qnT"))
        dump("qrT", qrT[:, :], [512, S], BF16, keys("qrT"))
        dump("krT", krT[:, :], [64, S], BF16, keys("krT"))
        dump("cosT", cosT[:, :], [64, S], F32, keys("cosT"))

    P.finish()
    return nc, stack, P


_CACHE = {}
_WEIGHTS = ["norm_mix", "w_in", "swa_sinks", "mla_q_norm", "mla_kv_norm", "w_uq", "w_ukv", "conv_w", "dn_a_log", "dn_dt_bias",
            "dn_norm", "w_branch", "w_out", "norm_ffn", "w_ffn_gate", "w_ffn_up", "w_ffn_down", "norm_ple", "w_ple_gate",
            "w_ple_proj", "w_router", "w_exp_gate", "w_exp_up", "w_exp_down"]


def kernel(**inputs):
    S, L = 8192, 2
    if "prog" not in _CACHE:
        _CACHE["prog"] = build(S, L)
    nc, stack, P = _CACHE["prog"]
    consts = host_consts()
    shared = {k: np.ascontiguousarray(np.asarray(inputs[k], dtype=np.float32)) for k in _WEIGHTS}
    shared["final_norm"] = np.ascontiguousarray(np.asarray(inputs["final_norm"], dtype=np.float32)[None])
    in_maps = []
    for b in range(2):
        m = dict(consts)
        m.update(shared)
        m["x"] = np.ascontiguousarray(np.asarray(inputs["x"], dtype=np.float32)[b])
        m["positions"] = np.ascontiguousarray(np.asarray(inputs["positions"])[b:b + 1].astype(np.int32))
        m["p"] = np.ascontiguousarray(np.asarray(inputs["p"], dtype=np.float32)[:, b])
        in_maps.append(m)
    res = run_bass_kernel_spmd(nc, in_maps, core_ids=[0, 1])
    return np.stack([np.asarray(res.results[b]["out"], dtype=np.float32) for b in range(2)])
```

```python
import contextlib
import numpy as np
import concourse.bass as bass
import concourse.mybir as mybir
from concourse.bass_utils import run_bass_kernel_spmd

F32 = mybir.dt.float32
BF16 = mybir.dt.bfloat16
I32 = mybir.dt.int32
ALU = mybir.AluOpType
AF = mybir.ActivationFunctionType
AX = mybir.AxisListType

D = 2048
KD = D // 128
IN_COLS = 12880
EPS = 1e-6

C_AQ, C_AK, C_AV = 0, 1024, 1280
C_BQKV, C_BZ, C_BB, C_BD = 1536, 4608, 5632, 5640
C_CQ, C_CKV, C_CKR, C_G = 5648, 6160, 6672, 6736


class Op:
    __slots__ = ("eng", "fn", "reads", "writes", "dma", "deps", "signal", "val", "sem", "lhs")

    def __init__(self, eng, fn, reads, writes, dma):
        self.eng, self.fn, self.reads, self.writes, self.dma = eng, fn, reads, writes, dma
        self.deps = ()
        self.signal = False
        self.val = 0
        self.sem = None
        self.lhs = None


def _norm(k):
    if isinstance(k, tuple):
        return tuple(_norm(x) for x in k)
    if isinstance(k, (str, int)):
        return k
    return "@" + k.name


class Prog:
    COMPUTE = ("pe", "act", "dve", "pool")

    def __init__(self, nc, stack):
        self.nc = nc
        self.stack = stack
        self.gstack = stack
        self.ops = []
        self.last_w = {}
        self.readers = {}
        self.n_alloc = 0

    def sb(self, shape, dtype, name=None):
        self.n_alloc += 1
        return self.stack.enter_context(self.nc.sbuf_tensor((name or "sb") + f"_{self.n_alloc}", list(shape), dtype))

    def ps(self, shape, dtype=F32, name=None):
        self.n_alloc += 1
        return self.stack.enter_context(self.nc.psum_tensor(name or f"ps{self.n_alloc}", list(shape), dtype))

    def dram(self, shape, dtype, name=None):
        self.n_alloc += 1
        return self.nc.dram_tensor(name or f"dr{self.n_alloc}", list(shape), dtype).ap()

    def _add(self, eng, fn, reads, writes, dma):
        rr = [_norm(r) for r in reads]
        ww = [_norm(w) for w in writes]
        ww += [r for r in rr if isinstance(r, tuple) and r[0] == "bank"]
        rr = [r for r in rr if not (isinstance(r, tuple) and r[0] == "bank")]
        op = Op(eng, fn, tuple(rr), tuple(ww), dma)
        deps = set()
        me = len(self.ops)
        for r in op.reads:
            w = self.last_w.get(r)
            if w is not None:
                deps.add((w, True))
        for w_ in op.writes:
            w = self.last_w.get(w_)
            if w is not None:
                deps.add((w, False))
            for rd in self.readers.get(w_, ()):
                deps.add((rd, False))
        keep = set()
        for (d, raw) in deps:
            dop = self.ops[d]
            if not dop.dma and dop.eng == eng and not dma:
                if eng == "pe" or not raw:
                    continue
            keep.add(d)
        op.deps = tuple(sorted(keep))
        if eng == "pe" and op.reads:
            lw = self.last_w.get(op.reads[0])
            if lw is not None and lw in keep:
                op.lhs = lw
        for d in op.deps:
            self.ops[d].signal = True
        for w_ in op.writes:
            self.last_w[w_] = me
            self.readers[w_] = []
        for r in op.reads:
            lst = self.readers.setdefault(r, [])
            if not dma:
                lst[:] = [x for x in lst if self.ops[x].dma or self.ops[x].eng != eng]
            lst.append(me)
        self.ops.append(op)
        return op

    def pe(self, fn, reads=(), writes=()):
        return self._add("pe", fn, reads, writes, False)

    def act(self, fn, reads=(), writes=()):
        return self._add("act", fn, reads, writes, False)

    def dve(self, fn, reads=(), writes=()):
        return self._add("dve", fn, reads, writes, False)

    def pool(self, fn, reads=(), writes=()):
        return self._add("pool", fn, reads, writes, False)

    def indirect(self, fn, reads=(), writes=()):
        op = self._add("pool", fn, reads, writes, True)
        op.lhs = "ind"
        return op

    def dma(self, out, in_, reads=(), writes=(), q="sp", slow=False):
        if slow:
            return self._add(q, lambda e: e.dma_start(out=out, in_=in_, allow_slow_non_contiguous=True), reads, writes, True)
        return self._add(q, lambda e: e.dma_start(out=out, in_=in_), reads, writes, True)

    def barrier(self):
        last = {}
        for i in range(len(self.ops) - 1, -1, -1):
            op = self.ops[i]
            if op.eng == "bar":
                break
            if not op.dma and op.eng not in last:
                last[op.eng] = i
                op.signal = True
            if len(last) == 4:
                break
        b = Op("bar", None, (), (), False)
        b.deps = tuple(last.values())
        self.ops.append(b)
        self.last_w = {}
        self.readers = {}

    @contextlib.contextmanager
    def phase(self):
        st = contextlib.ExitStack()
        old = self.stack
        self.stack = st
        try:
            yield
        finally:
            self.barrier()
            self.flush()
            st.close()
            self.stack = old

    def _init_emit(self, n_dma_sems=40):
        nc = self.nc
        self.engs = {"pe": nc.tensor, "act": nc.scalar, "dve": nc.vector, "pool": nc.gpsimd, "sp": nc.sync}
        st = self.gstack
        self.esem = {e: st.enter_context(nc.semaphore(f"s_{e}")) for e in self.COMPUTE}
        self.ecnt = {e: 0 for e in self.COMPUTE}
        self.dsem = [st.enter_context(nc.semaphore(f"s_d{i}")) for i in range(n_dma_sems)]
        self.dcnt = [0] * n_dma_sems
        self.dnext = 0
        self.waited = {}
        self.emitted = 0
        self.ind_hist = []

    def _wait(self, eng_name, sem, key, val):
        if self.waited.get((eng_name, key), 0) >= val:
            return
        self.waited[(eng_name, key)] = val
        self.engs[eng_name].wait_ge(sem, val)

    def flush(self):
        if not hasattr(self, "engs"):
            self._init_emit()
        engs, esem, ecnt, dsem, dcnt = self.engs, self.esem, self.ecnt, self.dsem, self.dcnt
        nd = len(dsem)
        for idx in range(self.emitted, len(self.ops)):
            op = self.ops[idx]
            en = op.eng
            if en == "bar":
                for q in ("pe", "act", "dve", "pool", "sp"):
                    for d in op.deps:
                        dop = self.ops[d]
                        if dop.eng != q:
                            self._wait(q, dop.sem[1], dop.sem[0], dop.val)
                    for k in range(nd):
                        if dcnt[k] > 0:
                            self._wait(q, dsem[k], ("d", k), 16 * dcnt[k])
                continue
            need = {}
            for d in op.deps:
                dop = self.ops[d]
                key, sem = dop.sem
                if key not in need or need[key][1] < dop.val:
                    need[key] = (sem, dop.val)
            for key, (sem, val) in need.items():
                self._wait(en, sem, key, val)
            if op.dma:
                if op.lhs == "ind":
                    self.ind_hist.append(None)
                    if len(self.ind_hist) > 6 and self.ind_hist[-7] is not None:
                        ksem, kkey, kval = self.ind_hist[-7]
                        self._wait(en, ksem, kkey, kval)
                k = self.dnext
                self.dnext = (self.dnext + 1) % nd
                if dcnt[k] > 0:
                    self._wait(en, dsem[k], ("d", k), 16 * dcnt[k])
                ins = op.fn(engs[en])
                dcnt[k] += 1
                ins.then_inc(dsem[k], 16)
                op.sem = (("d", k), dsem[k])
                op.val = 16 * dcnt[k]
                if op.lhs == "ind":
                    self.ind_hist[-1] = (dsem[k], ("d", k), op.val)
            else:
                ins = op.fn(engs[en])
                if op.lhs is not None:
                    dop = self.ops[op.lhs]
                    ins._wait_ge(dop.sem[1], dop.val)
                if op.signal:
                    ecnt[en] += 1
                    ins.then_inc(esem[en], 1)
                    op.sem = (("e", en), esem[en])
                    op.val = ecnt[en]
            op.fn = None
        self.emitted = len(self.ops)

    def finish(self):
        self.barrier()
        self.flush()
        self.counts = dict(self.ecnt)


def host_consts():
    c = {}
    c["ident_in"] = np.eye(128, dtype=np.float32)
    k = np.arange(128)[:, None]
    q = np.arange(128)[None, :]
    cur = (k <= q).astype(np.float32)
    prev = (k > q).astype(np.float32)
    c["m_swa"] = np.ascontiguousarray(np.stack([np.tile(prev, (1, 4)), np.tile(cur, (1, 4))]))
    q5 = np.arange(512)[None, :]
    c["m_mla"] = np.ascontiguousarray(np.stack([((128 * jj + k) <= q5).astype(np.float32) for jj in range(4)]))
    rot = np.zeros((64, 64), np.float32)
    for m in range(32):
        rot[m + 32, m] = -1.0
    for m in range(32, 64):
        rot[m - 32, m] = 1.0
    c["rot_in"] = rot
    c["tri_in"] = np.ascontiguousarray(np.stack([cur, prev]))
    sel = np.zeros((8, 8, 128), np.float32)
    for e_ in range(8):
        sel[e_, e_, :] = 1.0
    c["sel_in"] = sel
    c["tokid_in"] = (np.arange(64)[None, :] * 128 + np.arange(128)[:, None]).astype(np.float32)
    c["inv_freq"] = (10000.0 ** (-(np.arange(64) % 32) / 32.0)).astype(np.float32)[:, None]
    return c


def build(S, n_layers=2, dbg=None, phases=("p1", "swa", "mla", "dn", "p3")):
    dbg = dbg or {}
    nc = bass.Bass("TRN2", target_bir_lowering=False)
    NG = S // 512
    NT = S // 128
    stack = contextlib.ExitStack()
    P = Prog(nc, stack)
    L = n_layers

    def ext_in(name, shape, dt=F32):
        return nc.dram_tensor(name, list(shape), dt, kind="ExternalInput").ap()

    def ext_out(name, shape, dt=F32):
        return nc.dram_tensor(name, list(shape), dt, kind="ExternalOutput").ap()

    x_in = ext_in("x", [S, D])
    pos_in = ext_in("positions", [1, S], I32)
    ident_in = ext_in("ident_in", [128, 128])
    m_swa_in = ext_in("m_swa", [2, 128, 512])
    m_mla_in = ext_in("m_mla", [4, 128, 512])
    rot_in = ext_in("rot_in", [64, 64])
    inv_freq_in = ext_in("inv_freq", [64, 1])
    norm_mix = ext_in("norm_mix", [L, D])
    w_in = ext_in("w_in", [L, D, IN_COLS])
    swa_sinks = ext_in("swa_sinks", [L, 16])
    mla_q_norm = ext_in("mla_q_norm", [L, 512])
    mla_kv_norm = ext_in("mla_kv_norm", [L, 512])
    w_uq = ext_in("w_uq", [L, 512, 1536])
    w_ukv = ext_in("w_ukv", [L, 512, 2048])
    tri_in = ext_in("tri_in", [2, 128, 128])
    sel_in = ext_in("sel_in", [8, 8, 128])
    tokid_in = ext_in("tokid_in", [128, 64])
    p_in = ext_in("p", [L, S, 256])
    w_branch = ext_in("w_branch", [L, 3, 1024, D])
    w_out = ext_in("w_out", [L, D, D])
    norm_ffn = ext_in("norm_ffn", [L, D])
    w_ffn_gate = ext_in("w_ffn_gate", [1, D, 7168])
    w_ffn_up = ext_in("w_ffn_up", [1, D, 7168])
    w_ffn_down = ext_in("w_ffn_down", [1, 7168, D])
    norm_ple = ext_in("norm_ple", [L, D])
    w_ple_gate = ext_in("w_ple_gate", [L, D, D])
    w_ple_proj = ext_in("w_ple_proj", [L, 256, D])
    final_norm = ext_in("final_norm", [1, D])
    if L > 1 or "moe" in phases:
        w_router = ext_in("w_router", [1, D, 8])
        w_exp_gate = ext_in("w_exp_gate", [1, 8, D, 7168])
        w_exp_up = ext_in("w_exp_up", [1, 8, D, 7168])
        w_exp_down = ext_in("w_exp_down", [1, 8, 7168, D])
    out_ap = ext_out("out", [S, D])
    conv_w = ext_in("conv_w", [L, 4, 3072])
    dn_a_log = ext_in("dn_a_log", [L, 8])
    dn_dt_bias = ext_in("dn_dt_bias", [L, 8])
    dn_norm = ext_in("dn_norm", [L, 128])

    bank = [P.ps([128, 512], F32, f"bank{i}") for i in range(8)]

    ident = P.sb([128, 128], F32, "ident")
    P.dma(ident[:], ident_in[:, :], writes=[ident])
    eps_t = P.sb([128, 1], F32, "eps_t")
    P.pool(lambda e: e.memset(eps_t[:], EPS), writes=[eps_t])
    ones_bf = P.sb([128, 128], BF16, "ones_bf")
    P.pool(lambda e: e.memset(ones_bf[:], 1.0), writes=[ones_bf])
    m_swa = P.sb([128, 2, 512], BF16, "m_swa_t")
    m_mla = P.sb([128, 4, 512], BF16, "m_mla_t")
    for i in range(2):
        P.dma(m_swa[:, i, :], m_swa_in[i], writes=[m_swa], q="pool")
    for i in range(4):
        P.dma(m_mla[:, i, :], m_mla_in[i], writes=[m_mla], q="pool")
    rot = P.sb([64, 64], F32, "rot_t")
    P.dma(rot[:], rot_in[:, :], writes=[rot])
    inv_freq = P.sb([64, 1], F32, "inv_freq_t")
    P.dma(inv_freq[:], inv_freq_in[:, :], writes=[inv_freq])

    xT = P.dram([D, S], F32, "xT")
    projT = P.dram([C_G, S], F32, "projT")
    gatesT = P.dram([3 * D, S], BF16, "gatesT")
    vA = P.dram([S, 256], BF16, "vA")
    bd = P.dram([S, 16], F32, "bd")
    yT = P.dram([3 * 1024, S], BF16, "yT")
    cosT = P.dram([64, S], F32, "cosT")
    sinT = P.dram([64, S], F32, "sinT")
    qnT = P.dram([1024, S], BF16, "qnT")
    qrT = P.dram([512, S], BF16, "qrT")
    knT = P.dram([1024, S], BF16, "knT")
    krT = P.dram([64, S], BF16, "krT")
    vC = P.dram([S, 1024], BF16, "vC")
    w_in_bf = [P.dram([D, IN_COLS], BF16, f"w_in_bf{l}") for l in range(L)]
    w_branch_bf = [P.dram([3072, D], BF16, f"w_branch_bf{l}") for l in range(L)]
    w_out_bf = [P.dram([D, D], BF16, f"w_out_bf{l}") for l in range(L)]
    w_pg_bf = [P.dram([D, D], BF16, f"w_pg_bf{l}") for l in range(L)]
    w_pp_bf = [P.dram([256, D], BF16, f"w_pp_bf{l}") for l in range(L)]
    w_ffn_bf = [P.dram([D, 7168], BF16, "w_ffn_bf_g"), P.dram([D, 7168], BF16, "w_ffn_bf_u"), P.dram([7168, D], BF16, "w_ffn_bf_d")]
    if L > 1 or "moe" in phases:
        w_exp_bf = [P.dram([8, D, 7168], BF16, "w_exp_bf_g"), P.dram([8, D, 7168], BF16, "w_exp_bf_u"), P.dram([8, 7168, D], BF16, "w_exp_bf_d")]
    pT = [P.dram([256, S], BF16, f"pT{l}") for l in range(L)]

    ALLG = list(range(NG))

    def keys(name, gs=None):
        return [(name, g) for g in (ALLG if gs is None else gs)]

    def cast(dst, src, rows, key, step=256):
        for r in range(0, rows, step):
            P.dma(dst[r:r + step, :], src[r:r + step, :], writes=[key], q="pool")

    def expert_cast_jobs():
        jobs = []
        for ex in range(8):
            for (dst, src, rows, step) in ((w_exp_bf[0][ex], w_exp_gate[0, ex], D, 256), (w_exp_bf[1][ex], w_exp_up[0, ex], D, 256),
                                           (w_exp_bf[2][ex], w_exp_down[0, ex], 7168, 512)):
                for r in range(0, rows, step):
                    jobs.append((dst[r:r + step, :], src[r:r + step, :]))
        return jobs

    def casts_layer(l):
        cast(w_in_bf[l], w_in[l], D, ("w_in_bf", l))
        if "p3" in phases:
            cast(w_branch_bf[l], w_branch[l].rearrange("n k d -> (n k) d"), 3072, ("w_branch_bf", l), 512)
            cast(w_out_bf[l], w_out[l], D, ("w_out_bf", l), 512)
            if l == 0:
                cast(w_ffn_bf[0], w_ffn_gate[0], D, "w_ffn_bf")
                cast(w_ffn_bf[1], w_ffn_up[0], D, "w_ffn_bf")
                cast(w_ffn_bf[2], w_ffn_down[0], 7168, "w_ffn_bf", 512)
            cast(w_pg_bf[l], w_ple_gate[l], D, ("w_pg_bf", l), 512)
            cast(w_pp_bf[l], w_ple_proj[l], 256, ("w_pp_bf", l))

    with P.phase():
        xin4 = [P.sb([128, D], F32, f"xin4_{i}") for i in range(4)]
        tp_sb = [P.sb([128, 512], F32, f"tp_sb{i}") for i in range(2)]
        for g in range(NG):
            for t in range(4):
                tok0 = g * 512 + t * 128
                P.dma(xin4[t][:], x_in[tok0:tok0 + 128, :], writes=[xin4[t]])
            for k in range(KD):
                pp = bank[k % 2]
                sbuf = tp_sb[k % 2]
                for t in range(4):
                    P.pe(lambda e, pp=pp, t=t, k=k: e.transpose(pp[:, t * 128:(t + 1) * 128], xin4[t][:, k * 128:(k + 1) * 128], ident[:]),
                         reads=[xin4[t], ident], writes=[pp])
                P.act(lambda e, pp=pp, sbuf=sbuf: e.copy(out=sbuf[:], in_=pp[:]), reads=[pp], writes=[sbuf])
                P.dma(xT[k * 128:(k + 1) * 128, g * 512:(g + 1) * 512], sbuf[:], reads=[sbuf], writes=[("xT", g)], q="pool")
        if "p3" in phases:
            pin = [P.sb([128, 256], F32, f"pin{i}") for i in range(4)]
            psb = [P.sb([128, 512], BF16, f"psb{i}") for i in range(2)]
            np_ = 0
            for l in range(L):
                for g in range(NG):
                    for t in range(4):
                        tok0 = g * 512 + t * 128
                        P.dma(pin[t][:], p_in[l, tok0:tok0 + 128, :], writes=[pin[t]])
                    for kk in range(2):
                        pp = bank[2 + np_ % 2]
                        sbuf = psb[np_ % 2]
                        np_ += 1
                        for t in range(4):
                            P.pe(lambda e, pp=pp, t=t, kk=kk: e.transpose(pp[:, t * 128:(t + 1) * 128], pin[t][:, kk * 128:(kk + 1) * 128], ident[:]),
                                 reads=[pin[t], ident], writes=[pp])
                        P.act(lambda e, pp=pp, sbuf=sbuf: e.copy(out=sbuf[:], in_=pp[:]), reads=[pp], writes=[sbuf])
                        P.dma(pT[l][kk * 128:(kk + 1) * 128, g * 512:(g + 1) * 512], sbuf[:], reads=[sbuf], writes=[("pT", l)], q="pool")

    if "mla" in phases:
      with P.phase():
        pos_i = P.sb([64, 512], I32, "pos_i")
        ang = P.sb([64, 512], F32, "ang")
        ang2 = P.sb([64, 512], F32, "ang2")
        pos_k = P.sb([64, 512], I32, "pos_k")
        trig = [P.sb([64, 512], F32, f"trig{i}") for i in range(2)]
        negpi = P.sb([64, 1], F32, "negpi")
        P.pool(lambda e: e.memset(negpi[:], -float(np.pi)), writes=[negpi])
        TWO_PI = float(2 * np.pi)
        for g in range(NG):
            P.dma(pos_i[:], pos_in[0:1, g * 512:(g + 1) * 512].partition_broadcast(64), writes=[pos_i], slow=True)
            P.dve(lambda e: e.tensor_copy(out=ang[:], in_=pos_i[:]), reads=[pos_i], writes=[ang])
            P.dve(lambda e: e.tensor_scalar(out=ang[:], in0=ang[:], scalar1=inv_freq[:, 0:1], scalar2=1.0, op0=ALU.mult, op1=ALU.mult),
                  reads=[ang, inv_freq], writes=[ang])
            for which, shift, dst_d in ((0, 0.0, sinT), (1, float(np.pi / 2), cosT)):
                tr = trig[which]
                P.dve(lambda e, shift=shift: e.tensor_scalar(out=ang2[:], in0=ang[:], scalar1=shift, scalar2=float(1.0 / (2 * np.pi)), op0=ALU.add, op1=ALU.mult),
                      reads=[ang], writes=[ang2])
                P.dve(lambda e: e.tensor_copy(out=pos_k[:], in_=ang2[:]), reads=[ang2], writes=[pos_k])
                P.dve(lambda e: e.tensor_copy(out=ang2[:], in_=pos_k[:]), reads=[pos_k], writes=[ang2])
                P.dve(lambda e, tr=tr, shift=shift: e.tensor_scalar(out=tr[:], in0=ang[:], scalar1=shift, scalar2=1.0, op0=ALU.add, op1=ALU.mult),
                      reads=[ang], writes=[tr])
                P.dve(lambda e, tr=tr: e.scalar_tensor_tensor(out=tr[:], in0=ang2[:], scalar=-6.28125, in1=tr[:], op0=ALU.mult, op1=ALU.add),
                      reads=[ang2, tr], writes=[tr])
                P.dve(lambda e, tr=tr: e.scalar_tensor_tensor(out=tr[:], in0=ang2[:], scalar=-0.0019353071795864769, in1=tr[:], op0=ALU.mult, op1=ALU.add),
                      reads=[ang2, tr], writes=[tr])
                P.dve(lambda e, tr=tr: e.tensor_scalar(out=ang2[:], in0=tr[:], scalar1=float(np.pi), scalar2=TWO_PI, op0=ALU.is_gt, op1=ALU.mult),
                      reads=[tr], writes=[ang2])
                P.dve(lambda e, tr=tr: e.tensor_tensor(out=tr[:], in0=tr[:], in1=ang2[:], op=ALU.subtract), reads=[tr, ang2], writes=[tr])
                P.dve(lambda e, tr=tr: e.tensor_scalar(out=ang2[:], in0=tr[:], scalar1=-float(np.pi), scalar2=TWO_PI, op0=ALU.is_lt, op1=ALU.mult),
                      reads=[tr], writes=[ang2])
                P.dve(lambda e, tr=tr: e.tensor_tensor(out=tr[:], in0=tr[:], in1=ang2[:], op=ALU.add), reads=[tr, ang2], writes=[tr])
                P.dve(lambda e, tr=tr: e.tensor_scalar(out=tr[:], in0=tr[:], scalar1=float(np.pi), scalar2=-float(np.pi), op0=ALU.min, op1=ALU.max),
                      reads=[tr], writes=[tr])
                P.act(lambda e, tr=tr: e.activation(out=tr[:], in_=tr[:], func=AF.Sin), reads=[tr], writes=[tr])
                P.dma(dst_d[:, g * 512:(g + 1) * 512], tr[:], reads=[tr], writes=[("sinT" if which == 0 else "cosT", g)], q="pool")

    gain_mix = P.sb([128, L, KD], F32, "gain_mix")
    gain_q = P.sb([128, L, 4], F32, "gain_q")
    gain_kv = P.sb([128, L, 4], F32, "gain_kv")
    esink = P.sb([64, L, 16], F32, "esink")
    gain_ffn = P.sb([128, L, KD], F32, "gain_ffn")
    gain_ple = P.sb([128, L, KD], F32, "gain_ple")
    gain_fin = P.sb([128, KD], F32, "gain_fin")
    Wall = P.sb([128, NT, 8], F32, "Wall")
    Iall = P.sb([128, NT, 8], I32, "Iall")
    P.dma(gain_fin[:, :], final_norm[0].rearrange("(k p) -> p k", p=128), writes=[gain_fin], slow=True)
    for l in range(L):
        P.dma(gain_mix[:, l, :], norm_mix[l].rearrange("(k p) -> p k", p=128), writes=[gain_mix], slow=True)
        P.dma(gain_ffn[:, l, :], norm_ffn[l].rearrange("(k p) -> p k", p=128), writes=[gain_ffn], slow=True)
        P.dma(gain_ple[:, l, :], norm_ple[l].rearrange("(k p) -> p k", p=128), writes=[gain_ple], slow=True)
        P.dma(gain_q[:, l, :], mla_q_norm[l].rearrange("(k p) -> p k", p=128), writes=[gain_q], slow=True)
        P.dma(gain_kv[:, l, :], mla_kv_norm[l].rearrange("(k p) -> p k", p=128), writes=[gain_kv], slow=True)
        P.dma(esink[:, l, :], swa_sinks[l:l + 1, :].partition_broadcast(64), writes=[esink], slow=True)
    P.act(lambda e: e.activation(out=esink[:], in_=esink[:], func=AF.Exp), reads=[esink], writes=[esink])

    def rms_T(src, dst, nk, dim, gain, gkey, ss_bank, sq, inv):
        for k in range(nk):
            P.act(lambda e, k=k: e.activation(out=sq[:, k, :], in_=src[:, k, :], func=AF.Square), reads=[(src, k)], writes=[(sq, k)])
        for k in range(nk):
            P.pe(lambda e, k=k: e.matmul(ss_bank[:], lhsT=ones_bf[:], rhs=sq[:, k, :], start=(k == 0), stop=(k == nk - 1)),
                 reads=[ones_bf, (sq, k)], writes=[ss_bank])
        P.act(lambda e: e.activation(out=inv[:], in_=ss_bank[:], func=AF.Sqrt, scale=1.0 / dim, bias=eps_t[:, 0:1]),
              reads=[ss_bank, eps_t], writes=[inv])
        P.dve(lambda e: e.reciprocal(out=inv[:], in_=inv[:]), reads=[inv], writes=[inv])
        for k in range(nk):
            P.dve(lambda e, k=k: e.scalar_tensor_tensor(out=dst[:, k, :], in0=src[:, k, :], scalar=gain[:, k:k + 1], in1=inv[:],
                                                        op0=ALU.mult, op1=ALU.mult),
                  reads=[(src, k), inv, gkey], writes=[(dst, k)])

    blocks = [(0, 512, "f"), (512, 512, "f"), (C_AK, 256, "f"), (C_AV, 256, "t")] + \
             [(c, 512, "f") for c in range(C_BQKV, C_BB, 512)] + \
             [(C_BB, 16, "t"), (C_CQ, 512, "f"), (C_CKV, 512, "f"), (C_CKR, 64, "f")] + \
             [(c, 512, "f") for c in range(C_G, IN_COLS, 512)]

    cnt = {"blk": 0, "ev": 0}

    def p1(l):
        xg = P.sb([128, KD, 512], F32, "xg")
        sq = P.sb([128, KD, 512], BF16, "sq")
        hT = P.sb([128, KD, 512], BF16, "hT")
        inv = P.sb([128, 512], F32, "inv")
        wblk = [P.sb([128, KD, 512], BF16, f"wblk{i}") for i in range(2)]
        ev_sb = [P.sb([128, 512], F32, f"ev_sb{i}") for i in range(3)]
        evb_sb = [P.sb([128, 512], BF16, f"evb_sb{i}") for i in range(3)]
        for g in range(NG):
            for k in range(KD):
                P.dma(xg[:, k, :], xT[k * 128:(k + 1) * 128, g * 512:(g + 1) * 512], reads=[("xT", g)], writes=[(xg, k)])
            rms_T(xg, hT, KD, D, gain_mix[:, l, :], gain_mix, bank[2], sq, inv)
            for (c0, wd, mode) in blocks:
                wb = wblk[cnt["blk"] % 2]
                cnt["blk"] += 1
                for k in range(KD):
                    P.dma(wb[:, k, 0:wd], w_in_bf[l][k * 128:(k + 1) * 128, c0:c0 + wd], reads=[("w_in_bf", l)], writes=[wb])
                if mode == "t":
                    for t in range(4):
                        pp = bank[cnt["ev"] % 2]
                        for k in range(KD):
                            P.pe(lambda e, pp=pp, wb=wb, k=k, t=t, wd=wd: e.matmul(pp[:, 0:wd], lhsT=hT[:, k, t * 128:(t + 1) * 128], rhs=wb[:, k, 0:wd],
                                                                                     start=(k == 0), stop=(k == KD - 1)),
                                 reads=[(hT, k), wb], writes=[pp])
                        tok0 = g * 512 + t * 128
                        if c0 == C_AV:
                            ob = evb_sb[cnt["ev"] % 3]
                            P.act(lambda e, pp=pp, ob=ob, wd=wd: e.copy(out=ob[:, 0:wd], in_=pp[:, 0:wd]), reads=[pp], writes=[ob])
                            P.dma(vA[tok0:tok0 + 128, :], ob[:, 0:wd], reads=[ob], writes=[("vA", g)], q="pool")
                        else:
                            ob = ev_sb[cnt["ev"] % 3]
                            P.act(lambda e, pp=pp, ob=ob, wd=wd: e.copy(out=ob[:, 0:wd], in_=pp[:, 0:wd]), reads=[pp], writes=[ob])
                            P.dma(bd[tok0:tok0 + 128, :], ob[:, 0:wd], reads=[ob], writes=[("bd", g)], q="pool")
                        cnt["ev"] += 1
                    continue
                for j in range(0, wd, 128):
                    cw = min(128, wd - j)
                    pp = bank[cnt["ev"] % 2]
                    for k in range(KD):
                        P.pe(lambda e, pp=pp, wb=wb, k=k, j=j, cw=cw: e.matmul(pp[0:cw, :], lhsT=wb[:, k, j:j + cw], rhs=hT[:, k, :],
                                                                                start=(k == 0), stop=(k == KD - 1)),
                             reads=[wb, (hT, k)], writes=[pp])
                    col = c0 + j
                    if col >= C_G:
                        ob = evb_sb[cnt["ev"] % 3]
                        P.act(lambda e, pp=pp, ob=ob, cw=cw: e.activation(out=ob[0:cw, :], in_=pp[0:cw, :], func=AF.Sigmoid),
                              reads=[pp], writes=[ob])
                        P.dma(gatesT[col - C_G:col - C_G + cw, g * 512:(g + 1) * 512], ob[0:cw, :], reads=[ob],
                              writes=[("gatesT", g)], q="pool")
                    else:
                        ob = ev_sb[cnt["ev"] % 3]
                        P.act(lambda e, pp=pp, ob=ob, cw=cw: e.copy(out=ob[0:cw, :], in_=pp[0:cw, :]), reads=[pp], writes=[ob])
                        P.dma(projT[col:col + cw, g * 512:(g + 1) * 512], ob[0:cw, :], reads=[ob], writes=[("projT", g)], q="pool")
                    cnt["ev"] += 1

    def swa(l):
        swa_kT = P.sb([64, S], BF16, "swa_kT")
        swa_v = P.sb([128, NT, 64], BF16, "swa_v")
        swa_q = [P.sb([64, 4, 512], BF16, f"swa_q{i}") for i in range(2)]
        swa_p = [P.sb([128, 512], BF16, f"swa_p{i}") for i in range(4)]
        swa_den = P.sb([64, 512], F32, "swa_den")
        swa_y = [P.sb([64, 4, 512], BF16, f"swa_y{i}") for i in range(2)]

        npt = 0
        for hk in range(4):
            P.dma(swa_kT[:, :], projT[C_AK + hk * 64:C_AK + (hk + 1) * 64, :], reads=keys("projT"), writes=[swa_kT], q="pool")
            P.dma(swa_v[:, :, :], vA[:, hk * 64:(hk + 1) * 64].rearrange("(t p) d -> p t d", p=128), reads=keys("vA"), writes=[swa_v], slow=True)
            for g in range(NG):
                qt = swa_q[g % 2]
                yb = swa_y[g % 2]
                P.dma(qt[:, :, :], projT[hk * 256:(hk + 1) * 256, g * 512:(g + 1) * 512].rearrange("(i d) t -> d i t", d=64),
                      reads=[("projT", g)], writes=[qt], q="pool")
                for b in range(4):
                    n = g * 4 + b
                    o_ps = bank[4 + (n % 2)]
                    s_ps = bank[6 + (n % 2)]
                    jl = [j for j in (n - 1, n) if j >= 0]
                    for idx, j in enumerate(jl):
                        sc = bank[npt % 4]
                        pt = swa_p[npt % 4]
                        npt += 1
                        P.pe(lambda e, sc=sc, j=j, qt=qt, b=b: e.matmul(sc[:, :].rearrange("p (i t) -> p i t", i=4), lhsT=swa_kT[:, j * 128:(j + 1) * 128],
                                                                         rhs=qt[:, :, b * 128:(b + 1) * 128], start=True, stop=True),
                             reads=[swa_kT, qt], writes=[sc])
                        P.act(lambda e, sc=sc, pt=pt: e.activation(out=pt[:], in_=sc[:], func=AF.Exp, scale=0.125), reads=[sc], writes=[pt])
                        mi = 1 if j == n else 0
                        P.dve(lambda e, pt=pt, mi=mi: e.tensor_tensor(out=pt[:], in0=pt[:], in1=m_swa[:, mi, :], op=ALU.mult),
                              reads=[pt, m_swa], writes=[pt])
                        first, last = idx == 0, idx == len(jl) - 1
                        P.pe(lambda e, o_ps=o_ps, pt=pt, j=j, first=first, last=last: e.matmul(o_ps[0:64, :], lhsT=swa_v[:, j, :], rhs=pt[:], start=first, stop=last),
                             reads=[swa_v, pt], writes=[o_ps])
                        P.pe(lambda e, s_ps=s_ps, pt=pt, first=first, last=last: e.matmul(s_ps[0:64, :], lhsT=ones_bf[:, 0:64], rhs=pt[:], start=first, stop=last),
                             reads=[ones_bf, pt], writes=[s_ps])
                    for i in range(4):
                        h = hk * 4 + i
                        P.dve(lambda e, s_ps=s_ps, i=i, h=h: e.tensor_scalar(out=swa_den[:, i * 128:(i + 1) * 128], in0=s_ps[0:64, i * 128:(i + 1) * 128],
                                                                               scalar1=esink[:, l, h:h + 1], scalar2=1.0, op0=ALU.add, op1=ALU.mult),
                              reads=[s_ps, esink], writes=[swa_den])
                    P.dve(lambda e: e.reciprocal(out=swa_den[:], in_=swa_den[:]), reads=[swa_den], writes=[swa_den])
                    P.dve(lambda e, o_ps=o_ps, yb=yb, b=b: e.tensor_tensor(out=yb[:, :, b * 128:(b + 1) * 128],
                                                                           in0=o_ps[0:64, :].rearrange("p (i t) -> p i t", i=4),
                                                                           in1=swa_den[:, :].rearrange("p (i t) -> p i t", i=4), op=ALU.mult),
                          reads=[o_ps, swa_den], writes=[yb])
                P.dma(yT[hk * 256:(hk + 1) * 256, g * 512:(g + 1) * 512].rearrange("(i d) t -> d i t", d=64), yb[:, :, :],
                      reads=[yb], writes=[("yT_a", g)], q="pool")

    def mla1(l):
        wuq_sb = P.sb([128, 4, 1536], BF16, "wuq_sb")
        wukv_sb = P.sb([128, 4, 2048], BF16, "wukv_sb")
        lat = P.sb([128, 4, 512], F32, "lat")
        latn = [P.sb([128, 4, 512], BF16, f"latn{i}") for i in range(2)]
        sq = P.sb([128, 4, 512], BF16, "sq_m")
        inv = P.sb([128, 512], F32, "inv_m")
        rope_x = P.sb([64, 512], F32, "rope_x")
        rope_t = P.sb([64, 512], F32, "rope_t")
        rope_o = [P.sb([64, 512], BF16, f"rope_o{i}") for i in range(2)]
        cs_sb = P.sb([64, 2, 512], F32, "cs_sb")
        evb_sb = [P.sb([128, 512], BF16, f"evb_m{i}") for i in range(3)]
        def rope(src_ps_or_sb, src_key, nrows_dummy, dst, g):
            P.pe(lambda e: e.matmul(bank[3][0:64, :], lhsT=rot[:, :], rhs=rope_x[:, :], start=True, stop=True), reads=[rot, rope_x], writes=[bank[3]])
            P.dve(lambda e: e.tensor_tensor(out=rope_t[:], in0=bank[3][0:64, :], in1=cs_sb[:, 1, :], op=ALU.mult), reads=[bank[3], cs_sb], writes=[rope_t])
            P.dve(lambda e: e.tensor_tensor(out=rope_x[:], in0=rope_x[:], in1=cs_sb[:, 0, :], op=ALU.mult), reads=[rope_x, cs_sb], writes=[rope_x])
            P.dve(lambda e, dst=dst: e.tensor_tensor(out=dst[:], in0=rope_x[:], in1=rope_t[:], op=ALU.add), reads=[rope_x, rope_t], writes=[dst])

        for k in range(4):
            P.dma(wuq_sb[:, k, :], w_uq[l, k * 128:(k + 1) * 128, :], writes=[wuq_sb], q="pool")
            P.dma(wukv_sb[:, k, :], w_ukv[l, k * 128:(k + 1) * 128, :], writes=[wukv_sb], q="pool")
        nev = 0
        for g in range(NG):
            gs = slice(g * 512, (g + 1) * 512)
            P.dma(cs_sb[:, 0, :], cosT[:, gs], reads=[("cosT", g)], writes=[cs_sb])
            P.dma(cs_sb[:, 1, :], sinT[:, gs], reads=[("sinT", g)], writes=[cs_sb])
            for k in range(4):
                P.dma(lat[:, k, :], projT[C_CQ + k * 128:C_CQ + (k + 1) * 128, gs], reads=[("projT", g)], writes=[(lat, k)])
            rms_T(lat, latn[0], 4, 512, gain_q[:, l, :], gain_q, bank[2], sq, inv)
            for h in range(8):
                pp = bank[nev % 2]
                for k in range(4):
                    P.pe(lambda e, pp=pp, k=k, h=h: e.matmul(pp[:, :], lhsT=wuq_sb[:, k, h * 192:h * 192 + 128], rhs=latn[0][:, k, :], start=(k == 0), stop=(k == 3)),
                         reads=[wuq_sb, (latn[0], k)], writes=[pp])
                ob = evb_sb[nev % 3]
                P.act(lambda e, pp=pp, ob=ob: e.copy(out=ob[:], in_=pp[:]), reads=[pp], writes=[ob])
                P.dma(qnT[h * 128:(h + 1) * 128, gs], ob[:], reads=[ob], writes=[("qnT", g)], q="pool")
                nev += 1
                pp = bank[nev % 2]
                for k in range(4):
                    P.pe(lambda e, pp=pp, k=k, h=h: e.matmul(pp[0:64, :], lhsT=wuq_sb[:, k, h * 192 + 128:h * 192 + 192], rhs=latn[0][:, k, :], start=(k == 0), stop=(k == 3)),
                         reads=[wuq_sb, (latn[0], k)], writes=[pp])
                P.act(lambda e, pp=pp: e.copy(out=rope_x[:], in_=pp[0:64, :]), reads=[pp], writes=[rope_x])
                ro = rope_o[nev % 2]
                rope(None, None, None, ro, g)
                P.dma(qrT[h * 64:(h + 1) * 64, gs], ro[:], reads=[ro], writes=[("qrT", g)], q="pool")
                nev += 1
            P.dma(rope_x[:], projT[C_CKR:C_CKR + 64, gs], reads=[("projT", g)], writes=[rope_x])
            ro = rope_o[nev % 2]
            rope(None, None, None, ro, g)
            P.dma(krT[:, gs], ro[:], reads=[ro], writes=[("krT", g)], q="pool")
            nev += 1
            for k in range(4):
                P.dma(lat[:, k, :], projT[C_CKV + k * 128:C_CKV + (k + 1) * 128, gs], reads=[("projT", g)], writes=[(lat, k)])
            rms_T(lat, latn[1], 4, 512, gain_kv[:, l, :], gain_kv, bank[2], sq, inv)
            for h in range(8):
                pp = bank[nev % 2]
                for k in range(4):
                    P.pe(lambda e, pp=pp, k=k, h=h: e.matmul(pp[:, :], lhsT=wukv_sb[:, k, h * 256:h * 256 + 128], rhs=latn[1][:, k, :], start=(k == 0), stop=(k == 3)),
                         reads=[wukv_sb, (latn[1], k)], writes=[pp])
                ob = evb_sb[nev % 3]
                P.act(lambda e, pp=pp, ob=ob: e.copy(out=ob[:], in_=pp[:]), reads=[pp], writes=[ob])
                P.dma(knT[h * 128:(h + 1) * 128, gs], ob[:], reads=[ob], writes=[("knT", g)], q="pool")
                nev += 1
            for t in range(4):
                tok0 = g * 512 + t * 128
                for hh in range(2):
                    pp = bank[nev % 2]
                    for k in range(4):
                        P.pe(lambda e, pp=pp, k=k, t=t, hh=hh: e.matmul(pp[:, :].rearrange("p (h d) -> p h d", h=4), lhsT=latn[1][:, k, t * 128:(t + 1) * 128],
                                                                         rhs=wukv_sb[:, k, :].rearrange("p (h c) -> p h c", h=8)[:, hh * 4:(hh + 1) * 4, 128:256],
                                                                         start=(k == 0), stop=(k == 3)),
                             reads=[(latn[1], k), wukv_sb], writes=[pp])
                    ob = evb_sb[nev % 3]
                    P.act(lambda e, pp=pp, ob=ob: e.copy(out=ob[:], in_=pp[:]), reads=[pp], writes=[ob])
                    P.dma(vC[tok0:tok0 + 128, hh * 512:(hh + 1) * 512], ob[:], reads=[ob], writes=[("vC", g)], q="pool")
                    nev += 1

    def mla2(l):
        mla_k = P.sb([128, S], BF16, "mla_k")
        mla_kr = P.sb([64, S], BF16, "mla_kr")
        mla_v = P.sb([128, NT, 128], BF16, "mla_v")
        mla_qn = [P.sb([128, 512], BF16, f"mla_qn{i}") for i in range(2)]
        mla_qr = [P.sb([64, 512], BF16, f"mla_qr{i}") for i in range(2)]
        mla_p = [P.sb([128, 512], BF16, f"mla_p{i}") for i in range(4)]
        mla_rs = P.sb([128, 512], F32, "mla_rs")
        mla_y = [P.sb([128, 512], BF16, f"mla_y{i}") for i in range(2)]
        P.dma(mla_kr[:, :], krT[:, :], reads=keys("krT"), writes=[mla_kr])
        scale = float(192 ** -0.5)
        npt = 0
        nq = 0
        for h in range(8):
            P.dma(mla_k[:, :], knT[h * 128:(h + 1) * 128, :], reads=keys("knT"), writes=[mla_k])
            P.dma(mla_v[:, :, :], vC[:, h * 128:(h + 1) * 128].rearrange("(t p) d -> p t d", p=128), reads=keys("vC"), writes=[mla_v], slow=True)
            for g in range(NG):
                gs = slice(g * 512, (g + 1) * 512)
                qn = mla_qn[nq % 2]
                qr = mla_qr[nq % 2]
                yb = mla_y[nq % 2]
                o_ps = bank[4 + (nq % 2)]
                s_ps = bank[6 + (nq % 2)]
                nq += 1
                P.dma(qn[:, :], qnT[h * 128:(h + 1) * 128, gs], reads=[("qnT", g)], writes=[qn])
                P.dma(qr[:, :], qrT[h * 64:(h + 1) * 64, gs], reads=[("qrT", g)], writes=[qr])
                nj = 4 * g + 4

                def qk(j, slot):
                    sc = bank[slot % 4]
                    P.pe(lambda e, sc=sc, j=j, qn=qn: e.matmul(sc[:, :], lhsT=mla_k[:, j * 128:(j + 1) * 128], rhs=qn[:, :], start=True, stop=False),
                         reads=[mla_k, qn], writes=[sc])
                    P.pe(lambda e, sc=sc, j=j, qr=qr: e.matmul(sc[:, :], lhsT=mla_kr[:, j * 128:(j + 1) * 128], rhs=qr[:, :], start=False, stop=True),
                         reads=[mla_kr, qr], writes=[sc])

                qk(0, npt)
                for j in range(nj):
                    sc = bank[npt % 4]
                    pt = mla_p[npt % 4]
                    if j + 1 < nj:
                        qk(j + 1, npt + 1)
                    npt += 1
                    P.act(lambda e, sc=sc, pt=pt: e.activation(out=pt[:], in_=sc[:], func=AF.Exp, scale=scale), reads=[sc], writes=[pt])
                    if j >= 4 * g:
                        jj = j - 4 * g
                        P.dve(lambda e, pt=pt, jj=jj: e.tensor_tensor(out=pt[:], in0=pt[:], in1=m_mla[:, jj, :], op=ALU.mult),
                              reads=[pt, m_mla], writes=[pt])
                    first, last = j == 0, j == nj - 1
                    P.pe(lambda e, o_ps=o_ps, pt=pt, j=j, first=first, last=last: e.matmul(o_ps[:, :], lhsT=mla_v[:, j, :], rhs=pt[:], start=first, stop=last),
                         reads=[mla_v, pt], writes=[o_ps])
                    P.pe(lambda e, s_ps=s_ps, pt=pt, first=first, last=last: e.matmul(s_ps[:, :], lhsT=ones_bf[:, :], rhs=pt[:], start=first, stop=last),
                         reads=[ones_bf, pt], writes=[s_ps])
                P.dve(lambda e, s_ps=s_ps: e.reciprocal(out=mla_rs[:], in_=s_ps[:]), reads=[s_ps], writes=[mla_rs])
                P.dve(lambda e, o_ps=o_ps, yb=yb: e.tensor_tensor(out=yb[:], in0=o_ps[:], in1=mla_rs[:], op=ALU.mult), reads=[o_ps, mla_rs], writes=[yb])
                P.dma(yT[2048 + h * 128:2048 + (h + 1) * 128, gs], yb[:], reads=[yb], writes=[("yT_c", g)], q="pool")

    def dn(l):
        NB = 4
        stop = dbg.get('dn_stop', 99)
        ones_f = P.sb([128, 128], F32, "ones_f")
        P.pool(lambda e: e.memset(ones_f[:], 1.0), writes=[ones_f])
        one_t = P.sb([128, 1], F32, "one_t")
        P.pool(lambda e: e.memset(one_t[:], 1.0), writes=[one_t])
        eps6 = P.sb([128, 1], F32, "eps6")
        P.pool(lambda e: e.memset(eps6[:], 1e-6), writes=[eps6])
        tri = P.sb([128, 2, 128], F32, "tri")
        P.dma(tri[:, 0, :], tri_in[0], writes=[tri])
        P.dma(tri[:, 1, :], tri_in[1], writes=[tri])
        U = tri[:, 0, :]
        TL = tri[:, 1, :]
        convw = P.sb([128, 4, 24], F32, "convw")
        for j in range(4):
            P.dma(convw[:, j, :], conv_w[l, j].rearrange("(c p) -> p c", p=128), writes=[convw], slow=True)
        dtb = P.sb([128, 8], F32, "dtb")
        nA = P.sb([128, 8], F32, "nA")
        gn = P.sb([128, 1], F32, "gn")
        P.dma(dtb[:, :], dn_dt_bias[l:l + 1, :].partition_broadcast(128), writes=[dtb], slow=True)
        P.dma(nA[:, :], dn_a_log[l:l + 1, :].partition_broadcast(128), writes=[nA], slow=True)
        P.dma(gn[:, :], dn_norm[l].rearrange("(p o) -> p o", o=1), writes=[gn], slow=True)
        P.act(lambda e: e.activation(out=nA[:], in_=nA[:], func=AF.Exp), reads=[nA], writes=[nA])
        P.dve(lambda e: e.tensor_scalar(out=nA[:], in0=nA[:], scalar1=-1.0, scalar2=0.0, op0=ALU.mult, op1=ALU.add), reads=[nA], writes=[nA])
        bdt = P.sb([128, NT, 16], F32, "bdt")
        P.dma(bdt[:, :, :], bd[:, :].rearrange("(t p) c -> p t c", p=128), reads=keys("bd"), writes=[bdt], slow=True)
        betat = P.sb([128, NT, 8], F32, "betat")
        gt = P.sb([128, NT, 8], F32, "gt")
        P.act(lambda e: e.activation(out=betat[:, :, :], in_=bdt[:, :, 0:8], func=AF.Sigmoid), reads=[bdt], writes=[betat])
        for t in range(NT):
            P.dve(lambda e, t=t: e.tensor_tensor(out=gt[:, t, :], in0=bdt[:, t, 8:16], in1=dtb[:, :], op=ALU.add), reads=[bdt, dtb], writes=[gt])
        P.act(lambda e: e.activation(out=gt[:, :, :], in_=gt[:, :, :], func=AF.Exp), reads=[gt], writes=[gt])
        P.act(lambda e: e.activation(out=gt[:, :, :], in_=gt[:, :, :], func=AF.Ln, bias=one_t[:, 0:1], scale=1.0), reads=[gt, one_t], writes=[gt])
        for t in range(NT):
            P.dve(lambda e, t=t: e.tensor_tensor(out=gt[:, t, :], in0=gt[:, t, :], in1=nA[:, :], op=ALU.mult), reads=[gt, nA], writes=[gt])

        if stop <= 1:
            return
        if dbg.get("dn_dump"):
            dd = {n: ext_out("o_dn" + n, [1024, S]) for n in ("q", "k", "v", "o")}
            dgb = ext_out("o_dngb", [128, NT, 16])
            P.dma(dgb[:, :, 0:8], gt[:, :, :], reads=[gt], writes=["o_dngb"])
            P.dma(dgb[:, :, 8:16], betat[:, :, :], reads=[betat], writes=["o_dngb"])
        xin = [P.sb([128, 515], F32, f"dn_xin{i}") for i in range(2)]
        cacc = P.sb([128, 512], F32, "dn_cacc")
        qkvT = [P.sb([128, 512], F32, f"dn_qkvT{i}") for i in range(3)]
        sqb = P.sb([128, 512], BF16, "dn_sqb")
        rinv = P.sb([128, 512], F32, "dn_rinv")
        zT = P.sb([128, 512], F32, "dn_zT")
        oT = P.sb([128, 512], F32, "dn_oT")
        yb = [P.sb([128, 512], BF16, f"dn_yb{i}") for i in range(2)]
        St = P.sb([128, 128], F32, "dn_S")
        names = ["ktok", "vtok", "gcrow", "gccol", "t1", "t2", "Lm", "NTm", "Pa", "PaT", "Pb", "PbT", "R", "qkT", "qgT", "kbg", "vb", "kd",
                 "egrow", "sc1", "sc2", "u", "wT", "vnew"]
        W = [{n: P.sb([128, 128] if n not in ("gccol", "sc1", "sc2") else [128, 1], F32, f"dn_{n}{b}") for n in names} for b in range(NB)]
        nps = [0]

        def psq():
            i = nps[0] % 28
            nps[0] += 1
            bi, qi = i % 7, (i // 7) % 4
            return bank[bi][:, qi * 128:(qi + 1) * 128], ("bank", bi)

        def mmf(lhsT, rhs, rd, n=128):
            pp, key = psq()
            P.pe(lambda e, pp=pp, lhsT=lhsT, rhs=rhs, n=n: e.matmul(pp[:, 0:n], lhsT=lhsT, rhs=rhs, start=True, stop=True), reads=rd, writes=[key])
            return pp, key

        def tpf(src, rd):
            pp, key = psq()
            P.pe(lambda e, pp=pp, src=src: e.transpose(pp, src, ident[:]), reads=rd + [ident], writes=[key])
            return pp, key

        for h in range(8):
            P.dve(lambda e: e.memset(St[:], 0.0), writes=[St])
            for g in range(NG):
                gs = slice(g * 512, (g + 1) * 512)
                for ci in range(3):
                    chunk = ci * 8 + h
                    row0 = C_BQKV + chunk * 128
                    xi = xin[ci % 2]
                    if g == 0:
                        P.dve(lambda e, xi=xi: e.memset(xi[:, 0:3], 0.0), writes=[xi])
                        P.dma(xi[:, 3:515], projT[row0:row0 + 128, 0:512], reads=[("projT", 0)], writes=[xi])
                    else:
                        P.dma(xi[:, 0:515], projT[row0:row0 + 128, g * 512 - 3:(g + 1) * 512], reads=[("projT", g - 1), ("projT", g)], writes=[xi])
                    P.act(lambda e, xi=xi, chunk=chunk: e.activation(out=cacc[:], in_=xi[:, 0:512], func=AF.Copy, scale=convw[:, 0, chunk:chunk + 1]),
                          reads=[xi, convw], writes=[cacc])
                    for j in range(1, 4):
                        P.dve(lambda e, xi=xi, chunk=chunk, j=j: e.scalar_tensor_tensor(out=cacc[:], in0=xi[:, j:j + 512], scalar=convw[:, j, chunk:chunk + 1], in1=cacc[:],
                                                                                       op0=ALU.mult, op1=ALU.add), reads=[xi, convw, cacc], writes=[cacc])
                    dst = qkvT[ci]
                    P.act(lambda e, dst=dst: e.activation(out=dst[:], in_=cacc[:], func=AF.Silu), reads=[cacc], writes=[dst])
                    if ci < 2:
                        P.act(lambda e, dst=dst: e.activation(out=sqb[:], in_=dst[:], func=AF.Square), reads=[dst], writes=[sqb])
                        P.pe(lambda e: e.matmul(bank[7][:, :], lhsT=ones_bf[:], rhs=sqb[:], start=True, stop=True), reads=[ones_bf, sqb], writes=[("bank", 7)])
                        P.act(lambda e: e.activation(out=rinv[:], in_=bank[7][:, :], func=AF.Sqrt, bias=eps6[:, 0:1], scale=1.0),
                              reads=[("bank", 7), eps6], writes=[rinv])
                        P.dve(lambda e: e.reciprocal(out=rinv[:], in_=rinv[:]), reads=[rinv], writes=[rinv])
                        sc = float(128 ** -0.5) if ci == 0 else 1.0
                        P.dve(lambda e, dst=dst, sc=sc: e.scalar_tensor_tensor(out=dst[:], in0=dst[:], scalar=sc, in1=rinv[:], op0=ALU.mult, op1=ALU.mult),
                              reads=[dst, rinv], writes=[dst])
                qT_, kT_, vT_ = qkvT
                if dbg.get("dn_dump"):
                    for n_, t_ in (("q", qT_), ("k", kT_), ("v", vT_)):
                        P.dma(dd[n_][h * 128:(h + 1) * 128, gs], t_[:, :], reads=[t_], writes=["o_dn" + n_], q="pool")
                P.dma(zT[:, :], projT[C_BZ + h * 128:C_BZ + (h + 1) * 128, gs], reads=[("projT", g)], writes=[zT])
                P.act(lambda e: e.activation(out=zT[:], in_=zT[:], func=AF.Silu), reads=[zT], writes=[zT])
                if stop <= 2:
                    return
                for b in range(NB):
                    w = W[b]
                    t = g * 4 + b
                    cs = slice(b * 128, (b + 1) * 128)
                    gcol = gt[:, t, h:h + 1]
                    bcol = betat[:, t, h:h + 1]
                    pp, key = tpf(kT_[:, cs], [kT_])
                    P.act(lambda e, pp=pp, w=w: e.copy(out=w["ktok"][:], in_=pp), reads=[key], writes=[w["ktok"]])
                    pp, key = tpf(vT_[:, cs], [vT_])
                    P.act(lambda e, pp=pp, w=w: e.copy(out=w["vtok"][:], in_=pp), reads=[key], writes=[w["vtok"]])
                    P.dve(lambda e, w=w, gcol=gcol: e.tensor_scalar(out=w["t1"][:], in0=U, scalar1=gcol, scalar2=1.0, op0=ALU.mult, op1=ALU.mult), reads=[tri, gt], writes=[w["t1"]])
                    pp, key = mmf(ones_f[:, :], w["t1"][:], [ones_f, w["t1"]])
                    P.act(lambda e, pp=pp, w=w: e.copy(out=w["gcrow"][:], in_=pp), reads=[key], writes=[w["gcrow"]])
                    P.dve(lambda e, w=w: e.tensor_tensor(out=w["t2"][:], in0=w["gcrow"][:], in1=ident[:], op=ALU.mult), reads=[w["gcrow"], ident], writes=[w["t2"]])
                    P.dve(lambda e, w=w: e.reduce_sum(out=w["gccol"][:], in_=w["t2"][:], axis=AX.X), reads=[w["t2"]], writes=[w["gccol"]])
                    P.dve(lambda e, w=w: e.tensor_scalar(out=w["t1"][:], in0=w["gcrow"][:], scalar1=-1.0, scalar2=w["gccol"][:, 0:1], op0=ALU.mult, op1=ALU.add),
                          reads=[w["gcrow"], w["gccol"]], writes=[w["t1"]])
                    P.dve(lambda e, w=w: e.tensor_scalar(out=w["t1"][:], in0=w["t1"][:], scalar1=0.0, scalar2=1.0, op0=ALU.min, op1=ALU.mult), reads=[w["t1"]], writes=[w["t1"]])
                    P.act(lambda e, w=w: e.activation(out=w["t1"][:], in_=w["t1"][:], func=AF.Exp), reads=[w["t1"]], writes=[w["t1"]])
                    P.dve(lambda e, w=w, bcol=bcol: e.scalar_tensor_tensor(out=w["t1"][:], in0=w["t1"][:], scalar=bcol, in1=TL, op0=ALU.mult, op1=ALU.mult),
                          reads=[w["t1"], betat, tri], writes=[w["t1"]])
                    pp, key = mmf(kT_[:, cs], kT_[:, cs], [kT_])
                    P.dve(lambda e, pp=pp, w=w: e.tensor_tensor(out=w["Lm"][:], in0=pp, in1=w["t1"][:], op=ALU.mult), reads=[key, w["t1"]], writes=[w["Lm"]])
                    P.dve(lambda e, w=w: e.tensor_scalar(out=w["t2"][:], in0=w["gcrow"][:], scalar1=w["gccol"][:, 0:1], scalar2=0.0, op0=ALU.subtract, op1=ALU.min),
                          reads=[w["gcrow"], w["gccol"]], writes=[w["t2"]])
                    P.act(lambda e, w=w: e.activation(out=w["t2"][:], in_=w["t2"][:], func=AF.Exp), reads=[w["t2"]], writes=[w["t2"]])
                    P.dve(lambda e, w=w: e.tensor_tensor(out=w["t2"][:], in0=w["t2"][:], in1=U, op=ALU.mult), reads=[w["t2"], tri], writes=[w["t2"]])
                    pp, key = mmf(kT_[:, cs], qT_[:, cs], [kT_, qT_])
                    P.dve(lambda e, pp=pp, w=w: e.tensor_tensor(out=w["qkT"][:], in0=pp, in1=w["t2"][:], op=ALU.mult), reads=[key, w["t2"]], writes=[w["qkT"]])
                    P.act(lambda e, w=w: e.activation(out=w["egrow"][:], in_=w["gcrow"][:], func=AF.Exp), reads=[w["gcrow"]], writes=[w["egrow"]])
                    P.dve(lambda e, w=w, cs=cs: e.tensor_tensor(out=w["qgT"][:], in0=qT_[:, cs], in1=w["egrow"][:], op=ALU.mult), reads=[qT_, w["egrow"]], writes=[w["qgT"]])
                    P.act(lambda e, w=w: e.activation(out=w["sc1"][:], in_=w["gccol"][:], func=AF.Exp), reads=[w["gccol"]], writes=[w["sc1"]])
                    P.dve(lambda e, w=w, bcol=bcol: e.tensor_tensor(out=w["sc1"][:], in0=w["sc1"][:], in1=bcol, op=ALU.mult), reads=[w["sc1"], betat], writes=[w["sc1"]])
                    P.dve(lambda e, w=w: e.tensor_scalar(out=w["kbg"][:], in0=w["ktok"][:], scalar1=w["sc1"][:, 0:1], scalar2=1.0, op0=ALU.mult, op1=ALU.mult),
                          reads=[w["ktok"], w["sc1"]], writes=[w["kbg"]])
                    P.dve(lambda e, w=w, bcol=bcol: e.tensor_scalar(out=w["vb"][:], in0=w["vtok"][:], scalar1=bcol, scalar2=1.0, op0=ALU.mult, op1=ALU.mult),
                          reads=[w["vtok"], betat], writes=[w["vb"]])
                    P.act(lambda e, w=w: e.activation(out=w["sc2"][:], in_=w["gccol"][:], func=AF.Exp, scale=-1.0, bias=w["gcrow"][:, 127:128]),
                          reads=[w["gccol"], w["gcrow"]], writes=[w["sc2"]])
                    P.dve(lambda e, w=w: e.tensor_scalar(out=w["kd"][:], in0=w["ktok"][:], scalar1=w["sc2"][:, 0:1], scalar2=1.0, op0=ALU.mult, op1=ALU.mult),
                          reads=[w["ktok"], w["sc2"]], writes=[w["kd"]])
                    pp, key = tpf(w["Lm"][:], [w["Lm"]])
                    P.act(lambda e, pp=pp, w=w: e.copy(out=w["NTm"][:], in_=pp), reads=[key], writes=[w["NTm"]])
                    P.dve(lambda e, pp=pp, w=w: e.tensor_tensor(out=w["R"][:], in0=ident[:], in1=pp, op=ALU.subtract), reads=[key, ident], writes=[w["R"]])
                if stop <= 3:
                    return
                cur = [(W[b]["Lm"], W[b]["NTm"]) for b in range(NB)]
                for m in range(1, 7):
                    nxt = []
                    res = []
                    for b in range(NB):
                        w = W[b]
                        M_, MT_ = cur[b]
                        p1_, k1 = mmf(MT_[:], M_[:], [MT_, M_])
                        if m < 6:
                            p2_, k2 = mmf(M_[:], MT_[:], [M_, MT_])
                        else:
                            p2_, k2 = None, None
                        res.append((p1_, k1, p2_, k2))
                    for b in range(NB):
                        w = W[b]
                        p1_, k1, p2_, k2 = res[b]
                        Pn = w["Pa"] if m % 2 == 1 else w["Pb"]
                        PnT = w["PaT"] if m % 2 == 1 else w["PbT"]
                        P.act(lambda e, p1_=p1_, Pn=Pn: e.copy(out=Pn[:], in_=p1_), reads=[k1], writes=[Pn])
                        if m < 6:
                            P.dve(lambda e, p2_=p2_, PnT=PnT: e.tensor_copy(out=PnT[:], in_=p2_), reads=[k2], writes=[PnT])
                        nxt.append((Pn, PnT))
                    if dbg.get("dn_bar"):
                        P.barrier()
                    if dbg.get("dn_dump") and h == 1 and g == 0:
                        if m == 1:
                            oP = ext_out("o_dnP", [6, 4, 128, 128]); oPT = ext_out("o_dnPT", [6, 4, 128, 128]); oR = ext_out("o_dnR", [6, 4, 128, 128])
                        for b_ in range(NB):
                            P.dma(oP[m - 1, b_], nxt[b_][0][:, :], reads=[nxt[b_][0]], writes=["o_dnP"], q="pool")
                            if m < 6:
                                P.dma(oPT[m - 1, b_], nxt[b_][1][:, :], reads=[nxt[b_][1]], writes=["o_dnPT"], q="pool")
                            P.dma(oR[m - 1, b_], W[b_]["R"][:, :], reads=[W[b_]["R"]], writes=["o_dnR"], q="pool")
                    res = []
                    for b in range(NB):
                        w = W[b]
                        Pn, PnT = nxt[b]
                        res.append(mmf(Pn[:], w["R"][:], [Pn, w["R"]]))
                    for b in range(NB):
                        w = W[b]
                        pp, key = res[b]
                        P.dve(lambda e, pp=pp, w=w: e.tensor_tensor(out=w["R"][:], in0=w["R"][:], in1=pp, op=ALU.add), reads=[key, w["R"]], writes=[w["R"]])
                    cur = nxt
                if stop <= 4:
                    return
                if dbg.get("dn_dump") and h == 1 and g == 0:
                    for nm_ in ("R", "Lm", "NTm", "Pa", "PaT", "Pb", "PbT"):
                        oo = ext_out("o_dnW_" + nm_, [4, 128, 128])
                        for b_ in range(NB):
                            P.dma(oo[b_], W[b_][nm_][:, :], reads=[W[b_][nm_]], writes=["o_dnW_" + nm_], q="pool")
                for b in range(NB):
                    w = W[b]
                    pp, key = mmf(w["R"][:], w["vb"][:], [w["R"], w["vb"]])
                    P.act(lambda e, pp=pp, w=w: e.copy(out=w["u"][:], in_=pp), reads=[key], writes=[w["u"]])
                    pp, key = mmf(w["kbg"][:], w["R"][:], [w["kbg"], w["R"]])
                    P.act(lambda e, pp=pp, w=w: e.copy(out=w["wT"][:], in_=pp), reads=[key], writes=[w["wT"]])
                if stop <= 5:
                    return
                for b in range(NB):
                    w = W[b]
                    cs = slice(b * 128, (b + 1) * 128)
                    pp, key = mmf(w["wT"][:], St[:], [w["wT"], St])
                    P.dve(lambda e, pp=pp, w=w: e.tensor_tensor(out=w["vnew"][:], in0=w["u"][:], in1=pp, op=ALU.subtract), reads=[key, w["u"]], writes=[w["vnew"]])
                    po, keyo = psq()
                    P.pe(lambda e, po=po, w=w: e.matmul(po, lhsT=St[:], rhs=w["qgT"][:], start=True, stop=False), reads=[St, w["qgT"]], writes=[keyo])
                    P.pe(lambda e, po=po, w=w: e.matmul(po, lhsT=w["vnew"][:], rhs=w["qkT"][:], start=False, stop=True), reads=[w["vnew"], w["qkT"]], writes=[keyo])
                    P.act(lambda e, po=po, cs=cs: e.copy(out=oT[:, cs], in_=po), reads=[keyo], writes=[oT])
                    pp, key = mmf(w["kd"][:], w["vnew"][:], [w["kd"], w["vnew"]])
                    P.dve(lambda e, pp=pp, w=w: e.scalar_tensor_tensor(out=St[:], in0=St[:], scalar=w["egrow"][:, 127:128], in1=pp, op0=ALU.mult, op1=ALU.add),
                          reads=[key, St, w["egrow"]], writes=[St])
                if stop <= 6:
                    return
                if dbg.get("dn_dump"):
                    P.dma(dd["o"][h * 128:(h + 1) * 128, gs], oT[:, :], reads=[oT], writes=["o_dno"], q="pool")
                P.act(lambda e: e.activation(out=sqb[:], in_=oT[:], func=AF.Square), reads=[oT], writes=[sqb])
                bkeys = [("bank", 7)]
                P.pe(lambda e: e.matmul(bank[7][:, :], lhsT=ones_bf[:], rhs=sqb[:], start=True, stop=True), reads=[ones_bf, sqb], writes=bkeys)
                P.act(lambda e: e.activation(out=rinv[:], in_=bank[7][:, :], func=AF.Sqrt, bias=eps_t[:, 0:1], scale=1.0 / 128), reads=bkeys + [eps_t], writes=[rinv])
                P.dve(lambda e: e.reciprocal(out=rinv[:], in_=rinv[:]), reads=[rinv], writes=[rinv])
                P.dve(lambda e: e.scalar_tensor_tensor(out=oT[:], in0=oT[:], scalar=gn[:, 0:1], in1=rinv[:], op0=ALU.mult, op1=ALU.mult), reads=[oT, gn, rinv], writes=[oT])
                ybt = yb[g % 2]
                P.dve(lambda e, ybt=ybt: e.tensor_tensor(out=ybt[:], in0=oT[:], in1=zT[:], op=ALU.mult), reads=[oT, zT], writes=[ybt])
                P.dma(yT[1024 + h * 128:1024 + (h + 1) * 128, gs], ybt[:], reads=[ybt], writes=[("yT_b", g)], q="pool")

    def dump(name, src, shape, dt, rkeys):
        o = ext_out("o_" + name, shape, dt)
        P.dma(o, src, reads=rkeys, writes=["o_" + name])

    def load_x(xg, g):
        for k in range(KD):
            P.dma(xg[:, k, :], xT[k * 128:(k + 1) * 128, g * 512:(g + 1) * 512], reads=[("xT", g)], writes=[(xg, k)])

    def store_x(xg, g):
        for k in range(KD):
            P.dma(xT[k * 128:(k + 1) * 128, g * 512:(g + 1) * 512], xg[:, k, :], reads=[(xg, k)], writes=[("xT", g)], q="pool")

    def p3a(l):
        xg = P.sb([128, KD, 512], F32, "xg")
        yTt = P.sb([128, 24, 512], BF16, "yTt")
        gat = P.sb([128, KD, 512], BF16, "gat")
        merged = P.sb([128, KD, 512], F32, "merged")
        mergedb = P.sb([128, KD, 512], BF16, "mergedb")
        tmp = [P.sb([128, 512], F32, f"p3tmp{i}") for i in range(2)]
        wblk = [P.sb([128, KD, 512], BF16, f"wblk{i}") for i in range(2)]
        nb = 0
        ne = 0
        for g in range(NG):
            gs = slice(g * 512, (g + 1) * 512)
            load_x(xg, g)
            for c in range(24):
                P.dma(yTt[:, c, :], yT[c * 128:(c + 1) * 128, gs], reads=[("yT_a", g), ("yT_b", g), ("yT_c", g)], writes=[(yTt, c)])
            for n in range(3):
                for c in range(KD):
                    P.dma(gat[:, c, :], gatesT[n * D + c * 128:n * D + (c + 1) * 128, gs], reads=[("gatesT", g)], writes=[(gat, c)])
                for cb in range(4):
                    wb = wblk[nb % 2]
                    nb += 1
                    for k in range(8):
                        P.dma(wb[:, k, :], w_branch_bf[l][n * 1024 + k * 128:n * 1024 + (k + 1) * 128, cb * 512:(cb + 1) * 512],
                              reads=[("w_branch_bf", l)], writes=[wb])
                    for j in range(4):
                        dc = cb * 4 + j
                        pp = bank[ne % 4]
                        ne += 1
                        for k in range(8):
                            P.pe(lambda e, pp=pp, wb=wb, k=k, j=j, n=n: e.matmul(pp[:, :], lhsT=wb[:, k, j * 128:(j + 1) * 128], rhs=yTt[:, n * 8 + k, :],
                                                                                start=(k == 0), stop=(k == 7)), reads=[wb, (yTt, n * 8 + k)], writes=[pp])
                        if n == 0:
                            P.dve(lambda e, pp=pp, dc=dc: e.tensor_tensor(out=merged[:, dc, :], in0=pp[:, :], in1=gat[:, dc, :], op=ALU.mult),
                                  reads=[pp, (gat, dc)], writes=[(merged, dc)])
                        else:
                            tt = tmp[ne % 2]
                            P.dve(lambda e, pp=pp, dc=dc, tt=tt: e.tensor_tensor(out=tt[:], in0=pp[:, :], in1=gat[:, dc, :], op=ALU.mult),
                                  reads=[pp, (gat, dc)], writes=[tt])
                            P.pool(lambda e, dc=dc, tt=tt: e.tensor_tensor(out=merged[:, dc, :], in0=merged[:, dc, :], in1=tt[:], op=ALU.add),
                                   reads=[tt, (merged, dc)], writes=[(merged, dc)])
            for dc in range(KD):
                P.act(lambda e, dc=dc: e.copy(out=mergedb[:, dc, :], in_=merged[:, dc, :]), reads=[(merged, dc)], writes=[(mergedb, dc)])
            for cb in range(4):
                wb = wblk[nb % 2]
                nb += 1
                for k in range(KD):
                    P.dma(wb[:, k, :], w_out_bf[l][k * 128:(k + 1) * 128, cb * 512:(cb + 1) * 512], reads=[("w_out_bf", l)], writes=[wb])
                for j in range(4):
                    dc = cb * 4 + j
                    pp = bank[ne % 4]
                    ne += 1
                    for k in range(KD):
                        P.pe(lambda e, pp=pp, wb=wb, k=k, j=j: e.matmul(pp[:, :], lhsT=wb[:, k, j * 128:(j + 1) * 128], rhs=mergedb[:, k, :],
                                                                         start=(k == 0), stop=(k == KD - 1)), reads=[wb, (mergedb, k)], writes=[pp])
                    P.dve(lambda e, pp=pp, dc=dc: e.tensor_tensor(out=xg[:, dc, :], in0=xg[:, dc, :], in1=pp[:, :], op=ALU.add),
                          reads=[pp, (xg, dc)], writes=[(xg, dc)])
            store_x(xg, g)

    def ffn_core(wg_bf, wu_bf, wd_bf, wkey, hT, xg, hid, wblk, sgt, st, wbc=None):
        for fb in range(14):
            wbg = wblk[st["nb"] % len(wblk)]
            wbu = wblk[(st["nb"] + 1) % len(wblk)]
            st["nb"] += 2
            for k in range(KD):
                P.dma(wbg[:, k, :], wg_bf[k * 128:(k + 1) * 128, fb * 512:(fb + 1) * 512], reads=[wkey], writes=[wbg])
                P.dma(wbu[:, k, :], wu_bf[k * 128:(k + 1) * 128, fb * 512:(fb + 1) * 512], reads=[wkey], writes=[wbu])
            for j in range(4):
                fc = fb * 4 + j
                pg = bank[(st["ne"] % 2) * 2]
                pu = bank[(st["ne"] % 2) * 2 + 1]
                sg = sgt[st["ne"] % 2]
                st["ne"] += 1
                for k in range(KD):
                    P.pe(lambda e, pg=pg, wbg=wbg, k=k, j=j: e.matmul(pg[:, :], lhsT=wbg[:, k, j * 128:(j + 1) * 128], rhs=hT[:, k, :],
                                                                       start=(k == 0), stop=(k == KD - 1)), reads=[wbg, (hT, k)], writes=[pg])
                for k in range(KD):
                    P.pe(lambda e, pu=pu, wbu=wbu, k=k, j=j: e.matmul(pu[:, :], lhsT=wbu[:, k, j * 128:(j + 1) * 128], rhs=hT[:, k, :],
                                                                       start=(k == 0), stop=(k == KD - 1)), reads=[wbu, (hT, k)], writes=[pu])
                P.act(lambda e, pg=pg, sg=sg: e.activation(out=sg[:], in_=pg[:, :], func=AF.Silu), reads=[pg], writes=[sg])
                if wbc is not None:
                    P.pool(lambda e, sg=sg: e.tensor_tensor(out=sg[:], in0=sg[:], in1=wbc, op=ALU.mult), reads=[sg, "wbc"], writes=[sg])
                P.dve(lambda e, pu=pu, sg=sg, fc=fc: e.tensor_tensor(out=hid[:, fc, :], in0=pu[:, :], in1=sg[:], op=ALU.mult),
                      reads=[pu, sg], writes=[(hid, fc)])
        for cb in range(4):
            for seg in range(4):
                k0 = seg * 16
                nk = min(16, 56 - k0)
                wb = wblk[st["nb"] % len(wblk)]
                st["nb"] += 1
                for k in range(nk):
                    P.dma(wb[:, k, :], wd_bf[(k0 + k) * 128:(k0 + k + 1) * 128, cb * 512:(cb + 1) * 512], reads=[wkey], writes=[wb])
                for j in range(4):
                    pp = bank[4 + j]
                    for k in range(nk):
                        kk = k0 + k
                        P.pe(lambda e, pp=pp, wb=wb, k=k, j=j, kk=kk: e.matmul(pp[:, :], lhsT=wb[:, k, j * 128:(j + 1) * 128], rhs=hid[:, kk, :],
                                                                                start=(kk == 0), stop=(kk == 55)), reads=[wb, (hid, kk)], writes=[pp])
            for j in range(4):
                dc = cb * 4 + j
                pp = bank[4 + j]
                P.dve(lambda e, pp=pp, dc=dc: e.tensor_tensor(out=xg[:, dc, :], in0=xg[:, dc, :], in1=pp[:, :], op=ALU.add),
                      reads=[pp, (xg, dc)], writes=[(xg, dc)])

    def p3b_dense(l):
        xg = P.sb([128, KD, 512], F32, "xg")
        sq = P.sb([128, KD, 512], BF16, "sq")
        hT = P.sb([128, KD, 512], BF16, "hT")
        inv = P.sb([128, 512], F32, "inv")
        hid = P.sb([128, 56, 512], BF16, "hid")
        wblk = [P.sb([128, KD, 512], BF16, f"wblk{i}") for i in range(4)]
        sgt = [P.sb([128, 512], F32, f"sgt{i}") for i in range(2)]
        st = {"nb": 0, "ne": 0}
        jobs = expert_cast_jobs() if L > 1 else []
        per = -(-len(jobs) // NG) if jobs else 0
        for g in range(NG):
            for (dst_, src_) in jobs[g * per:(g + 1) * per]:
                P.dma(dst_, src_, writes=["w_exp_bf"], q="pool")
            load_x(xg, g)
            rms_T(xg, hT, KD, D, gain_ffn[:, l, :], gain_ffn, bank[0], sq, inv)
            ffn_core(w_ffn_bf[0], w_ffn_bf[1], w_ffn_bf[2], "w_ffn_bf", hT, xg, hid, wblk, sgt, st)
            store_x(xg, g)

    def p3b_moe(l):
        xg = P.sb([128, KD, 512], F32, "xg")
        sq = P.sb([128, KD, 512], BF16, "sq")
        hT = P.sb([128, KD, 512], BF16, "hT")
        inv = P.sb([128, 512], F32, "inv")
        hid = P.sb([128, 56, 512], BF16, "hid")
        wblk = [P.sb([128, KD, 512], BF16, f"wblk{i}") for i in range(3)]
        sgt = [P.sb([128, 512], F32, f"sgt{i}") for i in range(2)]
        hf = [P.sb([128, 512], F32, f"hf{i}") for i in range(2)]
        wr = P.sb([128, KD, 8], F32, "wr")
        P.dma(wr[:, :, :], w_router[0].rearrange("(k p) e -> p k e", p=128), writes=[wr], slow=True)
        sel = P.sb([8, 8, 128], F32, "sel")
        P.dma(sel[:, :, :], sel_in[:, :, :], writes=[sel])
        lg = P.sb([128, 4, 8], F32, "lg")
        m8 = P.sb([128, 8], F32, "m8")
        nm1 = P.sb([128, 1], F32, "nm1")
        msk = P.sb([128, 8], F32, "msk")
        den = P.sb([128, 1], F32, "den")
        wT8 = P.sb([8, 512], F32, "wT8")
        wbc = P.sb([128, 8, 512], BF16, "wbc")
        st = {"nb": 0, "ne": 0}
        for g in range(NG):
            load_x(xg, g)
            rms_T(xg, hT, KD, D, gain_ffn[:, l, :], gain_ffn, bank[0], sq, inv)
            for k in range(KD):
                hb = hf[k % 2]
                P.dve(lambda e, k=k, hb=hb: e.scalar_tensor_tensor(out=hb[:], in0=xg[:, k, :], scalar=gain_ffn[:, l, k:k + 1], in1=inv[:], op0=ALU.mult, op1=ALU.mult),
                      reads=[(xg, k), inv, gain_ffn], writes=[hb])
                for t in range(4):
                    P.pe(lambda e, k=k, hb=hb, t=t: e.matmul(bank[4 + t][:, 0:8], lhsT=hb[:, t * 128:(t + 1) * 128], rhs=wr[:, k, :], start=(k == 0), stop=(k == KD - 1)),
                         reads=[hb, wr], writes=[bank[4 + t]])
            for t in range(4):
                P.act(lambda e, t=t: e.copy(out=lg[:, t, :], in_=bank[4 + t][:, 0:8]), reads=[bank[4 + t]], writes=[lg])
            for t in range(4):
                P.dve(lambda e, t=t: e.max(out=m8[:, :], in_=lg[:, t, :]), reads=[lg], writes=[m8])
                P.dve(lambda e, t=t: e.tensor_scalar(out=msk[:, :], in0=lg[:, t, :], scalar1=m8[:, 1:2], scalar2=1.0, op0=ALU.is_ge, op1=ALU.mult), reads=[lg, m8], writes=[msk])
                P.dve(lambda e: e.tensor_scalar(out=nm1[:, :], in0=m8[:, 0:1], scalar1=-1.0, scalar2=0.0, op0=ALU.mult, op1=ALU.add), reads=[m8], writes=[nm1])
                P.act(lambda e, t=t: e.activation(out=lg[:, t, :], in_=lg[:, t, :], func=AF.Exp, bias=nm1[:, 0:1], scale=1.0), reads=[lg, nm1], writes=[lg])
                P.dve(lambda e, t=t: e.tensor_tensor(out=lg[:, t, :], in0=lg[:, t, :], in1=msk[:, :], op=ALU.mult), reads=[lg, msk], writes=[lg])
                P.dve(lambda e, t=t: e.reduce_sum(out=den[:, :], in_=lg[:, t, :], axis=AX.X), reads=[lg], writes=[den])
                P.dve(lambda e: e.reciprocal(out=den[:, :], in_=den[:, :]), reads=[den], writes=[den])
                P.dve(lambda e, t=t: e.tensor_scalar(out=lg[:, t, :], in0=lg[:, t, :], scalar1=den[:, 0:1], scalar2=1.0, op0=ALU.mult, op1=ALU.mult), reads=[lg, den], writes=[lg])
                P.pe(lambda e, t=t: e.transpose(bank[0][0:8, t * 128:(t + 1) * 128], lg[:, t, :], ident[:]), reads=[lg, ident], writes=[bank[0]])
            P.act(lambda e: e.copy(out=wT8[:, :], in_=bank[0][0:8, :]), reads=[bank[0]], writes=[wT8])
            for ex in range(8):
                pp = bank[ex % 4]
                P.pe(lambda e, pp=pp, ex=ex: e.matmul(pp[:, :], lhsT=sel[:, ex, :], rhs=wT8[:, :], start=True, stop=True), reads=[sel, wT8], writes=[pp])
                P.act(lambda e, pp=pp, ex=ex: e.copy(out=wbc[:, ex, :], in_=pp[:, :]), reads=[pp], writes=["wbc"])
            for ex in range(8):
                ffn_core(w_exp_bf[0][ex], w_exp_bf[1][ex], w_exp_bf[2][ex], "w_exp_bf", hT, xg, hid, wblk, sgt, st, wbc=wbc[:, ex, :])
            store_x(xg, g)

    CAP = max(512, -(-(S * 5 // 16) // 512) * 512)
    NSG = CAP // 512

    def moe_routed(l):
        Hsel = P.dram([8 * CAP + 128, D], BF16, "Hsel")
        hrow = P.dram([S, D], BF16, "hrow")
        Tslot = P.dram([8 * CAP + 128, 2], I32, "Tslot")
        Ytok = P.dram([2 * S + 128, D], F32, "Ytok")
        BIG2 = 2 * S
        pbf = bank[7][:, :].bitcast(BF16)
        with P.phase():
            xg = P.sb([128, KD, 512], F32, "xg")
            sq = P.sb([128, KD, 512], BF16, "sq")
            hT = P.sb([128, KD, 512], BF16, "hT")
            inv = P.sb([128, 512], F32, "inv")
            hf = [P.sb([128, 512], F32, f"hf{i}") for i in range(2)]
            wr = P.sb([128, KD, 8], F32, "wr")
            P.dma(wr[:, :, :], w_router[0].rearrange("(k p) e -> p k e", p=128), writes=[wr], slow=True)
            ident_bf = P.sb([128, 128], BF16, "ident_bf")
            P.dve(lambda e: e.tensor_copy(out=ident_bf[:], in_=ident[:]), reads=[ident], writes=[ident_bf])
            lg = P.sb([128, 4, 8], F32, "lg")
            m8 = P.sb([128, 8], F32, "m8")
            nm1 = P.sb([128, 1], F32, "nm1")
            den = P.sb([128, 1], F32, "den")
            Mall = P.sb([128, NT, 8], F32, "Mall")
            Tall = P.sb([128, NT, 8], F32, "Tall")
            TW = P.sb([128, NT, 8, 2], I32, "TW")
            tokid = P.sb([128, 64], F32, "tokid")
            P.dma(tokid[:, :], tokid_in[:, :], writes=[tokid])
            bigt = P.sb([128, 2 * 8 * CAP // 128], I32, "bigt")
            P.dve(lambda e: e.memset(bigt[:], BIG2), writes=[bigt])
            P.dma(Tslot[0:8 * CAP, :].rearrange("(p c) o -> p (c o)", p=128), bigt[:, :], reads=[bigt], writes=["Tslot"], q="pool")
            hro = [P.sb([128, D], BF16, f"hro{i}") for i in range(2)]
            nh = 0
            for g in range(NG):
                load_x(xg, g)
                rms_T(xg, hT, KD, D, gain_ffn[:, l, :], gain_ffn, bank[0], sq, inv)
                for k in range(KD):
                    hb = hf[k % 2]
                    P.dve(lambda e, k=k, hb=hb: e.scalar_tensor_tensor(out=hb[:], in0=xg[:, k, :], scalar=gain_ffn[:, l, k:k + 1], in1=inv[:], op0=ALU.mult, op1=ALU.mult),
                          reads=[(xg, k), inv, gain_ffn], writes=[hb])
                    for t in range(4):
                        P.pe(lambda e, k=k, hb=hb, t=t: e.matmul(bank[1 + t][:, 0:8], lhsT=hb[:, t * 128:(t + 1) * 128], rhs=wr[:, k, :], start=(k == 0), stop=(k == KD - 1)),
                             reads=[hb, wr], writes=[bank[1 + t]])
                for t in range(4):
                    P.act(lambda e, t=t: e.copy(out=lg[:, t, :], in_=bank[1 + t][:, 0:8]), reads=[bank[1 + t]], writes=[lg])
                for t in range(4):
                    tt = g * 4 + t
                    P.dve(lambda e, t=t: e.max(out=m8[:, :], in_=lg[:, t, :]), reads=[lg], writes=[m8])
                    P.dve(lambda e, t=t, tt=tt: e.tensor_scalar(out=Mall[:, tt, :], in0=lg[:, t, :], scalar1=m8[:, 1:2], scalar2=1.0, op0=ALU.is_ge, op1=ALU.mult),
                          reads=[lg, m8], writes=[Mall])
                    P.dve(lambda e, t=t, tt=tt: e.tensor_scalar(out=Tall[:, tt, :], in0=lg[:, t, :], scalar1=m8[:, 0:1], scalar2=float(-S), op0=ALU.is_ge, op1=ALU.mult),
                          reads=[lg, m8], writes=[Tall])
                    P.dve(lambda e, tt=tt: e.tensor_scalar(out=Tall[:, tt, :], in0=Tall[:, tt, :], scalar1=tokid[:, tt:tt + 1], scalar2=float(S), op0=ALU.add, op1=ALU.add),
                          reads=[Tall, tokid], writes=[Tall])
                    P.dve(lambda e: e.tensor_scalar(out=nm1[:, :], in0=m8[:, 0:1], scalar1=-1.0, scalar2=0.0, op0=ALU.mult, op1=ALU.add), reads=[m8], writes=[nm1])
                    P.act(lambda e, t=t: e.activation(out=lg[:, t, :], in_=lg[:, t, :], func=AF.Exp, bias=nm1[:, 0:1], scale=1.0), reads=[lg, nm1], writes=[lg])
                    P.dve(lambda e, t=t, tt=tt: e.tensor_tensor(out=lg[:, t, :], in0=lg[:, t, :], in1=Mall[:, tt, :], op=ALU.mult), reads=[lg, Mall], writes=[lg])
                    P.dve(lambda e, t=t: e.reduce_sum(out=den[:, :], in_=lg[:, t, :], axis=AX.X), reads=[lg], writes=[den])
                    P.dve(lambda e: e.reciprocal(out=den[:, :], in_=den[:, :]), reads=[den], writes=[den])
                    P.dve(lambda e, t=t, tt=tt: e.tensor_scalar(out=Wall[:, tt, :], in0=lg[:, t, :], scalar1=den[:, 0:1], scalar2=1.0, op0=ALU.mult, op1=ALU.mult),
                          reads=[lg, den], writes=[Wall])
                for t in range(4):
                    hr = hro[nh % 2]
                    nh += 1
                    for kb in range(2):
                        for kq in range(8):
                            k = kb * 8 + kq
                            P.pe(lambda e, k=k, kq=kq, t=t: e.transpose(pbf[:, kq * 128:(kq + 1) * 128], hT[:, k, t * 128:(t + 1) * 128], ident_bf[:]),
                                 reads=[(hT, k), ident_bf], writes=[bank[7]])
                        P.act(lambda e, hr=hr, kb=kb: e.copy(out=hr[:, kb * 1024:(kb + 1) * 1024], in_=pbf[:, :]), reads=[bank[7]], writes=[hr])
                    tok0 = g * 512 + t * 128
                    P.dma(hrow[tok0:tok0 + 128, :], hr[:, :], reads=[hr], writes=["hrow"], q="pool")
            us = P.sb([128, 128], F32, "us")
            ones_f = P.sb([128, 128], F32, "ones_f")
            P.pool(lambda e: e.memset(ones_f[:], 1.0), writes=[ones_f])
            P.dma(us[:, :], tri_in[0], writes=[us])
            P.dve(lambda e: e.tensor_tensor(out=us[:], in0=us[:], in1=ident[:], op=ALU.subtract), reads=[us, ident], writes=[us])
            cum = P.sb([128, NT, 8], F32, "cum")
            tot = P.sb([128, NT, 8], F32, "tot")
            base = P.sb([128, NT, 8], F32, "base")
            for c0 in range(0, NT * 8, 512):
                c1 = min(NT * 8, c0 + 512)
                mflat = Mall[:, :, :].rearrange("p t e -> p (t e)")
                P.pe(lambda e, c0=c0, c1=c1, mflat=mflat: e.matmul(bank[1][:, 0:c1 - c0], lhsT=us[:, :], rhs=mflat[:, c0:c1], start=True, stop=True), reads=[us, Mall], writes=[bank[1]])
                P.act(lambda e, c0=c0, c1=c1: e.copy(out=cum[:, :, :].rearrange("p t e -> p (t e)")[:, c0:c1], in_=bank[1][:, 0:c1 - c0]), reads=[bank[1]], writes=[cum])
                P.pe(lambda e, c0=c0, c1=c1, mflat=mflat: e.matmul(bank[2][:, 0:c1 - c0], lhsT=ones_f[:, :], rhs=mflat[:, c0:c1], start=True, stop=True), reads=[ones_f, Mall], writes=[bank[2]])
                P.act(lambda e, c0=c0, c1=c1: e.copy(out=tot[:, :, :].rearrange("p t e -> p (t e)")[:, c0:c1], in_=bank[2][:, 0:c1 - c0]), reads=[bank[2]], writes=[tot])
            for ex in range(8):
                P.dve(lambda e, ex=ex: e.memset(base[:, 0, ex:ex + 1], float(ex * CAP)), writes=[base])
            for t in range(1, NT):
                P.dve(lambda e, t=t: e.tensor_tensor(out=base[:, t, :], in0=base[:, t - 1, :], in1=tot[:, t - 1, :], op=ALU.add), reads=[base, tot], writes=[base])
            BIG = float(8 * CAP)
            P.dve(lambda e: e.tensor_tensor(out=cum[:, :, :], in0=cum[:, :, :], in1=base[:, :, :], op=ALU.add), reads=[cum, base], writes=[cum])
            P.dve(lambda e: e.tensor_scalar(out=cum[:, :, :], in0=cum[:, :, :], scalar1=-BIG, scalar2=1.0, op0=ALU.add, op1=ALU.mult), reads=[cum], writes=[cum])
            P.dve(lambda e: e.tensor_tensor(out=cum[:, :, :], in0=cum[:, :, :], in1=Mall[:, :, :], op=ALU.mult), reads=[cum, Mall], writes=[cum])
            P.dve(lambda e: e.tensor_scalar(out=cum[:, :, :], in0=cum[:, :, :], scalar1=BIG, scalar2=1.0, op0=ALU.add, op1=ALU.mult), reads=[cum], writes=[cum])
            P.dve(lambda e: e.tensor_copy(out=Iall[:, :, :], in_=cum[:, :, :]), reads=[cum], writes=[Iall])
            P.dve(lambda e: e.tensor_copy(out=TW[:, :, :, 0], in_=Tall[:, :, :]), reads=[Tall], writes=[TW])
            P.dve(lambda e: e.tensor_copy(out=TW[:, :, :, :].bitcast(F32)[:, :, :, 1], in_=Wall[:, :, :]), reads=[Wall, TW], writes=[TW])
            for t in range(NT):
                hr = hro[nh % 2]
                nh += 1
                P.dma(hr[:, :], hrow[t * 128:(t + 1) * 128, :], reads=["hrow"], writes=[hr])
                for ex in range(8):
                    P.indirect(lambda e, hr=hr, t=t, ex=ex: e.indirect_dma_start(
                        out=Hsel[:, :], out_offset=bass.IndirectOffsetOnAxis(ap=Iall[:, t, ex:ex + 1], axis=0),
                        in_=hr[:, :], in_offset=None),
                        [hr, Iall], ["Hsel"])
                    P.indirect(lambda e, t=t, ex=ex: e.indirect_dma_start(
                        out=Tslot[:, :], out_offset=bass.IndirectOffsetOnAxis(ap=Iall[:, t, ex:ex + 1], axis=0),
                        in_=TW[:, t, ex, :], in_offset=None),
                        [TW, Iall, "Tslot"], ["Tslot"])
        with P.phase():
            ident_bf = P.sb([128, 128], BF16, "ident_bf")
            P.dve(lambda e: e.tensor_copy(out=ident_bf[:], in_=ident[:]), reads=[ident], writes=[ident_bf])
            hs = P.sb([128, 4, D], BF16, "hs")
            hselT = P.sb([128, KD, 512], BF16, "hselT")
            hid = P.sb([128, 56, 512], BF16, "hid")
            wblk = [P.sb([128, KD, 512], BF16, f"wblk{i}") for i in range(3)]
            sgt = [P.sb([128, 512], F32, f"sgt{i}") for i in range(2)]
            osl = P.sb([128, 4, D], F32, "osl")
            tsl = P.sb([128, 4, 2], I32, "tsl")
            st = {"nb": 0, "ne": 0}
            for ex in range(8):
                for sg in range(NSG):
                    r0 = ex * CAP + sg * 512
                    for s4 in range(4):
                        P.dma(hs[:, s4, :], Hsel[r0 + s4 * 128:r0 + (s4 + 1) * 128, :], reads=["Hsel"], writes=[(hs, s4)])
                    P.dma(tsl[:, :, :], Tslot[r0:r0 + 512, :].rearrange("(s p) o -> p s o", p=128), reads=["Tslot"], writes=[tsl], slow=True)
                    for kp in range(KD // 2):
                        for kq in range(2):
                            k = kp * 2 + kq
                            for s4 in range(4):
                                P.pe(lambda e, k=k, kq=kq, s4=s4: e.transpose(pbf[:, kq * 512 + s4 * 128:kq * 512 + (s4 + 1) * 128], hs[:, s4, k * 128:(k + 1) * 128], ident_bf[:]),
                                     reads=[(hs, s4), ident_bf], writes=[bank[7]])
                        P.act(lambda e, kp=kp: e.copy(out=hselT[:, kp * 2:kp * 2 + 2, :].rearrange("p a b -> p (a b)"), in_=pbf[:, :]), reads=[bank[7]],
                              writes=[(hselT, kp * 2), (hselT, kp * 2 + 1)])
                    wg_bf, wu_bf, wd_bf = w_exp_bf[0][ex], w_exp_bf[1][ex], w_exp_bf[2][ex]
                    for fb in range(14):
                        wbg = wblk[st["nb"] % 3]
                        wbu = wblk[(st["nb"] + 1) % 3]
                        st["nb"] += 2
                        for k in range(KD):
                            P.dma(wbg[:, k, :], wg_bf[k * 128:(k + 1) * 128, fb * 512:(fb + 1) * 512], reads=["w_exp_bf"], writes=[wbg])
                            P.dma(wbu[:, k, :], wu_bf[k * 128:(k + 1) * 128, fb * 512:(fb + 1) * 512], reads=["w_exp_bf"], writes=[wbu])
                        for j in range(4):
                            fc = fb * 4 + j
                            pg = bank[(st["ne"] % 2) * 2]
                            pu = bank[(st["ne"] % 2) * 2 + 1]
                            sgq = sgt[st["ne"] % 2]
                            st["ne"] += 1
                            for k in range(KD):
                                P.pe(lambda e, pg=pg, wbg=wbg, k=k, j=j: e.matmul(pg[:, :], lhsT=wbg[:, k, j * 128:(j + 1) * 128], rhs=hselT[:, k, :],
                                                                                   start=(k == 0), stop=(k == KD - 1)), reads=[wbg, (hselT, k)], writes=[pg])
                            for k in range(KD):
                                P.pe(lambda e, pu=pu, wbu=wbu, k=k, j=j: e.matmul(pu[:, :], lhsT=wbu[:, k, j * 128:(j + 1) * 128], rhs=hselT[:, k, :],
                                                                                   start=(k == 0), stop=(k == KD - 1)), reads=[wbu, (hselT, k)], writes=[pu])
                            P.act(lambda e, pg=pg, sgq=sgq: e.activation(out=sgq[:], in_=pg[:, :], func=AF.Silu), reads=[pg], writes=[sgq])
                            P.dve(lambda e, pu=pu, sgq=sgq, fc=fc: e.tensor_tensor(out=hid[:, fc, :], in0=pu[:, :], in1=sgq[:], op=ALU.mult),
                                  reads=[pu, sgq], writes=[(hid, fc)])
                    for cb in range(4):
                        for seg in range(4):
                            k0 = seg * 16
                            nk = min(16, 56 - k0)
                            wb = wblk[st["nb"] % 3]
                            st["nb"] += 1
                            for k in range(nk):
                                P.dma(wb[:, k, :], wd_bf[(k0 + k) * 128:(k0 + k + 1) * 128, cb * 512:(cb + 1) * 512], reads=["w_exp_bf"], writes=[wb])
                            for s4 in range(4):
                                pp = bank[3 + s4]
                                for k in range(nk):
                                    kk = k0 + k
                                    P.pe(lambda e, pp=pp, wb=wb, k=k, s4=s4, kk=kk: e.matmul(pp[:, :], lhsT=hid[:, kk, s4 * 128:(s4 + 1) * 128], rhs=wb[:, k, :],
                                                                                          start=(kk == 0), stop=(kk == 55)), reads=[(hid, kk), wb], writes=[pp])
                        for s4 in range(4):
                            pp = bank[3 + s4]
                            P.act(lambda e, pp=pp, s4=s4, cb=cb: e.activation(out=osl[:, s4, cb * 512:(cb + 1) * 512], in_=pp[:, :], func=AF.Copy, scale=tsl[:, s4, 1:2].bitcast(F32)),
                                  reads=[pp, tsl], writes=[(osl, s4)])
                    for s4 in range(4):
                        P.indirect(lambda e, s4=s4: e.indirect_dma_start(
                            out=Ytok[:, :], out_offset=bass.IndirectOffsetOnAxis(ap=tsl[:, s4, 0:1], axis=0),
                            in_=osl[:, s4, :], in_offset=None),
                            [(osl, s4), tsl], ["Ytok"])
        with P.phase():
            xg = P.sb([128, KD, 512], F32, "xg")
            y1 = [P.sb([128, D], F32, f"y1_{i}") for i in range(4)]
            y2 = [P.sb([128, D], F32, f"y2_{i}") for i in range(4)]
            for g in range(NG):
                load_x(xg, g)
                for t in range(4):
                    tok0 = g * 512 + t * 128
                    P.dma(y1[t][:, :], Ytok[tok0:tok0 + 128, :], reads=["Ytok"], writes=[y1[t]])
                    P.dma(y2[t][:, :], Ytok[S + tok0:S + tok0 + 128, :], reads=["Ytok"], writes=[y2[t]])
                    P.pool(lambda e, t=t: e.tensor_tensor(out=y1[t][:], in0=y1[t][:], in1=y2[t][:], op=ALU.add), reads=[y1[t], y2[t]], writes=[y1[t]])
                for k in range(KD):
                    pp = bank[k % 4]
                    for t in range(4):
                        P.pe(lambda e, pp=pp, t=t, k=k: e.transpose(pp[:, t * 128:(t + 1) * 128], y1[t][:, k * 128:(k + 1) * 128], ident[:]),
                             reads=[y1[t], ident], writes=[pp])
                    P.dve(lambda e, pp=pp, k=k: e.tensor_tensor(out=xg[:, k, :], in0=xg[:, k, :], in1=pp[:, :], op=ALU.add), reads=[pp, (xg, k)], writes=[(xg, k)])
                store_x(xg, g)

    def p3c(l, last):
        xg = P.sb([128, KD, 512], F32, "xg")
        sq = P.sb([128, KD, 512], BF16, "sq")
        hT = P.sb([128, KD, 512], BF16, "hT")
        inv = P.sb([128, 512], F32, "inv")
        pTt = P.sb([128, 2, 512], BF16, "pTt")
        wblk = [P.sb([128, KD, 512], BF16, f"wblk{i}") for i in range(2)]
        wpb = [P.sb([128, 2, 512], BF16, f"wpb{i}") for i in range(2)]
        sgt = [P.sb([128, 512], F32, f"sgt{i}") for i in range(2)]
        if last:
            hfin = P.sb([128, KD, 512], F32, "hfin")
            osb = [P.sb([128, D], F32, f"osb{i}") for i in range(2)]
        nb = 0
        ne = 0
        no = 0
        for g in range(NG):
            gs = slice(g * 512, (g + 1) * 512)
            load_x(xg, g)
            rms_T(xg, hT, KD, D, gain_ple[:, l, :], gain_ple, bank[0], sq, inv)
            for kk in range(2):
                P.dma(pTt[:, kk, :], pT[l][kk * 128:(kk + 1) * 128, gs], reads=[("pT", l)], writes=[pTt])
            for cb in range(4):
                wb = wblk[nb % 2]
                wp = wpb[nb % 2]
                nb += 1
                for k in range(KD):
                    P.dma(wb[:, k, :], w_pg_bf[l][k * 128:(k + 1) * 128, cb * 512:(cb + 1) * 512], reads=[("w_pg_bf", l)], writes=[wb])
                for kk in range(2):
                    P.dma(wp[:, kk, :], w_pp_bf[l][kk * 128:(kk + 1) * 128, cb * 512:(cb + 1) * 512], reads=[("w_pp_bf", l)], writes=[wp])
                for j in range(4):
                    dc = cb * 4 + j
                    pgt = bank[2 + (ne % 2) * 2]
                    ppr = bank[3 + (ne % 2) * 2]
                    sg = sgt[ne % 2]
                    ne += 1
                    for k in range(KD):
                        P.pe(lambda e, pgt=pgt, wb=wb, k=k, j=j: e.matmul(pgt[:, :], lhsT=wb[:, k, j * 128:(j + 1) * 128], rhs=hT[:, k, :],
                                                                           start=(k == 0), stop=(k == KD - 1)), reads=[wb, (hT, k)], writes=[pgt])
                    for kk in range(2):
                        P.pe(lambda e, ppr=ppr, wp=wp, kk=kk, j=j: e.matmul(ppr[:, :], lhsT=wp[:, kk, j * 128:(j + 1) * 128], rhs=pTt[:, kk, :],
                                                                             start=(kk == 0), stop=(kk == 1)), reads=[wp, pTt], writes=[ppr])
                    P.act(lambda e, pgt=pgt, sg=sg: e.activation(out=sg[:], in_=pgt[:, :], func=AF.Sigmoid), reads=[pgt], writes=[sg])
                    P.dve(lambda e, ppr=ppr, sg=sg: e.tensor_tensor(out=sg[:], in0=ppr[:, :], in1=sg[:], op=ALU.mult), reads=[ppr, sg], writes=[sg])
                    P.pool(lambda e, sg=sg, dc=dc: e.tensor_tensor(out=xg[:, dc, :], in0=xg[:, dc, :], in1=sg[:], op=ALU.add), reads=[sg, (xg, dc)], writes=[(xg, dc)])
            if not last:
                store_x(xg, g)
                continue
            for k in range(KD):
                P.act(lambda e, k=k: e.activation(out=sq[:, k, :], in_=xg[:, k, :], func=AF.Square), reads=[(xg, k)], writes=[(sq, k)])
            for k in range(KD):
                P.pe(lambda e, k=k: e.matmul(bank[0][:], lhsT=ones_bf[:], rhs=sq[:, k, :], start=(k == 0), stop=(k == KD - 1)), reads=[ones_bf, (sq, k)], writes=[bank[0]])
            P.act(lambda e: e.activation(out=inv[:], in_=bank[0][:], func=AF.Sqrt, scale=1.0 / D, bias=eps_t[:, 0:1]), reads=[bank[0], eps_t], writes=[inv])
            P.dve(lambda e: e.reciprocal(out=inv[:], in_=inv[:]), reads=[inv], writes=[inv])
            for k in range(KD):
                P.dve(lambda e, k=k: e.scalar_tensor_tensor(out=hfin[:, k, :], in0=xg[:, k, :], scalar=gain_fin[:, k:k + 1], in1=inv[:], op0=ALU.mult, op1=ALU.mult),
                      reads=[(xg, k), inv, gain_fin], writes=[(hfin, k)])
            for t in range(4):
                ob = osb[no % 2]
                no += 1
                for kb in range(4):
                    pp = bank[4 + (kb % 4)]
                    for kq in range(4):
                        k = kb * 4 + kq
                        P.pe(lambda e, pp=pp, k=k, kq=kq, t=t: e.transpose(pp[:, kq * 128:(kq + 1) * 128], hfin[:, k, t * 128:(t + 1) * 128], ident[:]),
                             reads=[(hfin, k), ident], writes=[pp])
                    P.act(lambda e, pp=pp, ob=ob, kb=kb: e.copy(out=ob[:, kb * 512:(kb + 1) * 512], in_=pp[:, :]), reads=[pp], writes=[ob])
                tok0 = g * 512 + t * 128
                P.dma(out_ap[tok0:tok0 + 128, :], ob[:, :], reads=[ob], writes=["out"], q="pool")

    if "moe" in phases:
        for ex in range(8):
            cast(w_exp_bf[0][ex], w_exp_gate[0, ex], D, "w_exp_bf")
            cast(w_exp_bf[1][ex], w_exp_up[0, ex], D, "w_exp_bf")
            cast(w_exp_bf[2][ex], w_exp_down[0, ex], 7168, "w_exp_bf", 512)
        if dbg.get("dense_moe"):
            with P.phase():
                p3b_moe(0)
        else:
            moe_routed(0)
        dump("x2T", xT[:, :], [D, S], F32, [])
    for l in range(L):
        casts_layer(l)
    for l in range(L):
        if "p1" in phases:
            with P.phase():
                p1(l)
        if "swa" in phases:
            with P.phase():
                swa(l)
        if "mla" in phases:
            with P.phase():
                mla1(l)
            with P.phase():
                mla2(l)
        if "dn" in phases:
            with P.phase():
                dn(l)
        if "p3" in phases:
            with P.phase():
                p3a(l)
            if dbg.get("x1") and l == 0:
                dump("x1T", xT[:, :], [D, S], F32, [])
            if l % 2 == 0:
                with P.phase():
                    p3b_dense(l)
            elif dbg.get("dense_moe"):
                with P.phase():
                    p3b_moe(l)
            else:
                moe_routed(l)
            if dbg.get("x1") and l == 0:
                dump("x2T", xT[:, :], [D, S], F32, [])
            with P.phase():
                p3c(l, l == L - 1)

    if dbg.get("projT"):
        dump("projT", projT[:, :], [C_G, S], F32, keys("projT"))
        dump("gatesT", gatesT[:, :], [3 * D, S], BF16, keys("gatesT"))
    if dbg.get("xT"):
        dump("xT", xT[:, :], [D, S], F32, keys("xT"))
    if dbg.get("yT"):
        dump("yT", yT[:, :], [3072, S], BF16, keys("yT_a") + keys("yT_b") + keys("yT_c"))
    if dbg.get("mlaqk"):
        dump("qnT", qnT[:, :], [1024, S], BF16, keys("qnT"))
        dump("qrT", qrT[:, :], [512, S], BF16, keys("qrT"))
        dump("krT", krT[:, :], [64, S], BF16, keys("krT"))
        dump("cosT", cosT[:, :], [64, S], F32, keys("cosT"))

    P.finish()
    return nc, stack, P


_CACHE = {}
_WEIGHTS = ["norm_mix", "w_in", "swa_sinks", "mla_q_norm", "mla_kv_norm", "w_uq", "w_ukv", "conv_w", "dn_a_log", "dn_dt_bias",
            "dn_norm", "w_branch", "w_out", "norm_ffn", "w_ffn_gate", "w_ffn_up", "w_ffn_down", "norm_ple", "w_ple_gate",
            "w_ple_proj", "w_router", "w_exp_gate", "w_exp_up", "w_exp_down"]


def kernel(**inputs):
    S, L = 8192, 2
    if "prog" not in _CACHE:
        _CACHE["prog"] = build(S, L)
    nc, stack, P = _CACHE["prog"]
    consts = host_consts()
    shared = {k: np.ascontiguousarray(np.asarray(inputs[k], dtype=np.float32)) for k in _WEIGHTS}
    shared["final_norm"] = np.ascontiguousarray(np.asarray(inputs["final_norm"], dtype=np.float32)[None])
    in_maps = []
    for b in range(2):
        m = dict(consts)
        m.update(shared)
        m["x"] = np.ascontiguousarray(np.asarray(inputs["x"], dtype=np.float32)[b])
        m["positions"] = np.ascontiguousarray(np.asarray(inputs["positions"])[b:b + 1].astype(np.int32))
        m["p"] = np.ascontiguousarray(np.asarray(inputs["p"], dtype=np.float32)[:, b])
        in_maps.append(m)
    res = run_bass_kernel_spmd(nc, in_maps, core_ids=[0, 1])
    return np.stack([np.asarray(res.results[b]["out"], dtype=np.float32) for b in range(2)])
```

```python
import contextlib
import numpy as np
import concourse.bass as bass
import concourse.mybir as mybir
from concourse.bass_utils import run_bass_kernel_spmd

F32 = mybir.dt.float32
BF16 = mybir.dt.bfloat16
I32 = mybir.dt.int32
ALU = mybir.AluOpType
AF = mybir.ActivationFunctionType
AX = mybir.AxisListType

D = 2048
KD = D // 128
IN_COLS = 12880
EPS = 1e-6

C_AQ, C_AK, C_AV = 0, 1024, 1280
C_BQKV, C_BZ, C_BB, C_BD = 1536, 4608, 5632, 5640
C_CQ, C_CKV, C_CKR, C_G = 5648, 6160, 6672, 6736


class Op:
    __slots__ = ("eng", "fn", "reads", "writes", "dma", "deps", "signal", "val", "sem", "lhs")

    def __init__(self, eng, fn, reads, writes, dma):
        self.eng, self.fn, self.reads, self.writes, self.dma = eng, fn, reads, writes, dma
        self.deps = ()
        self.signal = False
        self.val = 0
        self.sem = None
        self.lhs = None


def _norm(k):
    if isinstance(k, tuple):
        return tuple(_norm(x) for x in k)
    if isinstance(k, (str, int)):
        return k
    return "@" + k.name


class Prog:
    COMPUTE = ("pe", "act", "dve", "pool")

    def __init__(self, nc, stack):
        self.nc = nc
        self.stack = stack
        self.gstack = stack
        self.ops = []
        self.last_w = {}
        self.readers = {}
        self.n_alloc = 0

    def sb(self, shape, dtype, name=None):
        self.n_alloc += 1
        return self.stack.enter_context(self.nc.sbuf_tensor((name or "sb") + f"_{self.n_alloc}", list(shape), dtype))

    def ps(self, shape, dtype=F32, name=None):
        self.n_alloc += 1
        return self.stack.enter_context(self.nc.psum_tensor(name or f"ps{self.n_alloc}", list(shape), dtype))

    def dram(self, shape, dtype, name=None):
        self.n_alloc += 1
        return self.nc.dram_tensor(name or f"dr{self.n_alloc}", list(shape), dtype).ap()

    def _add(self, eng, fn, reads, writes, dma):
        rr = [_norm(r) for r in reads]
        ww = [_norm(w) for w in writes]
        ww += [r for r in rr if isinstance(r, tuple) and r[0] == "bank"]
        rr = [r for r in rr if not (isinstance(r, tuple) and r[0] == "bank")]
        op = Op(eng, fn, tuple(rr), tuple(ww), dma)
        deps = set()
        me = len(self.ops)
        for r in op.reads:
            w = self.last_w.get(r)
            if w is not None:
                deps.add((w, True))
        for w_ in op.writes:
            w = self.last_w.get(w_)
            if w is not None:
                deps.add((w, False))
            for rd in self.readers.get(w_, ()):
                deps.add((rd, False))
        keep = set()
        for (d, raw) in deps:
            dop = self.ops[d]
            if not dop.dma and dop.eng == eng and not dma:
                if eng == "pe" or not raw:
                    continue
            keep.add(d)
        op.deps = tuple(sorted(keep))
        if eng == "pe" and op.reads:
            lw = self.last_w.get(op.reads[0])
            if lw is not None and lw in keep:
                op.lhs = lw
        for d in op.deps:
            self.ops[d].signal = True
        for w_ in op.writes:
            self.last_w[w_] = me
            self.readers[w_] = []
        for r in op.reads:
            lst = self.readers.setdefault(r, [])
            if not dma:
                lst[:] = [x for x in lst if self.ops[x].dma or self.ops[x].eng != eng]
            lst.append(me)
        self.ops.append(op)
        return op

    def pe(self, fn, reads=(), writes=()):
        return self._add("pe", fn, reads, writes, False)

    def act(self, fn, reads=(), writes=()):
        return self._add("act", fn, reads, writes, False)

    def dve(self, fn, reads=(), writes=()):
        return self._add("dve", fn, reads, writes, False)

    def pool(self, fn, reads=(), writes=()):
        return self._add("pool", fn, reads, writes, False)

    def indirect(self, fn, reads=(), writes=()):
        op = self._add("pool", fn, reads, writes, True)
        op.lhs = "ind"
        return op

    def dma(self, out, in_, reads=(), writes=(), q="sp", slow=False):
        if slow:
            return self._add(q, lambda e: e.dma_start(out=out, in_=in_, allow_slow_non_contiguous=True), reads, writes, True)
        return self._add(q, lambda e: e.dma_start(out=out, in_=in_), reads, writes, True)

    def barrier(self):
        last = {}
        for i in range(len(self.ops) - 1, -1, -1):
            op = self.ops[i]
            if op.eng == "bar":
                break
            if not op.dma and op.eng not in last:
                last[op.eng] = i
                op.signal = True
            if len(last) == 4:
                break
        b = Op("bar", None, (), (), False)
        b.deps = tuple(last.values())
        self.ops.append(b)
        self.last_w = {}
        self.readers = {}

    @contextlib.contextmanager
    def phase(self):
        st = contextlib.ExitStack()
        old = self.stack
        self.stack = st
        try:
            yield
        finally:
            self.barrier()
            self.flush()
            st.close()
            self.stack = old

    def _init_emit(self, n_dma_sems=40):
        nc = self.nc
        self.engs = {"pe": nc.tensor, "act": nc.scalar, "dve": nc.vector, "pool": nc.gpsimd, "sp": nc.sync}
        st = self.gstack
        self.esem = {e: st.enter_context(nc.semaphore(f"s_{e}")) for e in self.COMPUTE}
        self.ecnt = {e: 0 for e in self.COMPUTE}
        self.dsem = [st.enter_context(nc.semaphore(f"s_d{i}")) for i in range(n_dma_sems)]
        self.dcnt = [0] * n_dma_sems
        self.dnext = 0
        self.waited = {}
        self.emitted = 0
        self.ind_hist = []

    def _wait(self, eng_name, sem, key, val):
        if self.waited.get((eng_name, key), 0) >= val:
            return
        self.waited[(eng_name, key)] = val
        self.engs[eng_name].wait_ge(sem, val)

    def flush(self):
        if not hasattr(self, "engs"):
            self._init_emit()
        engs, esem, ecnt, dsem, dcnt = self.engs, self.esem, self.ecnt, self.dsem, self.dcnt
        nd = len(dsem)
        for idx in range(self.emitted, len(self.ops)):
            op = self.ops[idx]
            en = op.eng
            if en == "bar":
                for q in ("pe", "act", "dve", "pool", "sp"):
                    for d in op.deps:
                        dop = self.ops[d]
                        if dop.eng != q:
                            self._wait(q, dop.sem[1], dop.sem[0], dop.val)
                    for k in range(nd):
                        if dcnt[k] > 0:
                            self._wait(q, dsem[k], ("d", k), 16 * dcnt[k])
                continue
            need = {}
            for d in op.deps:
                dop = self.ops[d]
                key, sem = dop.sem
                if key not in need or need[key][1] < dop.val:
                    need[key] = (sem, dop.val)
            lhs_key = self.ops[op.lhs].sem[0] if (not op.dma and op.lhs is not None and op.lhs != "ind") else None
            for key, (sem, val) in need.items():
                if key == lhs_key and val == self.ops[op.lhs].val:
                    continue
                self._wait(en, sem, key, val)
            if op.dma:
                if op.lhs == "ind":
                    self.ind_hist.append(None)
                    if len(self.ind_hist) > 6 and self.ind_hist[-7] is not None:
                        ksem, kkey, kval = self.ind_hist[-7]
                        self._wait(en, ksem, kkey, kval)
                k = self.dnext
                self.dnext = (self.dnext + 1) % nd
                if dcnt[k] > 0:
                    self._wait(en, dsem[k], ("d", k), 16 * dcnt[k])
                ins = op.fn(engs[en])
                dcnt[k] += 1
                ins.then_inc(dsem[k], 16)
                op.sem = (("d", k), dsem[k])
                op.val = 16 * dcnt[k]
                if op.lhs == "ind":
                    self.ind_hist[-1] = (dsem[k], ("d", k), op.val)
            else:
                ins = op.fn(engs[en])
                if op.lhs is not None:
                    dop = self.ops[op.lhs]
                    if self.waited.get((en, dop.sem[0]), 0) < dop.val:
                        self.waited[(en, dop.sem[0])] = dop.val
                        ins._wait_ge(dop.sem[1], dop.val)
                if op.signal:
                    ecnt[en] += 1
                    ins.then_inc(esem[en], 1)
                    op.sem = (("e", en), esem[en])
                    op.val = ecnt[en]
            op.fn = None
        self.emitted = len(self.ops)

    def finish(self):
        self.barrier()
        self.flush()
        self.counts = dict(self.ecnt)


def host_consts():
    c = {}
    c["ident_in"] = np.eye(128, dtype=np.float32)
    k = np.arange(128)[:, None]
    q = np.arange(128)[None, :]
    cur = (k <= q).astype(np.float32)
    prev = (k > q).astype(np.float32)
    c["m_swa"] = np.ascontiguousarray(np.stack([np.tile(prev, (1, 4)), np.tile(cur, (1, 4))]))
    q5 = np.arange(512)[None, :]
    c["m_mla"] = np.ascontiguousarray(np.stack([((128 * jj + k) <= q5).astype(np.float32) for jj in range(4)]))
    rot = np.zeros((64, 64), np.float32)
    for m in range(32):
        rot[m + 32, m] = -1.0
    for m in range(32, 64):
        rot[m - 32, m] = 1.0
    c["rot_in"] = rot
    c["tri_in"] = np.ascontiguousarray(np.stack([cur, prev]))
    sel = np.zeros((8, 8, 128), np.float32)
    for e_ in range(8):
        sel[e_, e_, :] = 1.0
    c["sel_in"] = sel
    c["tokid_in"] = (np.arange(64)[None, :] * 128 + np.arange(128)[:, None]).astype(np.float32)
    c["inv_freq"] = (10000.0 ** (-(np.arange(64) % 32) / 32.0)).astype(np.float32)[:, None]
    return c


def build(S, n_layers=2, dbg=None, phases=("p1", "swa", "mla", "dn", "p3")):
    dbg = dbg or {}
    nc = bass.Bass("TRN2", target_bir_lowering=False)
    NG = S // 512
    NT = S // 128
    stack = contextlib.ExitStack()
    P = Prog(nc, stack)
    L = n_layers

    def ext_in(name, shape, dt=F32):
        return nc.dram_tensor(name, list(shape), dt, kind="ExternalInput").ap()

    def ext_out(name, shape, dt=F32):
        return nc.dram_tensor(name, list(shape), dt, kind="ExternalOutput").ap()

    x_in = ext_in("x", [S, D])
    pos_in = ext_in("positions", [1, S], I32)
    ident_in = ext_in("ident_in", [128, 128])
    m_swa_in = ext_in("m_swa", [2, 128, 512])
    m_mla_in = ext_in("m_mla", [4, 128, 512])
    rot_in = ext_in("rot_in", [64, 64])
    inv_freq_in = ext_in("inv_freq", [64, 1])
    norm_mix = ext_in("norm_mix", [L, D])
    w_in = ext_in("w_in", [L, D, IN_COLS])
    swa_sinks = ext_in("swa_sinks", [L, 16])
    mla_q_norm = ext_in("mla_q_norm", [L, 512])
    mla_kv_norm = ext_in("mla_kv_norm", [L, 512])
    w_uq = ext_in("w_uq", [L, 512, 1536])
    w_ukv = ext_in("w_ukv", [L, 512, 2048])
    tri_in = ext_in("tri_in", [2, 128, 128])
    sel_in = ext_in("sel_in", [8, 8, 128])
    tokid_in = ext_in("tokid_in", [128, 64])
    p_in = ext_in("p", [L, S, 256])
    w_branch = ext_in("w_branch", [L, 3, 1024, D])
    w_out = ext_in("w_out", [L, D, D])
    norm_ffn = ext_in("norm_ffn", [L, D])
    w_ffn_gate = ext_in("w_ffn_gate", [1, D, 7168])
    w_ffn_up = ext_in("w_ffn_up", [1, D, 7168])
    w_ffn_down = ext_in("w_ffn_down", [1, 7168, D])
    norm_ple = ext_in("norm_ple", [L, D])
    w_ple_gate = ext_in("w_ple_gate", [L, D, D])
    w_ple_proj = ext_in("w_ple_proj", [L, 256, D])
    final_norm = ext_in("final_norm", [1, D])
    if L > 1 or "moe" in phases:
        w_router = ext_in("w_router", [1, D, 8])
        w_exp_gate = ext_in("w_exp_gate", [1, 8, D, 7168])
        w_exp_up = ext_in("w_exp_up", [1, 8, D, 7168])
        w_exp_down = ext_in("w_exp_down", [1, 8, 7168, D])
    out_ap = ext_out("out", [S, D])
    conv_w = ext_in("conv_w", [L, 4, 3072])
    dn_a_log = ext_in("dn_a_log", [L, 8])
    dn_dt_bias = ext_in("dn_dt_bias", [L, 8])
    dn_norm = ext_in("dn_norm", [L, 128])

    bank = [P.ps([128, 512], F32, f"bank{i}") for i in range(8)]

    ident = P.sb([128, 128], F32, "ident")
    P.dma(ident[:], ident_in[:, :], writes=[ident])
    eps_t = P.sb([128, 1], F32, "eps_t")
    P.pool(lambda e: e.memset(eps_t[:], EPS), writes=[eps_t])
    ones_bf = P.sb([128, 128], BF16, "ones_bf")
    P.pool(lambda e: e.memset(ones_bf[:], 1.0), writes=[ones_bf])
    m_swa = P.sb([128, 2, 512], BF16, "m_swa_t")
    m_mla = P.sb([128, 4, 512], BF16, "m_mla_t")
    for i in range(2):
        P.dma(m_swa[:, i, :], m_swa_in[i], writes=[m_swa], q="pool")
    for i in range(4):
        P.dma(m_mla[:, i, :], m_mla_in[i], writes=[m_mla], q="pool")
    rot = P.sb([64, 64], F32, "rot_t")
    P.dma(rot[:], rot_in[:, :], writes=[rot])
    inv_freq = P.sb([64, 1], F32, "inv_freq_t")
    P.dma(inv_freq[:], inv_freq_in[:, :], writes=[inv_freq])

    xT = P.dram([D, S], F32, "xT")
    projT = P.dram([C_G, S], F32, "projT")
    gatesT = P.dram([3 * D, S], BF16, "gatesT")
    vA = P.dram([S, 256], BF16, "vA")
    bd = P.dram([S, 16], F32, "bd")
    yT = P.dram([3 * 1024, S], BF16, "yT")
    cosT = P.dram([64, S], F32, "cosT")
    sinT = P.dram([64, S], F32, "sinT")
    qnT = P.dram([1024, S], BF16, "qnT")
    qrT = P.dram([512, S], BF16, "qrT")
    knT = P.dram([1024, S], BF16, "knT")
    krT = P.dram([64, S], BF16, "krT")
    vC = P.dram([S, 1024], BF16, "vC")
    w_in_bf = [P.dram([32, 128, 16 * 512], BF16, f"w_in_bf{l}") for l in range(L)]
    w_branch_bf = [P.dram([3072, D], BF16, f"w_branch_bf{l}") for l in range(L)]
    w_out_bf = [P.dram([D, D], BF16, f"w_out_bf{l}") for l in range(L)]
    w_pg_bf = [P.dram([D, D], BF16, f"w_pg_bf{l}") for l in range(L)]
    w_pp_bf = [P.dram([256, D], BF16, f"w_pp_bf{l}") for l in range(L)]
    w_ffn_bf = [P.dram([14, 128, 16 * 512], BF16, "w_ffn_bf_g"), P.dram([14, 128, 16 * 512], BF16, "w_ffn_bf_u"), P.dram([16, 128, 16 * 512], BF16, "w_ffn_bf_d")]
    if L > 1 or "moe" in phases:
        w_exp_bf = [P.dram([8, 14, 128, 16 * 512], BF16, "w_exp_bf_g"), P.dram([8, 14, 128, 16 * 512], BF16, "w_exp_bf_u"), P.dram([8, 16, 128, 16 * 512], BF16, "w_exp_bf_d")]
    pT = [P.dram([256, S], BF16, f"pT{l}") for l in range(L)]

    ALLG = list(range(NG))

    def keys(name, gs=None):
        return [(name, g) for g in (ALLG if gs is None else gs)]

    def cast(dst, src, rows, key, step=256):
        for r in range(0, rows, step):
            P.dma(dst[r:r + step, :], src[r:r + step, :], writes=[key], q="pool")

    blocks = [(0, 512, "f"), (512, 512, "f"), (C_AK, 256, "f"), (C_AV, 256, "t")] + \
             [(c, 512, "f") for c in range(C_BQKV, C_BB, 512)] + \
             [(C_BB, 16, "t"), (C_CQ, 512, "f"), (C_CKV, 512, "f"), (C_CKR, 64, "f")] + \
             [(c, 512, "f") for c in range(C_G, IN_COLS, 512)]

    def blk3(ap2d, nk=16):
        return ap2d.rearrange("p (k c) -> p k c", k=16)[:, 0:nk, :]

    def up_jobs(dst_b, src):
        return [(blk3(dst_b[fb])[:, k, :], src[k * 128:(k + 1) * 128, fb * 512:(fb + 1) * 512]) for fb in range(14) for k in range(16)]

    def down_jobs(dst_b, src):
        jobs = []
        for cb in range(4):
            for seg in range(4):
                k0 = seg * 16
                nk = min(16, 56 - k0)
                for k in range(nk):
                    jobs.append((blk3(dst_b[cb * 4 + seg])[:, k, :], src[(k0 + k) * 128:(k0 + k + 1) * 128, cb * 512:(cb + 1) * 512]))
        return jobs

    def expert_cast_jobs():
        jobs = []
        for ex in range(8):
            jobs += up_jobs(w_exp_bf[0][ex], w_exp_gate[0, ex]) + up_jobs(w_exp_bf[1][ex], w_exp_up[0, ex]) + down_jobs(w_exp_bf[2][ex], w_exp_down[0, ex])
        return jobs

    def expert_cast_jobs_old():
        jobs = []
        for ex in range(8):
            for (dst, src, rows, step) in ((w_exp_bf[0][ex], w_exp_gate[0, ex], D, 256), (w_exp_bf[1][ex], w_exp_up[0, ex], D, 256),
                                           (w_exp_bf[2][ex], w_exp_down[0, ex], 7168, 512)):
                for r in range(0, rows, step):
                    jobs.append((dst[r:r + step, :], src[r:r + step, :]))
        return jobs

    def casts_layer(l):
        for bi, (c0, wd, mode) in enumerate(blocks):
            for k in range(16):
                P.dma(blk3(w_in_bf[l][bi])[:, k, 0:wd], w_in[l][k * 128:(k + 1) * 128, c0:c0 + wd], writes=[("w_in_bf", l)], q="pool")
        if "p3" in phases:
            cast(w_branch_bf[l], w_branch[l].rearrange("n k d -> (n k) d"), 3072, ("w_branch_bf", l), 512)
            cast(w_out_bf[l], w_out[l], D, ("w_out_bf", l), 512)
            if l == 0:
                for (dst_, src_) in up_jobs(w_ffn_bf[0], w_ffn_gate[0]) + up_jobs(w_ffn_bf[1], w_ffn_up[0]) + down_jobs(w_ffn_bf[2], w_ffn_down[0]):
                    P.dma(dst_, src_, writes=["w_ffn_bf"], q="pool")
            cast(w_pg_bf[l], w_ple_gate[l], D, ("w_pg_bf", l), 512)
            cast(w_pp_bf[l], w_ple_proj[l], 256, ("w_pp_bf", l))

    with P.phase():
        xin4 = [P.sb([128, D], F32, f"xin4_{i}") for i in range(4)]
        tp_sb = [P.sb([128, 512], F32, f"tp_sb{i}") for i in range(2)]
        for g in range(NG):
            for t in range(4):
                tok0 = g * 512 + t * 128
                P.dma(xin4[t][:], x_in[tok0:tok0 + 128, :], writes=[xin4[t]])
            for k in range(KD):
                pp = bank[k % 2]
                sbuf = tp_sb[k % 2]
                for t in range(4):
                    P.pe(lambda e, pp=pp, t=t, k=k: e.transpose(pp[:, t * 128:(t + 1) * 128], xin4[t][:, k * 128:(k + 1) * 128], ident[:]),
                         reads=[xin4[t], ident], writes=[pp])
                P.act(lambda e, pp=pp, sbuf=sbuf: e.copy(out=sbuf[:], in_=pp[:]), reads=[pp], writes=[sbuf])
                P.dma(xT[k * 128:(k + 1) * 128, g * 512:(g + 1) * 512], sbuf[:], reads=[sbuf], writes=[("xT", g)], q="pool")
        if "p3" in phases:
            pin = [P.sb([128, 256], F32, f"pin{i}") for i in range(4)]
            psb = [P.sb([128, 512], BF16, f"psb{i}") for i in range(2)]
            np_ = 0
            for l in range(L):
                for g in range(NG):
                    for t in range(4):
                        tok0 = g * 512 + t * 128
                        P.dma(pin[t][:], p_in[l, tok0:tok0 + 128, :], writes=[pin[t]])
                    for kk in range(2):
                        pp = bank[2 + np_ % 2]
                        sbuf = psb[np_ % 2]
                        np_ += 1
                        for t in range(4):
                            P.pe(lambda e, pp=pp, t=t, kk=kk: e.transpose(pp[:, t * 128:(t + 1) * 128], pin[t][:, kk * 128:(kk + 1) * 128], ident[:]),
                                 reads=[pin[t], ident], writes=[pp])
                        P.act(lambda e, pp=pp, sbuf=sbuf: e.copy(out=sbuf[:], in_=pp[:]), reads=[pp], writes=[sbuf])
                        P.dma(pT[l][kk * 128:(kk + 1) * 128, g * 512:(g + 1) * 512], sbuf[:], reads=[sbuf], writes=[("pT", l)], q="pool")

    if "mla" in phases:
      with P.phase():
        pos_i = P.sb([64, 512], I32, "pos_i")
        ang = P.sb([64, 512], F32, "ang")
        ang2 = P.sb([64, 512], F32, "ang2")
        pos_k = P.sb([64, 512], I32, "pos_k")
        trig = [P.sb([64, 512], F32, f"trig{i}") for i in range(2)]
        negpi = P.sb([64, 1], F32, "negpi")
        P.pool(lambda e: e.memset(negpi[:], -float(np.pi)), writes=[negpi])
        TWO_PI = float(2 * np.pi)
        for g in range(NG):
            P.dma(pos_i[:], pos_in[0:1, g * 512:(g + 1) * 512].partition_broadcast(64), writes=[pos_i], slow=True)
            P.dve(lambda e: e.tensor_copy(out=ang[:], in_=pos_i[:]), reads=[pos_i], writes=[ang])
            P.dve(lambda e: e.tensor_scalar(out=ang[:], in0=ang[:], scalar1=inv_freq[:, 0:1], scalar2=1.0, op0=ALU.mult, op1=ALU.mult),
                  reads=[ang, inv_freq], writes=[ang])
            for which, shift, dst_d in ((0, 0.0, sinT), (1, float(np.pi / 2), cosT)):
                tr = trig[which]
                P.dve(lambda e, shift=shift: e.tensor_scalar(out=ang2[:], in0=ang[:], scalar1=shift, scalar2=float(1.0 / (2 * np.pi)), op0=ALU.add, op1=ALU.mult),
                      reads=[ang], writes=[ang2])
                P.dve(lambda e: e.tensor_copy(out=pos_k[:], in_=ang2[:]), reads=[ang2], writes=[pos_k])
                P.dve(lambda e: e.tensor_copy(out=ang2[:], in_=pos_k[:]), reads=[pos_k], writes=[ang2])
                P.dve(lambda e, tr=tr, shift=shift: e.tensor_scalar(out=tr[:], in0=ang[:], scalar1=shift, scalar2=1.0, op0=ALU.add, op1=ALU.mult),
                      reads=[ang], writes=[tr])
                P.dve(lambda e, tr=tr: e.scalar_tensor_tensor(out=tr[:], in0=ang2[:], scalar=-6.28125, in1=tr[:], op0=ALU.mult, op1=ALU.add),
                      reads=[ang2, tr], writes=[tr])
                P.dve(lambda e, tr=tr: e.scalar_tensor_tensor(out=tr[:], in0=ang2[:], scalar=-0.0019353071795864769, in1=tr[:], op0=ALU.mult, op1=ALU.add),
                      reads=[ang2, tr], writes=[tr])
                P.dve(lambda e, tr=tr: e.tensor_scalar(out=ang2[:], in0=tr[:], scalar1=float(np.pi), scalar2=TWO_PI, op0=ALU.is_gt, op1=ALU.mult),
                      reads=[tr], writes=[ang2])
                P.dve(lambda e, tr=tr: e.tensor_tensor(out=tr[:], in0=tr[:], in1=ang2[:], op=ALU.subtract), reads=[tr, ang2], writes=[tr])
                P.dve(lambda e, tr=tr: e.tensor_scalar(out=ang2[:], in0=tr[:], scalar1=-float(np.pi), scalar2=TWO_PI, op0=ALU.is_lt, op1=ALU.mult),
                      reads=[tr], writes=[ang2])
                P.dve(lambda e, tr=tr: e.tensor_tensor(out=tr[:], in0=tr[:], in1=ang2[:], op=ALU.add), reads=[tr, ang2], writes=[tr])
                P.dve(lambda e, tr=tr: e.tensor_scalar(out=tr[:], in0=tr[:], scalar1=float(np.pi), scalar2=-float(np.pi), op0=ALU.min, op1=ALU.max),
                      reads=[tr], writes=[tr])
                P.act(lambda e, tr=tr: e.activation(out=tr[:], in_=tr[:], func=AF.Sin), reads=[tr], writes=[tr])
                P.dma(dst_d[:, g * 512:(g + 1) * 512], tr[:], reads=[tr], writes=[("sinT" if which == 0 else "cosT", g)], q="pool")

    gain_mix = P.sb([128, L, KD], F32, "gain_mix")
    gain_q = P.sb([128, L, 4], F32, "gain_q")
    gain_kv = P.sb([128, L, 4], F32, "gain_kv")
    esink = P.sb([64, L, 16], F32, "esink")
    gain_ffn = P.sb([128, L, KD], F32, "gain_ffn")
    gain_ple = P.sb([128, L, KD], F32, "gain_ple")
    gain_fin = P.sb([128, KD], F32, "gain_fin")
    Wall = P.sb([128, NT, 8], F32, "Wall")
    Iall = P.sb([128, NT, 8], I32, "Iall")
    P.dma(gain_fin[:, :], final_norm[0].rearrange("(k p) -> p k", p=128), writes=[gain_fin], slow=True)
    for l in range(L):
        P.dma(gain_mix[:, l, :], norm_mix[l].rearrange("(k p) -> p k", p=128), writes=[gain_mix], slow=True)
        P.dma(gain_ffn[:, l, :], norm_ffn[l].rearrange("(k p) -> p k", p=128), writes=[gain_ffn], slow=True)
        P.dma(gain_ple[:, l, :], norm_ple[l].rearrange("(k p) -> p k", p=128), writes=[gain_ple], slow=True)
        P.dma(gain_q[:, l, :], mla_q_norm[l].rearrange("(k p) -> p k", p=128), writes=[gain_q], slow=True)
        P.dma(gain_kv[:, l, :], mla_kv_norm[l].rearrange("(k p) -> p k", p=128), writes=[gain_kv], slow=True)
        P.dma(esink[:, l, :], swa_sinks[l:l + 1, :].partition_broadcast(64), writes=[esink], slow=True)
    P.act(lambda e: e.activation(out=esink[:], in_=esink[:], func=AF.Exp), reads=[esink], writes=[esink])

    def rms_T(src, dst, nk, dim, gain, gkey, ss_bank, sq, inv):
        for k in range(nk):
            P.act(lambda e, k=k: e.activation(out=sq[:, k, :], in_=src[:, k, :], func=AF.Square), reads=[(src, k)], writes=[(sq, k)])
        for k in range(nk):
            P.pe(lambda e, k=k: e.matmul(ss_bank[:], lhsT=ones_bf[:], rhs=sq[:, k, :], start=(k == 0), stop=(k == nk - 1)),
                 reads=[ones_bf, (sq, k)], writes=[ss_bank])
        P.act(lambda e: e.activation(out=inv[:], in_=ss_bank[:], func=AF.Sqrt, scale=1.0 / dim, bias=eps_t[:, 0:1]),
              reads=[ss_bank, eps_t], writes=[inv])
        P.dve(lambda e: e.reciprocal(out=inv[:], in_=inv[:]), reads=[inv], writes=[inv])
        for k in range(nk):
            P.dve(lambda e, k=k: e.scalar_tensor_tensor(out=dst[:, k, :], in0=src[:, k, :], scalar=gain[:, k:k + 1], in1=inv[:],
                                                        op0=ALU.mult, op1=ALU.mult),
                  reads=[(src, k), inv, gkey], writes=[(dst, k)])

    cnt = {"blk": 0, "ev": 0}

    def p1(l):
        xg = P.sb([128, KD, 512], F32, "xg")
        sq = P.sb([128, KD, 512], BF16, "sq")
        hT = P.sb([128, KD, 512], BF16, "hT")
        inv = P.sb([128, 512], F32, "inv")
        wblk = [P.sb([128, KD, 512], BF16, f"wblk{i}") for i in range(2)]
        ev_sb = [P.sb([128, 512], F32, f"ev_sb{i}") for i in range(3)]
        evb_sb = [P.sb([128, 512], BF16, f"evb_sb{i}") for i in range(3)]
        for g in range(NG):
            for k in range(KD):
                P.dma(xg[:, k, :], xT[k * 128:(k + 1) * 128, g * 512:(g + 1) * 512], reads=[("xT", g)], writes=[(xg, k)])
            rms_T(xg, hT, KD, D, gain_mix[:, l, :], gain_mix, bank[2], sq, inv)
            for bi, (c0, wd, mode) in enumerate(blocks):
                wb = wblk[cnt["blk"] % 2]
                cnt["blk"] += 1
                P.dma(wb[:, :, :], blk3(w_in_bf[l][bi]), reads=[("w_in_bf", l)], writes=[wb])
                if mode == "t":
                    for t in range(4):
                        pp = bank[cnt["ev"] % 2]
                        for k in range(KD):
                            P.pe(lambda e, pp=pp, wb=wb, k=k, t=t, wd=wd: e.matmul(pp[:, 0:wd], lhsT=hT[:, k, t * 128:(t + 1) * 128], rhs=wb[:, k, 0:wd],
                                                                                     start=(k == 0), stop=(k == KD - 1)),
                                 reads=[(hT, k), wb], writes=[pp])
                        tok0 = g * 512 + t * 128
                        if c0 == C_AV:
                            ob = evb_sb[cnt["ev"] % 3]
                            P.act(lambda e, pp=pp, ob=ob, wd=wd: e.copy(out=ob[:, 0:wd], in_=pp[:, 0:wd]), reads=[pp], writes=[ob])
                            P.dma(vA[tok0:tok0 + 128, :], ob[:, 0:wd], reads=[ob], writes=[("vA", g)], q="pool")
                        else:
                            ob = ev_sb[cnt["ev"] % 3]
                            P.act(lambda e, pp=pp, ob=ob, wd=wd: e.copy(out=ob[:, 0:wd], in_=pp[:, 0:wd]), reads=[pp], writes=[ob])
                            P.dma(bd[tok0:tok0 + 128, :], ob[:, 0:wd], reads=[ob], writes=[("bd", g)], q="pool")
                        cnt["ev"] += 1
                    continue
                for j in range(0, wd, 128):
                    cw = min(128, wd - j)
                    pp = bank[cnt["ev"] % 2]
                    for k in range(KD):
                        P.pe(lambda e, pp=pp, wb=wb, k=k, j=j, cw=cw: e.matmul(pp[0:cw, :], lhsT=wb[:, k, j:j + cw], rhs=hT[:, k, :],
                                                                                start=(k == 0), stop=(k == KD - 1)),
                             reads=[wb, (hT, k)], writes=[pp])
                    col = c0 + j
                    if col >= C_G:
                        ob = evb_sb[cnt["ev"] % 3]
                        P.act(lambda e, pp=pp, ob=ob, cw=cw: e.activation(out=ob[0:cw, :], in_=pp[0:cw, :], func=AF.Sigmoid),
                              reads=[pp], writes=[ob])
                        P.dma(gatesT[col - C_G:col - C_G + cw, g * 512:(g + 1) * 512], ob[0:cw, :], reads=[ob],
                              writes=[("gatesT", g)], q="pool")
                    else:
                        ob = ev_sb[cnt["ev"] % 3]
                        P.act(lambda e, pp=pp, ob=ob, cw=cw: e.copy(out=ob[0:cw, :], in_=pp[0:cw, :]), reads=[pp], writes=[ob])
                        P.dma(projT[col:col + cw, g * 512:(g + 1) * 512], ob[0:cw, :], reads=[ob], writes=[("projT", g)], q="pool")
                    cnt["ev"] += 1

    def swa(l):
        swa_kT = P.sb([64, S], BF16, "swa_kT")
        swa_v = P.sb([128, NT, 64], BF16, "swa_v")
        swa_q = [P.sb([64, 4, 512], BF16, f"swa_q{i}") for i in range(2)]
        swa_p = [P.sb([128, 512], BF16, f"swa_p{i}") for i in range(4)]
        swa_den = P.sb([64, 512], F32, "swa_den")
        swa_y = [P.sb([64, 4, 512], BF16, f"swa_y{i}") for i in range(2)]

        npt = 0
        for hk in range(4):
            P.dma(swa_kT[:, :], projT[C_AK + hk * 64:C_AK + (hk + 1) * 64, :], reads=keys("projT"), writes=[swa_kT], q="pool")
            P.dma(swa_v[:, :, :], vA[:, hk * 64:(hk + 1) * 64].rearrange("(t p) d -> p t d", p=128), reads=keys("vA"), writes=[swa_v], slow=True)
            for g in range(NG):
                qt = swa_q[g % 2]
                yb = swa_y[g % 2]
                P.dma(qt[:, :, :], projT[hk * 256:(hk + 1) * 256, g * 512:(g + 1) * 512].rearrange("(i d) t -> d i t", d=64),
                      reads=[("projT", g)], writes=[qt], q="pool")
                for b in range(4):
                    n = g * 4 + b
                    o_ps = bank[4 + (n % 2)]
                    s_ps = bank[6 + (n % 2)]
                    jl = [j for j in (n - 1, n) if j >= 0]
                    for idx, j in enumerate(jl):
                        sc = bank[npt % 4]
                        pt = swa_p[npt % 4]
                        npt += 1
                        P.pe(lambda e, sc=sc, j=j, qt=qt, b=b: e.matmul(sc[:, :].rearrange("p (i t) -> p i t", i=4), lhsT=swa_kT[:, j * 128:(j + 1) * 128],
                                                                         rhs=qt[:, :, b * 128:(b + 1) * 128], start=True, stop=True),
                             reads=[swa_kT, qt], writes=[sc])
                        P.act(lambda e, sc=sc, pt=pt: e.activation(out=pt[:], in_=sc[:], func=AF.Exp, scale=0.125), reads=[sc], writes=[pt])
                        mi = 1 if j == n else 0
                        P.dve(lambda e, pt=pt, mi=mi: e.tensor_tensor(out=pt[:], in0=pt[:], in1=m_swa[:, mi, :], op=ALU.mult),
                              reads=[pt, m_swa], writes=[pt])
                        first, last = idx == 0, idx == len(jl) - 1
                        P.pe(lambda e, o_ps=o_ps, pt=pt, j=j, first=first, last=last: e.matmul(o_ps[0:64, :], lhsT=swa_v[:, j, :], rhs=pt[:], start=first, stop=last),
                             reads=[swa_v, pt], writes=[o_ps])
                        P.pe(lambda e, s_ps=s_ps, pt=pt, first=first, last=last: e.matmul(s_ps[0:64, :], lhsT=ones_bf[:, 0:64], rhs=pt[:], start=first, stop=last),
                             reads=[ones_bf, pt], writes=[s_ps])
                    for i in range(4):
                        h = hk * 4 + i
                        P.dve(lambda e, s_ps=s_ps, i=i, h=h: e.tensor_scalar(out=swa_den[:, i * 128:(i + 1) * 128], in0=s_ps[0:64, i * 128:(i + 1) * 128],
                                                                               scalar1=esink[:, l, h:h + 1], scalar2=1.0, op0=ALU.add, op1=ALU.mult),
                              reads=[s_ps, esink], writes=[swa_den])
                    P.dve(lambda e: e.reciprocal(out=swa_den[:], in_=swa_den[:]), reads=[swa_den], writes=[swa_den])
                    P.dve(lambda e, o_ps=o_ps, yb=yb, b=b: e.tensor_tensor(out=yb[:, :, b * 128:(b + 1) * 128],
                                                                           in0=o_ps[0:64, :].rearrange("p (i t) -> p i t", i=4),
                                                                           in1=swa_den[:, :].rearrange("p (i t) -> p i t", i=4), op=ALU.mult),
                          reads=[o_ps, swa_den], writes=[yb])
                P.dma(yT[hk * 256:(hk + 1) * 256, g * 512:(g + 1) * 512].rearrange("(i d) t -> d i t", d=64), yb[:, :, :],
                      reads=[yb], writes=[("yT_a", g)], q="pool")

    def mla1(l):
        wuq_sb = P.sb([128, 4, 1536], BF16, "wuq_sb")
        wukv_sb = P.sb([128, 4, 2048], BF16, "wukv_sb")
        lat = P.sb([128, 4, 512], F32, "lat")
        latn = [P.sb([128, 4, 512], BF16, f"latn{i}") for i in range(2)]
        sq = P.sb([128, 4, 512], BF16, "sq_m")
        inv = P.sb([128, 512], F32, "inv_m")
        rope_x = P.sb([64, 512], F32, "rope_x")
        rope_t = P.sb([64, 512], F32, "rope_t")
        rope_o = [P.sb([64, 512], BF16, f"rope_o{i}") for i in range(2)]
        cs_sb = P.sb([64, 2, 512], F32, "cs_sb")
        evb_sb = [P.sb([128, 512], BF16, f"evb_m{i}") for i in range(3)]
        def rope(src_ps_or_sb, src_key, nrows_dummy, dst, g):
            P.pe(lambda e: e.matmul(bank[3][0:64, :], lhsT=rot[:, :], rhs=rope_x[:, :], start=True, stop=True), reads=[rot, rope_x], writes=[bank[3]])
            P.dve(lambda e: e.tensor_tensor(out=rope_t[:], in0=bank[3][0:64, :], in1=cs_sb[:, 1, :], op=ALU.mult), reads=[bank[3], cs_sb], writes=[rope_t])
            P.dve(lambda e: e.tensor_tensor(out=rope_x[:], in0=rope_x[:], in1=cs_sb[:, 0, :], op=ALU.mult), reads=[rope_x, cs_sb], writes=[rope_x])
            P.dve(lambda e, dst=dst: e.tensor_tensor(out=dst[:], in0=rope_x[:], in1=rope_t[:], op=ALU.add), reads=[rope_x, rope_t], writes=[dst])

        for k in range(4):
            P.dma(wuq_sb[:, k, :], w_uq[l, k * 128:(k + 1) * 128, :], writes=[wuq_sb], q="pool")
            P.dma(wukv_sb[:, k, :], w_ukv[l, k * 128:(k + 1) * 128, :], writes=[wukv_sb], q="pool")
        nev = 0
        for g in range(NG):
            gs = slice(g * 512, (g + 1) * 512)
            P.dma(cs_sb[:, 0, :], cosT[:, gs], reads=[("cosT", g)], writes=[cs_sb])
            P.dma(cs_sb[:, 1, :], sinT[:, gs], reads=[("sinT", g)], writes=[cs_sb])
            for k in range(4):
                P.dma(lat[:, k, :], projT[C_CQ + k * 128:C_CQ + (k + 1) * 128, gs], reads=[("projT", g)], writes=[(lat, k)])
            rms_T(lat, latn[0], 4, 512, gain_q[:, l, :], gain_q, bank[2], sq, inv)
            for h in range(8):
                pp = bank[nev % 2]
                for k in range(4):
                    P.pe(lambda e, pp=pp, k=k, h=h: e.matmul(pp[:, :], lhsT=wuq_sb[:, k, h * 192:h * 192 + 128], rhs=latn[0][:, k, :], start=(k == 0), stop=(k == 3)),
                         reads=[wuq_sb, (latn[0], k)], writes=[pp])
                ob = evb_sb[nev % 3]
                P.act(lambda e, pp=pp, ob=ob: e.copy(out=ob[:], in_=pp[:]), reads=[pp], writes=[ob])
                P.dma(qnT[h * 128:(h + 1) * 128, gs], ob[:], reads=[ob], writes=[("qnT", g)], q="pool")
                nev += 1
                pp = bank[nev % 2]
                for k in range(4):
                    P.pe(lambda e, pp=pp, k=k, h=h: e.matmul(pp[0:64, :], lhsT=wuq_sb[:, k, h * 192 + 128:h * 192 + 192], rhs=latn[0][:, k, :], start=(k == 0), stop=(k == 3)),
                         reads=[wuq_sb, (latn[0], k)], writes=[pp])
                P.act(lambda e, pp=pp: e.copy(out=rope_x[:], in_=pp[0:64, :]), reads=[pp], writes=[rope_x])
                ro = rope_o[nev % 2]
                rope(None, None, None, ro, g)
                P.dma(qrT[h * 64:(h + 1) * 64, gs], ro[:], reads=[ro], writes=[("qrT", g)], q="pool")
                nev += 1
            P.dma(rope_x[:], projT[C_CKR:C_CKR + 64, gs], reads=[("projT", g)], writes=[rope_x])
            ro = rope_o[nev % 2]
            rope(None, None, None, ro, g)
            P.dma(krT[:, gs], ro[:], reads=[ro], writes=[("krT", g)], q="pool")
            nev += 1
            for k in range(4):
                P.dma(lat[:, k, :], projT[C_CKV + k * 128:C_CKV + (k + 1) * 128, gs], reads=[("projT", g)], writes=[(lat, k)])
            rms_T(lat, latn[1], 4, 512, gain_kv[:, l, :], gain_kv, bank[2], sq, inv)
            for h in range(8):
                pp = bank[nev % 2]
                for k in range(4):
                    P.pe(lambda e, pp=pp, k=k, h=h: e.matmul(pp[:, :], lhsT=wukv_sb[:, k, h * 256:h * 256 + 128], rhs=latn[1][:, k, :], start=(k == 0), stop=(k == 3)),
                         reads=[wukv_sb, (latn[1], k)], writes=[pp])
                ob = evb_sb[nev % 3]
                P.act(lambda e, pp=pp, ob=ob: e.copy(out=ob[:], in_=pp[:]), reads=[pp], writes=[ob])
                P.dma(knT[h * 128:(h + 1) * 128, gs], ob[:], reads=[ob], writes=[("knT", g)], q="pool")
                nev += 1
            for t in range(4):
                tok0 = g * 512 + t * 128
                for hh in range(2):
                    pp = bank[nev % 2]
                    for k in range(4):
                        P.pe(lambda e, pp=pp, k=k, t=t, hh=hh: e.matmul(pp[:, :].rearrange("p (h d) -> p h d", h=4), lhsT=latn[1][:, k, t * 128:(t + 1) * 128],
                                                                         rhs=wukv_sb[:, k, :].rearrange("p (h c) -> p h c", h=8)[:, hh * 4:(hh + 1) * 4, 128:256],
                                                                         start=(k == 0), stop=(k == 3)),
                             reads=[(latn[1], k), wukv_sb], writes=[pp])
                    ob = evb_sb[nev % 3]
                    P.act(lambda e, pp=pp, ob=ob: e.copy(out=ob[:], in_=pp[:]), reads=[pp], writes=[ob])
                    P.dma(vC[tok0:tok0 + 128, hh * 512:(hh + 1) * 512], ob[:], reads=[ob], writes=[("vC", g)], q="pool")
                    nev += 1

    def mla2(l):
        mla_k = P.sb([128, S], BF16, "mla_k")
        mla_kr = P.sb([64, S], BF16, "mla_kr")
        mla_v = P.sb([128, NT, 128], BF16, "mla_v")
        mla_qn = [P.sb([128, 512], BF16, f"mla_qn{i}") for i in range(2)]
        mla_qr = [P.sb([64, 512], BF16, f"mla_qr{i}") for i in range(2)]
        mla_p = [P.sb([128, 512], BF16, f"mla_p{i}") for i in range(4)]
        mla_rs = P.sb([128, 512], F32, "mla_rs")
        mla_y = [P.sb([128, 512], BF16, f"mla_y{i}") for i in range(2)]
        P.dma(mla_kr[:, :], krT[:, :], reads=keys("krT"), writes=[mla_kr])
        scale = float(192 ** -0.5)
        npt = 0
        nq = 0
        for h in range(8):
            P.dma(mla_k[:, :], knT[h * 128:(h + 1) * 128, :], reads=keys("knT"), writes=[mla_k])
            P.dma(mla_v[:, :, :], vC[:, h * 128:(h + 1) * 128].rearrange("(t p) d -> p t d", p=128), reads=keys("vC"), writes=[mla_v], slow=True)
            for g in range(NG):
                gs = slice(g * 512, (g + 1) * 512)
                qn = mla_qn[nq % 2]
                qr = mla_qr[nq % 2]
                yb = mla_y[nq % 2]
                o_ps = bank[4 + (nq % 2)]
                s_ps = bank[6 + (nq % 2)]
                nq += 1
                P.dma(qn[:, :], qnT[h * 128:(h + 1) * 128, gs], reads=[("qnT", g)], writes=[qn])
                P.dma(qr[:, :], qrT[h * 64:(h + 1) * 64, gs], reads=[("qrT", g)], writes=[qr])
                nj = 4 * g + 4

                def qk(j, slot):
                    sc = bank[slot % 4]
                    P.pe(lambda e, sc=sc, j=j, qn=qn: e.matmul(sc[:, :], lhsT=mla_k[:, j * 128:(j + 1) * 128], rhs=qn[:, :], start=True, stop=False),
                         reads=[mla_k, qn], writes=[sc])
                    P.pe(lambda e, sc=sc, j=j, qr=qr: e.matmul(sc[:, :], lhsT=mla_kr[:, j * 128:(j + 1) * 128], rhs=qr[:, :], start=False, stop=True),
                         reads=[mla_kr, qr], writes=[sc])

                qk(0, npt)
                for j in range(nj):
                    sc = bank[npt % 4]
                    pt = mla_p[npt % 4]
                    if j + 1 < nj:
                        qk(j + 1, npt + 1)
                    npt += 1
                    P.act(lambda e, sc=sc, pt=pt: e.activation(out=pt[:], in_=sc[:], func=AF.Exp, scale=scale), reads=[sc], writes=[pt])
                    if j >= 4 * g:
                        jj = j - 4 * g
                        P.dve(lambda e, pt=pt, jj=jj: e.tensor_tensor(out=pt[:], in0=pt[:], in1=m_mla[:, jj, :], op=ALU.mult),
                              reads=[pt, m_mla], writes=[pt])
                    first, last = j == 0, j == nj - 1
                    P.pe(lambda e, o_ps=o_ps, pt=pt, j=j, first=first, last=last: e.matmul(o_ps[:, :], lhsT=mla_v[:, j, :], rhs=pt[:], start=first, stop=last),
                         reads=[mla_v, pt], writes=[o_ps])
                    P.pe(lambda e, s_ps=s_ps, pt=pt, first=first, last=last: e.matmul(s_ps[:, :], lhsT=ones_bf[:, :], rhs=pt[:], start=first, stop=last),
                         reads=[ones_bf, pt], writes=[s_ps])
                P.dve(lambda e, s_ps=s_ps: e.reciprocal(out=mla_rs[:], in_=s_ps[:]), reads=[s_ps], writes=[mla_rs])
                P.dve(lambda e, o_ps=o_ps, yb=yb: e.tensor_tensor(out=yb[:], in0=o_ps[:], in1=mla_rs[:], op=ALU.mult), reads=[o_ps, mla_rs], writes=[yb])
                P.dma(yT[2048 + h * 128:2048 + (h + 1) * 128, gs], yb[:], reads=[yb], writes=[("yT_c", g)], q="pool")

    def dn(l):
        NB = 4
        stop = dbg.get('dn_stop', 99)
        ones_f = P.sb([128, 128], F32, "ones_f")
        P.pool(lambda e: e.memset(ones_f[:], 1.0), writes=[ones_f])
        one_t = P.sb([128, 1], F32, "one_t")
        P.pool(lambda e: e.memset(one_t[:], 1.0), writes=[one_t])
        eps6 = P.sb([128, 1], F32, "eps6")
        P.pool(lambda e: e.memset(eps6[:], 1e-6), writes=[eps6])
        tri = P.sb([128, 2, 128], F32, "tri")
        P.dma(tri[:, 0, :], tri_in[0], writes=[tri])
        P.dma(tri[:, 1, :], tri_in[1], writes=[tri])
        U = tri[:, 0, :]
        TL = tri[:, 1, :]
        convw = P.sb([128, 4, 24], F32, "convw")
        for j in range(4):
            P.dma(convw[:, j, :], conv_w[l, j].rearrange("(c p) -> p c", p=128), writes=[convw], slow=True)
        dtb = P.sb([128, 8], F32, "dtb")
        nA = P.sb([128, 8], F32, "nA")
        gn = P.sb([128, 1], F32, "gn")
        P.dma(dtb[:, :], dn_dt_bias[l:l + 1, :].partition_broadcast(128), writes=[dtb], slow=True)
        P.dma(nA[:, :], dn_a_log[l:l + 1, :].partition_broadcast(128), writes=[nA], slow=True)
        P.dma(gn[:, :], dn_norm[l].rearrange("(p o) -> p o", o=1), writes=[gn], slow=True)
        P.act(lambda e: e.activation(out=nA[:], in_=nA[:], func=AF.Exp), reads=[nA], writes=[nA])
        P.dve(lambda e: e.tensor_scalar(out=nA[:], in0=nA[:], scalar1=-1.0, scalar2=0.0, op0=ALU.mult, op1=ALU.add), reads=[nA], writes=[nA])
        bdt = P.sb([128, NT, 16], F32, "bdt")
        P.dma(bdt[:, :, :], bd[:, :].rearrange("(t p) c -> p t c", p=128), reads=keys("bd"), writes=[bdt], slow=True)
        betat = P.sb([128, NT, 8], F32, "betat")
        gt = P.sb([128, NT, 8], F32, "gt")
        P.act(lambda e: e.activation(out=betat[:, :, :], in_=bdt[:, :, 0:8], func=AF.Sigmoid), reads=[bdt], writes=[betat])
        for t in range(NT):
            P.dve(lambda e, t=t: e.tensor_tensor(out=gt[:, t, :], in0=bdt[:, t, 8:16], in1=dtb[:, :], op=ALU.add), reads=[bdt, dtb], writes=[gt])
        P.act(lambda e: e.activation(out=gt[:, :, :], in_=gt[:, :, :], func=AF.Exp), reads=[gt], writes=[gt])
        P.act(lambda e: e.activation(out=gt[:, :, :], in_=gt[:, :, :], func=AF.Ln, bias=one_t[:, 0:1], scale=1.0), reads=[gt, one_t], writes=[gt])
        for t in range(NT):
            P.dve(lambda e, t=t: e.tensor_tensor(out=gt[:, t, :], in0=gt[:, t, :], in1=nA[:, :], op=ALU.mult), reads=[gt, nA], writes=[gt])

        if stop <= 1:
            return
        if dbg.get("dn_dump"):
            dd = {n: ext_out("o_dn" + n, [1024, S]) for n in ("q", "k", "v", "o")}
            dgb = ext_out("o_dngb", [128, NT, 16])
            P.dma(dgb[:, :, 0:8], gt[:, :, :], reads=[gt], writes=["o_dngb"])
            P.dma(dgb[:, :, 8:16], betat[:, :, :], reads=[betat], writes=["o_dngb"])
        xin = [P.sb([128, 515], F32, f"dn_xin{i}") for i in range(2)]
        cacc = P.sb([128, 512], F32, "dn_cacc")
        qkvT = [P.sb([128, 512], F32, f"dn_qkvT{i}") for i in range(3)]
        sqb = P.sb([128, 512], BF16, "dn_sqb")
        rinv = P.sb([128, 512], F32, "dn_rinv")
        zT = P.sb([128, 512], F32, "dn_zT")
        oT = P.sb([128, 512], F32, "dn_oT")
        yb = [P.sb([128, 512], BF16, f"dn_yb{i}") for i in range(2)]
        St = P.sb([128, 128], F32, "dn_S")
        names = ["ktok", "vtok", "gcrow", "gccol", "t1", "t2", "Lm", "NTm", "Pa", "PaT", "Pb", "PbT", "R", "qkT", "qgT", "kbg", "vb", "kd",
                 "egrow", "sc1", "sc2", "u", "wT", "vnew"]
        W = [{n: P.sb([128, 128] if n not in ("gccol", "sc1", "sc2") else [128, 1], F32, f"dn_{n}{b}") for n in names} for b in range(NB)]
        nps = [0]

        def psq():
            i = nps[0] % 28
            nps[0] += 1
            bi, qi = i % 7, (i // 7) % 4
            return bank[bi][:, qi * 128:(qi + 1) * 128], ("bank", bi)

        def mmf(lhsT, rhs, rd, n=128):
            pp, key = psq()
            P.pe(lambda e, pp=pp, lhsT=lhsT, rhs=rhs, n=n: e.matmul(pp[:, 0:n], lhsT=lhsT, rhs=rhs, start=True, stop=True), reads=rd, writes=[key])
            return pp, key

        def tpf(src, rd):
            pp, key = psq()
            P.pe(lambda e, pp=pp, src=src: e.transpose(pp, src, ident[:]), reads=rd + [ident], writes=[key])
            return pp, key

        for h in range(8):
            P.dve(lambda e: e.memset(St[:], 0.0), writes=[St])
            for g in range(NG):
                gs = slice(g * 512, (g + 1) * 512)
                for ci in range(3):
                    chunk = ci * 8 + h
                    row0 = C_BQKV + chunk * 128
                    xi = xin[ci % 2]
                    if g == 0:
                        P.dve(lambda e, xi=xi: e.memset(xi[:, 0:3], 0.0), writes=[xi])
                        P.dma(xi[:, 3:515], projT[row0:row0 + 128, 0:512], reads=[("projT", 0)], writes=[xi])
                    else:
                        P.dma(xi[:, 0:515], projT[row0:row0 + 128, g * 512 - 3:(g + 1) * 512], reads=[("projT", g - 1), ("projT", g)], writes=[xi])
                    P.act(lambda e, xi=xi, chunk=chunk: e.activation(out=cacc[:], in_=xi[:, 0:512], func=AF.Copy, scale=convw[:, 0, chunk:chunk + 1]),
                          reads=[xi, convw], writes=[cacc])
                    for j in range(1, 4):
                        P.dve(lambda e, xi=xi, chunk=chunk, j=j: e.scalar_tensor_tensor(out=cacc[:], in0=xi[:, j:j + 512], scalar=convw[:, j, chunk:chunk + 1], in1=cacc[:],
                                                                                       op0=ALU.mult, op1=ALU.add), reads=[xi, convw, cacc], writes=[cacc])
                    dst = qkvT[ci]
                    P.act(lambda e, dst=dst: e.activation(out=dst[:], in_=cacc[:], func=AF.Silu), reads=[cacc], writes=[dst])
                    if ci < 2:
                        P.act(lambda e, dst=dst: e.activation(out=sqb[:], in_=dst[:], func=AF.Square), reads=[dst], writes=[sqb])
                        P.pe(lambda e: e.matmul(bank[7][:, :], lhsT=ones_bf[:], rhs=sqb[:], start=True, stop=True), reads=[ones_bf, sqb], writes=[("bank", 7)])
                        P.act(lambda e: e.activation(out=rinv[:], in_=bank[7][:, :], func=AF.Sqrt, bias=eps6[:, 0:1], scale=1.0),
                              reads=[("bank", 7), eps6], writes=[rinv])
                        P.dve(lambda e: e.reciprocal(out=rinv[:], in_=rinv[:]), reads=[rinv], writes=[rinv])
                        sc = float(128 ** -0.5) if ci == 0 else 1.0
                        P.dve(lambda e, dst=dst, sc=sc: e.scalar_tensor_tensor(out=dst[:], in0=dst[:], scalar=sc, in1=rinv[:], op0=ALU.mult, op1=ALU.mult),
                              reads=[dst, rinv], writes=[dst])
                qT_, kT_, vT_ = qkvT
                if dbg.get("dn_dump"):
                    for n_, t_ in (("q", qT_), ("k", kT_), ("v", vT_)):
                        P.dma(dd[n_][h * 128:(h + 1) * 128, gs], t_[:, :], reads=[t_], writes=["o_dn" + n_], q="pool")
                P.dma(zT[:, :], projT[C_BZ + h * 128:C_BZ + (h + 1) * 128, gs], reads=[("projT", g)], writes=[zT])
                P.act(lambda e: e.activation(out=zT[:], in_=zT[:], func=AF.Silu), reads=[zT], writes=[zT])
                if stop <= 2:
                    return
                for b in range(NB):
                    w = W[b]
                    t = g * 4 + b
                    cs = slice(b * 128, (b + 1) * 128)
                    gcol = gt[:, t, h:h + 1]
                    bcol = betat[:, t, h:h + 1]
                    pp, key = tpf(kT_[:, cs], [kT_])
                    P.act(lambda e, pp=pp, w=w: e.copy(out=w["ktok"][:], in_=pp), reads=[key], writes=[w["ktok"]])
                    pp, key = tpf(vT_[:, cs], [vT_])
                    P.act(lambda e, pp=pp, w=w: e.copy(out=w["vtok"][:], in_=pp), reads=[key], writes=[w["vtok"]])
                    P.dve(lambda e, w=w, gcol=gcol: e.tensor_scalar(out=w["t1"][:], in0=U, scalar1=gcol, scalar2=1.0, op0=ALU.mult, op1=ALU.mult), reads=[tri, gt], writes=[w["t1"]])
                    pp, key = mmf(ones_f[:, :], w["t1"][:], [ones_f, w["t1"]])
                    P.act(lambda e, pp=pp, w=w: e.copy(out=w["gcrow"][:], in_=pp), reads=[key], writes=[w["gcrow"]])
                    P.dve(lambda e, w=w: e.tensor_tensor(out=w["t2"][:], in0=w["gcrow"][:], in1=ident[:], op=ALU.mult), reads=[w["gcrow"], ident], writes=[w["t2"]])
                    P.dve(lambda e, w=w: e.reduce_sum(out=w["gccol"][:], in_=w["t2"][:], axis=AX.X), reads=[w["t2"]], writes=[w["gccol"]])
                    P.dve(lambda e, w=w: e.tensor_scalar(out=w["t1"][:], in0=w["gcrow"][:], scalar1=-1.0, scalar2=w["gccol"][:, 0:1], op0=ALU.mult, op1=ALU.add),
                          reads=[w["gcrow"], w["gccol"]], writes=[w["t1"]])
                    P.dve(lambda e, w=w: e.tensor_scalar(out=w["t1"][:], in0=w["t1"][:], scalar1=0.0, scalar2=1.0, op0=ALU.min, op1=ALU.mult), reads=[w["t1"]], writes=[w["t1"]])
                    P.act(lambda e, w=w: e.activation(out=w["t1"][:], in_=w["t1"][:], func=AF.Exp), reads=[w["t1"]], writes=[w["t1"]])
                    P.dve(lambda e, w=w, bcol=bcol: e.scalar_tensor_tensor(out=w["t1"][:], in0=w["t1"][:], scalar=bcol, in1=TL, op0=ALU.mult, op1=ALU.mult),
                          reads=[w["t1"], betat, tri], writes=[w["t1"]])
                    pp, key = mmf(kT_[:, cs], kT_[:, cs], [kT_])
                    P.dve(lambda e, pp=pp, w=w: e.tensor_tensor(out=w["Lm"][:], in0=pp, in1=w["t1"][:], op=ALU.mult), reads=[key, w["t1"]], writes=[w["Lm"]])
                    P.dve(lambda e, w=w: e.tensor_scalar(out=w["t2"][:], in0=w["gcrow"][:], scalar1=w["gccol"][:, 0:1], scalar2=0.0, op0=ALU.subtract, op1=ALU.min),
                          reads=[w["gcrow"], w["gccol"]], writes=[w["t2"]])
                    P.act(lambda e, w=w: e.activation(out=w["t2"][:], in_=w["t2"][:], func=AF.Exp), reads=[w["t2"]], writes=[w["t2"]])
                    P.dve(lambda e, w=w: e.tensor_tensor(out=w["t2"][:], in0=w["t2"][:], in1=U, op=ALU.mult), reads=[w["t2"], tri], writes=[w["t2"]])
                    pp, key = mmf(kT_[:, cs], qT_[:, cs], [kT_, qT_])
                    P.dve(lambda e, pp=pp, w=w: e.tensor_tensor(out=w["qkT"][:], in0=pp, in1=w["t2"][:], op=ALU.mult), reads=[key, w["t2"]], writes=[w["qkT"]])
                    P.act(lambda e, w=w: e.activation(out=w["egrow"][:], in_=w["gcrow"][:], func=AF.Exp), reads=[w["gcrow"]], writes=[w["egrow"]])
                    P.dve(lambda e, w=w, cs=cs: e.tensor_tensor(out=w["qgT"][:], in0=qT_[:, cs], in1=w["egrow"][:], op=ALU.mult), reads=[qT_, w["egrow"]], writes=[w["qgT"]])
                    P.act(lambda e, w=w: e.activation(out=w["sc1"][:], in_=w["gccol"][:], func=AF.Exp), reads=[w["gccol"]], writes=[w["sc1"]])
                    P.dve(lambda e, w=w, bcol=bcol: e.tensor_tensor(out=w["sc1"][:], in0=w["sc1"][:], in1=bcol, op=ALU.mult), reads=[w["sc1"], betat], writes=[w["sc1"]])
                    P.dve(lambda e, w=w: e.tensor_scalar(out=w["kbg"][:], in0=w["ktok"][:], scalar1=w["sc1"][:, 0:1], scalar2=1.0, op0=ALU.mult, op1=ALU.mult),
                          reads=[w["ktok"], w["sc1"]], writes=[w["kbg"]])
                    P.dve(lambda e, w=w, bcol=bcol: e.tensor_scalar(out=w["vb"][:], in0=w["vtok"][:], scalar1=bcol, scalar2=1.0, op0=ALU.mult, op1=ALU.mult),
                          reads=[w["vtok"], betat], writes=[w["vb"]])
                    P.act(lambda e, w=w: e.activation(out=w["sc2"][:], in_=w["gccol"][:], func=AF.Exp, scale=-1.0, bias=w["gcrow"][:, 127:128]),
                          reads=[w["gccol"], w["gcrow"]], writes=[w["sc2"]])
                    P.dve(lambda e, w=w: e.tensor_scalar(out=w["kd"][:], in0=w["ktok"][:], scalar1=w["sc2"][:, 0:1], scalar2=1.0, op0=ALU.mult, op1=ALU.mult),
                          reads=[w["ktok"], w["sc2"]], writes=[w["kd"]])
                    pp, key = tpf(w["Lm"][:], [w["Lm"]])
                    P.act(lambda e, pp=pp, w=w: e.copy(out=w["NTm"][:], in_=pp), reads=[key], writes=[w["NTm"]])
                    P.dve(lambda e, pp=pp, w=w: e.tensor_tensor(out=w["R"][:], in0=ident[:], in1=pp, op=ALU.subtract), reads=[key, ident], writes=[w["R"]])
                if stop <= 3:
                    return
                cur = [(W[b]["Lm"], W[b]["NTm"]) for b in range(NB)]
                for m in range(1, 7):
                    nxt = []
                    res = []
                    for b in range(NB):
                        w = W[b]
                        M_, MT_ = cur[b]
                        p1_, k1 = mmf(MT_[:], M_[:], [MT_, M_])
                        if m < 6:
                            p2_, k2 = mmf(M_[:], MT_[:], [M_, MT_])
                        else:
                            p2_, k2 = None, None
                        res.append((p1_, k1, p2_, k2))
                    for b in range(NB):
                        w = W[b]
                        p1_, k1, p2_, k2 = res[b]
                        Pn = w["Pa"] if m % 2 == 1 else w["Pb"]
                        PnT = w["PaT"] if m % 2 == 1 else w["PbT"]
                        P.act(lambda e, p1_=p1_, Pn=Pn: e.copy(out=Pn[:], in_=p1_), reads=[k1], writes=[Pn])
                        if m < 6:
                            P.dve(lambda e, p2_=p2_, PnT=PnT: e.tensor_copy(out=PnT[:], in_=p2_), reads=[k2], writes=[PnT])
                        nxt.append((Pn, PnT))
                    if dbg.get("dn_bar"):
                        P.barrier()
                    if dbg.get("dn_dump") and h == 1 and g == 0:
                        if m == 1:
                            oP = ext_out("o_dnP", [6, 4, 128, 128]); oPT = ext_out("o_dnPT", [6, 4, 128, 128]); oR = ext_out("o_dnR", [6, 4, 128, 128])
                        for b_ in range(NB):
                            P.dma(oP[m - 1, b_], nxt[b_][0][:, :], reads=[nxt[b_][0]], writes=["o_dnP"], q="pool")
                            if m < 6:
                                P.dma(oPT[m - 1, b_], nxt[b_][1][:, :], reads=[nxt[b_][1]], writes=["o_dnPT"], q="pool")
                            P.dma(oR[m - 1, b_], W[b_]["R"][:, :], reads=[W[b_]["R"]], writes=["o_dnR"], q="pool")
                    res = []
                    for b in range(NB):
                        w = W[b]
                        Pn, PnT = nxt[b]
                        res.append(mmf(Pn[:], w["R"][:], [Pn, w["R"]]))
                    for b in range(NB):
                        w = W[b]
                        pp, key = res[b]
                        P.dve(lambda e, pp=pp, w=w: e.tensor_tensor(out=w["R"][:], in0=w["R"][:], in1=pp, op=ALU.add), reads=[key, w["R"]], writes=[w["R"]])
                    cur = nxt
                if stop <= 4:
                    return
                if dbg.get("dn_dump") and h == 1 and g == 0:
                    for nm_ in ("R", "Lm", "NTm", "Pa", "PaT", "Pb", "PbT"):
                        oo = ext_out("o_dnW_" + nm_, [4, 128, 128])
                        for b_ in range(NB):
                            P.dma(oo[b_], W[b_][nm_][:, :], reads=[W[b_][nm_]], writes=["o_dnW_" + nm_], q="pool")
                for b in range(NB):
                    w = W[b]
                    pp, key = mmf(w["R"][:], w["vb"][:], [w["R"], w["vb"]])
                    P.act(lambda e, pp=pp, w=w: e.copy(out=w["u"][:], in_=pp), reads=[key], writes=[w["u"]])
                    pp, key = mmf(w["kbg"][:], w["R"][:], [w["kbg"], w["R"]])
                    P.act(lambda e, pp=pp, w=w: e.copy(out=w["wT"][:], in_=pp), reads=[key], writes=[w["wT"]])
                if stop <= 5:
                    return
                for b in range(NB):
                    w = W[b]
                    cs = slice(b * 128, (b + 1) * 128)
                    pp, key = mmf(w["wT"][:], St[:], [w["wT"], St])
                    P.dve(lambda e, pp=pp, w=w: e.tensor_tensor(out=w["vnew"][:], in0=w["u"][:], in1=pp, op=ALU.subtract), reads=[key, w["u"]], writes=[w["vnew"]])
                    po, keyo = psq()
                    P.pe(lambda e, po=po, w=w: e.matmul(po, lhsT=St[:], rhs=w["qgT"][:], start=True, stop=False), reads=[St, w["qgT"]], writes=[keyo])
                    P.pe(lambda e, po=po, w=w: e.matmul(po, lhsT=w["vnew"][:], rhs=w["qkT"][:], start=False, stop=True), reads=[w["vnew"], w["qkT"]], writes=[keyo])
                    P.act(lambda e, po=po, cs=cs: e.copy(out=oT[:, cs], in_=po), reads=[keyo], writes=[oT])
                    pp, key = mmf(w["kd"][:], w["vnew"][:], [w["kd"], w["vnew"]])
                    P.dve(lambda e, pp=pp, w=w: e.scalar_tensor_tensor(out=St[:], in0=St[:], scalar=w["egrow"][:, 127:128], in1=pp, op0=ALU.mult, op1=ALU.add),
                          reads=[key, St, w["egrow"]], writes=[St])
                if stop <= 6:
                    return
                if dbg.get("dn_dump"):
                    P.dma(dd["o"][h * 128:(h + 1) * 128, gs], oT[:, :], reads=[oT], writes=["o_dno"], q="pool")
                P.act(lambda e: e.activation(out=sqb[:], in_=oT[:], func=AF.Square), reads=[oT], writes=[sqb])
                bkeys = [("bank", 7)]
                P.pe(lambda e: e.matmul(bank[7][:, :], lhsT=ones_bf[:], rhs=sqb[:], start=True, stop=True), reads=[ones_bf, sqb], writes=bkeys)
                P.act(lambda e: e.activation(out=rinv[:], in_=bank[7][:, :], func=AF.Sqrt, bias=eps_t[:, 0:1], scale=1.0 / 128), reads=bkeys + [eps_t], writes=[rinv])
                P.dve(lambda e: e.reciprocal(out=rinv[:], in_=rinv[:]), reads=[rinv], writes=[rinv])
                P.dve(lambda e: e.scalar_tensor_tensor(out=oT[:], in0=oT[:], scalar=gn[:, 0:1], in1=rinv[:], op0=ALU.mult, op1=ALU.mult), reads=[oT, gn, rinv], writes=[oT])
                ybt = yb[g % 2]
                P.dve(lambda e, ybt=ybt: e.tensor_tensor(out=ybt[:], in0=oT[:], in1=zT[:], op=ALU.mult), reads=[oT, zT], writes=[ybt])
                P.dma(yT[1024 + h * 128:1024 + (h + 1) * 128, gs], ybt[:], reads=[ybt], writes=[("yT_b", g)], q="pool")

    def dump(name, src, shape, dt, rkeys):
        o = ext_out("o_" + name, shape, dt)
        P.dma(o, src, reads=rkeys, writes=["o_" + name])

    def load_x(xg, g):
        for k in range(KD):
            P.dma(xg[:, k, :], xT[k * 128:(k + 1) * 128, g * 512:(g + 1) * 512], reads=[("xT", g)], writes=[(xg, k)])

    def store_x(xg, g):
        for k in range(KD):
            P.dma(xT[k * 128:(k + 1) * 128, g * 512:(g + 1) * 512], xg[:, k, :], reads=[(xg, k)], writes=[("xT", g)], q="pool")

    def p3a(l):
        xg = P.sb([128, KD, 512], F32, "xg")
        yTt = P.sb([128, 24, 512], BF16, "yTt")
        gat = P.sb([128, KD, 512], BF16, "gat")
        merged = P.sb([128, KD, 512], F32, "merged")
        mergedb = P.sb([128, KD, 512], BF16, "mergedb")
        tmp = [P.sb([128, 512], F32, f"p3tmp{i}") for i in range(2)]
        wblk = [P.sb([128, KD, 512], BF16, f"wblk{i}") for i in range(2)]
        nb = 0
        ne = 0
        for g in range(NG):
            gs = slice(g * 512, (g + 1) * 512)
            load_x(xg, g)
            for c in range(24):
                P.dma(yTt[:, c, :], yT[c * 128:(c + 1) * 128, gs], reads=[("yT_a", g), ("yT_b", g), ("yT_c", g)], writes=[(yTt, c)])
            for n in range(3):
                for c in range(KD):
                    P.dma(gat[:, c, :], gatesT[n * D + c * 128:n * D + (c + 1) * 128, gs], reads=[("gatesT", g)], writes=[(gat, c)])
                for cb in range(4):
                    wb = wblk[nb % 2]
                    nb += 1
                    for k in range(8):
                        P.dma(wb[:, k, :], w_branch_bf[l][n * 1024 + k * 128:n * 1024 + (k + 1) * 128, cb * 512:(cb + 1) * 512],
                              reads=[("w_branch_bf", l)], writes=[wb])
                    for j in range(4):
                        dc = cb * 4 + j
                        pp = bank[ne % 4]
                        ne += 1
                        for k in range(8):
                            P.pe(lambda e, pp=pp, wb=wb, k=k, j=j, n=n: e.matmul(pp[:, :], lhsT=wb[:, k, j * 128:(j + 1) * 128], rhs=yTt[:, n * 8 + k, :],
                                                                                start=(k == 0), stop=(k == 7)), reads=[wb, (yTt, n * 8 + k)], writes=[pp])
                        if n == 0:
                            P.dve(lambda e, pp=pp, dc=dc: e.tensor_tensor(out=merged[:, dc, :], in0=pp[:, :], in1=gat[:, dc, :], op=ALU.mult),
                                  reads=[pp, (gat, dc)], writes=[(merged, dc)])
                        else:
                            tt = tmp[ne % 2]
                            P.dve(lambda e, pp=pp, dc=dc, tt=tt: e.tensor_tensor(out=tt[:], in0=pp[:, :], in1=gat[:, dc, :], op=ALU.mult),
                                  reads=[pp, (gat, dc)], writes=[tt])
                            P.pool(lambda e, dc=dc, tt=tt: e.tensor_tensor(out=merged[:, dc, :], in0=merged[:, dc, :], in1=tt[:], op=ALU.add),
                                   reads=[tt, (merged, dc)], writes=[(merged, dc)])
            for dc in range(KD):
                P.act(lambda e, dc=dc: e.copy(out=mergedb[:, dc, :], in_=merged[:, dc, :]), reads=[(merged, dc)], writes=[(mergedb, dc)])
            for cb in range(4):
                wb = wblk[nb % 2]
                nb += 1
                for k in range(KD):
                    P.dma(wb[:, k, :], w_out_bf[l][k * 128:(k + 1) * 128, cb * 512:(cb + 1) * 512], reads=[("w_out_bf", l)], writes=[wb])
                for j in range(4):
                    dc = cb * 4 + j
                    pp = bank[ne % 4]
                    ne += 1
                    for k in range(KD):
                        P.pe(lambda e, pp=pp, wb=wb, k=k, j=j: e.matmul(pp[:, :], lhsT=wb[:, k, j * 128:(j + 1) * 128], rhs=mergedb[:, k, :],
                                                                         start=(k == 0), stop=(k == KD - 1)), reads=[wb, (mergedb, k)], writes=[pp])
                    P.dve(lambda e, pp=pp, dc=dc: e.tensor_tensor(out=xg[:, dc, :], in0=xg[:, dc, :], in1=pp[:, :], op=ALU.add),
                          reads=[pp, (xg, dc)], writes=[(xg, dc)])
            store_x(xg, g)

    def ffn_core(wg_bf, wu_bf, wd_bf, wkey, hT, xg, hid, wblk, sgt, st, wbc=None):
        for fb in range(14):
            wbg = wblk[st["nb"] % len(wblk)]
            wbu = wblk[(st["nb"] + 1) % len(wblk)]
            st["nb"] += 2
            P.dma(wbg[:, :, :], blk3(wg_bf[fb]), reads=[wkey], writes=[wbg])
            P.dma(wbu[:, :, :], blk3(wu_bf[fb]), reads=[wkey], writes=[wbu])
            for j in range(4):
                fc = fb * 4 + j
                pg = bank[(st["ne"] % 2) * 2]
                pu = bank[(st["ne"] % 2) * 2 + 1]
                sg = sgt[st["ne"] % 2]
                st["ne"] += 1
                for k in range(KD):
                    P.pe(lambda e, pg=pg, wbg=wbg, k=k, j=j: e.matmul(pg[:, :], lhsT=wbg[:, k, j * 128:(j + 1) * 128], rhs=hT[:, k, :],
                                                                       start=(k == 0), stop=(k == KD - 1)), reads=[wbg, (hT, k)], writes=[pg])
                for k in range(KD):
                    P.pe(lambda e, pu=pu, wbu=wbu, k=k, j=j: e.matmul(pu[:, :], lhsT=wbu[:, k, j * 128:(j + 1) * 128], rhs=hT[:, k, :],
                                                                       start=(k == 0), stop=(k == KD - 1)), reads=[wbu, (hT, k)], writes=[pu])
                P.act(lambda e, pg=pg, sg=sg: e.activation(out=sg[:], in_=pg[:, :], func=AF.Silu), reads=[pg], writes=[sg])
                if wbc is not None:
                    P.pool(lambda e, sg=sg: e.tensor_tensor(out=sg[:], in0=sg[:], in1=wbc, op=ALU.mult), reads=[sg, "wbc"], writes=[sg])
                P.dve(lambda e, pu=pu, sg=sg, fc=fc: e.tensor_tensor(out=hid[:, fc, :], in0=pu[:, :], in1=sg[:], op=ALU.mult),
                      reads=[pu, sg], writes=[(hid, fc)])
        for cb in range(4):
            for seg in range(4):
                k0 = seg * 16
                nk = min(16, 56 - k0)
                wb = wblk[st["nb"] % len(wblk)]
                st["nb"] += 1
                P.dma(wb[:, 0:nk, :], blk3(wd_bf[cb * 4 + seg], nk), reads=[wkey], writes=[wb])
                for j in range(4):
                    pp = bank[4 + j]
                    for k in range(nk):
                        kk = k0 + k
                        P.pe(lambda e, pp=pp, wb=wb, k=k, j=j, kk=kk: e.matmul(pp[:, :], lhsT=wb[:, k, j * 128:(j + 1) * 128], rhs=hid[:, kk, :],
                                                                                start=(kk == 0), stop=(kk == 55)), reads=[wb, (hid, kk)], writes=[pp])
            for j in range(4):
                dc = cb * 4 + j
                pp = bank[4 + j]
                P.dve(lambda e, pp=pp, dc=dc: e.tensor_tensor(out=xg[:, dc, :], in0=xg[:, dc, :], in1=pp[:, :], op=ALU.add),
                      reads=[pp, (xg, dc)], writes=[(xg, dc)])

    def p3b_dense(l):
        xg = P.sb([128, KD, 512], F32, "xg")
        sq = P.sb([128, KD, 512], BF16, "sq")
        hT = P.sb([128, KD, 512], BF16, "hT")
        inv = P.sb([128, 512], F32, "inv")
        hid = P.sb([128, 56, 512], BF16, "hid")
        wblk = [P.sb([128, KD, 512], BF16, f"wblk{i}") for i in range(4)]
        sgt = [P.sb([128, 512], F32, f"sgt{i}") for i in range(2)]
        st = {"nb": 0, "ne": 0}
        jobs = expert_cast_jobs() if L > 1 else []
        per = -(-len(jobs) // NG) if jobs else 0
        for g in range(NG):
            for (dst_, src_) in jobs[g * per:(g + 1) * per]:
                P.dma(dst_, src_, writes=["w_exp_bf"], q="pool")
            load_x(xg, g)
            rms_T(xg, hT, KD, D, gain_ffn[:, l, :], gain_ffn, bank[0], sq, inv)
            ffn_core(w_ffn_bf[0], w_ffn_bf[1], w_ffn_bf[2], "w_ffn_bf", hT, xg, hid, wblk, sgt, st)
            store_x(xg, g)

    def p3b_moe(l):
        xg = P.sb([128, KD, 512], F32, "xg")
        sq = P.sb([128, KD, 512], BF16, "sq")
        hT = P.sb([128, KD, 512], BF16, "hT")
        inv = P.sb([128, 512], F32, "inv")
        hid = P.sb([128, 56, 512], BF16, "hid")
        wblk = [P.sb([128, KD, 512], BF16, f"wblk{i}") for i in range(3)]
        sgt = [P.sb([128, 512], F32, f"sgt{i}") for i in range(2)]
        hf = [P.sb([128, 512], F32, f"hf{i}") for i in range(2)]
        wr = P.sb([128, KD, 8], F32, "wr")
        P.dma(wr[:, :, :], w_router[0].rearrange("(k p) e -> p k e", p=128), writes=[wr], slow=True)
        sel = P.sb([8, 8, 128], F32, "sel")
        P.dma(sel[:, :, :], sel_in[:, :, :], writes=[sel])
        lg = P.sb([128, 4, 8], F32, "lg")
        m8 = P.sb([128, 8], F32, "m8")
        nm1 = P.sb([128, 1], F32, "nm1")
        msk = P.sb([128, 8], F32, "msk")
        den = P.sb([128, 1], F32, "den")
        wT8 = P.sb([8, 512], F32, "wT8")
        wbc = P.sb([128, 8, 512], BF16, "wbc")
        st = {"nb": 0, "ne": 0}
        for g in range(NG):
            load_x(xg, g)
            rms_T(xg, hT, KD, D, gain_ffn[:, l, :], gain_ffn, bank[0], sq, inv)
            for k in range(KD):
                hb = hf[k % 2]
                P.dve(lambda e, k=k, hb=hb: e.scalar_tensor_tensor(out=hb[:], in0=xg[:, k, :], scalar=gain_ffn[:, l, k:k + 1], in1=inv[:], op0=ALU.mult, op1=ALU.mult),
                      reads=[(xg, k), inv, gain_ffn], writes=[hb])
                for t in range(4):
                    P.pe(lambda e, k=k, hb=hb, t=t: e.matmul(bank[4 + t][:, 0:8], lhsT=hb[:, t * 128:(t + 1) * 128], rhs=wr[:, k, :], start=(k == 0), stop=(k == KD - 1)),
                         reads=[hb, wr], writes=[bank[4 + t]])
            for t in range(4):
                P.act(lambda e, t=t: e.copy(out=lg[:, t, :], in_=bank[4 + t][:, 0:8]), reads=[bank[4 + t]], writes=[lg])
            for t in range(4):
                P.dve(lambda e, t=t: e.max(out=m8[:, :], in_=lg[:, t, :]), reads=[lg], writes=[m8])
                P.dve(lambda e, t=t: e.tensor_scalar(out=msk[:, :], in0=lg[:, t, :], scalar1=m8[:, 1:2], scalar2=1.0, op0=ALU.is_ge, op1=ALU.mult), reads=[lg, m8], writes=[msk])
                P.dve(lambda e: e.tensor_scalar(out=nm1[:, :], in0=m8[:, 0:1], scalar1=-1.0, scalar2=0.0, op0=ALU.mult, op1=ALU.add), reads=[m8], writes=[nm1])
                P.act(lambda e, t=t: e.activation(out=lg[:, t, :], in_=lg[:, t, :], func=AF.Exp, bias=nm1[:, 0:1], scale=1.0), reads=[lg, nm1], writes=[lg])
                P.dve(lambda e, t=t: e.tensor_tensor(out=lg[:, t, :], in0=lg[:, t, :], in1=msk[:, :], op=ALU.mult), reads=[lg, msk], writes=[lg])
                P.dve(lambda e, t=t: e.reduce_sum(out=den[:, :], in_=lg[:, t, :], axis=AX.X), reads=[lg], writes=[den])
                P.dve(lambda e: e.reciprocal(out=den[:, :], in_=den[:, :]), reads=[den], writes=[den])
                P.dve(lambda e, t=t: e.tensor_scalar(out=lg[:, t, :], in0=lg[:, t, :], scalar1=den[:, 0:1], scalar2=1.0, op0=ALU.mult, op1=ALU.mult), reads=[lg, den], writes=[lg])
                P.pe(lambda e, t=t: e.transpose(bank[0][0:8, t * 128:(t + 1) * 128], lg[:, t, :], ident[:]), reads=[lg, ident], writes=[bank[0]])
            P.act(lambda e: e.copy(out=wT8[:, :], in_=bank[0][0:8, :]), reads=[bank[0]], writes=[wT8])
            for ex in range(8):
                pp = bank[ex % 4]
                P.pe(lambda e, pp=pp, ex=ex: e.matmul(pp[:, :], lhsT=sel[:, ex, :], rhs=wT8[:, :], start=True, stop=True), reads=[sel, wT8], writes=[pp])
                P.act(lambda e, pp=pp, ex=ex: e.copy(out=wbc[:, ex, :], in_=pp[:, :]), reads=[pp], writes=["wbc"])
            for ex in range(8):
                ffn_core(w_exp_bf[0][ex], w_exp_bf[1][ex], w_exp_bf[2][ex], "w_exp_bf", hT, xg, hid, wblk, sgt, st, wbc=wbc[:, ex, :])
            store_x(xg, g)

    CAP = max(512, -(-(S * 5 // 16) // 512) * 512)
    NSG = CAP // 512

    def moe_routed(l):
        Hsel = P.dram([8 * CAP + 128, D], BF16, "Hsel")
        hrow = P.dram([S, D], BF16, "hrow")
        Tslot = P.dram([8 * CAP + 128, 2], I32, "Tslot")
        Ytok = P.dram([2 * S + 128, D], F32, "Ytok")
        BIG2 = 2 * S
        pbf = bank[7][:, :].bitcast(BF16)
        with P.phase():
            xg = P.sb([128, KD, 512], F32, "xg")
            sq = P.sb([128, KD, 512], BF16, "sq")
            hT = P.sb([128, KD, 512], BF16, "hT")
            inv = P.sb([128, 512], F32, "inv")
            hf = [P.sb([128, 512], F32, f"hf{i}") for i in range(2)]
            wr = P.sb([128, KD, 8], F32, "wr")
            P.dma(wr[:, :, :], w_router[0].rearrange("(k p) e -> p k e", p=128), writes=[wr], slow=True)
            ident_bf = P.sb([128, 128], BF16, "ident_bf")
            P.dve(lambda e: e.tensor_copy(out=ident_bf[:], in_=ident[:]), reads=[ident], writes=[ident_bf])
            lg = P.sb([128, 4, 8], F32, "lg")
            m8 = P.sb([128, 8], F32, "m8")
            nm1 = P.sb([128, 1], F32, "nm1")
            den = P.sb([128, 1], F32, "den")
            Mall = P.sb([128, NT, 8], F32, "Mall")
            Tall = P.sb([128, NT, 8], F32, "Tall")
            TW = P.sb([128, NT, 8, 2], I32, "TW")
            tokid = P.sb([128, 64], F32, "tokid")
            P.dma(tokid[:, :], tokid_in[:, :], writes=[tokid])
            bigt = P.sb([128, 2 * 8 * CAP // 128], I32, "bigt")
            P.dve(lambda e: e.memset(bigt[:], BIG2), writes=[bigt])
            P.dma(Tslot[0:8 * CAP, :].rearrange("(p c) o -> p (c o)", p=128), bigt[:, :], reads=[bigt], writes=["Tslot"], q="pool")
            hro = [P.sb([128, D], BF16, f"hro{i}") for i in range(2)]
            nh = 0
            for g in range(NG):
                load_x(xg, g)
                rms_T(xg, hT, KD, D, gain_ffn[:, l, :], gain_ffn, bank[0], sq, inv)
                for k in range(KD):
                    hb = hf[k % 2]
                    P.dve(lambda e, k=k, hb=hb: e.scalar_tensor_tensor(out=hb[:], in0=xg[:, k, :], scalar=gain_ffn[:, l, k:k + 1], in1=inv[:], op0=ALU.mult, op1=ALU.mult),
                          reads=[(xg, k), inv, gain_ffn], writes=[hb])
                    for t in range(4):
                        P.pe(lambda e, k=k, hb=hb, t=t: e.matmul(bank[1 + t][:, 0:8], lhsT=hb[:, t * 128:(t + 1) * 128], rhs=wr[:, k, :], start=(k == 0), stop=(k == KD - 1)),
                             reads=[hb, wr], writes=[bank[1 + t]])
                for t in range(4):
                    P.act(lambda e, t=t: e.copy(out=lg[:, t, :], in_=bank[1 + t][:, 0:8]), reads=[bank[1 + t]], writes=[lg])
                for t in range(4):
                    tt = g * 4 + t
                    P.dve(lambda e, t=t: e.max(out=m8[:, :], in_=lg[:, t, :]), reads=[lg], writes=[m8])
                    P.dve(lambda e, t=t, tt=tt: e.tensor_scalar(out=Mall[:, tt, :], in0=lg[:, t, :], scalar1=m8[:, 1:2], scalar2=1.0, op0=ALU.is_ge, op1=ALU.mult),
                          reads=[lg, m8], writes=[Mall])
                    P.dve(lambda e, t=t, tt=tt: e.tensor_scalar(out=Tall[:, tt, :], in0=lg[:, t, :], scalar1=m8[:, 0:1], scalar2=float(-S), op0=ALU.is_ge, op1=ALU.mult),
                          reads=[lg, m8], writes=[Tall])
                    P.dve(lambda e, tt=tt: e.tensor_scalar(out=Tall[:, tt, :], in0=Tall[:, tt, :], scalar1=tokid[:, tt:tt + 1], scalar2=float(S), op0=ALU.add, op1=ALU.add),
                          reads=[Tall, tokid], writes=[Tall])
                    P.dve(lambda e: e.tensor_scalar(out=nm1[:, :], in0=m8[:, 0:1], scalar1=-1.0, scalar2=0.0, op0=ALU.mult, op1=ALU.add), reads=[m8], writes=[nm1])
                    P.act(lambda e, t=t: e.activation(out=lg[:, t, :], in_=lg[:, t, :], func=AF.Exp, bias=nm1[:, 0:1], scale=1.0), reads=[lg, nm1], writes=[lg])
                    P.dve(lambda e, t=t, tt=tt: e.tensor_tensor(out=lg[:, t, :], in0=lg[:, t, :], in1=Mall[:, tt, :], op=ALU.mult), reads=[lg, Mall], writes=[lg])
                    P.dve(lambda e, t=t: e.reduce_sum(out=den[:, :], in_=lg[:, t, :], axis=AX.X), reads=[lg], writes=[den])
                    P.dve(lambda e: e.reciprocal(out=den[:, :], in_=den[:, :]), reads=[den], writes=[den])
                    P.dve(lambda e, t=t, tt=tt: e.tensor_scalar(out=Wall[:, tt, :], in0=lg[:, t, :], scalar1=den[:, 0:1], scalar2=1.0, op0=ALU.mult, op1=ALU.mult),
                          reads=[lg, den], writes=[Wall])
                for t in range(4):
                    hr = hro[nh % 2]
                    nh += 1
                    for kb in range(2):
                        for kq in range(8):
                            k = kb * 8 + kq
                            P.pe(lambda e, k=k, kq=kq, t=t: e.transpose(pbf[:, kq * 128:(kq + 1) * 128], hT[:, k, t * 128:(t + 1) * 128], ident_bf[:]),
                                 reads=[(hT, k), ident_bf], writes=[bank[7]])
                        P.act(lambda e, hr=hr, kb=kb: e.copy(out=hr[:, kb * 1024:(kb + 1) * 1024], in_=pbf[:, :]), reads=[bank[7]], writes=[hr])
                    tok0 = g * 512 + t * 128
                    P.dma(hrow[tok0:tok0 + 128, :], hr[:, :], reads=[hr], writes=["hrow"], q="pool")
            us = P.sb([128, 128], F32, "us")
            ones_f = P.sb([128, 128], F32, "ones_f")
            P.pool(lambda e: e.memset(ones_f[:], 1.0), writes=[ones_f])
            P.dma(us[:, :], tri_in[0], writes=[us])
            P.dve(lambda e: e.tensor_tensor(out=us[:], in0=us[:], in1=ident[:], op=ALU.subtract), reads=[us, ident], writes=[us])
            cum = P.sb([128, NT, 8], F32, "cum")
            tot = P.sb([128, NT, 8], F32, "tot")
            base = P.sb([128, NT, 8], F32, "base")
            for c0 in range(0, NT * 8, 512):
                c1 = min(NT * 8, c0 + 512)
                mflat = Mall[:, :, :].rearrange("p t e -> p (t e)")
                P.pe(lambda e, c0=c0, c1=c1, mflat=mflat: e.matmul(bank[1][:, 0:c1 - c0], lhsT=us[:, :], rhs=mflat[:, c0:c1], start=True, stop=True), reads=[us, Mall], writes=[bank[1]])
                P.act(lambda e, c0=c0, c1=c1: e.copy(out=cum[:, :, :].rearrange("p t e -> p (t e)")[:, c0:c1], in_=bank[1][:, 0:c1 - c0]), reads=[bank[1]], writes=[cum])
                P.pe(lambda e, c0=c0, c1=c1, mflat=mflat: e.matmul(bank[2][:, 0:c1 - c0], lhsT=ones_f[:, :], rhs=mflat[:, c0:c1], start=True, stop=True), reads=[ones_f, Mall], writes=[bank[2]])
                P.act(lambda e, c0=c0, c1=c1: e.copy(out=tot[:, :, :].rearrange("p t e -> p (t e)")[:, c0:c1], in_=bank[2][:, 0:c1 - c0]), reads=[bank[2]], writes=[tot])
            for ex in range(8):
                P.dve(lambda e, ex=ex: e.memset(base[:, 0, ex:ex + 1], float(ex * CAP)), writes=[base])
            for t in range(1, NT):
                P.dve(lambda e, t=t: e.tensor_tensor(out=base[:, t, :], in0=base[:, t - 1, :], in1=tot[:, t - 1, :], op=ALU.add), reads=[base, tot], writes=[base])
            BIG = float(8 * CAP)
            P.dve(lambda e: e.tensor_tensor(out=cum[:, :, :], in0=cum[:, :, :], in1=base[:, :, :], op=ALU.add), reads=[cum, base], writes=[cum])
            P.dve(lambda e: e.tensor_scalar(out=cum[:, :, :], in0=cum[:, :, :], scalar1=-BIG, scalar2=1.0, op0=ALU.add, op1=ALU.mult), reads=[cum], writes=[cum])
            P.dve(lambda e: e.tensor_tensor(out=cum[:, :, :], in0=cum[:, :, :], in1=Mall[:, :, :], op=ALU.mult), reads=[cum, Mall], writes=[cum])
            P.dve(lambda e: e.tensor_scalar(out=cum[:, :, :], in0=cum[:, :, :], scalar1=BIG, scalar2=1.0, op0=ALU.add, op1=ALU.mult), reads=[cum], writes=[cum])
            P.dve(lambda e: e.tensor_copy(out=Iall[:, :, :], in_=cum[:, :, :]), reads=[cum], writes=[Iall])
            P.dve(lambda e: e.tensor_copy(out=TW[:, :, :, 0], in_=Tall[:, :, :]), reads=[Tall], writes=[TW])
            P.dve(lambda e: e.tensor_copy(out=TW[:, :, :, :].bitcast(F32)[:, :, :, 1], in_=Wall[:, :, :]), reads=[Wall, TW], writes=[TW])
            for t in range(NT):
                hr = hro[nh % 2]
                nh += 1
                P.dma(hr[:, :], hrow[t * 128:(t + 1) * 128, :], reads=["hrow"], writes=[hr])
                for ex in range(8):
                    P.indirect(lambda e, hr=hr, t=t, ex=ex: e.indirect_dma_start(
                        out=Hsel[:, :], out_offset=bass.IndirectOffsetOnAxis(ap=Iall[:, t, ex:ex + 1], axis=0),
                        in_=hr[:, :], in_offset=None),
                        [hr, Iall], ["Hsel"])
                    P.indirect(lambda e, t=t, ex=ex: e.indirect_dma_start(
                        out=Tslot[:, :], out_offset=bass.IndirectOffsetOnAxis(ap=Iall[:, t, ex:ex + 1], axis=0),
                        in_=TW[:, t, ex, :], in_offset=None),
                        [TW, Iall, "Tslot"], ["Tslot"])
        with P.phase():
            ident_bf = P.sb([128, 128], BF16, "ident_bf")
            P.dve(lambda e: e.tensor_copy(out=ident_bf[:], in_=ident[:]), reads=[ident], writes=[ident_bf])
            hs = P.sb([128, 4, D], BF16, "hs")
            hselT = P.sb([128, KD, 512], BF16, "hselT")
            hid = P.sb([128, 56, 512], BF16, "hid")
            wblk = [P.sb([128, KD, 512], BF16, f"wblk{i}") for i in range(3)]
            sgt = [P.sb([128, 512], F32, f"sgt{i}") for i in range(2)]
            osl = P.sb([128, 4, D], F32, "osl")
            tsl = P.sb([128, 4, 2], I32, "tsl")
            st = {"nb": 0, "ne": 0}
            for ex in range(8):
                for sg in range(NSG):
                    r0 = ex * CAP + sg * 512
                    for s4 in range(4):
                        P.dma(hs[:, s4, :], Hsel[r0 + s4 * 128:r0 + (s4 + 1) * 128, :], reads=["Hsel"], writes=[(hs, s4)])
                    P.dma(tsl[:, :, :], Tslot[r0:r0 + 512, :].rearrange("(s p) o -> p s o", p=128), reads=["Tslot"], writes=[tsl], slow=True)
                    for kp in range(KD // 2):
                        for kq in range(2):
                            k = kp * 2 + kq
                            for s4 in range(4):
                                P.pe(lambda e, k=k, kq=kq, s4=s4: e.transpose(pbf[:, kq * 512 + s4 * 128:kq * 512 + (s4 + 1) * 128], hs[:, s4, k * 128:(k + 1) * 128], ident_bf[:]),
                                     reads=[(hs, s4), ident_bf], writes=[bank[7]])
                        P.act(lambda e, kp=kp: e.copy(out=hselT[:, kp * 2:kp * 2 + 2, :].rearrange("p a b -> p (a b)"), in_=pbf[:, :]), reads=[bank[7]],
                              writes=[(hselT, kp * 2), (hselT, kp * 2 + 1)])
                    wg_bf, wu_bf, wd_bf = w_exp_bf[0][ex], w_exp_bf[1][ex], w_exp_bf[2][ex]
                    for fb in range(14):
                        wbg = wblk[st["nb"] % 3]
                        wbu = wblk[(st["nb"] + 1) % 3]
                        st["nb"] += 2
                        P.dma(wbg[:, :, :], blk3(wg_bf[fb]), reads=["w_exp_bf"], writes=[wbg])
                        P.dma(wbu[:, :, :], blk3(wu_bf[fb]), reads=["w_exp_bf"], writes=[wbu])
                        for j in range(4):
                            fc = fb * 4 + j
                            pg = bank[(st["ne"] % 2) * 2]
                            pu = bank[(st["ne"] % 2) * 2 + 1]
                            sgq = sgt[st["ne"] % 2]
                            st["ne"] += 1
                            for k in range(KD):
                                P.pe(lambda e, pg=pg, wbg=wbg, k=k, j=j: e.matmul(pg[:, :], lhsT=wbg[:, k, j * 128:(j + 1) * 128], rhs=hselT[:, k, :],
                                                                                   start=(k == 0), stop=(k == KD - 1)), reads=[wbg, (hselT, k)], writes=[pg])
                            for k in range(KD):
                                P.pe(lambda e, pu=pu, wbu=wbu, k=k, j=j: e.matmul(pu[:, :], lhsT=wbu[:, k, j * 128:(j + 1) * 128], rhs=hselT[:, k, :],
                                                                                   start=(k == 0), stop=(k == KD - 1)), reads=[wbu, (hselT, k)], writes=[pu])
                            P.act(lambda e, pg=pg, sgq=sgq: e.activation(out=sgq[:], in_=pg[:, :], func=AF.Silu), reads=[pg], writes=[sgq])
                            P.dve(lambda e, pu=pu, sgq=sgq, fc=fc: e.tensor_tensor(out=hid[:, fc, :], in0=pu[:, :], in1=sgq[:], op=ALU.mult),
                                  reads=[pu, sgq], writes=[(hid, fc)])
                    for cb in range(4):
                        for seg in range(4):
                            k0 = seg * 16
                            nk = min(16, 56 - k0)
                            wb = wblk[st["nb"] % 3]
                            st["nb"] += 1
                            P.dma(wb[:, 0:nk, :], blk3(wd_bf[cb * 4 + seg], nk), reads=["w_exp_bf"], writes=[wb])
                            for s4 in range(4):
                                pp = bank[3 + s4]
                                for k in range(nk):
                                    kk = k0 + k
                                    P.pe(lambda e, pp=pp, wb=wb, k=k, s4=s4, kk=kk: e.matmul(pp[:, :], lhsT=hid[:, kk, s4 * 128:(s4 + 1) * 128], rhs=wb[:, k, :],
                                                                                          start=(kk == 0), stop=(kk == 55)), reads=[(hid, kk), wb], writes=[pp])
                        for s4 in range(4):
                            pp = bank[3 + s4]
                            P.act(lambda e, pp=pp, s4=s4, cb=cb: e.activation(out=osl[:, s4, cb * 512:(cb + 1) * 512], in_=pp[:, :], func=AF.Copy, scale=tsl[:, s4, 1:2].bitcast(F32)),
                                  reads=[pp, tsl], writes=[(osl, s4)])
                    for s4 in range(4):
                        P.indirect(lambda e, s4=s4: e.indirect_dma_start(
                            out=Ytok[:, :], out_offset=bass.IndirectOffsetOnAxis(ap=tsl[:, s4, 0:1], axis=0),
                            in_=osl[:, s4, :], in_offset=None),
                            [(osl, s4), tsl], ["Ytok"])
        with P.phase():
            xg = P.sb([128, KD, 512], F32, "xg")
            y1 = [P.sb([128, D], F32, f"y1_{i}") for i in range(4)]
            y2 = [P.sb([128, D], F32, f"y2_{i}") for i in range(4)]
            for g in range(NG):
                load_x(xg, g)
                for t in range(4):
                    tok0 = g * 512 + t * 128
                    P.dma(y1[t][:, :], Ytok[tok0:tok0 + 128, :], reads=["Ytok"], writes=[y1[t]])
                    P.dma(y2[t][:, :], Ytok[S + tok0:S + tok0 + 128, :], reads=["Ytok"], writes=[y2[t]])
                    P.pool(lambda e, t=t: e.tensor_tensor(out=y1[t][:], in0=y1[t][:], in1=y2[t][:], op=ALU.add), reads=[y1[t], y2[t]], writes=[y1[t]])
                for k in range(KD):
                    pp = bank[k % 4]
                    for t in range(4):
                        P.pe(lambda e, pp=pp, t=t, k=k: e.transpose(pp[:, t * 128:(t + 1) * 128], y1[t][:, k * 128:(k + 1) * 128], ident[:]),
                             reads=[y1[t], ident], writes=[pp])
                    P.dve(lambda e, pp=pp, k=k: e.tensor_tensor(out=xg[:, k, :], in0=xg[:, k, :], in1=pp[:, :], op=ALU.add), reads=[pp, (xg, k)], writes=[(xg, k)])
                store_x(xg, g)

    def p3c(l, last):
        xg = P.sb([128, KD, 512], F32, "xg")
        sq = P.sb([128, KD, 512], BF16, "sq")
        hT = P.sb([128, KD, 512], BF16, "hT")
        inv = P.sb([128, 512], F32, "inv")
        pTt = P.sb([128, 2, 512], BF16, "pTt")
        wblk = [P.sb([128, KD, 512], BF16, f"wblk{i}") for i in range(2)]
        wpb = [P.sb([128, 2, 512], BF16, f"wpb{i}") for i in range(2)]
        sgt = [P.sb([128, 512], F32, f"sgt{i}") for i in range(2)]
        if last:
            hfin = P.sb([128, KD, 512], F32, "hfin")
            osb = [P.sb([128, D], F32, f"osb{i}") for i in range(2)]
        nb = 0
        ne = 0
        no = 0
        for g in range(NG):
            gs = slice(g * 512, (g + 1) * 512)
            load_x(xg, g)
            rms_T(xg, hT, KD, D, gain_ple[:, l, :], gain_ple, bank[0], sq, inv)
            for kk in range(2):
                P.dma(pTt[:, kk, :], pT[l][kk * 128:(kk + 1) * 128, gs], reads=[("pT", l)], writes=[pTt])
            for cb in range(4):
                wb = wblk[nb % 2]
                wp = wpb[nb % 2]
                nb += 1
                for k in range(KD):
                    P.dma(wb[:, k, :], w_pg_bf[l][k * 128:(k + 1) * 128, cb * 512:(cb + 1) * 512], reads=[("w_pg_bf", l)], writes=[wb])
                for kk in range(2):
                    P.dma(wp[:, kk, :], w_pp_bf[l][kk * 128:(kk + 1) * 128, cb * 512:(cb + 1) * 512], reads=[("w_pp_bf", l)], writes=[wp])
                for j in range(4):
                    dc = cb * 4 + j
                    pgt = bank[2 + (ne % 2) * 2]
                    ppr = bank[3 + (ne % 2) * 2]
                    sg = sgt[ne % 2]
                    ne += 1
                    for k in range(KD):
                        P.pe(lambda e, pgt=pgt, wb=wb, k=k, j=j: e.matmul(pgt[:, :], lhsT=wb[:, k, j * 128:(j + 1) * 128], rhs=hT[:, k, :],
                                                                           start=(k == 0), stop=(k == KD - 1)), reads=[wb, (hT, k)], writes=[pgt])
                    for kk in range(2):
                        P.pe(lambda e, ppr=ppr, wp=wp, kk=kk, j=j: e.matmul(ppr[:, :], lhsT=wp[:, kk, j * 128:(j + 1) * 128], rhs=pTt[:, kk, :],
                                                                             start=(kk == 0), stop=(kk == 1)), reads=[wp, pTt], writes=[ppr])
                    P.act(lambda e, pgt=pgt, sg=sg: e.activation(out=sg[:], in_=pgt[:, :], func=AF.Sigmoid), reads=[pgt], writes=[sg])
                    P.dve(lambda e, ppr=ppr, sg=sg: e.tensor_tensor(out=sg[:], in0=ppr[:, :], in1=sg[:], op=ALU.mult), reads=[ppr, sg], writes=[sg])
                    P.pool(lambda e, sg=sg, dc=dc: e.tensor_tensor(out=xg[:, dc, :], in0=xg[:, dc, :], in1=sg[:], op=ALU.add), reads=[sg, (xg, dc)], writes=[(xg, dc)])
            if not last:
                store_x(xg, g)
                continue
            for k in range(KD):
                P.act(lambda e, k=k: e.activation(out=sq[:, k, :], in_=xg[:, k, :], func=AF.Square), reads=[(xg, k)], writes=[(sq, k)])
            for k in range(KD):
                P.pe(lambda e, k=k: e.matmul(bank[0][:], lhsT=ones_bf[:], rhs=sq[:, k, :], start=(k == 0), stop=(k == KD - 1)), reads=[ones_bf, (sq, k)], writes=[bank[0]])
            P.act(lambda e: e.activation(out=inv[:], in_=bank[0][:], func=AF.Sqrt, scale=1.0 / D, bias=eps_t[:, 0:1]), reads=[bank[0], eps_t], writes=[inv])
            P.dve(lambda e: e.reciprocal(out=inv[:], in_=inv[:]), reads=[inv], writes=[inv])
            for k in range(KD):
                P.dve(lambda e, k=k: e.scalar_tensor_tensor(out=hfin[:, k, :], in0=xg[:, k, :], scalar=gain_fin[:, k:k + 1], in1=inv[:], op0=ALU.mult, op1=ALU.mult),
                      reads=[(xg, k), inv, gain_fin], writes=[(hfin, k)])
            for t in range(4):
                ob = osb[no % 2]
                no += 1
                for kb in range(4):
                    pp = bank[4 + (kb % 4)]
                    for kq in range(4):
                        k = kb * 4 + kq
                        P.pe(lambda e, pp=pp, k=k, kq=kq, t=t: e.transpose(pp[:, kq * 128:(kq + 1) * 128], hfin[:, k, t * 128:(t + 1) * 128], ident[:]),
                             reads=[(hfin, k), ident], writes=[pp])
                    P.act(lambda e, pp=pp, ob=ob, kb=kb: e.copy(out=ob[:, kb * 512:(kb + 1) * 512], in_=pp[:, :]), reads=[pp], writes=[ob])
                tok0 = g * 512 + t * 128
                P.dma(out_ap[tok0:tok0 + 128, :], ob[:, :], reads=[ob], writes=["out"], q="pool")

    if "moe" in phases:
        for (dst_, src_) in expert_cast_jobs():
            P.dma(dst_, src_, writes=["w_exp_bf"], q="pool")
        if dbg.get("dense_moe"):
            with P.phase():
                p3b_moe(0)
        else:
            moe_routed(0)
        dump("x2T", xT[:, :], [D, S], F32, [])
    for l in range(L):
        casts_layer(l)
    for l in range(L):
        if "p1" in phases:
            with P.phase():
                p1(l)
        if "swa" in phases:
            with P.phase():
                swa(l)
        if "mla" in phases:
            with P.phase():
                mla1(l)
            with P.phase():
                mla2(l)
        if "dn" in phases:
            with P.phase():
                dn(l)
        if "p3" in phases:
            with P.phase():
                p3a(l)
            if dbg.get("x1") and l == 0:
                dump("x1T", xT[:, :], [D, S], F32, [])
            if l % 2 == 0:
                with P.phase():
                    p3b_dense(l)
            elif dbg.get("dense_moe"):
                with P.phase():
                    p3b_moe(l)
            else:
                moe_routed(l)
            if dbg.get("x1") and l == 0:
                dump("x2T", xT[:, :], [D, S], F32, [])
            with P.phase():
                p3c(l, l == L - 1)

    if dbg.get("projT"):
        dump("projT", projT[:, :], [C_G, S], F32, keys("projT"))
        dump("gatesT", gatesT[:, :], [3 * D, S], BF16, keys("gatesT"))
    if dbg.get("xT"):
        dump("xT", xT[:, :], [D, S], F32, keys("xT"))
    if dbg.get("yT"):
        dump("yT", yT[:, :], [3072, S], BF16, keys("yT_a") + keys("yT_b") + keys("yT_c"))
    if dbg.get("mlaqk"):
        dump("qnT", qnT[:, :], [1024, S], BF16, keys("qnT"))
        dump("qrT", qrT[:, :], [512, S], BF16, keys("qrT"))
        dump("krT", krT[:, :], [64, S], BF16, keys("krT"))
        dump("cosT", cosT[:, :], [64, S], F32, keys("cosT"))

    P.finish()
    return nc, stack, P


_CACHE = {}
_WEIGHTS = ["norm_mix", "w_in", "swa_sinks", "mla_q_norm", "mla_kv_norm", "w_uq", "w_ukv", "conv_w", "dn_a_log", "dn_dt_bias",
            "dn_norm", "w_branch", "w_out", "norm_ffn", "w_ffn_gate", "w_ffn_up", "w_ffn_down", "norm_ple", "w_ple_gate",
            "w_ple_proj", "w_router", "w_exp_gate", "w_exp_up", "w_exp_down"]


def kernel(**inputs):
    S, L = 8192, 2
    if "prog" not in _CACHE:
        _CACHE["prog"] = build(S, L)
    nc, stack, P = _CACHE["prog"]
    consts = host_consts()
    shared = {k: np.ascontiguousarray(np.asarray(inputs[k], dtype=np.float32)) for k in _WEIGHTS}
    shared["final_norm"] = np.ascontiguousarray(np.asarray(inputs["final_norm"], dtype=np.float32)[None])
    in_maps = []
    for b in range(2):
        m = dict(consts)
        m.update(shared)
        m["x"] = np.ascontiguousarray(np.asarray(inputs["x"], dtype=np.float32)[b])
        m["positions"] = np.ascontiguousarray(np.asarray(inputs["positions"])[b:b + 1].astype(np.int32))
        m["p"] = np.ascontiguousarray(np.asarray(inputs["p"], dtype=np.float32)[:, b])
        in_maps.append(m)
    res = run_bass_kernel_spmd(nc, in_maps, core_ids=[0, 1])
    return np.stack([np.asarray(res.results[b]["out"], dtype=np.float32) for b in range(2)])
```

```python
import contextlib
import numpy as np
import concourse.bass as bass
import concourse.mybir as mybir
from concourse.bass_utils import run_bass_kernel_spmd

F32 = mybir.dt.float32
BF16 = mybir.dt.bfloat16
I32 = mybir.dt.int32
ALU = mybir.AluOpType
AF = mybir.ActivationFunctionType
AX = mybir.AxisListType

D = 2048
KD = D // 128
IN_COLS = 12880
EPS = 1e-6

C_AQ, C_AK, C_AV = 0, 1024, 1280
C_BQKV, C_BZ, C_BB, C_BD = 1536, 4608, 5632, 5640
C_CQ, C_CKV, C_CKR, C_G = 5648, 6160, 6672, 6736


class Op:
    __slots__ = ("eng", "fn", "reads", "writes", "dma", "deps", "signal", "val", "sem", "lhs")

    def __init__(self, eng, fn, reads, writes, dma):
        self.eng, self.fn, self.reads, self.writes, self.dma = eng, fn, reads, writes, dma
        self.deps = ()
        self.signal = False
        self.val = 0
        self.sem = None
        self.lhs = None


def _norm(k):
    if isinstance(k, tuple):
        return tuple(_norm(x) for x in k)
    if isinstance(k, (str, int)):
        return k
    return "@" + k.name


class Prog:
    COMPUTE = ("pe", "act", "dve", "pool")

    def __init__(self, nc, stack):
        self.nc = nc
        self.stack = stack
        self.gstack = stack
        self.ops = []
        self.last_w = {}
        self.readers = {}
        self.n_alloc = 0

    def sb(self, shape, dtype, name=None):
        self.n_alloc += 1
        return self.stack.enter_context(self.nc.sbuf_tensor((name or "sb") + f"_{self.n_alloc}", list(shape), dtype))

    def ps(self, shape, dtype=F32, name=None):
        self.n_alloc += 1
        return self.stack.enter_context(self.nc.psum_tensor(name or f"ps{self.n_alloc}", list(shape), dtype))

    def dram(self, shape, dtype, name=None):
        self.n_alloc += 1
        return self.nc.dram_tensor(name or f"dr{self.n_alloc}", list(shape), dtype).ap()

    def _add(self, eng, fn, reads, writes, dma):
        rr = [_norm(r) for r in reads]
        ww = [_norm(w) for w in writes]
        ww += [r for r in rr if isinstance(r, tuple) and r[0] == "bank"]
        rr = [r for r in rr if not (isinstance(r, tuple) and r[0] == "bank")]
        op = Op(eng, fn, tuple(rr), tuple(ww), dma)
        deps = set()
        me = len(self.ops)
        for r in op.reads:
            w = self.last_w.get(r)
            if w is not None:
                deps.add((w, True))
        for w_ in op.writes:
            w = self.last_w.get(w_)
            if w is not None:
                deps.add((w, False))
            for rd in self.readers.get(w_, ()):
                deps.add((rd, False))
        keep = set()
        for (d, raw) in deps:
            dop = self.ops[d]
            if not dop.dma and dop.eng == eng and not dma:
                if eng == "pe" or not raw:
                    continue
            keep.add(d)
        op.deps = tuple(sorted(keep))
        if eng == "pe" and op.reads:
            lw = self.last_w.get(op.reads[0])
            if lw is not None and lw in keep:
                op.lhs = lw
        for d in op.deps:
            self.ops[d].signal = True
        for w_ in op.writes:
            self.last_w[w_] = me
            self.readers[w_] = []
        for r in op.reads:
            lst = self.readers.setdefault(r, [])
            if not dma:
                lst[:] = [x for x in lst if self.ops[x].dma or self.ops[x].eng != eng]
            lst.append(me)
        self.ops.append(op)
        return op

    def pe(self, fn, reads=(), writes=()):
        return self._add("pe", fn, reads, writes, False)

    def act(self, fn, reads=(), writes=()):
        return self._add("act", fn, reads, writes, False)

    def dve(self, fn, reads=(), writes=()):
        return self._add("dve", fn, reads, writes, False)

    def pool(self, fn, reads=(), writes=()):
        return self._add("pool", fn, reads, writes, False)

    def indirect(self, fn, reads=(), writes=()):
        op = self._add("pool", fn, reads, writes, True)
        op.lhs = "ind"
        return op

    def dma(self, out, in_, reads=(), writes=(), q="sp", slow=False):
        if slow:
            return self._add(q, lambda e: e.dma_start(out=out, in_=in_, allow_slow_non_contiguous=True), reads, writes, True)
        return self._add(q, lambda e: e.dma_start(out=out, in_=in_), reads, writes, True)

    def barrier(self):
        last = {}
        for i in range(len(self.ops) - 1, -1, -1):
            op = self.ops[i]
            if op.eng == "bar":
                break
            if not op.dma and op.eng not in last:
                last[op.eng] = i
                op.signal = True
            if len(last) == 4:
                break
        b = Op("bar", None, (), (), False)
        b.deps = tuple(last.values())
        self.ops.append(b)
        self.last_w = {}
        self.readers = {}

    @contextlib.contextmanager
    def phase(self):
        st = contextlib.ExitStack()
        old = self.stack
        self.stack = st
        try:
            yield
        finally:
            self.barrier()
            self.flush()
            st.close()
            self.stack = old

    def _init_emit(self, n_dma_sems=40):
        nc = self.nc
        self.engs = {"pe": nc.tensor, "act": nc.scalar, "dve": nc.vector, "pool": nc.gpsimd, "sp": nc.sync}
        st = self.gstack
        self.esem = {e: st.enter_context(nc.semaphore(f"s_{e}")) for e in self.COMPUTE}
        self.ecnt = {e: 0 for e in self.COMPUTE}
        self.dsem = [st.enter_context(nc.semaphore(f"s_d{i}")) for i in range(n_dma_sems)]
        self.dcnt = [0] * n_dma_sems
        self.dnext = 0
        self.waited = {}
        self.emitted = 0
        self.ind_hist = []

    def _wait(self, eng_name, sem, key, val):
        if self.waited.get((eng_name, key), 0) >= val:
            return
        self.waited[(eng_name, key)] = val
        self.engs[eng_name].wait_ge(sem, val)

    def flush(self):
        if not hasattr(self, "engs"):
            self._init_emit()
        engs, esem, ecnt, dsem, dcnt = self.engs, self.esem, self.ecnt, self.dsem, self.dcnt
        nd = len(dsem)
        for idx in range(self.emitted, len(self.ops)):
            op = self.ops[idx]
            en = op.eng
            if en == "bar":
                for q in ("pe", "act", "dve", "pool", "sp"):
                    for d in op.deps:
                        dop = self.ops[d]
                        if dop.eng != q:
                            self._wait(q, dop.sem[1], dop.sem[0], dop.val)
                    for k in range(nd):
                        if dcnt[k] > 0:
                            self._wait(q, dsem[k], ("d", k), 16 * dcnt[k])
                continue
            need = {}
            for d in op.deps:
                dop = self.ops[d]
                key, sem = dop.sem
                if key not in need or need[key][1] < dop.val:
                    need[key] = (sem, dop.val)
            lhs_key = self.ops[op.lhs].sem[0] if (not op.dma and op.lhs is not None and op.lhs != "ind") else None
            for key, (sem, val) in need.items():
                if key == lhs_key and val == self.ops[op.lhs].val:
                    continue
                self._wait(en, sem, key, val)
            if op.dma:
                if op.lhs == "ind":
                    self.ind_hist.append(None)
                    if len(self.ind_hist) > 6 and self.ind_hist[-7] is not None:
                        ksem, kkey, kval = self.ind_hist[-7]
                        self._wait(en, ksem, kkey, kval)
                k = self.dnext
                self.dnext = (self.dnext + 1) % nd
                if dcnt[k] > 0:
                    self._wait(en, dsem[k], ("d", k), 16 * dcnt[k])
                ins = op.fn(engs[en])
                dcnt[k] += 1
                ins.then_inc(dsem[k], 16)
                op.sem = (("d", k), dsem[k])
                op.val = 16 * dcnt[k]
                if op.lhs == "ind":
                    self.ind_hist[-1] = (dsem[k], ("d", k), op.val)
            else:
                ins = op.fn(engs[en])
                if op.lhs is not None:
                    dop = self.ops[op.lhs]
                    if self.waited.get((en, dop.sem[0]), 0) < dop.val:
                        self.waited[(en, dop.sem[0])] = dop.val
                        ins._wait_ge(dop.sem[1], dop.val)
                if op.signal:
                    ecnt[en] += 1
                    ins.then_inc(esem[en], 1)
                    op.sem = (("e", en), esem[en])
                    op.val = ecnt[en]
            op.fn = None
        self.emitted = len(self.ops)

    def finish(self):
        self.barrier()
        self.flush()
        self.counts = dict(self.ecnt)


def host_consts():
    c = {}
    c["ident_in"] = np.eye(128, dtype=np.float32)
    k = np.arange(128)[:, None]
    q = np.arange(128)[None, :]
    cur = (k <= q).astype(np.float32)
    prev = (k > q).astype(np.float32)
    c["m_swa"] = np.ascontiguousarray(np.stack([np.tile(prev, (1, 4)), np.tile(cur, (1, 4))]))
    q5 = np.arange(512)[None, :]
    c["m_mla"] = np.ascontiguousarray(np.stack([((128 * jj + k) <= q5).astype(np.float32) for jj in range(4)]))
    rot = np.zeros((64, 64), np.float32)
    for m in range(32):
        rot[m + 32, m] = -1.0
    for m in range(32, 64):
        rot[m - 32, m] = 1.0
    c["rot_in"] = rot
    c["tri_in"] = np.ascontiguousarray(np.stack([cur, prev]))
    sel = np.zeros((8, 8, 128), np.float32)
    for e_ in range(8):
        sel[e_, e_, :] = 1.0
    c["sel_in"] = sel
    c["tokid_in"] = (np.arange(64)[None, :] * 128 + np.arange(128)[:, None]).astype(np.float32)
    c["inv_freq"] = (10000.0 ** (-(np.arange(64) % 32) / 32.0)).astype(np.float32)[:, None]
    return c


def build(S, n_layers=2, dbg=None, phases=("p1", "swa", "mla", "dn", "p3")):
    dbg = dbg or {}
    nc = bass.Bass("TRN2", target_bir_lowering=False)
    NG = S // 512
    NT = S // 128
    stack = contextlib.ExitStack()
    P = Prog(nc, stack)
    L = n_layers

    def ext_in(name, shape, dt=F32):
        return nc.dram_tensor(name, list(shape), dt, kind="ExternalInput").ap()

    def ext_out(name, shape, dt=F32):
        return nc.dram_tensor(name, list(shape), dt, kind="ExternalOutput").ap()

    x_in = ext_in("x", [S, D])
    pos_in = ext_in("positions", [1, S], I32)
    ident_in = ext_in("ident_in", [128, 128])
    m_swa_in = ext_in("m_swa", [2, 128, 512])
    m_mla_in = ext_in("m_mla", [4, 128, 512])
    rot_in = ext_in("rot_in", [64, 64])
    inv_freq_in = ext_in("inv_freq", [64, 1])
    norm_mix = ext_in("norm_mix", [L, D])
    w_in = ext_in("w_in", [L, D, IN_COLS])
    swa_sinks = ext_in("swa_sinks", [L, 16])
    mla_q_norm = ext_in("mla_q_norm", [L, 512])
    mla_kv_norm = ext_in("mla_kv_norm", [L, 512])
    w_uq = ext_in("w_uq", [L, 512, 1536])
    w_ukv = ext_in("w_ukv", [L, 512, 2048])
    tri_in = ext_in("tri_in", [2, 128, 128])
    sel_in = ext_in("sel_in", [8, 8, 128])
    tokid_in = ext_in("tokid_in", [128, 64])
    p_in = ext_in("p", [L, S, 256])
    w_branch = ext_in("w_branch", [L, 3, 1024, D])
    w_out = ext_in("w_out", [L, D, D])
    norm_ffn = ext_in("norm_ffn", [L, D])
    w_ffn_gate = ext_in("w_ffn_gate", [1, D, 7168])
    w_ffn_up = ext_in("w_ffn_up", [1, D, 7168])
    w_ffn_down = ext_in("w_ffn_down", [1, 7168, D])
    norm_ple = ext_in("norm_ple", [L, D])
    w_ple_gate = ext_in("w_ple_gate", [L, D, D])
    w_ple_proj = ext_in("w_ple_proj", [L, 256, D])
    final_norm = ext_in("final_norm", [1, D])
    if L > 1 or "moe" in phases:
        w_router = ext_in("w_router", [1, D, 8])
        w_exp_gate = ext_in("w_exp_gate", [1, 8, D, 7168])
        w_exp_up = ext_in("w_exp_up", [1, 8, D, 7168])
        w_exp_down = ext_in("w_exp_down", [1, 8, 7168, D])
    out_ap = ext_out("out", [S, D])
    conv_w = ext_in("conv_w", [L, 4, 3072])
    dn_a_log = ext_in("dn_a_log", [L, 8])
    dn_dt_bias = ext_in("dn_dt_bias", [L, 8])
    dn_norm = ext_in("dn_norm", [L, 128])

    bank = [P.ps([128, 512], F32, f"bank{i}") for i in range(8)]

    ident = P.sb([128, 128], F32, "ident")
    P.dma(ident[:], ident_in[:, :], writes=[ident])
    eps_t = P.sb([128, 1], F32, "eps_t")
    P.pool(lambda e: e.memset(eps_t[:], EPS), writes=[eps_t])
    ones_bf = P.sb([128, 128], BF16, "ones_bf")
    P.pool(lambda e: e.memset(ones_bf[:], 1.0), writes=[ones_bf])
    m_swa = P.sb([128, 2, 512], BF16, "m_swa_t")
    m_mla = P.sb([128, 4, 512], BF16, "m_mla_t")
    for i in range(2):
        P.dma(m_swa[:, i, :], m_swa_in[i], writes=[m_swa], q="pool")
    for i in range(4):
        P.dma(m_mla[:, i, :], m_mla_in[i], writes=[m_mla], q="pool")
    rot = P.sb([64, 64], F32, "rot_t")
    P.dma(rot[:], rot_in[:, :], writes=[rot])
    inv_freq = P.sb([64, 1], F32, "inv_freq_t")
    P.dma(inv_freq[:], inv_freq_in[:, :], writes=[inv_freq])

    xT = P.dram([D, S], F32, "xT")
    projT = P.dram([C_G, S], F32, "projT")
    gatesT = P.dram([3 * D, S], BF16, "gatesT")
    vA = P.dram([S, 256], BF16, "vA")
    bd = P.dram([S, 16], F32, "bd")
    yT = P.dram([3 * 1024, S], BF16, "yT")
    cosT = P.dram([64, S], F32, "cosT")
    sinT = P.dram([64, S], F32, "sinT")
    qnT = P.dram([1024, S], BF16, "qnT")
    qrT = P.dram([512, S], BF16, "qrT")
    knT = P.dram([1024, S], BF16, "knT")
    krT = P.dram([64, S], BF16, "krT")
    vC = P.dram([S, 1024], BF16, "vC")
    w_in_bf = [P.dram([32, 128, 16 * 512], BF16, f"w_in_bf{l}") for l in range(L)]
    w_branch_bf = [P.dram([3072, D], BF16, f"w_branch_bf{l}") for l in range(L)]
    w_out_bf = [P.dram([D, D], BF16, f"w_out_bf{l}") for l in range(L)]
    w_pg_bf = [P.dram([D, D], BF16, f"w_pg_bf{l}") for l in range(L)]
    w_pp_bf = [P.dram([256, D], BF16, f"w_pp_bf{l}") for l in range(L)]
    w_ffn_bf = [P.dram([14, 128, 16 * 512], BF16, "w_ffn_bf_g"), P.dram([14, 128, 16 * 512], BF16, "w_ffn_bf_u"), P.dram([16, 128, 16 * 512], BF16, "w_ffn_bf_d")]
    if L > 1 or "moe" in phases:
        w_exp_bf = [P.dram([8, 14, 128, 16 * 512], BF16, "w_exp_bf_g"), P.dram([8, 14, 128, 16 * 512], BF16, "w_exp_bf_u"), P.dram([8, 16, 128, 16 * 512], BF16, "w_exp_bf_d")]
    pT = [P.dram([256, S], BF16, f"pT{l}") for l in range(L)]

    ALLG = list(range(NG))

    def keys(name, gs=None):
        return [(name, g) for g in (ALLG if gs is None else gs)]

    def cast(dst, src, rows, key, step=256):
        for r in range(0, rows, step):
            P.dma(dst[r:r + step, :], src[r:r + step, :], writes=[key], q="pool")

    blocks = [(0, 512, "f"), (512, 512, "f"), (C_AK, 256, "f"), (C_AV, 256, "t")] + \
             [(c, 512, "f") for c in range(C_BQKV, C_BB, 512)] + \
             [(C_BB, 16, "t"), (C_CQ, 512, "f"), (C_CKV, 512, "f"), (C_CKR, 64, "f")] + \
             [(c, 512, "f") for c in range(C_G, IN_COLS, 512)]

    def blk3(ap2d, nk=16):
        return ap2d.rearrange("p (k c) -> p k c", k=16)[:, 0:nk, :]

    def up_jobs(dst_b, src):
        return [(blk3(dst_b[fb])[:, k, :], src[k * 128:(k + 1) * 128, fb * 512:(fb + 1) * 512]) for fb in range(14) for k in range(16)]

    def down_jobs(dst_b, src):
        jobs = []
        for cb in range(4):
            for seg in range(4):
                k0 = seg * 16
                nk = min(16, 56 - k0)
                for k in range(nk):
                    jobs.append((blk3(dst_b[cb * 4 + seg])[:, k, :], src[(k0 + k) * 128:(k0 + k + 1) * 128, cb * 512:(cb + 1) * 512]))
        return jobs

    def expert_cast_jobs():
        jobs = []
        for ex in range(8):
            jobs += up_jobs(w_exp_bf[0][ex], w_exp_gate[0, ex]) + up_jobs(w_exp_bf[1][ex], w_exp_up[0, ex]) + down_jobs(w_exp_bf[2][ex], w_exp_down[0, ex])
        return jobs

    def expert_cast_jobs_old():
        jobs = []
        for ex in range(8):
            for (dst, src, rows, step) in ((w_exp_bf[0][ex], w_exp_gate[0, ex], D, 256), (w_exp_bf[1][ex], w_exp_up[0, ex], D, 256),
                                           (w_exp_bf[2][ex], w_exp_down[0, ex], 7168, 512)):
                for r in range(0, rows, step):
                    jobs.append((dst[r:r + step, :], src[r:r + step, :]))
        return jobs

    ejobs = {"jobs": None, "i": 0}

    def issue_casts(n, l=0, rest=False):
        if L <= 1 or l != 0:
            return
        if ejobs["jobs"] is None:
            ejobs["jobs"] = expert_cast_jobs()
        jobs = ejobs["jobs"]
        hi = len(jobs) if rest else min(len(jobs), ejobs["i"] + n)
        for (dst_, src_) in jobs[ejobs["i"]:hi]:
            P.dma(dst_, src_, writes=["w_exp_bf"], q="pool")
        ejobs["i"] = hi

    def casts_layer(l):
        for bi, (c0, wd, mode) in enumerate(blocks):
            for k in range(16):
                P.dma(blk3(w_in_bf[l][bi])[:, k, 0:wd], w_in[l][k * 128:(k + 1) * 128, c0:c0 + wd], writes=[("w_in_bf", l)], q="pool")
        if "p3" in phases:
            cast(w_branch_bf[l], w_branch[l].rearrange("n k d -> (n k) d"), 3072, ("w_branch_bf", l), 512)
            cast(w_out_bf[l], w_out[l], D, ("w_out_bf", l), 512)
            if l == 0:
                for (dst_, src_) in up_jobs(w_ffn_bf[0], w_ffn_gate[0]) + up_jobs(w_ffn_bf[1], w_ffn_up[0]) + down_jobs(w_ffn_bf[2], w_ffn_down[0]):
                    P.dma(dst_, src_, writes=["w_ffn_bf"], q="pool")
            cast(w_pg_bf[l], w_ple_gate[l], D, ("w_pg_bf", l), 512)
            cast(w_pp_bf[l], w_ple_proj[l], 256, ("w_pp_bf", l))

    with P.phase():
        xin4 = [P.sb([128, D], F32, f"xin4_{i}") for i in range(4)]
        tp_sb = [P.sb([128, 512], F32, f"tp_sb{i}") for i in range(2)]
        for g in range(NG):
            for t in range(4):
                tok0 = g * 512 + t * 128
                P.dma(xin4[t][:], x_in[tok0:tok0 + 128, :], writes=[xin4[t]])
            for k in range(KD):
                pp = bank[k % 2]
                sbuf = tp_sb[k % 2]
                for t in range(4):
                    P.pe(lambda e, pp=pp, t=t, k=k: e.transpose(pp[:, t * 128:(t + 1) * 128], xin4[t][:, k * 128:(k + 1) * 128], ident[:]),
                         reads=[xin4[t], ident], writes=[pp])
                P.act(lambda e, pp=pp, sbuf=sbuf: e.copy(out=sbuf[:], in_=pp[:]), reads=[pp], writes=[sbuf])
                P.dma(xT[k * 128:(k + 1) * 128, g * 512:(g + 1) * 512], sbuf[:], reads=[sbuf], writes=[("xT", g)], q="pool")
        if "p3" in phases:
            pin = [P.sb([128, 256], F32, f"pin{i}") for i in range(4)]
            psb = [P.sb([128, 512], BF16, f"psb{i}") for i in range(2)]
            np_ = 0
            for l in range(L):
                for g in range(NG):
                    for t in range(4):
                        tok0 = g * 512 + t * 128
                        P.dma(pin[t][:], p_in[l, tok0:tok0 + 128, :], writes=[pin[t]])
                    for kk in range(2):
                        pp = bank[2 + np_ % 2]
                        sbuf = psb[np_ % 2]
                        np_ += 1
                        for t in range(4):
                            P.pe(lambda e, pp=pp, t=t, kk=kk: e.transpose(pp[:, t * 128:(t + 1) * 128], pin[t][:, kk * 128:(kk + 1) * 128], ident[:]),
                                 reads=[pin[t], ident], writes=[pp])
                        P.act(lambda e, pp=pp, sbuf=sbuf: e.copy(out=sbuf[:], in_=pp[:]), reads=[pp], writes=[sbuf])
                        P.dma(pT[l][kk * 128:(kk + 1) * 128, g * 512:(g + 1) * 512], sbuf[:], reads=[sbuf], writes=[("pT", l)], q="pool")

    if "mla" in phases:
      with P.phase():
        pos_i = P.sb([64, 512], I32, "pos_i")
        ang = P.sb([64, 512], F32, "ang")
        ang2 = P.sb([64, 512], F32, "ang2")
        pos_k = P.sb([64, 512], I32, "pos_k")
        trig = [P.sb([64, 512], F32, f"trig{i}") for i in range(2)]
        negpi = P.sb([64, 1], F32, "negpi")
        P.pool(lambda e: e.memset(negpi[:], -float(np.pi)), writes=[negpi])
        TWO_PI = float(2 * np.pi)
        for g in range(NG):
            P.dma(pos_i[:], pos_in[0:1, g * 512:(g + 1) * 512].partition_broadcast(64), writes=[pos_i], slow=True)
            P.dve(lambda e: e.tensor_copy(out=ang[:], in_=pos_i[:]), reads=[pos_i], writes=[ang])
            P.dve(lambda e: e.tensor_scalar(out=ang[:], in0=ang[:], scalar1=inv_freq[:, 0:1], scalar2=1.0, op0=ALU.mult, op1=ALU.mult),
                  reads=[ang, inv_freq], writes=[ang])
            for which, shift, dst_d in ((0, 0.0, sinT), (1, float(np.pi / 2), cosT)):
                tr = trig[which]
                P.dve(lambda e, shift=shift: e.tensor_scalar(out=ang2[:], in0=ang[:], scalar1=shift, scalar2=float(1.0 / (2 * np.pi)), op0=ALU.add, op1=ALU.mult),
                      reads=[ang], writes=[ang2])
                P.dve(lambda e: e.tensor_copy(out=pos_k[:], in_=ang2[:]), reads=[ang2], writes=[pos_k])
                P.dve(lambda e: e.tensor_copy(out=ang2[:], in_=pos_k[:]), reads=[pos_k], writes=[ang2])
                P.dve(lambda e, tr=tr, shift=shift: e.tensor_scalar(out=tr[:], in0=ang[:], scalar1=shift, scalar2=1.0, op0=ALU.add, op1=ALU.mult),
                      reads=[ang], writes=[tr])
                P.dve(lambda e, tr=tr: e.scalar_tensor_tensor(out=tr[:], in0=ang2[:], scalar=-6.28125, in1=tr[:], op0=ALU.mult, op1=ALU.add),
                      reads=[ang2, tr], writes=[tr])
                P.dve(lambda e, tr=tr: e.scalar_tensor_tensor(out=tr[:], in0=ang2[:], scalar=-0.0019353071795864769, in1=tr[:], op0=ALU.mult, op1=ALU.add),
                      reads=[ang2, tr], writes=[tr])
                P.dve(lambda e, tr=tr: e.tensor_scalar(out=ang2[:], in0=tr[:], scalar1=float(np.pi), scalar2=TWO_PI, op0=ALU.is_gt, op1=ALU.mult),
                      reads=[tr], writes=[ang2])
                P.dve(lambda e, tr=tr: e.tensor_tensor(out=tr[:], in0=tr[:], in1=ang2[:], op=ALU.subtract), reads=[tr, ang2], writes=[tr])
                P.dve(lambda e, tr=tr: e.tensor_scalar(out=ang2[:], in0=tr[:], scalar1=-float(np.pi), scalar2=TWO_PI, op0=ALU.is_lt, op1=ALU.mult),
                      reads=[tr], writes=[ang2])
                P.dve(lambda e, tr=tr: e.tensor_tensor(out=tr[:], in0=tr[:], in1=ang2[:], op=ALU.add), reads=[tr, ang2], writes=[tr])
                P.dve(lambda e, tr=tr: e.tensor_scalar(out=tr[:], in0=tr[:], scalar1=float(np.pi), scalar2=-float(np.pi), op0=ALU.min, op1=ALU.max),
                      reads=[tr], writes=[tr])
                P.act(lambda e, tr=tr: e.activation(out=tr[:], in_=tr[:], func=AF.Sin), reads=[tr], writes=[tr])
                P.dma(dst_d[:, g * 512:(g + 1) * 512], tr[:], reads=[tr], writes=[("sinT" if which == 0 else "cosT", g)], q="pool")

    gain_mix = P.sb([128, L, KD], F32, "gain_mix")
    gain_q = P.sb([128, L, 4], F32, "gain_q")
    gain_kv = P.sb([128, L, 4], F32, "gain_kv")
    esink = P.sb([64, L, 16], F32, "esink")
    gain_ffn = P.sb([128, L, KD], F32, "gain_ffn")
    gain_ple = P.sb([128, L, KD], F32, "gain_ple")
    gain_fin = P.sb([128, KD], F32, "gain_fin")
    Wall = P.sb([128, NT, 8], F32, "Wall")
    Iall = P.sb([128, NT, 8], I32, "Iall")
    P.dma(gain_fin[:, :], final_norm[0].rearrange("(k p) -> p k", p=128), writes=[gain_fin], slow=True)
    for l in range(L):
        P.dma(gain_mix[:, l, :], norm_mix[l].rearrange("(k p) -> p k", p=128), writes=[gain_mix], slow=True)
        P.dma(gain_ffn[:, l, :], norm_ffn[l].rearrange("(k p) -> p k", p=128), writes=[gain_ffn], slow=True)
        P.dma(gain_ple[:, l, :], norm_ple[l].rearrange("(k p) -> p k", p=128), writes=[gain_ple], slow=True)
        P.dma(gain_q[:, l, :], mla_q_norm[l].rearrange("(k p) -> p k", p=128), writes=[gain_q], slow=True)
        P.dma(gain_kv[:, l, :], mla_kv_norm[l].rearrange("(k p) -> p k", p=128), writes=[gain_kv], slow=True)
        P.dma(esink[:, l, :], swa_sinks[l:l + 1, :].partition_broadcast(64), writes=[esink], slow=True)
    P.act(lambda e: e.activation(out=esink[:], in_=esink[:], func=AF.Exp), reads=[esink], writes=[esink])

    def rms_T(src, dst, nk, dim, gain, gkey, ss_bank, sq, inv):
        for k in range(nk):
            P.act(lambda e, k=k: e.activation(out=sq[:, k, :], in_=src[:, k, :], func=AF.Square), reads=[(src, k)], writes=[(sq, k)])
        for k in range(nk):
            P.pe(lambda e, k=k: e.matmul(ss_bank[:], lhsT=ones_bf[:], rhs=sq[:, k, :], start=(k == 0), stop=(k == nk - 1)),
                 reads=[ones_bf, (sq, k)], writes=[ss_bank])
        P.act(lambda e: e.activation(out=inv[:], in_=ss_bank[:], func=AF.Sqrt, scale=1.0 / dim, bias=eps_t[:, 0:1]),
              reads=[ss_bank, eps_t], writes=[inv])
        P.dve(lambda e: e.reciprocal(out=inv[:], in_=inv[:]), reads=[inv], writes=[inv])
        for k in range(nk):
            P.dve(lambda e, k=k: e.scalar_tensor_tensor(out=dst[:, k, :], in0=src[:, k, :], scalar=gain[:, k:k + 1], in1=inv[:],
                                                        op0=ALU.mult, op1=ALU.mult),
                  reads=[(src, k), inv, gkey], writes=[(dst, k)])

    cnt = {"blk": 0, "ev": 0}

    def p1(l):
        xg = P.sb([128, KD, 512], F32, "xg")
        sq = P.sb([128, KD, 512], BF16, "sq")
        hT = P.sb([128, KD, 512], BF16, "hT")
        inv = P.sb([128, 512], F32, "inv")
        wblk = [P.sb([128, KD, 512], BF16, f"wblk{i}") for i in range(2)]
        ev_sb = [P.sb([128, 512], F32, f"ev_sb{i}") for i in range(3)]
        evb_sb = [P.sb([128, 512], BF16, f"evb_sb{i}") for i in range(3)]
        for g in range(NG):
            issue_casts(1120 // NG, l)
            for k in range(KD):
                P.dma(xg[:, k, :], xT[k * 128:(k + 1) * 128, g * 512:(g + 1) * 512], reads=[("xT", g)], writes=[(xg, k)])
            rms_T(xg, hT, KD, D, gain_mix[:, l, :], gain_mix, bank[2], sq, inv)
            for bi, (c0, wd, mode) in enumerate(blocks):
                wb = wblk[cnt["blk"] % 2]
                cnt["blk"] += 1
                P.dma(wb[:, :, :], blk3(w_in_bf[l][bi]), reads=[("w_in_bf", l)], writes=[wb])
                if mode == "t":
                    for t in range(4):
                        pp = bank[cnt["ev"] % 2]
                        for k in range(KD):
                            P.pe(lambda e, pp=pp, wb=wb, k=k, t=t, wd=wd: e.matmul(pp[:, 0:wd], lhsT=hT[:, k, t * 128:(t + 1) * 128], rhs=wb[:, k, 0:wd],
                                                                                     start=(k == 0), stop=(k == KD - 1)),
                                 reads=[(hT, k), wb], writes=[pp])
                        tok0 = g * 512 + t * 128
                        if c0 == C_AV:
                            ob = evb_sb[cnt["ev"] % 3]
                            P.act(lambda e, pp=pp, ob=ob, wd=wd: e.copy(out=ob[:, 0:wd], in_=pp[:, 0:wd]), reads=[pp], writes=[ob])
                            P.dma(vA[tok0:tok0 + 128, :], ob[:, 0:wd], reads=[ob], writes=[("vA", g)], q="pool")
                        else:
                            ob = ev_sb[cnt["ev"] % 3]
                            P.act(lambda e, pp=pp, ob=ob, wd=wd: e.copy(out=ob[:, 0:wd], in_=pp[:, 0:wd]), reads=[pp], writes=[ob])
                            P.dma(bd[tok0:tok0 + 128, :], ob[:, 0:wd], reads=[ob], writes=[("bd", g)], q="pool")
                        cnt["ev"] += 1
                    continue
                for j in range(0, wd, 128):
                    cw = min(128, wd - j)
                    pp = bank[cnt["ev"] % 2]
                    for k in range(KD):
                        P.pe(lambda e, pp=pp, wb=wb, k=k, j=j, cw=cw: e.matmul(pp[0:cw, :], lhsT=wb[:, k, j:j + cw], rhs=hT[:, k, :],
                                                                                start=(k == 0), stop=(k == KD - 1)),
                             reads=[wb, (hT, k)], writes=[pp])
                    col = c0 + j
                    if col >= C_G:
                        ob = evb_sb[cnt["ev"] % 3]
                        P.act(lambda e, pp=pp, ob=ob, cw=cw: e.activation(out=ob[0:cw, :], in_=pp[0:cw, :], func=AF.Sigmoid),
                              reads=[pp], writes=[ob])
                        P.dma(gatesT[col - C_G:col - C_G + cw, g * 512:(g + 1) * 512], ob[0:cw, :], reads=[ob],
                              writes=[("gatesT", g)], q="pool")
                    else:
                        ob = ev_sb[cnt["ev"] % 3]
                        P.act(lambda e, pp=pp, ob=ob, cw=cw: e.copy(out=ob[0:cw, :], in_=pp[0:cw, :]), reads=[pp], writes=[ob])
                        P.dma(projT[col:col + cw, g * 512:(g + 1) * 512], ob[0:cw, :], reads=[ob], writes=[("projT", g)], q="pool")
                    cnt["ev"] += 1

    def swa(l):
        swa_kT = P.sb([64, S], BF16, "swa_kT")
        swa_v = P.sb([128, NT, 64], BF16, "swa_v")
        swa_q = [P.sb([64, 4, 512], BF16, f"swa_q{i}") for i in range(2)]
        swa_p = [P.sb([128, 512], BF16, f"swa_p{i}") for i in range(4)]
        swa_den = P.sb([64, 512], F32, "swa_den")
        swa_y = [P.sb([64, 4, 512], BF16, f"swa_y{i}") for i in range(2)]

        npt = 0
        for hk in range(4):
            P.dma(swa_kT[:, :], projT[C_AK + hk * 64:C_AK + (hk + 1) * 64, :], reads=keys("projT"), writes=[swa_kT], q="pool")
            P.dma(swa_v[:, :, :], vA[:, hk * 64:(hk + 1) * 64].rearrange("(t p) d -> p t d", p=128), reads=keys("vA"), writes=[swa_v], slow=True)
            for g in range(NG):
                qt = swa_q[g % 2]
                yb = swa_y[g % 2]
                P.dma(qt[:, :, :], projT[hk * 256:(hk + 1) * 256, g * 512:(g + 1) * 512].rearrange("(i d) t -> d i t", d=64),
                      reads=[("projT", g)], writes=[qt], q="pool")
                for b in range(4):
                    n = g * 4 + b
                    o_ps = bank[4 + (n % 2)]
                    s_ps = bank[6 + (n % 2)]
                    jl = [j for j in (n - 1, n) if j >= 0]
                    for idx, j in enumerate(jl):
                        sc = bank[npt % 4]
                        pt = swa_p[npt % 4]
                        npt += 1
                        P.pe(lambda e, sc=sc, j=j, qt=qt, b=b: e.matmul(sc[:, :].rearrange("p (i t) -> p i t", i=4), lhsT=swa_kT[:, j * 128:(j + 1) * 128],
                                                                         rhs=qt[:, :, b * 128:(b + 1) * 128], start=True, stop=True),
                             reads=[swa_kT, qt], writes=[sc])
                        P.act(lambda e, sc=sc, pt=pt: e.activation(out=pt[:], in_=sc[:], func=AF.Exp, scale=0.125), reads=[sc], writes=[pt])
                        mi = 1 if j == n else 0
                        P.dve(lambda e, pt=pt, mi=mi: e.tensor_tensor(out=pt[:], in0=pt[:], in1=m_swa[:, mi, :], op=ALU.mult),
                              reads=[pt, m_swa], writes=[pt])
                        first, last = idx == 0, idx == len(jl) - 1
                        P.pe(lambda e, o_ps=o_ps, pt=pt, j=j, first=first, last=last: e.matmul(o_ps[0:64, :], lhsT=swa_v[:, j, :], rhs=pt[:], start=first, stop=last),
                             reads=[swa_v, pt], writes=[o_ps])
                        P.pe(lambda e, s_ps=s_ps, pt=pt, first=first, last=last: e.matmul(s_ps[0:64, :], lhsT=ones_bf[:, 0:64], rhs=pt[:], start=first, stop=last),
                             reads=[ones_bf, pt], writes=[s_ps])
                    for i in range(4):
                        h = hk * 4 + i
                        P.dve(lambda e, s_ps=s_ps, i=i, h=h: e.tensor_scalar(out=swa_den[:, i * 128:(i + 1) * 128], in0=s_ps[0:64, i * 128:(i + 1) * 128],
                                                                               scalar1=esink[:, l, h:h + 1], scalar2=1.0, op0=ALU.add, op1=ALU.mult),
                              reads=[s_ps, esink], writes=[swa_den])
                    P.dve(lambda e: e.reciprocal(out=swa_den[:], in_=swa_den[:]), reads=[swa_den], writes=[swa_den])
                    P.dve(lambda e, o_ps=o_ps, yb=yb, b=b: e.tensor_tensor(out=yb[:, :, b * 128:(b + 1) * 128],
                                                                           in0=o_ps[0:64, :].rearrange("p (i t) -> p i t", i=4),
                                                                           in1=swa_den[:, :].rearrange("p (i t) -> p i t", i=4), op=ALU.mult),
                          reads=[o_ps, swa_den], writes=[yb])
                P.dma(yT[hk * 256:(hk + 1) * 256, g * 512:(g + 1) * 512].rearrange("(i d) t -> d i t", d=64), yb[:, :, :],
                      reads=[yb], writes=[("yT_a", g)], q="pool")

    def mla1(l):
        wuq_sb = P.sb([128, 4, 1536], BF16, "wuq_sb")
        wukv_sb = P.sb([128, 4, 2048], BF16, "wukv_sb")
        lat = P.sb([128, 4, 512], F32, "lat")
        latn = [P.sb([128, 4, 512], BF16, f"latn{i}") for i in range(2)]
        sq = P.sb([128, 4, 512], BF16, "sq_m")
        inv = P.sb([128, 512], F32, "inv_m")
        rope_x = P.sb([64, 512], F32, "rope_x")
        rope_t = P.sb([64, 512], F32, "rope_t")
        rope_o = [P.sb([64, 512], BF16, f"rope_o{i}") for i in range(2)]
        cs_sb = P.sb([64, 2, 512], F32, "cs_sb")
        evb_sb = [P.sb([128, 512], BF16, f"evb_m{i}") for i in range(3)]
        def rope(src_ps_or_sb, src_key, nrows_dummy, dst, g):
            P.pe(lambda e: e.matmul(bank[3][0:64, :], lhsT=rot[:, :], rhs=rope_x[:, :], start=True, stop=True), reads=[rot, rope_x], writes=[bank[3]])
            P.dve(lambda e: e.tensor_tensor(out=rope_t[:], in0=bank[3][0:64, :], in1=cs_sb[:, 1, :], op=ALU.mult), reads=[bank[3], cs_sb], writes=[rope_t])
            P.dve(lambda e: e.tensor_tensor(out=rope_x[:], in0=rope_x[:], in1=cs_sb[:, 0, :], op=ALU.mult), reads=[rope_x, cs_sb], writes=[rope_x])
            P.dve(lambda e, dst=dst: e.tensor_tensor(out=dst[:], in0=rope_x[:], in1=rope_t[:], op=ALU.add), reads=[rope_x, rope_t], writes=[dst])

        for k in range(4):
            P.dma(wuq_sb[:, k, :], w_uq[l, k * 128:(k + 1) * 128, :], writes=[wuq_sb], q="pool")
            P.dma(wukv_sb[:, k, :], w_ukv[l, k * 128:(k + 1) * 128, :], writes=[wukv_sb], q="pool")
        nev = 0
        for g in range(NG):
            gs = slice(g * 512, (g + 1) * 512)
            P.dma(cs_sb[:, 0, :], cosT[:, gs], reads=[("cosT", g)], writes=[cs_sb])
            P.dma(cs_sb[:, 1, :], sinT[:, gs], reads=[("sinT", g)], writes=[cs_sb])
            for k in range(4):
                P.dma(lat[:, k, :], projT[C_CQ + k * 128:C_CQ + (k + 1) * 128, gs], reads=[("projT", g)], writes=[(lat, k)])
            rms_T(lat, latn[0], 4, 512, gain_q[:, l, :], gain_q, bank[2], sq, inv)
            for h in range(8):
                pp = bank[nev % 2]
                for k in range(4):
                    P.pe(lambda e, pp=pp, k=k, h=h: e.matmul(pp[:, :], lhsT=wuq_sb[:, k, h * 192:h * 192 + 128], rhs=latn[0][:, k, :], start=(k == 0), stop=(k == 3)),
                         reads=[wuq_sb, (latn[0], k)], writes=[pp])
                ob = evb_sb[nev % 3]
                P.act(lambda e, pp=pp, ob=ob: e.copy(out=ob[:], in_=pp[:]), reads=[pp], writes=[ob])
                P.dma(qnT[h * 128:(h + 1) * 128, gs], ob[:], reads=[ob], writes=[("qnT", g)], q="pool")
                nev += 1
                pp = bank[nev % 2]
                for k in range(4):
                    P.pe(lambda e, pp=pp, k=k, h=h: e.matmul(pp[0:64, :], lhsT=wuq_sb[:, k, h * 192 + 128:h * 192 + 192], rhs=latn[0][:, k, :], start=(k == 0), stop=(k == 3)),
                         reads=[wuq_sb, (latn[0], k)], writes=[pp])
                P.act(lambda e, pp=pp: e.copy(out=rope_x[:], in_=pp[0:64, :]), reads=[pp], writes=[rope_x])
                ro = rope_o[nev % 2]
                rope(None, None, None, ro, g)
                P.dma(qrT[h * 64:(h + 1) * 64, gs], ro[:], reads=[ro], writes=[("qrT", g)], q="pool")
                nev += 1
            P.dma(rope_x[:], projT[C_CKR:C_CKR + 64, gs], reads=[("projT", g)], writes=[rope_x])
            ro = rope_o[nev % 2]
            rope(None, None, None, ro, g)
            P.dma(krT[:, gs], ro[:], reads=[ro], writes=[("krT", g)], q="pool")
            nev += 1
            for k in range(4):
                P.dma(lat[:, k, :], projT[C_CKV + k * 128:C_CKV + (k + 1) * 128, gs], reads=[("projT", g)], writes=[(lat, k)])
            rms_T(lat, latn[1], 4, 512, gain_kv[:, l, :], gain_kv, bank[2], sq, inv)
            for h in range(8):
                pp = bank[nev % 2]
                for k in range(4):
                    P.pe(lambda e, pp=pp, k=k, h=h: e.matmul(pp[:, :], lhsT=wukv_sb[:, k, h * 256:h * 256 + 128], rhs=latn[1][:, k, :], start=(k == 0), stop=(k == 3)),
                         reads=[wukv_sb, (latn[1], k)], writes=[pp])
                ob = evb_sb[nev % 3]
                P.act(lambda e, pp=pp, ob=ob: e.copy(out=ob[:], in_=pp[:]), reads=[pp], writes=[ob])
                P.dma(knT[h * 128:(h + 1) * 128, gs], ob[:], reads=[ob], writes=[("knT", g)], q="pool")
                nev += 1
            for t in range(4):
                tok0 = g * 512 + t * 128
                for hh in range(2):
                    pp = bank[nev % 2]
                    for k in range(4):
                        P.pe(lambda e, pp=pp, k=k, t=t, hh=hh: e.matmul(pp[:, :].rearrange("p (h d) -> p h d", h=4), lhsT=latn[1][:, k, t * 128:(t + 1) * 128],
                                                                         rhs=wukv_sb[:, k, :].rearrange("p (h c) -> p h c", h=8)[:, hh * 4:(hh + 1) * 4, 128:256],
                                                                         start=(k == 0), stop=(k == 3)),
                             reads=[(latn[1], k), wukv_sb], writes=[pp])
                    ob = evb_sb[nev % 3]
                    P.act(lambda e, pp=pp, ob=ob: e.copy(out=ob[:], in_=pp[:]), reads=[pp], writes=[ob])
                    P.dma(vC[tok0:tok0 + 128, hh * 512:(hh + 1) * 512], ob[:], reads=[ob], writes=[("vC", g)], q="pool")
                    nev += 1

    def mla2(l):
        mla_k = P.sb([128, S], BF16, "mla_k")
        mla_kr = P.sb([64, S], BF16, "mla_kr")
        mla_v = P.sb([128, NT, 128], BF16, "mla_v")
        mla_qn = [P.sb([128, 512], BF16, f"mla_qn{i}") for i in range(2)]
        mla_qr = [P.sb([64, 512], BF16, f"mla_qr{i}") for i in range(2)]
        mla_p = [P.sb([128, 512], BF16, f"mla_p{i}") for i in range(4)]
        mla_rs = P.sb([128, 512], F32, "mla_rs")
        mla_y = [P.sb([128, 512], BF16, f"mla_y{i}") for i in range(2)]
        P.dma(mla_kr[:, :], krT[:, :], reads=keys("krT"), writes=[mla_kr])
        scale = float(192 ** -0.5)
        npt = 0
        nq = 0
        for h in range(8):
            issue_casts(70, l)
            P.dma(mla_k[:, :], knT[h * 128:(h + 1) * 128, :], reads=keys("knT"), writes=[mla_k])
            P.dma(mla_v[:, :, :], vC[:, h * 128:(h + 1) * 128].rearrange("(t p) d -> p t d", p=128), reads=keys("vC"), writes=[mla_v], slow=True)
            for g in range(NG):
                gs = slice(g * 512, (g + 1) * 512)
                qn = mla_qn[nq % 2]
                qr = mla_qr[nq % 2]
                yb = mla_y[nq % 2]
                o_ps = bank[4 + (nq % 2)]
                s_ps = bank[6 + (nq % 2)]
                nq += 1
                P.dma(qn[:, :], qnT[h * 128:(h + 1) * 128, gs], reads=[("qnT", g)], writes=[qn])
                P.dma(qr[:, :], qrT[h * 64:(h + 1) * 64, gs], reads=[("qrT", g)], writes=[qr])
                nj = 4 * g + 4

                def qk(j, slot):
                    sc = bank[slot % 4]
                    P.pe(lambda e, sc=sc, j=j, qn=qn: e.matmul(sc[:, :], lhsT=mla_k[:, j * 128:(j + 1) * 128], rhs=qn[:, :], start=True, stop=False),
                         reads=[mla_k, qn], writes=[sc])
                    P.pe(lambda e, sc=sc, j=j, qr=qr: e.matmul(sc[:, :], lhsT=mla_kr[:, j * 128:(j + 1) * 128], rhs=qr[:, :], start=False, stop=True),
                         reads=[mla_kr, qr], writes=[sc])

                qk(0, npt)
                for j in range(nj):
                    sc = bank[npt % 4]
                    pt = mla_p[npt % 4]
                    if j + 1 < nj:
                        qk(j + 1, npt + 1)
                    npt += 1
                    P.act(lambda e, sc=sc, pt=pt: e.activation(out=pt[:], in_=sc[:], func=AF.Exp, scale=scale), reads=[sc], writes=[pt])
                    if j >= 4 * g:
                        jj = j - 4 * g
                        P.dve(lambda e, pt=pt, jj=jj: e.tensor_tensor(out=pt[:], in0=pt[:], in1=m_mla[:, jj, :], op=ALU.mult),
                              reads=[pt, m_mla], writes=[pt])
                    first, last = j == 0, j == nj - 1
                    P.pe(lambda e, o_ps=o_ps, pt=pt, j=j, first=first, last=last: e.matmul(o_ps[:, :], lhsT=mla_v[:, j, :], rhs=pt[:], start=first, stop=last),
                         reads=[mla_v, pt], writes=[o_ps])
                    P.pe(lambda e, s_ps=s_ps, pt=pt, first=first, last=last: e.matmul(s_ps[:, :], lhsT=ones_bf[:, :], rhs=pt[:], start=first, stop=last),
                         reads=[ones_bf, pt], writes=[s_ps])
                P.dve(lambda e, s_ps=s_ps: e.reciprocal(out=mla_rs[:], in_=s_ps[:]), reads=[s_ps], writes=[mla_rs])
                P.dve(lambda e, o_ps=o_ps, yb=yb: e.tensor_tensor(out=yb[:], in0=o_ps[:], in1=mla_rs[:], op=ALU.mult), reads=[o_ps, mla_rs], writes=[yb])
                P.dma(yT[2048 + h * 128:2048 + (h + 1) * 128, gs], yb[:], reads=[yb], writes=[("yT_c", g)], q="pool")

    def dn(l):
        NB = 4
        stop = dbg.get('dn_stop', 99)
        ones_f = P.sb([128, 128], F32, "ones_f")
        P.pool(lambda e: e.memset(ones_f[:], 1.0), writes=[ones_f])
        one_t = P.sb([128, 1], F32, "one_t")
        P.pool(lambda e: e.memset(one_t[:], 1.0), writes=[one_t])
        eps6 = P.sb([128, 1], F32, "eps6")
        P.pool(lambda e: e.memset(eps6[:], 1e-6), writes=[eps6])
        tri = P.sb([128, 2, 128], F32, "tri")
        P.dma(tri[:, 0, :], tri_in[0], writes=[tri])
        P.dma(tri[:, 1, :], tri_in[1], writes=[tri])
        U = tri[:, 0, :]
        TL = tri[:, 1, :]
        convw = P.sb([128, 4, 24], F32, "convw")
        for j in range(4):
            P.dma(convw[:, j, :], conv_w[l, j].rearrange("(c p) -> p c", p=128), writes=[convw], slow=True)
        dtb = P.sb([128, 8], F32, "dtb")
        nA = P.sb([128, 8], F32, "nA")
        gn = P.sb([128, 1], F32, "gn")
        P.dma(dtb[:, :], dn_dt_bias[l:l + 1, :].partition_broadcast(128), writes=[dtb], slow=True)
        P.dma(nA[:, :], dn_a_log[l:l + 1, :].partition_broadcast(128), writes=[nA], slow=True)
        P.dma(gn[:, :], dn_norm[l].rearrange("(p o) -> p o", o=1), writes=[gn], slow=True)
        P.act(lambda e: e.activation(out=nA[:], in_=nA[:], func=AF.Exp), reads=[nA], writes=[nA])
        P.dve(lambda e: e.tensor_scalar(out=nA[:], in0=nA[:], scalar1=-1.0, scalar2=0.0, op0=ALU.mult, op1=ALU.add), reads=[nA], writes=[nA])
        bdt = P.sb([128, NT, 16], F32, "bdt")
        P.dma(bdt[:, :, :], bd[:, :].rearrange("(t p) c -> p t c", p=128), reads=keys("bd"), writes=[bdt], slow=True)
        betat = P.sb([128, NT, 8], F32, "betat")
        gt = P.sb([128, NT, 8], F32, "gt")
        P.act(lambda e: e.activation(out=betat[:, :, :], in_=bdt[:, :, 0:8], func=AF.Sigmoid), reads=[bdt], writes=[betat])
        for t in range(NT):
            P.dve(lambda e, t=t: e.tensor_tensor(out=gt[:, t, :], in0=bdt[:, t, 8:16], in1=dtb[:, :], op=ALU.add), reads=[bdt, dtb], writes=[gt])
        P.act(lambda e: e.activation(out=gt[:, :, :], in_=gt[:, :, :], func=AF.Exp), reads=[gt], writes=[gt])
        P.act(lambda e: e.activation(out=gt[:, :, :], in_=gt[:, :, :], func=AF.Ln, bias=one_t[:, 0:1], scale=1.0), reads=[gt, one_t], writes=[gt])
        for t in range(NT):
            P.dve(lambda e, t=t: e.tensor_tensor(out=gt[:, t, :], in0=gt[:, t, :], in1=nA[:, :], op=ALU.mult), reads=[gt, nA], writes=[gt])

        if stop <= 1:
            return
        if dbg.get("dn_dump"):
            dd = {n: ext_out("o_dn" + n, [1024, S]) for n in ("q", "k", "v", "o")}
            dgb = ext_out("o_dngb", [128, NT, 16])
            P.dma(dgb[:, :, 0:8], gt[:, :, :], reads=[gt], writes=["o_dngb"])
            P.dma(dgb[:, :, 8:16], betat[:, :, :], reads=[betat], writes=["o_dngb"])
        xin = [P.sb([128, 515], F32, f"dn_xin{i}") for i in range(2)]
        cacc = P.sb([128, 512], F32, "dn_cacc")
        qkvT = [P.sb([128, 512], F32, f"dn_qkvT{i}") for i in range(3)]
        sqb = P.sb([128, 512], BF16, "dn_sqb")
        rinv = P.sb([128, 512], F32, "dn_rinv")
        zT = P.sb([128, 512], F32, "dn_zT")
        oT = P.sb([128, 512], F32, "dn_oT")
        yb = [P.sb([128, 512], BF16, f"dn_yb{i}") for i in range(2)]
        St = P.sb([128, 128], F32, "dn_S")
        names = ["ktok", "vtok", "gcrow", "gccol", "t1", "t2", "Lm", "NTm", "Pa", "PaT", "Pb", "PbT", "R", "qkT", "qgT", "kbg", "vb", "kd",
                 "egrow", "sc1", "sc2", "u", "wT", "vnew"]
        W = [{n: P.sb([128, 128] if n not in ("gccol", "sc1", "sc2") else [128, 1], F32, f"dn_{n}{b}") for n in names} for b in range(NB)]
        nps = [0]

        def psq():
            i = nps[0] % 28
            nps[0] += 1
            bi, qi = i % 7, (i // 7) % 4
            return bank[bi][:, qi * 128:(qi + 1) * 128], ("bank", bi)

        def mmf(lhsT, rhs, rd, n=128):
            pp, key = psq()
            P.pe(lambda e, pp=pp, lhsT=lhsT, rhs=rhs, n=n: e.matmul(pp[:, 0:n], lhsT=lhsT, rhs=rhs, start=True, stop=True), reads=rd, writes=[key])
            return pp, key

        def tpf(src, rd):
            pp, key = psq()
            P.pe(lambda e, pp=pp, src=src: e.transpose(pp, src, ident[:]), reads=rd + [ident], writes=[key])
            return pp, key

        for h in range(8):
            P.dve(lambda e: e.memset(St[:], 0.0), writes=[St])
            for g in range(NG):
                gs = slice(g * 512, (g + 1) * 512)
                issue_casts(1664 // (8 * NG), l)
                for ci in range(3):
                    chunk = ci * 8 + h
                    row0 = C_BQKV + chunk * 128
                    xi = xin[ci % 2]
                    if g == 0:
                        P.dve(lambda e, xi=xi: e.memset(xi[:, 0:3], 0.0), writes=[xi])
                        P.dma(xi[:, 3:515], projT[row0:row0 + 128, 0:512], reads=[("projT", 0)], writes=[xi])
                    else:
                        P.dma(xi[:, 0:515], projT[row0:row0 + 128, g * 512 - 3:(g + 1) * 512], reads=[("projT", g - 1), ("projT", g)], writes=[xi])
                    P.act(lambda e, xi=xi, chunk=chunk: e.activation(out=cacc[:], in_=xi[:, 0:512], func=AF.Copy, scale=convw[:, 0, chunk:chunk + 1]),
                          reads=[xi, convw], writes=[cacc])
                    for j in range(1, 4):
                        P.dve(lambda e, xi=xi, chunk=chunk, j=j: e.scalar_tensor_tensor(out=cacc[:], in0=xi[:, j:j + 512], scalar=convw[:, j, chunk:chunk + 1], in1=cacc[:],
                                                                                       op0=ALU.mult, op1=ALU.add), reads=[xi, convw, cacc], writes=[cacc])
                    dst = qkvT[ci]
                    P.act(lambda e, dst=dst: e.activation(out=dst[:], in_=cacc[:], func=AF.Silu), reads=[cacc], writes=[dst])
                    if ci < 2:
                        P.act(lambda e, dst=dst: e.activation(out=sqb[:], in_=dst[:], func=AF.Square), reads=[dst], writes=[sqb])
                        P.pe(lambda e: e.matmul(bank[7][:, :], lhsT=ones_bf[:], rhs=sqb[:], start=True, stop=True), reads=[ones_bf, sqb], writes=[("bank", 7)])
                        P.act(lambda e: e.activation(out=rinv[:], in_=bank[7][:, :], func=AF.Sqrt, bias=eps6[:, 0:1], scale=1.0),
                              reads=[("bank", 7), eps6], writes=[rinv])
                        P.dve(lambda e: e.reciprocal(out=rinv[:], in_=rinv[:]), reads=[rinv], writes=[rinv])
                        sc = float(128 ** -0.5) if ci == 0 else 1.0
                        P.dve(lambda e, dst=dst, sc=sc: e.scalar_tensor_tensor(out=dst[:], in0=dst[:], scalar=sc, in1=rinv[:], op0=ALU.mult, op1=ALU.mult),
                              reads=[dst, rinv], writes=[dst])
                qT_, kT_, vT_ = qkvT
                if dbg.get("dn_dump"):
                    for n_, t_ in (("q", qT_), ("k", kT_), ("v", vT_)):
                        P.dma(dd[n_][h * 128:(h + 1) * 128, gs], t_[:, :], reads=[t_], writes=["o_dn" + n_], q="pool")
                P.dma(zT[:, :], projT[C_BZ + h * 128:C_BZ + (h + 1) * 128, gs], reads=[("projT", g)], writes=[zT])
                P.act(lambda e: e.activation(out=zT[:], in_=zT[:], func=AF.Silu), reads=[zT], writes=[zT])
                if stop <= 2:
                    return
                for b in range(NB):
                    w = W[b]
                    t = g * 4 + b
                    cs = slice(b * 128, (b + 1) * 128)
                    gcol = gt[:, t, h:h + 1]
                    bcol = betat[:, t, h:h + 1]
                    pp, key = tpf(kT_[:, cs], [kT_])
                    P.act(lambda e, pp=pp, w=w: e.copy(out=w["ktok"][:], in_=pp), reads=[key], writes=[w["ktok"]])
                    pp, key = tpf(vT_[:, cs], [vT_])
                    P.act(lambda e, pp=pp, w=w: e.copy(out=w["vtok"][:], in_=pp), reads=[key], writes=[w["vtok"]])
                    P.dve(lambda e, w=w, gcol=gcol: e.tensor_scalar(out=w["t1"][:], in0=U, scalar1=gcol, scalar2=1.0, op0=ALU.mult, op1=ALU.mult), reads=[tri, gt], writes=[w["t1"]])
                    pp, key = mmf(ones_f[:, :], w["t1"][:], [ones_f, w["t1"]])
                    P.act(lambda e, pp=pp, w=w: e.copy(out=w["gcrow"][:], in_=pp), reads=[key], writes=[w["gcrow"]])
                    P.dve(lambda e, w=w: e.tensor_tensor(out=w["t2"][:], in0=w["gcrow"][:], in1=ident[:], op=ALU.mult), reads=[w["gcrow"], ident], writes=[w["t2"]])
                    P.dve(lambda e, w=w: e.reduce_sum(out=w["gccol"][:], in_=w["t2"][:], axis=AX.X), reads=[w["t2"]], writes=[w["gccol"]])
                    P.dve(lambda e, w=w: e.tensor_scalar(out=w["t1"][:], in0=w["gcrow"][:], scalar1=-1.0, scalar2=w["gccol"][:, 0:1], op0=ALU.mult, op1=ALU.add),
                          reads=[w["gcrow"], w["gccol"]], writes=[w["t1"]])
                    P.dve(lambda e, w=w: e.tensor_scalar(out=w["t1"][:], in0=w["t1"][:], scalar1=0.0, scalar2=1.0, op0=ALU.min, op1=ALU.mult), reads=[w["t1"]], writes=[w["t1"]])
                    P.act(lambda e, w=w: e.activation(out=w["t1"][:], in_=w["t1"][:], func=AF.Exp), reads=[w["t1"]], writes=[w["t1"]])
                    P.dve(lambda e, w=w, bcol=bcol: e.scalar_tensor_tensor(out=w["t1"][:], in0=w["t1"][:], scalar=bcol, in1=TL, op0=ALU.mult, op1=ALU.mult),
                          reads=[w["t1"], betat, tri], writes=[w["t1"]])
                    pp, key = mmf(kT_[:, cs], kT_[:, cs], [kT_])
                    P.dve(lambda e, pp=pp, w=w: e.tensor_tensor(out=w["Lm"][:], in0=pp, in1=w["t1"][:], op=ALU.mult), reads=[key, w["t1"]], writes=[w["Lm"]])
                    P.dve(lambda e, w=w: e.tensor_scalar(out=w["t2"][:], in0=w["gcrow"][:], scalar1=w["gccol"][:, 0:1], scalar2=0.0, op0=ALU.subtract, op1=ALU.min),
                          reads=[w["gcrow"], w["gccol"]], writes=[w["t2"]])
                    P.act(lambda e, w=w: e.activation(out=w["t2"][:], in_=w["t2"][:], func=AF.Exp), reads=[w["t2"]], writes=[w["t2"]])
                    P.dve(lambda e, w=w: e.tensor_tensor(out=w["t2"][:], in0=w["t2"][:], in1=U, op=ALU.mult), reads=[w["t2"], tri], writes=[w["t2"]])
                    pp, key = mmf(kT_[:, cs], qT_[:, cs], [kT_, qT_])
                    P.dve(lambda e, pp=pp, w=w: e.tensor_tensor(out=w["qkT"][:], in0=pp, in1=w["t2"][:], op=ALU.mult), reads=[key, w["t2"]], writes=[w["qkT"]])
                    P.act(lambda e, w=w: e.activation(out=w["egrow"][:], in_=w["gcrow"][:], func=AF.Exp), reads=[w["gcrow"]], writes=[w["egrow"]])
                    P.dve(lambda e, w=w, cs=cs: e.tensor_tensor(out=w["qgT"][:], in0=qT_[:, cs], in1=w["egrow"][:], op=ALU.mult), reads=[qT_, w["egrow"]], writes=[w["qgT"]])
                    P.act(lambda e, w=w: e.activation(out=w["sc1"][:], in_=w["gccol"][:], func=AF.Exp), reads=[w["gccol"]], writes=[w["sc1"]])
                    P.dve(lambda e, w=w, bcol=bcol: e.tensor_tensor(out=w["sc1"][:], in0=w["sc1"][:], in1=bcol, op=ALU.mult), reads=[w["sc1"], betat], writes=[w["sc1"]])
                    P.dve(lambda e, w=w: e.tensor_scalar(out=w["kbg"][:], in0=w["ktok"][:], scalar1=w["sc1"][:, 0:1], scalar2=1.0, op0=ALU.mult, op1=ALU.mult),
                          reads=[w["ktok"], w["sc1"]], writes=[w["kbg"]])
                    P.dve(lambda e, w=w, bcol=bcol: e.tensor_scalar(out=w["vb"][:], in0=w["vtok"][:], scalar1=bcol, scalar2=1.0, op0=ALU.mult, op1=ALU.mult),
                          reads=[w["vtok"], betat], writes=[w["vb"]])
                    P.act(lambda e, w=w: e.activation(out=w["sc2"][:], in_=w["gccol"][:], func=AF.Exp, scale=-1.0, bias=w["gcrow"][:, 127:128]),
                          reads=[w["gccol"], w["gcrow"]], writes=[w["sc2"]])
                    P.dve(lambda e, w=w: e.tensor_scalar(out=w["kd"][:], in0=w["ktok"][:], scalar1=w["sc2"][:, 0:1], scalar2=1.0, op0=ALU.mult, op1=ALU.mult),
                          reads=[w["ktok"], w["sc2"]], writes=[w["kd"]])
                    pp, key = tpf(w["Lm"][:], [w["Lm"]])
                    P.act(lambda e, pp=pp, w=w: e.copy(out=w["NTm"][:], in_=pp), reads=[key], writes=[w["NTm"]])
                    P.dve(lambda e, pp=pp, w=w: e.tensor_tensor(out=w["R"][:], in0=ident[:], in1=pp, op=ALU.subtract), reads=[key, ident], writes=[w["R"]])
                if stop <= 3:
                    return
                cur = [(W[b]["Lm"], W[b]["NTm"]) for b in range(NB)]
                for m in range(1, 7):
                    nxt = []
                    res = []
                    for b in range(NB):
                        w = W[b]
                        M_, MT_ = cur[b]
                        p1_, k1 = mmf(MT_[:], M_[:], [MT_, M_])
                        if m < 6:
                            p2_, k2 = mmf(M_[:], MT_[:], [M_, MT_])
                        else:
                            p2_, k2 = None, None
                        res.append((p1_, k1, p2_, k2))
                    for b in range(NB):
                        w = W[b]
                        p1_, k1, p2_, k2 = res[b]
                        Pn = w["Pa"] if m % 2 == 1 else w["Pb"]
                        PnT = w["PaT"] if m % 2 == 1 else w["PbT"]
                        P.act(lambda e, p1_=p1_, Pn=Pn: e.copy(out=Pn[:], in_=p1_), reads=[k1], writes=[Pn])
                        if m < 6:
                            P.dve(lambda e, p2_=p2_, PnT=PnT: e.tensor_copy(out=PnT[:], in_=p2_), reads=[k2], writes=[PnT])
                        nxt.append((Pn, PnT))
                    if dbg.get("dn_bar"):
                        P.barrier()
                    if dbg.get("dn_dump") and h == 1 and g == 0:
                        if m == 1:
                            oP = ext_out("o_dnP", [6, 4, 128, 128]); oPT = ext_out("o_dnPT", [6, 4, 128, 128]); oR = ext_out("o_dnR", [6, 4, 128, 128])
                        for b_ in range(NB):
                            P.dma(oP[m - 1, b_], nxt[b_][0][:, :], reads=[nxt[b_][0]], writes=["o_dnP"], q="pool")
                            if m < 6:
                                P.dma(oPT[m - 1, b_], nxt[b_][1][:, :], reads=[nxt[b_][1]], writes=["o_dnPT"], q="pool")
                            P.dma(oR[m - 1, b_], W[b_]["R"][:, :], reads=[W[b_]["R"]], writes=["o_dnR"], q="pool")
                    res = []
                    for b in range(NB):
                        w = W[b]
                        Pn, PnT = nxt[b]
                        res.append(mmf(Pn[:], w["R"][:], [Pn, w["R"]]))
                    for b in range(NB):
                        w = W[b]
                        pp, key = res[b]
                        P.dve(lambda e, pp=pp, w=w: e.tensor_tensor(out=w["R"][:], in0=w["R"][:], in1=pp, op=ALU.add), reads=[key, w["R"]], writes=[w["R"]])
                    cur = nxt
                if stop <= 4:
                    return
                if dbg.get("dn_dump") and h == 1 and g == 0:
                    for nm_ in ("R", "Lm", "NTm", "Pa", "PaT", "Pb", "PbT"):
                        oo = ext_out("o_dnW_" + nm_, [4, 128, 128])
                        for b_ in range(NB):
                            P.dma(oo[b_], W[b_][nm_][:, :], reads=[W[b_][nm_]], writes=["o_dnW_" + nm_], q="pool")
                for b in range(NB):
                    w = W[b]
                    pp, key = mmf(w["R"][:], w["vb"][:], [w["R"], w["vb"]])
                    P.act(lambda e, pp=pp, w=w: e.copy(out=w["u"][:], in_=pp), reads=[key], writes=[w["u"]])
                    pp, key = mmf(w["kbg"][:], w["R"][:], [w["kbg"], w["R"]])
                    P.act(lambda e, pp=pp, w=w: e.copy(out=w["wT"][:], in_=pp), reads=[key], writes=[w["wT"]])
                if stop <= 5:
                    return
                for b in range(NB):
                    w = W[b]
                    cs = slice(b * 128, (b + 1) * 128)
                    pp, key = mmf(w["wT"][:], St[:], [w["wT"], St])
                    P.dve(lambda e, pp=pp, w=w: e.tensor_tensor(out=w["vnew"][:], in0=w["u"][:], in1=pp, op=ALU.subtract), reads=[key, w["u"]], writes=[w["vnew"]])
                    po, keyo = psq()
                    P.pe(lambda e, po=po, w=w: e.matmul(po, lhsT=St[:], rhs=w["qgT"][:], start=True, stop=False), reads=[St, w["qgT"]], writes=[keyo])
                    P.pe(lambda e, po=po, w=w: e.matmul(po, lhsT=w["vnew"][:], rhs=w["qkT"][:], start=False, stop=True), reads=[w["vnew"], w["qkT"]], writes=[keyo])
                    P.act(lambda e, po=po, cs=cs: e.copy(out=oT[:, cs], in_=po), reads=[keyo], writes=[oT])
                    pp, key = mmf(w["kd"][:], w["vnew"][:], [w["kd"], w["vnew"]])
                    P.dve(lambda e, pp=pp, w=w: e.scalar_tensor_tensor(out=St[:], in0=St[:], scalar=w["egrow"][:, 127:128], in1=pp, op0=ALU.mult, op1=ALU.add),
                          reads=[key, St, w["egrow"]], writes=[St])
                if stop <= 6:
                    return
                if dbg.get("dn_dump"):
                    P.dma(dd["o"][h * 128:(h + 1) * 128, gs], oT[:, :], reads=[oT], writes=["o_dno"], q="pool")
                P.act(lambda e: e.activation(out=sqb[:], in_=oT[:], func=AF.Square), reads=[oT], writes=[sqb])
                bkeys = [("bank", 7)]
                P.pe(lambda e: e.matmul(bank[7][:, :], lhsT=ones_bf[:], rhs=sqb[:], start=True, stop=True), reads=[ones_bf, sqb], writes=bkeys)
                P.act(lambda e: e.activation(out=rinv[:], in_=bank[7][:, :], func=AF.Sqrt, bias=eps_t[:, 0:1], scale=1.0 / 128), reads=bkeys + [eps_t], writes=[rinv])
                P.dve(lambda e: e.reciprocal(out=rinv[:], in_=rinv[:]), reads=[rinv], writes=[rinv])
                P.dve(lambda e: e.scalar_tensor_tensor(out=oT[:], in0=oT[:], scalar=gn[:, 0:1], in1=rinv[:], op0=ALU.mult, op1=ALU.mult), reads=[oT, gn, rinv], writes=[oT])
                ybt = yb[g % 2]
                P.dve(lambda e, ybt=ybt: e.tensor_tensor(out=ybt[:], in0=oT[:], in1=zT[:], op=ALU.mult), reads=[oT, zT], writes=[ybt])
                P.dma(yT[1024 + h * 128:1024 + (h + 1) * 128, gs], ybt[:], reads=[ybt], writes=[("yT_b", g)], q="pool")

    def dump(name, src, shape, dt, rkeys):
        o = ext_out("o_" + name, shape, dt)
        P.dma(o, src, reads=rkeys, writes=["o_" + name])

    def load_x(xg, g):
        for k in range(KD):
            P.dma(xg[:, k, :], xT[k * 128:(k + 1) * 128, g * 512:(g + 1) * 512], reads=[("xT", g)], writes=[(xg, k)])

    def store_x(xg, g):
        for k in range(KD):
            P.dma(xT[k * 128:(k + 1) * 128, g * 512:(g + 1) * 512], xg[:, k, :], reads=[(xg, k)], writes=[("xT", g)], q="pool")

    def p3a(l):
        xg = P.sb([128, KD, 512], F32, "xg")
        yTt = P.sb([128, 24, 512], BF16, "yTt")
        gat = P.sb([128, KD, 512], BF16, "gat")
        merged = P.sb([128, KD, 512], F32, "merged")
        mergedb = P.sb([128, KD, 512], BF16, "mergedb")
        tmp = [P.sb([128, 512], F32, f"p3tmp{i}") for i in range(2)]
        wblk = [P.sb([128, KD, 512], BF16, f"wblk{i}") for i in range(2)]
        nb = 0
        ne = 0
        for g in range(NG):
            gs = slice(g * 512, (g + 1) * 512)
            issue_casts(560 // NG, l)
            load_x(xg, g)
            for c in range(24):
                P.dma(yTt[:, c, :], yT[c * 128:(c + 1) * 128, gs], reads=[("yT_a", g), ("yT_b", g), ("yT_c", g)], writes=[(yTt, c)])
            for n in range(3):
                for c in range(KD):
                    P.dma(gat[:, c, :], gatesT[n * D + c * 128:n * D + (c + 1) * 128, gs], reads=[("gatesT", g)], writes=[(gat, c)])
                for cb in range(4):
                    wb = wblk[nb % 2]
                    nb += 1
                    for k in range(8):
                        P.dma(wb[:, k, :], w_branch_bf[l][n * 1024 + k * 128:n * 1024 + (k + 1) * 128, cb * 512:(cb + 1) * 512],
                              reads=[("w_branch_bf", l)], writes=[wb])
                    for j in range(4):
                        dc = cb * 4 + j
                        pp = bank[ne % 4]
                        ne += 1
                        for k in range(8):
                            P.pe(lambda e, pp=pp, wb=wb, k=k, j=j, n=n: e.matmul(pp[:, :], lhsT=wb[:, k, j * 128:(j + 1) * 128], rhs=yTt[:, n * 8 + k, :],
                                                                                start=(k == 0), stop=(k == 7)), reads=[wb, (yTt, n * 8 + k)], writes=[pp])
                        if n == 0:
                            P.dve(lambda e, pp=pp, dc=dc: e.tensor_tensor(out=merged[:, dc, :], in0=pp[:, :], in1=gat[:, dc, :], op=ALU.mult),
                                  reads=[pp, (gat, dc)], writes=[(merged, dc)])
                        else:
                            tt = tmp[ne % 2]
                            P.dve(lambda e, pp=pp, dc=dc, tt=tt: e.tensor_tensor(out=tt[:], in0=pp[:, :], in1=gat[:, dc, :], op=ALU.mult),
                                  reads=[pp, (gat, dc)], writes=[tt])
                            P.pool(lambda e, dc=dc, tt=tt: e.tensor_tensor(out=merged[:, dc, :], in0=merged[:, dc, :], in1=tt[:], op=ALU.add),
                                   reads=[tt, (merged, dc)], writes=[(merged, dc)])
            for dc in range(KD):
                P.act(lambda e, dc=dc: e.copy(out=mergedb[:, dc, :], in_=merged[:, dc, :]), reads=[(merged, dc)], writes=[(mergedb, dc)])
            for cb in range(4):
                wb = wblk[nb % 2]
                nb += 1
                for k in range(KD):
                    P.dma(wb[:, k, :], w_out_bf[l][k * 128:(k + 1) * 128, cb * 512:(cb + 1) * 512], reads=[("w_out_bf", l)], writes=[wb])
                for j in range(4):
                    dc = cb * 4 + j
                    pp = bank[ne % 4]
                    ne += 1
                    for k in range(KD):
                        P.pe(lambda e, pp=pp, wb=wb, k=k, j=j: e.matmul(pp[:, :], lhsT=wb[:, k, j * 128:(j + 1) * 128], rhs=mergedb[:, k, :],
                                                                         start=(k == 0), stop=(k == KD - 1)), reads=[wb, (mergedb, k)], writes=[pp])
                    P.dve(lambda e, pp=pp, dc=dc: e.tensor_tensor(out=xg[:, dc, :], in0=xg[:, dc, :], in1=pp[:, :], op=ALU.add),
                          reads=[pp, (xg, dc)], writes=[(xg, dc)])
            store_x(xg, g)

    def ffn_core(wg_bf, wu_bf, wd_bf, wkey, hT, xg, hid, wblk, sgt, st, wbc=None):
        for fb in range(14):
            wbg = wblk[st["nb"] % len(wblk)]
            wbu = wblk[(st["nb"] + 1) % len(wblk)]
            st["nb"] += 2
            P.dma(wbg[:, :, :], blk3(wg_bf[fb]), reads=[wkey], writes=[wbg])
            P.dma(wbu[:, :, :], blk3(wu_bf[fb]), reads=[wkey], writes=[wbu])
            for j in range(4):
                fc = fb * 4 + j
                pg = bank[(st["ne"] % 2) * 2]
                pu = bank[(st["ne"] % 2) * 2 + 1]
                sg = sgt[st["ne"] % 2]
                st["ne"] += 1
                for k in range(KD):
                    P.pe(lambda e, pg=pg, wbg=wbg, k=k, j=j: e.matmul(pg[:, :], lhsT=wbg[:, k, j * 128:(j + 1) * 128], rhs=hT[:, k, :],
                                                                       start=(k == 0), stop=(k == KD - 1)), reads=[wbg, (hT, k)], writes=[pg])
                for k in range(KD):
                    P.pe(lambda e, pu=pu, wbu=wbu, k=k, j=j: e.matmul(pu[:, :], lhsT=wbu[:, k, j * 128:(j + 1) * 128], rhs=hT[:, k, :],
                                                                       start=(k == 0), stop=(k == KD - 1)), reads=[wbu, (hT, k)], writes=[pu])
                P.act(lambda e, pg=pg, sg=sg: e.activation(out=sg[:], in_=pg[:, :], func=AF.Silu), reads=[pg], writes=[sg])
                if wbc is not None:
                    P.pool(lambda e, sg=sg: e.tensor_tensor(out=sg[:], in0=sg[:], in1=wbc, op=ALU.mult), reads=[sg, "wbc"], writes=[sg])
                P.dve(lambda e, pu=pu, sg=sg, fc=fc: e.tensor_tensor(out=hid[:, fc, :], in0=pu[:, :], in1=sg[:], op=ALU.mult),
                      reads=[pu, sg], writes=[(hid, fc)])
        for cb in range(4):
            for seg in range(4):
                k0 = seg * 16
                nk = min(16, 56 - k0)
                wb = wblk[st["nb"] % len(wblk)]
                st["nb"] += 1
                P.dma(wb[:, 0:nk, :], blk3(wd_bf[cb * 4 + seg], nk), reads=[wkey], writes=[wb])
                for j in range(4):
                    pp = bank[4 + j]
                    for k in range(nk):
                        kk = k0 + k
                        P.pe(lambda e, pp=pp, wb=wb, k=k, j=j, kk=kk: e.matmul(pp[:, :], lhsT=wb[:, k, j * 128:(j + 1) * 128], rhs=hid[:, kk, :],
                                                                                start=(kk == 0), stop=(kk == 55)), reads=[wb, (hid, kk)], writes=[pp])
            for j in range(4):
                dc = cb * 4 + j
                pp = bank[4 + j]
                P.dve(lambda e, pp=pp, dc=dc: e.tensor_tensor(out=xg[:, dc, :], in0=xg[:, dc, :], in1=pp[:, :], op=ALU.add),
                      reads=[pp, (xg, dc)], writes=[(xg, dc)])

    def p3b_dense(l):
        xg = P.sb([128, KD, 512], F32, "xg")
        sq = P.sb([128, KD, 512], BF16, "sq")
        hT = P.sb([128, KD, 512], BF16, "hT")
        inv = P.sb([128, 512], F32, "inv")
        hid = P.sb([128, 56, 512], BF16, "hid")
        wblk = [P.sb([128, KD, 512], BF16, f"wblk{i}") for i in range(4)]
        sgt = [P.sb([128, 512], F32, f"sgt{i}") for i in range(2)]
        st = {"nb": 0, "ne": 0}
        for g in range(NG):
            issue_casts(1760 // NG, l, rest=(g == NG - 1))
            load_x(xg, g)
            rms_T(xg, hT, KD, D, gain_ffn[:, l, :], gain_ffn, bank[0], sq, inv)
            ffn_core(w_ffn_bf[0], w_ffn_bf[1], w_ffn_bf[2], "w_ffn_bf", hT, xg, hid, wblk, sgt, st)
            store_x(xg, g)

    def p3b_moe(l):
        xg = P.sb([128, KD, 512], F32, "xg")
        sq = P.sb([128, KD, 512], BF16, "sq")
        hT = P.sb([128, KD, 512], BF16, "hT")
        inv = P.sb([128, 512], F32, "inv")
        hid = P.sb([128, 56, 512], BF16, "hid")
        wblk = [P.sb([128, KD, 512], BF16, f"wblk{i}") for i in range(3)]
        sgt = [P.sb([128, 512], F32, f"sgt{i}") for i in range(2)]
        hf = [P.sb([128, 512], F32, f"hf{i}") for i in range(2)]
        wr = P.sb([128, KD, 8], F32, "wr")
        P.dma(wr[:, :, :], w_router[0].rearrange("(k p) e -> p k e", p=128), writes=[wr], slow=True)
        sel = P.sb([8, 8, 128], F32, "sel")
        P.dma(sel[:, :, :], sel_in[:, :, :], writes=[sel])
        lg = P.sb([128, 4, 8], F32, "lg")
        m8 = P.sb([128, 8], F32, "m8")
        nm1 = P.sb([128, 1], F32, "nm1")
        msk = P.sb([128, 8], F32, "msk")
        den = P.sb([128, 1], F32, "den")
        wT8 = P.sb([8, 512], F32, "wT8")
        wbc = P.sb([128, 8, 512], BF16, "wbc")
        st = {"nb": 0, "ne": 0}
        for g in range(NG):
            load_x(xg, g)
            rms_T(xg, hT, KD, D, gain_ffn[:, l, :], gain_ffn, bank[0], sq, inv)
            for k in range(KD):
                hb = hf[k % 2]
                P.dve(lambda e, k=k, hb=hb: e.scalar_tensor_tensor(out=hb[:], in0=xg[:, k, :], scalar=gain_ffn[:, l, k:k + 1], in1=inv[:], op0=ALU.mult, op1=ALU.mult),
                      reads=[(xg, k), inv, gain_ffn], writes=[hb])
                for t in range(4):
                    P.pe(lambda e, k=k, hb=hb, t=t: e.matmul(bank[4 + t][:, 0:8], lhsT=hb[:, t * 128:(t + 1) * 128], rhs=wr[:, k, :], start=(k == 0), stop=(k == KD - 1)),
                         reads=[hb, wr], writes=[bank[4 + t]])
            for t in range(4):
                P.act(lambda e, t=t: e.copy(out=lg[:, t, :], in_=bank[4 + t][:, 0:8]), reads=[bank[4 + t]], writes=[lg])
            for t in range(4):
                P.dve(lambda e, t=t: e.max(out=m8[:, :], in_=lg[:, t, :]), reads=[lg], writes=[m8])
                P.dve(lambda e, t=t: e.tensor_scalar(out=msk[:, :], in0=lg[:, t, :], scalar1=m8[:, 1:2], scalar2=1.0, op0=ALU.is_ge, op1=ALU.mult), reads=[lg, m8], writes=[msk])
                P.dve(lambda e: e.tensor_scalar(out=nm1[:, :], in0=m8[:, 0:1], scalar1=-1.0, scalar2=0.0, op0=ALU.mult, op1=ALU.add), reads=[m8], writes=[nm1])
                P.act(lambda e, t=t: e.activation(out=lg[:, t, :], in_=lg[:, t, :], func=AF.Exp, bias=nm1[:, 0:1], scale=1.0), reads=[lg, nm1], writes=[lg])
                P.dve(lambda e, t=t: e.tensor_tensor(out=lg[:, t, :], in0=lg[:, t, :], in1=msk[:, :], op=ALU.mult), reads=[lg, msk], writes=[lg])
                P.dve(lambda e, t=t: e.reduce_sum(out=den[:, :], in_=lg[:, t, :], axis=AX.X), reads=[lg], writes=[den])
                P.dve(lambda e: e.reciprocal(out=den[:, :], in_=den[:, :]), reads=[den], writes=[den])
                P.dve(lambda e, t=t: e.tensor_scalar(out=lg[:, t, :], in0=lg[:, t, :], scalar1=den[:, 0:1], scalar2=1.0, op0=ALU.mult, op1=ALU.mult), reads=[lg, den], writes=[lg])
                P.pe(lambda e, t=t: e.transpose(bank[0][0:8, t * 128:(t + 1) * 128], lg[:, t, :], ident[:]), reads=[lg, ident], writes=[bank[0]])
            P.act(lambda e: e.copy(out=wT8[:, :], in_=bank[0][0:8, :]), reads=[bank[0]], writes=[wT8])
            for ex in range(8):
                pp = bank[ex % 4]
                P.pe(lambda e, pp=pp, ex=ex: e.matmul(pp[:, :], lhsT=sel[:, ex, :], rhs=wT8[:, :], start=True, stop=True), reads=[sel, wT8], writes=[pp])
                P.act(lambda e, pp=pp, ex=ex: e.copy(out=wbc[:, ex, :], in_=pp[:, :]), reads=[pp], writes=["wbc"])
            for ex in range(8):
                ffn_core(w_exp_bf[0][ex], w_exp_bf[1][ex], w_exp_bf[2][ex], "w_exp_bf", hT, xg, hid, wblk, sgt, st, wbc=wbc[:, ex, :])
            store_x(xg, g)

    CAP = max(512, -(-(S * 5 // 16) // 512) * 512)
    NSG = CAP // 512

    def moe_routed(l):
        Hsel = P.dram([8 * CAP + 128, D], BF16, "Hsel")
        hrow = P.dram([S, D], BF16, "hrow")
        Tslot = P.dram([8 * CAP + 128, 2], I32, "Tslot")
        Ytok = P.dram([2 * S + 128, D], F32, "Ytok")
        BIG2 = 2 * S
        pbf = bank[7][:, :].bitcast(BF16)
        with P.phase():
            xg = P.sb([128, KD, 512], F32, "xg")
            sq = P.sb([128, KD, 512], BF16, "sq")
            hT = P.sb([128, KD, 512], BF16, "hT")
            inv = P.sb([128, 512], F32, "inv")
            hf = [P.sb([128, 512], F32, f"hf{i}") for i in range(2)]
            wr = P.sb([128, KD, 8], F32, "wr")
            P.dma(wr[:, :, :], w_router[0].rearrange("(k p) e -> p k e", p=128), writes=[wr], slow=True)
            ident_bf = P.sb([128, 128], BF16, "ident_bf")
            P.dve(lambda e: e.tensor_copy(out=ident_bf[:], in_=ident[:]), reads=[ident], writes=[ident_bf])
            lg = P.sb([128, 4, 8], F32, "lg")
            m8 = P.sb([128, 8], F32, "m8")
            nm1 = P.sb([128, 1], F32, "nm1")
            den = P.sb([128, 1], F32, "den")
            Mall = P.sb([128, NT, 8], F32, "Mall")
            Tall = P.sb([128, NT, 8], F32, "Tall")
            TW = P.sb([128, NT, 8, 2], I32, "TW")
            tokid = P.sb([128, 64], F32, "tokid")
            P.dma(tokid[:, :], tokid_in[:, :], writes=[tokid])
            bigt = P.sb([128, 2 * 8 * CAP // 128], I32, "bigt")
            P.dve(lambda e: e.memset(bigt[:], BIG2), writes=[bigt])
            P.dma(Tslot[0:8 * CAP, :].rearrange("(p c) o -> p (c o)", p=128), bigt[:, :], reads=[bigt], writes=["Tslot"], q="pool")
            hro = [P.sb([128, D], BF16, f"hro{i}") for i in range(2)]
            nh = 0
            for g in range(NG):
                load_x(xg, g)
                rms_T(xg, hT, KD, D, gain_ffn[:, l, :], gain_ffn, bank[0], sq, inv)
                for k in range(KD):
                    hb = hf[k % 2]
                    P.dve(lambda e, k=k, hb=hb: e.scalar_tensor_tensor(out=hb[:], in0=xg[:, k, :], scalar=gain_ffn[:, l, k:k + 1], in1=inv[:], op0=ALU.mult, op1=ALU.mult),
                          reads=[(xg, k), inv, gain_ffn], writes=[hb])
                    for t in range(4):
                        P.pe(lambda e, k=k, hb=hb, t=t: e.matmul(bank[1 + t][:, 0:8], lhsT=hb[:, t * 128:(t + 1) * 128], rhs=wr[:, k, :], start=(k == 0), stop=(k == KD - 1)),
                             reads=[hb, wr], writes=[bank[1 + t]])
                for t in range(4):
                    P.act(lambda e, t=t: e.copy(out=lg[:, t, :], in_=bank[1 + t][:, 0:8]), reads=[bank[1 + t]], writes=[lg])
                for t in range(4):
                    tt = g * 4 + t
                    P.dve(lambda e, t=t: e.max(out=m8[:, :], in_=lg[:, t, :]), reads=[lg], writes=[m8])
                    P.dve(lambda e, t=t, tt=tt: e.tensor_scalar(out=Mall[:, tt, :], in0=lg[:, t, :], scalar1=m8[:, 1:2], scalar2=1.0, op0=ALU.is_ge, op1=ALU.mult),
                          reads=[lg, m8], writes=[Mall])
                    P.dve(lambda e, t=t, tt=tt: e.tensor_scalar(out=Tall[:, tt, :], in0=lg[:, t, :], scalar1=m8[:, 0:1], scalar2=float(-S), op0=ALU.is_ge, op1=ALU.mult),
                          reads=[lg, m8], writes=[Tall])
                    P.dve(lambda e, tt=tt: e.tensor_scalar(out=Tall[:, tt, :], in0=Tall[:, tt, :], scalar1=tokid[:, tt:tt + 1], scalar2=float(S), op0=ALU.add, op1=ALU.add),
                          reads=[Tall, tokid], writes=[Tall])
                    P.dve(lambda e: e.tensor_scalar(out=nm1[:, :], in0=m8[:, 0:1], scalar1=-1.0, scalar2=0.0, op0=ALU.mult, op1=ALU.add), reads=[m8], writes=[nm1])
                    P.act(lambda e, t=t: e.activation(out=lg[:, t, :], in_=lg[:, t, :], func=AF.Exp, bias=nm1[:, 0:1], scale=1.0), reads=[lg, nm1], writes=[lg])
                    P.dve(lambda e, t=t, tt=tt: e.tensor_tensor(out=lg[:, t, :], in0=lg[:, t, :], in1=Mall[:, tt, :], op=ALU.mult), reads=[lg, Mall], writes=[lg])
                    P.dve(lambda e, t=t: e.reduce_sum(out=den[:, :], in_=lg[:, t, :], axis=AX.X), reads=[lg], writes=[den])
                    P.dve(lambda e: e.reciprocal(out=den[:, :], in_=den[:, :]), reads=[den], writes=[den])
                    P.dve(lambda e, t=t, tt=tt: e.tensor_scalar(out=Wall[:, tt, :], in0=lg[:, t, :], scalar1=den[:, 0:1], scalar2=1.0, op0=ALU.mult, op1=ALU.mult),
                          reads=[lg, den], writes=[Wall])
                for t in range(4):
                    hr = hro[nh % 2]
                    nh += 1
                    for kb in range(2):
                        for kq in range(8):
                            k = kb * 8 + kq
                            P.pe(lambda e, k=k, kq=kq, t=t: e.transpose(pbf[:, kq * 128:(kq + 1) * 128], hT[:, k, t * 128:(t + 1) * 128], ident_bf[:]),
                                 reads=[(hT, k), ident_bf], writes=[bank[7]])
                        P.act(lambda e, hr=hr, kb=kb: e.copy(out=hr[:, kb * 1024:(kb + 1) * 1024], in_=pbf[:, :]), reads=[bank[7]], writes=[hr])
                    tok0 = g * 512 + t * 128
                    P.dma(hrow[tok0:tok0 + 128, :], hr[:, :], reads=[hr], writes=["hrow"], q="pool")
            us = P.sb([128, 128], F32, "us")
            ones_f = P.sb([128, 128], F32, "ones_f")
            P.pool(lambda e: e.memset(ones_f[:], 1.0), writes=[ones_f])
            P.dma(us[:, :], tri_in[0], writes=[us])
            P.dve(lambda e: e.tensor_tensor(out=us[:], in0=us[:], in1=ident[:], op=ALU.subtract), reads=[us, ident], writes=[us])
            cum = P.sb([128, NT, 8], F32, "cum")
            tot = P.sb([128, NT, 8], F32, "tot")
            base = P.sb([128, NT, 8], F32, "base")
            for c0 in range(0, NT * 8, 512):
                c1 = min(NT * 8, c0 + 512)
                mflat = Mall[:, :, :].rearrange("p t e -> p (t e)")
                P.pe(lambda e, c0=c0, c1=c1, mflat=mflat: e.matmul(bank[1][:, 0:c1 - c0], lhsT=us[:, :], rhs=mflat[:, c0:c1], start=True, stop=True), reads=[us, Mall], writes=[bank[1]])
                P.act(lambda e, c0=c0, c1=c1: e.copy(out=cum[:, :, :].rearrange("p t e -> p (t e)")[:, c0:c1], in_=bank[1][:, 0:c1 - c0]), reads=[bank[1]], writes=[cum])
                P.pe(lambda e, c0=c0, c1=c1, mflat=mflat: e.matmul(bank[2][:, 0:c1 - c0], lhsT=ones_f[:, :], rhs=mflat[:, c0:c1], start=True, stop=True), reads=[ones_f, Mall], writes=[bank[2]])
                P.act(lambda e, c0=c0, c1=c1: e.copy(out=tot[:, :, :].rearrange("p t e -> p (t e)")[:, c0:c1], in_=bank[2][:, 0:c1 - c0]), reads=[bank[2]], writes=[tot])
            for ex in range(8):
                P.dve(lambda e, ex=ex: e.memset(base[:, 0, ex:ex + 1], float(ex * CAP)), writes=[base])
            for t in range(1, NT):
                P.dve(lambda e, t=t: e.tensor_tensor(out=base[:, t, :], in0=base[:, t - 1, :], in1=tot[:, t - 1, :], op=ALU.add), reads=[base, tot], writes=[base])
            BIG = float(8 * CAP)
            P.dve(lambda e: e.tensor_tensor(out=cum[:, :, :], in0=cum[:, :, :], in1=base[:, :, :], op=ALU.add), reads=[cum, base], writes=[cum])
            P.dve(lambda e: e.tensor_scalar(out=cum[:, :, :], in0=cum[:, :, :], scalar1=-BIG, scalar2=1.0, op0=ALU.add, op1=ALU.mult), reads=[cum], writes=[cum])
            P.dve(lambda e: e.tensor_tensor(out=cum[:, :, :], in0=cum[:, :, :], in1=Mall[:, :, :], op=ALU.mult), reads=[cum, Mall], writes=[cum])
            P.dve(lambda e: e.tensor_scalar(out=cum[:, :, :], in0=cum[:, :, :], scalar1=BIG, scalar2=1.0, op0=ALU.add, op1=ALU.mult), reads=[cum], writes=[cum])
            P.dve(lambda e: e.tensor_copy(out=Iall[:, :, :], in_=cum[:, :, :]), reads=[cum], writes=[Iall])
            P.dve(lambda e: e.tensor_copy(out=TW[:, :, :, 0], in_=Tall[:, :, :]), reads=[Tall], writes=[TW])
            P.dve(lambda e: e.tensor_copy(out=TW[:, :, :, :].bitcast(F32)[:, :, :, 1], in_=Wall[:, :, :]), reads=[Wall, TW], writes=[TW])
            for t in range(NT):
                hr = hro[nh % 2]
                nh += 1
                P.dma(hr[:, :], hrow[t * 128:(t + 1) * 128, :], reads=["hrow"], writes=[hr])
                for ex in range(8):
                    P.indirect(lambda e, hr=hr, t=t, ex=ex: e.indirect_dma_start(
                        out=Hsel[:, :], out_offset=bass.IndirectOffsetOnAxis(ap=Iall[:, t, ex:ex + 1], axis=0),
                        in_=hr[:, :], in_offset=None),
                        [hr, Iall], ["Hsel"])
                    P.indirect(lambda e, t=t, ex=ex: e.indirect_dma_start(
                        out=Tslot[:, :], out_offset=bass.IndirectOffsetOnAxis(ap=Iall[:, t, ex:ex + 1], axis=0),
                        in_=TW[:, t, ex, :], in_offset=None),
                        [TW, Iall, "Tslot"], ["Tslot"])
        with P.phase():
            ident_bf = P.sb([128, 128], BF16, "ident_bf")
            P.dve(lambda e: e.tensor_copy(out=ident_bf[:], in_=ident[:]), reads=[ident], writes=[ident_bf])
            hs = P.sb([128, 4, D], BF16, "hs")
            hselT = P.sb([128, KD, 512], BF16, "hselT")
            hid = P.sb([128, 56, 512], BF16, "hid")
            wblk = [P.sb([128, KD, 512], BF16, f"wblk{i}") for i in range(3)]
            sgt = [P.sb([128, 512], F32, f"sgt{i}") for i in range(2)]
            osl = P.sb([128, 4, D], F32, "osl")
            tsl = P.sb([128, 4, 2], I32, "tsl")
            st = {"nb": 0, "ne": 0}
            for ex in range(8):
                for sg in range(NSG):
                    r0 = ex * CAP + sg * 512
                    for s4 in range(4):
                        P.dma(hs[:, s4, :], Hsel[r0 + s4 * 128:r0 + (s4 + 1) * 128, :], reads=["Hsel"], writes=[(hs, s4)])
                    P.dma(tsl[:, :, :], Tslot[r0:r0 + 512, :].rearrange("(s p) o -> p s o", p=128), reads=["Tslot"], writes=[tsl], slow=True)
                    for kp in range(KD // 2):
                        for kq in range(2):
                            k = kp * 2 + kq
                            for s4 in range(4):
                                P.pe(lambda e, k=k, kq=kq, s4=s4: e.transpose(pbf[:, kq * 512 + s4 * 128:kq * 512 + (s4 + 1) * 128], hs[:, s4, k * 128:(k + 1) * 128], ident_bf[:]),
                                     reads=[(hs, s4), ident_bf], writes=[bank[7]])
                        P.act(lambda e, kp=kp: e.copy(out=hselT[:, kp * 2:kp * 2 + 2, :].rearrange("p a b -> p (a b)"), in_=pbf[:, :]), reads=[bank[7]],
                              writes=[(hselT, kp * 2), (hselT, kp * 2 + 1)])
                    wg_bf, wu_bf, wd_bf = w_exp_bf[0][ex], w_exp_bf[1][ex], w_exp_bf[2][ex]
                    for fb in range(14):
                        wbg = wblk[st["nb"] % 3]
                        wbu = wblk[(st["nb"] + 1) % 3]
                        st["nb"] += 2
                        P.dma(wbg[:, :, :], blk3(wg_bf[fb]), reads=["w_exp_bf"], writes=[wbg])
                        P.dma(wbu[:, :, :], blk3(wu_bf[fb]), reads=["w_exp_bf"], writes=[wbu])
                        for j in range(4):
                            fc = fb * 4 + j
                            pg = bank[(st["ne"] % 2) * 2]
                            pu = bank[(st["ne"] % 2) * 2 + 1]
                            sgq = sgt[st["ne"] % 2]
                            st["ne"] += 1
                            for k in range(KD):
                                P.pe(lambda e, pg=pg, wbg=wbg, k=k, j=j: e.matmul(pg[:, :], lhsT=wbg[:, k, j * 128:(j + 1) * 128], rhs=hselT[:, k, :],
                                                                                   start=(k == 0), stop=(k == KD - 1)), reads=[wbg, (hselT, k)], writes=[pg])
                            for k in range(KD):
                                P.pe(lambda e, pu=pu, wbu=wbu, k=k, j=j: e.matmul(pu[:, :], lhsT=wbu[:, k, j * 128:(j + 1) * 128], rhs=hselT[:, k, :],
                                                                                   start=(k == 0), stop=(k == KD - 1)), reads=[wbu, (hselT, k)], writes=[pu])
                            P.act(lambda e, pg=pg, sgq=sgq: e.activation(out=sgq[:], in_=pg[:, :], func=AF.Silu), reads=[pg], writes=[sgq])
                            P.dve(lambda e, pu=pu, sgq=sgq, fc=fc: e.tensor_tensor(out=hid[:, fc, :], in0=pu[:, :], in1=sgq[:], op=ALU.mult),
                                  reads=[pu, sgq], writes=[(hid, fc)])
                    for cb in range(4):
                        for seg in range(4):
                            k0 = seg * 16
                            nk = min(16, 56 - k0)
                            wb = wblk[st["nb"] % 3]
                            st["nb"] += 1
                            P.dma(wb[:, 0:nk, :], blk3(wd_bf[cb * 4 + seg], nk), reads=["w_exp_bf"], writes=[wb])
                            for s4 in range(4):
                                pp = bank[3 + s4]
                                for k in range(nk):
                                    kk = k0 + k
                                    P.pe(lambda e, pp=pp, wb=wb, k=k, s4=s4, kk=kk: e.matmul(pp[:, :], lhsT=hid[:, kk, s4 * 128:(s4 + 1) * 128], rhs=wb[:, k, :],
                                                                                          start=(kk == 0), stop=(kk == 55)), reads=[(hid, kk), wb], writes=[pp])
                        for s4 in range(4):
                            pp = bank[3 + s4]
                            P.act(lambda e, pp=pp, s4=s4, cb=cb: e.activation(out=osl[:, s4, cb * 512:(cb + 1) * 512], in_=pp[:, :], func=AF.Copy, scale=tsl[:, s4, 1:2].bitcast(F32)),
                                  reads=[pp, tsl], writes=[(osl, s4)])
                    for s4 in range(4):
                        P.indirect(lambda e, s4=s4: e.indirect_dma_start(
                            out=Ytok[:, :], out_offset=bass.IndirectOffsetOnAxis(ap=tsl[:, s4, 0:1], axis=0),
                            in_=osl[:, s4, :], in_offset=None),
                            [(osl, s4), tsl], ["Ytok"])
        with P.phase():
            xg = P.sb([128, KD, 512], F32, "xg")
            y1 = [P.sb([128, D], F32, f"y1_{i}") for i in range(4)]
            y2 = [P.sb([128, D], F32, f"y2_{i}") for i in range(4)]
            for g in range(NG):
                load_x(xg, g)
                for t in range(4):
                    tok0 = g * 512 + t * 128
                    P.dma(y1[t][:, :], Ytok[tok0:tok0 + 128, :], reads=["Ytok"], writes=[y1[t]])
                    P.dma(y2[t][:, :], Ytok[S + tok0:S + tok0 + 128, :], reads=["Ytok"], writes=[y2[t]])
                    P.pool(lambda e, t=t: e.tensor_tensor(out=y1[t][:], in0=y1[t][:], in1=y2[t][:], op=ALU.add), reads=[y1[t], y2[t]], writes=[y1[t]])
                for k in range(KD):
                    pp = bank[k % 4]
                    for t in range(4):
                        P.pe(lambda e, pp=pp, t=t, k=k: e.transpose(pp[:, t * 128:(t + 1) * 128], y1[t][:, k * 128:(k + 1) * 128], ident[:]),
                             reads=[y1[t], ident], writes=[pp])
                    P.dve(lambda e, pp=pp, k=k: e.tensor_tensor(out=xg[:, k, :], in0=xg[:, k, :], in1=pp[:, :], op=ALU.add), reads=[pp, (xg, k)], writes=[(xg, k)])
                store_x(xg, g)

    def p3c(l, last):
        xg = P.sb([128, KD, 512], F32, "xg")
        sq = P.sb([128, KD, 512], BF16, "sq")
        hT = P.sb([128, KD, 512], BF16, "hT")
        inv = P.sb([128, 512], F32, "inv")
        pTt = P.sb([128, 2, 512], BF16, "pTt")
        wblk = [P.sb([128, KD, 512], BF16, f"wblk{i}") for i in range(2)]
        wpb = [P.sb([128, 2, 512], BF16, f"wpb{i}") for i in range(2)]
        sgt = [P.sb([128, 512], F32, f"sgt{i}") for i in range(2)]
        if last:
            hfin = P.sb([128, KD, 512], F32, "hfin")
            osb = [P.sb([128, D], F32, f"osb{i}") for i in range(2)]
        nb = 0
        ne = 0
        no = 0
        for g in range(NG):
            gs = slice(g * 512, (g + 1) * 512)
            load_x(xg, g)
            rms_T(xg, hT, KD, D, gain_ple[:, l, :], gain_ple, bank[0], sq, inv)
            for kk in range(2):
                P.dma(pTt[:, kk, :], pT[l][kk * 128:(kk + 1) * 128, gs], reads=[("pT", l)], writes=[pTt])
            for cb in range(4):
                wb = wblk[nb % 2]
                wp = wpb[nb % 2]
                nb += 1
                for k in range(KD):
                    P.dma(wb[:, k, :], w_pg_bf[l][k * 128:(k + 1) * 128, cb * 512:(cb + 1) * 512], reads=[("w_pg_bf", l)], writes=[wb])
                for kk in range(2):
                    P.dma(wp[:, kk, :], w_pp_bf[l][kk * 128:(kk + 1) * 128, cb * 512:(cb + 1) * 512], reads=[("w_pp_bf", l)], writes=[wp])
                for j in range(4):
                    dc = cb * 4 + j
                    pgt = bank[2 + (ne % 2) * 2]
                    ppr = bank[3 + (ne % 2) * 2]
                    sg = sgt[ne % 2]
                    ne += 1
                    for k in range(KD):
                        P.pe(lambda e, pgt=pgt, wb=wb, k=k, j=j: e.matmul(pgt[:, :], lhsT=wb[:, k, j * 128:(j + 1) * 128], rhs=hT[:, k, :],
                                                                           start=(k == 0), stop=(k == KD - 1)), reads=[wb, (hT, k)], writes=[pgt])
                    for kk in range(2):
                        P.pe(lambda e, ppr=ppr, wp=wp, kk=kk, j=j: e.matmul(ppr[:, :], lhsT=wp[:, kk, j * 128:(j + 1) * 128], rhs=pTt[:, kk, :],
                                                                             start=(kk == 0), stop=(kk == 1)), reads=[wp, pTt], writes=[ppr])
                    P.act(lambda e, pgt=pgt, sg=sg: e.activation(out=sg[:], in_=pgt[:, :], func=AF.Sigmoid), reads=[pgt], writes=[sg])
                    P.dve(lambda e, ppr=ppr, sg=sg: e.tensor_tensor(out=sg[:], in0=ppr[:, :], in1=sg[:], op=ALU.mult), reads=[ppr, sg], writes=[sg])
                    P.pool(lambda e, sg=sg, dc=dc: e.tensor_tensor(out=xg[:, dc, :], in0=xg[:, dc, :], in1=sg[:], op=ALU.add), reads=[sg, (xg, dc)], writes=[(xg, dc)])
            if not last:
                store_x(xg, g)
                continue
            for k in range(KD):
                P.act(lambda e, k=k: e.activation(out=sq[:, k, :], in_=xg[:, k, :], func=AF.Square), reads=[(xg, k)], writes=[(sq, k)])
            for k in range(KD):
                P.pe(lambda e, k=k: e.matmul(bank[0][:], lhsT=ones_bf[:], rhs=sq[:, k, :], start=(k == 0), stop=(k == KD - 1)), reads=[ones_bf, (sq, k)], writes=[bank[0]])
            P.act(lambda e: e.activation(out=inv[:], in_=bank[0][:], func=AF.Sqrt, scale=1.0 / D, bias=eps_t[:, 0:1]), reads=[bank[0], eps_t], writes=[inv])
            P.dve(lambda e: e.reciprocal(out=inv[:], in_=inv[:]), reads=[inv], writes=[inv])
            for k in range(KD):
                P.dve(lambda e, k=k: e.scalar_tensor_tensor(out=hfin[:, k, :], in0=xg[:, k, :], scalar=gain_fin[:, k:k + 1], in1=inv[:], op0=ALU.mult, op1=ALU.mult),
                      reads=[(xg, k), inv, gain_fin], writes=[(hfin, k)])
            for t in range(4):
                ob = osb[no % 2]
                no += 1
                for kb in range(4):
                    pp = bank[4 + (kb % 4)]
                    for kq in range(4):
                        k = kb * 4 + kq
                        P.pe(lambda e, pp=pp, k=k, kq=kq, t=t: e.transpose(pp[:, kq * 128:(kq + 1) * 128], hfin[:, k, t * 128:(t + 1) * 128], ident[:]),
                             reads=[(hfin, k), ident], writes=[pp])
                    P.act(lambda e, pp=pp, ob=ob, kb=kb: e.copy(out=ob[:, kb * 512:(kb + 1) * 512], in_=pp[:, :]), reads=[pp], writes=[ob])
                tok0 = g * 512 + t * 128
                P.dma(out_ap[tok0:tok0 + 128, :], ob[:, :], reads=[ob], writes=["out"], q="pool")

    if "moe" in phases:
        for (dst_, src_) in expert_cast_jobs():
            P.dma(dst_, src_, writes=["w_exp_bf"], q="pool")
        if dbg.get("dense_moe"):
            with P.phase():
                p3b_moe(0)
        else:
            moe_routed(0)
        dump("x2T", xT[:, :], [D, S], F32, [])
    for l in range(L):
        casts_layer(l)
    for l in range(L):
        if "p1" in phases:
            with P.phase():
                p1(l)
        if "swa" in phases:
            with P.phase():
                swa(l)
        if "mla" in phases:
            with P.phase():
                mla1(l)
            with P.phase():
                mla2(l)
        if "dn" in phases:
            with P.phase():
                dn(l)
        if "p3" in phases:
            with P.phase():
                p3a(l)
            if dbg.get("x1") and l == 0:
                dump("x1T", xT[:, :], [D, S], F32, [])
            if l % 2 == 0:
                with P.phase():
                    p3b_dense(l)
            elif dbg.get("dense_moe"):
                with P.phase():
                    p3b_moe(l)
            else:
                moe_routed(l)
            if dbg.get("x1") and l == 0:
                dump("x2T", xT[:, :], [D, S], F32, [])
            with P.phase():
                p3c(l, l == L - 1)

    if dbg.get("projT"):
        dump("projT", projT[:, :], [C_G, S], F32, keys("projT"))
        dump("gatesT", gatesT[:, :], [3 * D, S], BF16, keys("gatesT"))
    if dbg.get("xT"):
        dump("xT", xT[:, :], [D, S], F32, keys("xT"))
    if dbg.get("yT"):
        dump("yT", yT[:, :], [3072, S], BF16, keys("yT_a") + keys("yT_b") + keys("yT_c"))
    if dbg.get("mlaqk"):
        dump("qnT", qnT[:, :], [1024, S], BF16, keys("qnT"))
        dump("qrT", qrT[:, :], [512, S], BF16, keys("qrT"))
        dump("krT", krT[:, :], [64, S], BF16, keys("krT"))
        dump("cosT", cosT[:, :], [64, S], F32, keys("cosT"))

    P.finish()
    return nc, stack, P


_CACHE = {}
_WEIGHTS = ["norm_mix", "w_in", "swa_sinks", "mla_q_norm", "mla_kv_norm", "w_uq", "w_ukv", "conv_w", "dn_a_log", "dn_dt_bias",
            "dn_norm", "w_branch", "w_out", "norm_ffn", "w_ffn_gate", "w_ffn_up", "w_ffn_down", "norm_ple", "w_ple_gate",
            "w_ple_proj", "w_router", "w_exp_gate", "w_exp_up", "w_exp_down"]


def kernel(**inputs):
    S, L = 8192, 2
    if "prog" not in _CACHE:
        _CACHE["prog"] = build(S, L)
    nc, stack, P = _CACHE["prog"]
    consts = host_consts()
    shared = {k: np.ascontiguousarray(np.asarray(inputs[k], dtype=np.float32)) for k in _WEIGHTS}
    shared["final_norm"] = np.ascontiguousarray(np.asarray(inputs["final_norm"], dtype=np.float32)[None])
    in_maps = []
    for b in range(2):
        m = dict(consts)
        m.update(shared)
        m["x"] = np.ascontiguousarray(np.asarray(inputs["x"], dtype=np.float32)[b])
        m["positions"] = np.ascontiguousarray(np.asarray(inputs["positions"])[b:b + 1].astype(np.int32))
        m["p"] = np.ascontiguousarray(np.asarray(inputs["p"], dtype=np.float32)[:, b])
        in_maps.append(m)
    res = run_bass_kernel_spmd(nc, in_maps, core_ids=[0, 1])
    return np.stack([np.asarray(res.results[b]["out"], dtype=np.float32) for b in range(2)])
```
